# Optimizing a Trainium2 kernel written in Bass

```python
import jax, jax.numpy as jnp
from jax import lax
import numpy as np

D_MODEL = 2048
BATCH = 2
SEQ = 4096
DEPTH = 1

MIX_WIDTH = D_MODEL
NSA_HEADS = 8
NSA_KV_HEADS = 2
HEAD_DIM = MIX_WIDTH // 2 // NSA_HEADS
NSA_WIDTH = NSA_HEADS * HEAD_DIM
GQA_GROUP = NSA_HEADS // NSA_KV_HEADS
KV_WIDTH = NSA_KV_HEADS * HEAD_DIM
ROPE_DIM = HEAD_DIM // 4
ROPE_THETA = 500000.0
CMP_LEN = 32
CMP_STRIDE = 16
CMP_HIDDEN = 2 * HEAD_DIM
SEL_LEN = 64
SEL_TOPK = 16
WINDOW = 512
Q_BLOCK = 128
N_GATES = 3
LRU_WIDTH = MIX_WIDTH - NSA_WIDTH
LRU_BLOCKS = 8
LRU_BLOCK_DIM = LRU_WIDTH // LRU_BLOCKS
CONV_WIDTH = 4
LRU_C = 8.0
D_FF = 5632
PLE_DIM = 256
IN_WIDTH = NSA_WIDTH + 6 * KV_WIDTH + NSA_HEADS * N_GATES + 2 * LRU_WIDTH
RMS_EPS = 1e-6
NEG = -1e30
SEL_FORCE = 1e4

kernel_name = 'hybrid_nsa_rglru_macaron_block'


def rmsnorm(x, g):
    xf = x.astype(jnp.float32)
    y = xf * lax.rsqrt(jnp.mean(xf * xf, axis=-1, keepdims=True) + RMS_EPS)
    return (y * g.astype(jnp.float32)).astype(x.dtype)


def swiglu(x, w_gate, w_up, w_down):
    return (jax.nn.silu(x @ w_gate) * (x @ w_up)) @ w_down


def partial_rope(x, positions):
    half = ROPE_DIM // 2
    inv_freq = ROPE_THETA ** (-jnp.arange(half, dtype=jnp.float32) / half)
    ang = positions.astype(jnp.float32)[..., None] * inv_freq
    cos = jnp.cos(ang)[:, :, None, :]
    sin = jnp.sin(ang)[:, :, None, :]
    xr = x[..., :ROPE_DIM].astype(jnp.float32)
    x1, x2 = xr[..., :half], xr[..., half:]
    rot = jnp.concatenate([x1 * cos - x2 * sin, x2 * cos + x1 * sin], axis=-1)
    return jnp.concatenate([rot.astype(x.dtype), x[..., ROPE_DIM:]], axis=-1)


def masked_softmax(s, mask):
    s = jnp.where(mask, s.astype(jnp.float32), NEG)
    return jax.nn.softmax(s, axis=-1) * mask


def compress(kv, pos_emb, w1, w2):
    B, T, G, dh = kv.shape
    n_cmp = (T - CMP_LEN) // CMP_STRIDE + 1
    idx = jnp.arange(n_cmp)[:, None] * CMP_STRIDE + jnp.arange(CMP_LEN)[None, :]
    blocks = kv[:, idx] + pos_emb[:, None, :]
    blocks = blocks.transpose(0, 1, 3, 2, 4).reshape(B, n_cmp, G, CMP_LEN * dh)
    return jax.nn.gelu(blocks @ w1) @ w2


def nsa_attention(q, k_cmp, v_cmp, k_sel, v_sel, k_win, v_win, gates,
                  pos_k, pos_v, k_w1, k_w2, v_w1, v_w2):
    B, T = q.shape[:2]
    G, R, dh = NSA_KV_HEADS, GQA_GROUP, HEAD_DIM
    scale = HEAD_DIM ** -0.5
    kc = compress(k_cmp, pos_k, k_w1, k_w2)
    vc = compress(v_cmp, pos_v, v_w1, v_w2)
    n_cmp = kc.shape[1]
    cmp_start = jnp.arange(n_cmp) * CMP_STRIDE
    cmp_end = cmp_start + CMP_LEN - 1
    n_sel = T // SEL_LEN
    top_k = min(SEL_TOPK, n_sel)
    sel_start = jnp.arange(n_sel) * SEL_LEN
    overlap = ((cmp_start[:, None] < sel_start[None, :] + SEL_LEN)
               & (cmp_end[:, None] >= sel_start[None, :])).astype(jnp.float32)
    kb = k_sel.reshape(B, n_sel, SEL_LEN, G, dh).transpose(0, 3, 1, 2, 4)
    vb = v_sel.reshape(B, n_sel, SEL_LEN, G, dh).transpose(0, 3, 1, 2, 4)
    kw = jnp.pad(k_win, ((0, 0), (WINDOW, 0), (0, 0), (0, 0)))
    vw = jnp.pad(v_win, ((0, 0), (WINDOW, 0), (0, 0), (0, 0)))
    qg = q.reshape(B, T, G, R, dh)
    bi = jnp.arange(B)[:, None, None, None]
    gi = jnp.arange(G)[None, :, None, None]
    blk = jnp.arange(n_sel)

    def query_block(i):
        t0 = i * Q_BLOCK
        qb = lax.dynamic_slice_in_dim(qg, t0, Q_BLOCK, 1)
        gb = lax.dynamic_slice_in_dim(gates, t0, Q_BLOCK, 1).reshape(B, Q_BLOCK, G, R, N_GATES)
        t = t0 + jnp.arange(Q_BLOCK)
        s = jnp.einsum('btgrd,bcgd->bgrtc', qb, kc) * scale
        pc = masked_softmax(s, cmp_end[None, :] <= t[:, None])
        o_cmp = jnp.einsum('bgrtc,bcgd->btgrd', pc.astype(vc.dtype), vc)
        imp = jnp.einsum('bgrtc,cs->bgts', pc, overlap)
        forced = (blk[None, :] == (t // SEL_LEN)[:, None]) | (blk[None, :] == 0)
        valid = sel_start[None, :] <= t[:, None]
        imp = jnp.where(forced, SEL_FORCE, jnp.where(valid, imp, -SEL_FORCE))
        _, idx = lax.top_k(imp, top_k)
        ks = kb[bi, gi, idx]
        vs = vb[bi, gi, idx]
        kpos = idx[..., None] * SEL_LEN + jnp.arange(SEL_LEN)
        ms = (kpos <= t[:, None, None]).reshape(B, G, 1, Q_BLOCK, top_k * SEL_LEN)
        s = jnp.einsum('btgrd,bgtkjd->bgrtkj', qb, ks).reshape(B, G, R, Q_BLOCK, top_k * SEL_LEN) * scale
        ps = masked_softmax(s, ms).reshape(B, G, R, Q_BLOCK, top_k, SEL_LEN)
        o_sel = jnp.einsum('bgrtkj,bgtkjd->btgrd', ps.astype(vs.dtype), vs)
        kwb = lax.dynamic_slice_in_dim(kw, t0, WINDOW + Q_BLOCK, 1)
        vwb = lax.dynamic_slice_in_dim(vw, t0, WINDOW + Q_BLOCK, 1)
        kp = t0 - WINDOW + jnp.arange(WINDOW + Q_BLOCK)
        mw = (kp[None, :] <= t[:, None]) & (kp[None, :] > t[:, None] - WINDOW) & (kp[None, :] >= 0)
        s = jnp.einsum('btgrd,bsgd->bgrts', qb, kwb) * scale
        pw = masked_softmax(s, mw)
        o_win = jnp.einsum('bgrts,bsgd->btgrd', pw.astype(vwb.dtype), vwb)
        o = gb[..., 0, None] * o_cmp + gb[..., 1, None] * o_sel + gb[..., 2, None] * o_win
        return o.reshape(B, Q_BLOCK, NSA_WIDTH)

    out = lax.map(query_block, jnp.arange(T // Q_BLOCK))
    return out.transpose(1, 0, 2, 3).reshape(B, T, NSA_WIDTH)


def rglru_block(xb, yb, conv_w, conv_b, w_a, b_a, w_i, b_i, lam):
    B, T, C = xb.shape
    xc = lax.conv_general_dilated(xb, conv_w[:, None, :], window_strides=(1,),
                                  padding=[(CONV_WIDTH - 1, 0)],
                                  dimension_numbers=('NWC', 'WIO', 'NWC'),
                                  feature_group_count=C) + conv_b
    xh = xc.reshape(B, T, LRU_BLOCKS, LRU_BLOCK_DIM)
    r = jax.nn.sigmoid(jnp.einsum('bthi,hij->bthj', xh, w_a).reshape(B, T, C) + b_a)
    ig = jax.nn.sigmoid(jnp.einsum('bthi,hij->bthj', xh, w_i).reshape(B, T, C) + b_i)
    log_a = -LRU_C * r.astype(jnp.float32) * jax.nn.softplus(-lam.astype(jnp.float32))
    a = jnp.exp(log_a)
    u = jnp.sqrt(-jnp.expm1(2.0 * log_a)) * (ig * xc).astype(jnp.float32)

    def combine(left, right):
        a1, b1 = left
        a2, b2 = right
        return a1 * a2, a2 * b1 + b2

    _, h = lax.associative_scan(combine, (a, u), axis=1)
    return h.astype(xb.dtype) * jax.nn.gelu(yb)


def setup_inputs(seed: int = 0) -> dict:
    key = jax.random.key(seed)
    ks = iter(jax.random.split(key, 64))
    L = DEPTH
    f32 = jnp.float32

    def nrm(shape, fan_in):
        return jax.random.normal(next(ks), shape, f32) * fan_in ** -0.5

    def gain(shape):
        return 1.0 + 0.02 * jax.random.normal(next(ks), shape, f32)

    def small(shape, s=0.01):
        return s * jax.random.normal(next(ks), shape, f32)

    x = jax.random.normal(next(ks), (BATCH, SEQ, D_MODEL), f32)
    p = jax.random.normal(next(ks), (DEPTH, BATCH, SEQ, PLE_DIM), f32)
    positions = (jax.random.randint(next(ks), (BATCH, 1), 0, 1024) + jnp.arange(SEQ)[None, :]).astype(jnp.int32)
    d = {'x': x, 'p': p, 'positions': positions}
    d['ff1_pre_g'] = gain((L, D_MODEL))
    d['ff1_post_g'] = gain((L, D_MODEL))
    d['ff1_w_gate'] = nrm((L, D_MODEL, D_FF), D_MODEL)
    d['ff1_w_up'] = nrm((L, D_MODEL, D_FF), D_MODEL)
    d['ff1_w_down'] = nrm((L, D_FF, D_MODEL), D_FF)
    d['mix_pre_g'] = gain((L, D_MODEL))
    d['mix_post_g'] = gain((L, D_MODEL))
    d['w_in'] = nrm((L, D_MODEL, IN_WIDTH), D_MODEL)
    d['cmp_pos_k'] = small((L, CMP_LEN, HEAD_DIM), 0.1)
    d['cmp_pos_v'] = small((L, CMP_LEN, HEAD_DIM), 0.1)
    d['cmp_k_w1'] = nrm((L, CMP_LEN * HEAD_DIM, CMP_HIDDEN), CMP_LEN * HEAD_DIM)
    d['cmp_k_w2'] = nrm((L, CMP_HIDDEN, HEAD_DIM), CMP_HIDDEN)
    d['cmp_v_w1'] = nrm((L, CMP_LEN * HEAD_DIM, CMP_HIDDEN), CMP_LEN * HEAD_DIM)
    d['cmp_v_w2'] = nrm((L, CMP_HIDDEN, HEAD_DIM), CMP_HIDDEN)
    d['nsa_gate_b'] = small((L, NSA_HEADS, N_GATES))
    d['conv_w'] = nrm((L, CONV_WIDTH, LRU_WIDTH), CONV_WIDTH)
    d['conv_b'] = small((L, LRU_WIDTH))
    d['rg_w_a'] = nrm((L, LRU_BLOCKS, LRU_BLOCK_DIM, LRU_BLOCK_DIM), LRU_BLOCK_DIM)
    d['rg_b_a'] = small((L, LRU_WIDTH))
    d['rg_w_i'] = nrm((L, LRU_BLOCKS, LRU_BLOCK_DIM, LRU_BLOCK_DIM), LRU_BLOCK_DIM)
    d['rg_b_i'] = small((L, LRU_WIDTH))
    a_c = jax.random.uniform(next(ks), (L, LRU_WIDTH), f32, 0.9, 0.999)
    a0 = a_c ** (1.0 / LRU_C)
    d['rg_lambda'] = jnp.log(a0) - jnp.log1p(-a0)
    d['attn_out_g'] = gain((L, NSA_WIDTH))
    d['rec_out_g'] = gain((L, LRU_WIDTH))
    d['w_out'] = nrm((L, MIX_WIDTH, D_MODEL), MIX_WIDTH)
    d['ff2_pre_g'] = gain((L, D_MODEL))
    d['ff2_post_g'] = gain((L, D_MODEL))
    d['ff2_w_gate'] = nrm((L, D_MODEL, D_FF), D_MODEL)
    d['ff2_w_up'] = nrm((L, D_MODEL, D_FF), D_MODEL)
    d['ff2_w_down'] = nrm((L, D_FF, D_MODEL), D_FF)
    d['ple_pre_g'] = gain((L, D_MODEL))
    d['ple_post_g'] = gain((L, D_MODEL))
    d['w_ple_gate'] = nrm((L, D_MODEL, D_MODEL), D_MODEL)
    d['w_ple_proj'] = nrm((L, PLE_DIM, D_MODEL), PLE_DIM)
    return d


def reference(x, p, positions,
              ff1_pre_g, ff1_post_g, ff1_w_gate, ff1_w_up, ff1_w_down,
              mix_pre_g, mix_post_g, w_in,
              cmp_pos_k, cmp_pos_v, cmp_k_w1, cmp_k_w2, cmp_v_w1, cmp_v_w2, nsa_gate_b,
              conv_w, conv_b, rg_w_a, rg_b_a, rg_w_i, rg_b_i, rg_lambda,
              attn_out_g, rec_out_g, w_out,
              ff2_pre_g, ff2_post_g, ff2_w_gate, ff2_w_up, ff2_w_down,
              ple_pre_g, ple_post_g, w_ple_gate, w_ple_proj):
    B, T, _ = x.shape
    G = NSA_KV_HEADS
    offs = np.cumsum([NSA_WIDTH] + [KV_WIDTH] * 6 + [NSA_HEADS * N_GATES, LRU_WIDTH]).tolist()

    def heads(t, n):
        return t.reshape(B, T, n, HEAD_DIM)

    h = x
    for i in range(DEPTH):
        u = rmsnorm(h, ff1_pre_g[i])
        h = h + 0.5 * rmsnorm(swiglu(u, ff1_w_gate[i], ff1_w_up[i], ff1_w_down[i]), ff1_post_g[i])
        u = rmsnorm(h, mix_pre_g[i])
        z = u @ w_in[i]
        zq, zkc, zvc, zks, zvs, zkw, zvw, zg, zx, zy = jnp.split(z, offs, axis=-1)
        q = partial_rope(heads(zq, NSA_HEADS), positions)
        k_cmp = partial_rope(heads(zkc, G), positions)
        k_sel = partial_rope(heads(zks, G), positions)
        k_win = partial_rope(heads(zkw, G), positions)
        gates = jax.nn.sigmoid(zg.reshape(B, T, NSA_HEADS, N_GATES) + nsa_gate_b[i])
        o_attn = nsa_attention(q, k_cmp, heads(zvc, G), k_sel, heads(zvs, G), k_win, heads(zvw, G), gates,
                               cmp_pos_k[i], cmp_pos_v[i], cmp_k_w1[i], cmp_k_w2[i], cmp_v_w1[i], cmp_v_w2[i])
        o_rec = rglru_block(zx, zy, conv_w[i], conv_b[i], rg_w_a[i], rg_b_a[i],
                            rg_w_i[i], rg_b_i[i], rg_lambda[i])
        o = jnp.concatenate([rmsnorm(o_attn, attn_out_g[i]), rmsnorm(o_rec, rec_out_g[i])], axis=-1)
        h = h + rmsnorm(o @ w_out[i], mix_post_g[i])
        u = rmsnorm(h, ff2_pre_g[i])
        h = h + 0.5 * rmsnorm(swiglu(u, ff2_w_gate[i], ff2_w_up[i], ff2_w_down[i]), ff2_post_g[i])
        gate = jax.nn.sigmoid(rmsnorm(h, ple_pre_g[i]) @ w_ple_gate[i])
        h = h + rmsnorm(gate * (p[i] @ w_ple_proj[i]), ple_post_g[i])
    return h
```

```python
import numpy as np
import concourse.bass as bass
import concourse.mybir as mybir
from concourse.bass_utils import run_bass_kernel_spmd

F32 = mybir.dt.float32
BF16 = mybir.dt.bfloat16
I32 = mybir.dt.int32
AF = mybir.ActivationFunctionType
ALU = mybir.AluOpType
AX = mybir.AxisListType

COMPUTE = ("pe", "act", "dve", "pool")
NRING = 12


class Op:
    __slots__ = ("eng", "fn", "deps", "is_dma", "signal", "idx", "ring", "ringval", "prev_ring", "is_cc")

    def __init__(self, eng, fn, is_dma):
        self.eng = eng
        self.fn = fn
        self.is_dma = is_dma
        self.deps = []
        self.signal = 0
        self.ring = None
        self.ringval = 0
        self.prev_ring = None
        self.idx = 0
        self.is_cc = False


class Prog:
    def __init__(self, nc):
        self.nc = nc
        self.ops = {k: [] for k in ("pe", "act", "dve", "pool", "sp")}
        self.last_w = {}
        self.readers = {}
        self.nops = 0
        self.pend = {}
        self.since = []
        self.lastc = {}

    def barrier(self):
        deps = list(self.lastc.values()) + list(self.since)
        self.since = []
        for e in self.ops:
            self.pend[e] = list(self.pend.get(e, [])) + deps

    def _add(self, eng, fn, reads, writes, is_dma):
        op = Op(eng, fn, is_dma)
        op.idx = self.nops
        self.nops += 1
        deps = {}
        for r in reads:
            w = self.last_w.get(r)
            if w is not None:
                deps[id(w)] = w
        for wr in writes:
            w = self.last_w.get(wr)
            if w is not None:
                deps[id(w)] = w
            for rd in self.readers.get(wr, ()):
                deps[id(rd)] = rd
        for d in deps.values():
            if d is op:
                continue
            if d.eng == "pe" and eng == "pe":
                continue
            op.deps.append(d)
        if eng in self.pend:
            for d in self.pend.pop(eng):
                if not (d.eng == eng and not d.is_dma):
                    op.deps.append(d)
        if is_dma:
            self.since.append(op)
        else:
            self.lastc[eng] = op
        for r in reads:
            self.readers.setdefault(r, []).append(op)
        for wr in writes:
            self.last_w[wr] = op
            self.readers[wr] = []
        self.ops[eng].append(op)
        return op

    def pe(self, fn, reads=(), writes=()):
        return self._add("pe", fn, reads, writes, False)

    def act(self, fn, reads=(), writes=()):
        return self._add("act", fn, reads, writes, False)

    def dve(self, fn, reads=(), writes=()):
        return self._add("dve", fn, reads, writes, False)

    def pool(self, fn, reads=(), writes=()):
        return self._add("pool", fn, reads, writes, False)

    def dma(self, fn, reads=(), writes=(), q="sp"):
        return self._add(q, fn, reads, writes, True)

    def cc(self, fn, reads=(), writes=()):
        op = self._add("pool", fn, reads, writes, True)
        op.is_cc = True
        return op

    def emit(self, final_wait_ops=()):
        nc = self.nc
        needed = set()
        for e, lst in self.ops.items():
            for op in lst:
                for d in op.deps:
                    needed.add(id(d))
        for op in final_wait_ops:
            needed.add(id(op))
        ringcount = {}
        for e, lst in self.ops.items():
            n = 0
            k = 0
            last_on_ring = {}
            for op in lst:
                if id(op) not in needed:
                    continue
                if op.is_cc:
                    op.ring = ("cc", op.idx)
                    op.ringval = 1
                    op.prev_ring = None
                elif op.is_dma:
                    slot = k % NRING
                    k += 1
                    op.ring = (e, slot)
                    ringcount[(e, slot)] = ringcount.get((e, slot), 0) + 16
                    op.ringval = ringcount[(e, slot)]
                    op.prev_ring = last_on_ring.get(slot)
                    last_on_ring[slot] = op
                else:
                    n += 1
                    op.signal = n
        engmap = {"pe": nc.tensor, "act": nc.scalar, "dve": nc.vector, "pool": nc.gpsimd, "sp": nc.sync}
        import contextlib
        with contextlib.ExitStack() as st:
            csem = {e: st.enter_context(nc.semaphore("s_" + e)) for e in ("pe", "act", "dve", "pool")}
            rsem = {}
            for e in ("sp", "pool"):
                for s in range(NRING):
                    rsem[(e, s)] = st.enter_context(nc.semaphore("r_%s_%d" % (e, s)))
            for e, lst in self.ops.items():
                for op in lst:
                    if op.is_cc and op.ring is not None:
                        rsem[op.ring] = st.enter_context(nc.semaphore("cc_%d" % op.idx))
            block = st.enter_context(nc.Block())

            def run(ename):
                def body(eng):
                    waited = {}
                    lst = self.ops[ename]
                    for op in lst:
                        deps = list(op.deps)
                        if op.is_dma and op.prev_ring is not None:
                            deps.append(op.prev_ring)
                        need = {}
                        for d in deps:
                            if d.is_dma:
                                key = ("r",) + d.ring
                                val = d.ringval
                                sem = rsem[d.ring]
                            else:
                                key = ("c", d.eng)
                                val = d.signal
                                sem = csem[d.eng]
                            assert val > 0, (ename, d.eng)
                            if need.get(key, (0, None))[0] < val:
                                need[key] = (val, sem)
                        for key, (val, sem) in need.items():
                            if waited.get(key, 0) >= val:
                                continue
                            waited[key] = val
                            eng.wait_ge(sem, val)
                        ins = op.fn(eng)
                        if op.is_cc:
                            if op.ring is not None:
                                ins.then_inc(rsem[op.ring], 1)
                        elif op.is_dma:
                            if op.ring is not None:
                                ins.then_inc(rsem[op.ring], 16)
                        elif op.signal:
                            ins.then_inc(csem[ename], 1)
                    if ename == "sp":
                        for d in final_wait_ops:
                            if d.is_dma:
                                eng.wait_ge(rsem[d.ring], d.ringval)
                            else:
                                eng.wait_ge(csem[d.eng], d.signal)
                return body

            block.tensor(run("pe"))
            block.scalar(run("act"))
            block.vector(run("dve"))
            block.gpsimd(run("pool"))
            block.sync(run("sp"))


D = 2048
NT = 1024
NTILE = 8
DFF = 5632
NF = DFF // 128
KD = D // 128
EPS = 1e-6
IN_WIDTH = 4632


def bcast_rows(ap, n, parts=128):
    return bass.AP(ap.tensor, ap.offset, [[0, parts], [1, n]])


class Ctx:
    pass


def build(stop=99, debug=False):
    import contextlib
    nc = bass.Bass("TRN2", target_bir_lowering=False)
    C = Ctx()
    C.nc = nc
    P = Prog(nc)
    C.P = P

    def din(name, shape, dt=F32):
        return nc.dram_tensor(name, list(shape), dt, kind="ExternalInput").ap()

    x = din("x", [NT, D])
    pin = din("p", [NT, 256])
    pos = din("pos", [1, NT], I32)
    WSHAPES = dict([("ff1_pre_g", [1, D]), ("ff1_post_g", [1, D]), ("ff1_w_gate", [D, DFF]), ("ff1_w_up", [D, DFF]),
                    ("ff1_w_down", [DFF, D]), ("mix_pre_g", [1, D]), ("mix_post_g", [1, D]), ("w_in", [D, IN_WIDTH]),
                    ("cmp_pos_k", [32, 128]), ("cmp_pos_v", [32, 128]), ("cmp_k_w1", [4096, 256]), ("cmp_k_w2", [256, 128]),
                    ("cmp_v_w1", [4096, 256]), ("cmp_v_w2", [256, 128]), ("nsa_gate_b", [1, 24]),
                    ("conv_w", [4, 1024]), ("conv_b", [1, 1024]), ("rg_w_a", [8, 128, 128]), ("rg_b_a", [1, 1024]),
                    ("rg_w_i", [8, 128, 128]), ("rg_b_i", [1, 1024]), ("rg_lambda", [1, 1024]),
                    ("attn_out_g", [1, 1024]), ("rec_out_g", [1, 1024]), ("w_out", [D, D]),
                    ("ff2_pre_g", [1, D]), ("ff2_post_g", [1, D]), ("ff2_w_gate", [D, DFF]), ("ff2_w_up", [D, DFF]),
                    ("ff2_w_down", [DFF, D]), ("ple_pre_g", [1, D]), ("ple_post_g", [1, D]),
                    ("w_ple_gate", [D, D]), ("w_ple_proj", [256, D])])

    class LazyW(dict):
        def __missing__(self, nm):
            v = din(nm, WSHAPES[nm])
            self[nm] = v
            return v
    W = LazyW()
    C.W = W
    out = nc.dram_tensor("out", [NT, D], F32, kind="ExternalOutput").ap()
    h1d = nc.dram_tensor("h1d", [NT, D], F32, kind="Internal").ap()
    h2d = nc.dram_tensor("h2d", [NT, D], F32, kind="Internal").ap()
    h3d = nc.dram_tensor("h3d", [NT, D], F32, kind="Internal").ap()
    dbg = {}
    if debug:
        dbg["uT"] = nc.dram_tensor("dbg_uT", [128, KD, NT], BF16, kind="ExternalOutput").ap()

    finals = []
    with contextlib.ExitStack() as st:
        used_names = {}

        def uniq(name):
            n = used_names.get(name, 0)
            used_names[name] = n + 1
            return name if n == 0 else "%s_v%d" % (name, n)

        def sb(name, shape, dt, stack=st):
            return stack.enter_context(nc.sbuf_tensor(uniq(name), list(shape), dt))

        def ps(name, shape, dt, stack=st):
            return stack.enter_context(nc.psum_tensor(uniq(name), list(shape), dt))

        identf = sb("identf", [128, 128], F32)
        ident = sb("ident", [128, 128], BF16)
        AT = sb("AT", [128, KD, NT], BF16)
        P.pool(lambda e: e.memset(identf[:], 1.0), writes=["identf"])
        P.pool(lambda e: e.affine_select(out=identf[:], in_=identf[:], pattern=[[-1, 128]], compare_op=ALU.is_equal,
                                         fill=0.0, base=0, channel_multiplier=1), reads=["identf"], writes=["identf"])
        P.dve(lambda e: e.tensor_copy(out=ident[:], in_=identf[:]), reads=["identf"], writes=["ident"])
        C.ident, C.identf, C.AT = ident, identf, AT
        C.sb, C.ps = sb, ps

        def norm_transpose(S, tg, ht, hkey, gb, gkey, t, scr):
            sq, ss, ub, pT = scr["sq"], scr["ss"], scr["ub"], scr["pT"]
            P.act(lambda e: e.activation(out=sq[:], in_=ht[:], func=AF.Square, accum_out=ss[:, 0:1]),
                  reads=[hkey], writes=[tg + "sq", tg + "ss"])
            P.act(lambda e: e.activation(out=ss[:, 1:2], in_=ss[:, 0:1], func=AF.Sqrt, scale=1.0 / D, bias=scr["eps"][:, 0:1]),
                  reads=[tg + "ss"], writes=[tg + "ss1"])
            P.dve(lambda e: e.reciprocal(out=ss[:, 2:3], in_=ss[:, 1:2]), reads=[tg + "ss1"], writes=[tg + "ss2"])
            P.dve(lambda e: e.scalar_tensor_tensor(out=ub[:], in0=ht[:], scalar=ss[:, 2:3], in1=gb[:], op0=ALU.mult, op1=ALU.mult),
                  reads=[hkey, tg + "ss2", gkey], writes=[tg + "ub"])
            for k in range(KD):
                P.pe(lambda e, k=k: e.transpose(out=pT[:, k, :], in_=ub[:, k * 128:(k + 1) * 128], identity=ident[:]),
                     reads=[tg + "ub", "ident"], writes=[tg + "pT"])
            P.act(lambda e: e.copy(out=AT[:, :, t * 128:(t + 1) * 128], in_=pT[:]), reads=[tg + "pT"], writes=[("AT", t)])

        C.norm_transpose = norm_transpose

        def ffn(tg, h_src, pre_g, post_g, wg, wu, wd, h_dst, next_g, first):
            with contextlib.ExitStack() as S:
                hid = sb(tg + "hid", [128, NF, NT], BF16, S)
                epsT = sb(tg + "eps", [128, 1], F32, S)
                P.pool(lambda e: e.memset(epsT[:], EPS), writes=[tg + "eps"])
                if first:
                    with contextlib.ExitStack() as S0:
                        gb = sb(tg + "gb", [128, D], F32, S0)
                        P.dma(lambda e: e.dma_start(out=gb[:], in_=bcast_rows(pre_g, D)), writes=[tg + "gb"])
                        scr = [dict(sq=sb(tg + "sq%d" % i, [128, D], BF16, S0), ss=sb(tg + "ss%d" % i, [128, 4], F32, S0),
                                    ub=sb(tg + "ub%d" % i, [128, D], BF16, S0), pT=ps(tg + "pT%d" % i, [128, KD, 128], BF16, S0),
                                    eps=epsT) for i in range(2)]
                        hts = [sb(tg + "ht%d" % i, [128, D], F32, S0) for i in range(2)]
                        for t in range(NTILE):
                            i = t % 2
                            P.dma(lambda e, t=t, i=i: e.dma_start(out=hts[i][:], in_=h_src[t * 128:(t + 1) * 128, :]),
                                  writes=[tg + "ht%d" % i])
                            norm_transpose(S0, tg + "n%d" % i, hts[i], tg + "ht%d" % i, gb, tg + "gb", t, scr[i])
                P.barrier()
                with contextlib.ExitStack() as S1:
                    wgp = [sb(tg + "wgp%d" % i, [128, KD, 512], BF16, S1) for i in range(2)]
                    wup = [sb(tg + "wup%d" % i, [128, KD, 512], BF16, S1) for i in range(2)]
                    sgt = [sb(tg + "sg%d" % i, [128, 512], BF16, S1) for i in range(2)]
                    pg = [[ps(tg + "pg%d%d" % (i, hf), [128, 512], F32, S1) for hf in range(2)] for i in range(2)]
                    pu = [[ps(tg + "pu%d%d" % (i, hf), [128, 512], F32, S1) for hf in range(2)] for i in range(2)]
                    wgv = wg.rearrange("(kc p) m -> p kc m", p=128)
                    wuv = wu.rearrange("(kc p) m -> p kc m", p=128)
                    ATall = [("AT", t) for t in range(NTILE)]
                    for pi in range(NF // 4):
                        b = pi % 2
                        P.dma(lambda e, pi=pi, b=b: e.dma_start(out=wgp[b][:], in_=wgv[:, :, pi * 512:(pi + 1) * 512]),
                              writes=[tg + "wgp%d" % b], q="pool")
                        P.dma(lambda e, pi=pi, b=b: e.dma_start(out=wup[b][:], in_=wuv[:, :, pi * 512:(pi + 1) * 512]),
                              writes=[tg + "wup%d" % b], q="pool")
                        for fl in range(4):
                            f = pi * 4 + fl
                            r = f % 2
                            for hf in range(2):
                                for k in range(KD):
                                    P.pe(lambda e, k=k, hf=hf, r=r, b=b, fl=fl: e.matmul(
                                        pg[r][hf][:], lhsT=wgp[b][:, k, fl * 128:(fl + 1) * 128], rhs=AT[:, k, hf * 512:(hf + 1) * 512],
                                        start=(k == 0), stop=(k == KD - 1)),
                                        reads=[tg + "wgp%d" % b] + ATall[hf * 4:hf * 4 + 4], writes=[tg + "pg%d%d" % (r, hf)])
                                for k in range(KD):
                                    P.pe(lambda e, k=k, hf=hf, r=r, b=b, fl=fl: e.matmul(
                                        pu[r][hf][:], lhsT=wup[b][:, k, fl * 128:(fl + 1) * 128], rhs=AT[:, k, hf * 512:(hf + 1) * 512],
                                        start=(k == 0), stop=(k == KD - 1)),
                                        reads=[tg + "wup%d" % b] + ATall[hf * 4:hf * 4 + 4], writes=[tg + "pu%d%d" % (r, hf)])
                                P.act(lambda e, hf=hf, r=r: e.activation(out=sgt[hf][:], in_=pg[r][hf][:], func=AF.Silu),
                                      reads=[tg + "pg%d%d" % (r, hf)], writes=[tg + "sg%d" % hf])
                                P.dve(lambda e, hf=hf, r=r, f=f: e.tensor_tensor(out=hid[:, f, hf * 512:(hf + 1) * 512], in0=sgt[hf][:],
                                                                               in1=pu[r][hf][:], op=ALU.mult),
                                      reads=[tg + "sg%d" % hf, tg + "pu%d%d" % (r, hf)], writes=[(tg + "hid", f)])
                P.barrier()
                if debug and tg == "f1":
                    dbg["hid"] = nc.dram_tensor("dbg_hid", [128, NF, NT], BF16, kind="ExternalOutput").ap()
                    C.dbg_extra = [P.dma(lambda e: e.dma_start(out=dbg["hid"][:, :, :], in_=hid[:]), reads=[(tg + "hid", f) for f in range(NF)], writes=["dbg_hid"])]
                with contextlib.ExitStack() as S2:
                    NP = 4
                    FP = NF // NP
                    wdp = [sb(tg + "wdp%d" % i, [128, FP, 256], BF16, S2) for i in range(4)]
                    py = [[[ps(tg + "py%d%d%d" % (i, mc, hf), [128, 512], F32, S2) for hf in range(2)] for mc in range(2)] for i in range(2)]
                    wdv = wd.rearrange("(fc p) m -> p fc m", p=128)
                    cnt = 0
                    for cb in range(8):
                        r = cb % 2
                        for pc in range(NP):
                            bi = cnt % 4
                            cnt += 1
                            P.dma(lambda e, cb=cb, pc=pc, bi=bi: e.dma_start(out=wdp[bi][:], in_=wdv[:, pc * FP:(pc + 1) * FP, cb * 256:(cb + 1) * 256]),
                                  writes=[tg + "wdp%d" % bi], q="pool")
                            for mc in range(2):
                                for hf in range(2):
                                    for fi in range(FP):
                                        f = pc * FP + fi
                                        P.pe(lambda e, mc=mc, hf=hf, fi=fi, f=f, bi=bi, r=r: e.matmul(
                                            py[r][mc][hf][:], lhsT=wdp[bi][:, fi, mc * 128:(mc + 1) * 128], rhs=hid[:, f, hf * 512:(hf + 1) * 512],
                                            start=(f == 0), stop=(f == NF - 1)),
                                            reads=[tg + "wdp%d" % bi, (tg + "hid", f)], writes=[tg + "py%d%d%d" % (r, mc, hf)])
                        for mc in range(2):
                            for hf in range(2):
                                m = cb * 2 + mc
                                P.act(lambda e, m=m, mc=mc, hf=hf, r=r: e.copy(out=AT[:, m, hf * 512:(hf + 1) * 512], in_=py[r][mc][hf][:]),
                                      reads=[tg + "py%d%d%d" % (r, mc, hf)], writes=[("AT", hf * 4 + q) for q in range(4)])
            if debug and tg == "f1":
                P.barrier()
                dbg["yT"] = nc.dram_tensor("dbg_yT", [128, KD, NT], BF16, kind="ExternalOutput").ap()
                C.dbg_extra.append(P.dma(lambda e: e.dma_start(out=dbg["yT"][:, :, :], in_=AT[:]), reads=[("AT", t) for t in range(NTILE)], writes=["dbg_yT"]))
            post(tg, AT, h_src, post_g, 0.5, h_dst, next_g)

        def post(tg, YT, h_src, post_g, coef, h_dst, next_g):
            P.barrier()
            with contextlib.ExitStack() as S3:
                epsT = sb(tg + "eps3", [128, 1], F32, S3)
                P.pool(lambda e: e.memset(epsT[:], EPS), writes=[tg + "eps3"])
                gpo = sb(tg + "gpo", [128, D], F32, S3)
                P.dma(lambda e: e.dma_start(out=gpo[:], in_=bcast_rows(post_g, D)), writes=[tg + "gpo"])
                gnx = None
                if next_g is not None:
                    gnx = sb(tg + "gnx", [128, D], F32, S3)
                    P.dma(lambda e: e.dma_start(out=gnx[:], in_=bcast_rows(next_g, D)), writes=[tg + "gnx"])
                scr = [dict(sq=sb(tg + "psq%d" % i, [128, D], BF16, S3), ss=sb(tg + "pss%d" % i, [128, 4], F32, S3),
                            ub=sb(tg + "pub%d" % i, [128, D], BF16, S3), pT=ps(tg + "ppT%d" % i, [128, KD, 128], BF16, S3),
                            eps=epsT) for i in range(2)]
                xt = [sb(tg + "xt%d" % i, [128, D], F32, S3) for i in range(2)]
                tt = [sb(tg + "tt%d" % i, [128, D], F32, S3) for i in range(2)]
                s2 = [sb(tg + "s2%d" % i, [128, 4], F32, S3) for i in range(2)]
                pyT = [ps(tg + "pyT%d" % i, [128, KD, 128], BF16, S3) for i in range(2)]
                ykey = YT.name
                for t in range(NTILE):
                    i = t % 2
                    P.dma(lambda e, t=t, i=i: e.dma_start(out=xt[i][:], in_=h_src[t * 128:(t + 1) * 128, :]),
                          reads=[("hd", h_src.tensor.name, t)], writes=[tg + "xt%d" % i])
                    for m in range(KD):
                        P.pe(lambda e, m=m, t=t, i=i: e.transpose(out=pyT[i][:, m, :], in_=YT[:, m, t * 128:(t + 1) * 128], identity=ident[:]),
                             reads=[(ykey, t), "ident"], writes=[tg + "pyT%d" % i])
                    yv = pyT[i][:].rearrange("p k c -> p (k c)")
                    P.act(lambda e, i=i, yv=yv: e.activation(out=scr[i]["sq"][:], in_=yv, func=AF.Square, accum_out=s2[i][:, 0:1]),
                          reads=[tg + "pyT%d" % i], writes=[tg + "n%dsq" % i, tg + "s2a%d" % i])
                    P.act(lambda e, i=i: e.activation(out=s2[i][:, 1:2], in_=s2[i][:, 0:1], func=AF.Sqrt, scale=1.0 / D, bias=epsT[:, 0:1]),
                          reads=[tg + "s2a%d" % i, tg + "eps3"], writes=[tg + "s2b%d" % i])
                    P.dve(lambda e, i=i: e.reciprocal(out=s2[i][:, 2:3], in_=s2[i][:, 1:2]), reads=[tg + "s2b%d" % i], writes=[tg + "s2c%d" % i])
                    P.dve(lambda e, i=i, yv=yv: e.scalar_tensor_tensor(out=tt[i][:], in0=yv, scalar=s2[i][:, 2:3], in1=gpo[:], op0=ALU.mult, op1=ALU.mult),
                          reads=[tg + "pyT%d" % i, tg + "s2c%d" % i, tg + "gpo"], writes=[tg + "tt%d" % i])
                    P.dve(lambda e, i=i: e.scalar_tensor_tensor(out=xt[i][:], in0=tt[i][:], scalar=float(coef), in1=xt[i][:], op0=ALU.mult, op1=ALU.add),
                          reads=[tg + "tt%d" % i, tg + "xt%d" % i], writes=[tg + "xt%d" % i])
                    o = P.dma(lambda e, t=t, i=i: e.dma_start(out=h_dst[t * 128:(t + 1) * 128, :], in_=xt[i][:]), reads=[tg + "xt%d" % i],
                              writes=[("hd", h_dst.tensor.name, t)])
                    if next_g is not None:
                        norm_transpose(S3, tg + "n%d" % i, xt[i], tg + "xt%d" % i, gnx, tg + "gnx", t, scr[i])
                    else:
                        finals.append(o)
            P.barrier()

        C.post = post
        C.x, C.pin, C.pos, C.out, C.h1d, C.h2d, C.h3d, C.dbg, C.finals = x, pin, pos, out, h1d, h2d, h3d, dbg, finals
        C.bcast_rows = bcast_rows
        C.contextlib = contextlib
        C.stop = stop
        C.debug = debug

        if stop not in (20, 21, 22):
            ffn("f1", x, W["ff1_pre_g"], W["ff1_post_g"], W["ff1_w_gate"], W["ff1_w_up"], W["ff1_w_down"], h1d, W["mix_pre_g"], True)
        if stop <= 1:
            C.dbg_extra = getattr(C, "dbg_extra", [])
            o = P.dma(lambda e: e.dma_start(out=dbg["uT"][:, :, :], in_=AT[:]), reads=[("AT", t) for t in range(NTILE)], writes=["dbg_uT"])
            o2 = P.dma(lambda e: e.dma_start(out=out[:, :], in_=h1d[:, :]), reads=[("hd", "h1d", t) for t in range(NTILE)], writes=["out"])
            P.emit(final_wait_ops=[o, o2] + C.dbg_extra)
            return nc
        if stop in (20, 21, 22):
            with contextlib.ExitStack() as S0:
                epsT = sb("eps0", [128, 1], F32, S0)
                P.pool(lambda e: e.memset(epsT[:], EPS), writes=["eps0"])
                gb = sb("gb0", [128, D], F32, S0)
                P.dma(lambda e: e.dma_start(out=gb[:], in_=bcast_rows(W["mix_pre_g"], D)), writes=["gb0"])
                scr = [dict(sq=sb("sq0%d" % i, [128, D], BF16, S0), ss=sb("ss0%d" % i, [128, 4], F32, S0), ub=sb("ub0%d" % i, [128, D], BF16, S0),
                            pT=ps("pT0%d" % i, [128, KD, 128], BF16, S0), eps=epsT) for i in range(2)]
                hts = [sb("ht0%d" % i, [128, D], F32, S0) for i in range(2)]
                for t in range(NTILE):
                    i = t % 2
                    P.dma(lambda e, t=t, i=i: e.dma_start(out=hts[i][:], in_=x[t * 128:(t + 1) * 128, :]), writes=["ht0%d" % i])
                    P.dma(lambda e, t=t, i=i: e.dma_start(out=h1d[t * 128:(t + 1) * 128, :], in_=hts[i][:]), reads=["ht0%d" % i], writes=[("hd", "h1d", t)])
                    norm_transpose(S0, "n0%d" % i, hts[i], "ht0%d" % i, gb, "gb0", t, scr[i])
            P.barrier()
        if mixer(C) == "stop":
            return nc
        if stop <= 2 or stop in (20, 22):
            o2 = P.dma(lambda e: e.dma_start(out=out[:, :], in_=h2d[:, :]), reads=[("hd", "h2d", t) for t in range(NTILE)], writes=["out"])
            P.emit(final_wait_ops=[o2] + C.dbg_ops)
            return nc
        ffn("f2", h2d, None, W["ff2_post_g"], W["ff2_w_gate"], W["ff2_w_up"], W["ff2_w_down"], h3d, W["ple_pre_g"], False)
        ple(C)
        P.emit(final_wait_ops=finals)
    return nc


WNAMES = ["ff1_pre_g", "ff1_post_g", "ff1_w_gate", "ff1_w_up", "ff1_w_down", "mix_pre_g", "mix_post_g", "w_in",
          "cmp_pos_k", "cmp_pos_v", "cmp_k_w1", "cmp_k_w2", "cmp_v_w1", "cmp_v_w2", "nsa_gate_b",
          "conv_w", "conv_b", "rg_w_a", "rg_b_a", "rg_w_i", "rg_b_i", "rg_lambda",
          "attn_out_g", "rec_out_g", "w_out", "ff2_pre_g", "ff2_post_g", "ff2_w_gate", "ff2_w_up", "ff2_w_down",
          "ple_pre_g", "ple_post_g", "w_ple_gate", "w_ple_proj"]


def shard_tokens(a, b, j):
    T = a.shape[0]
    r = a.reshape(T // 512, 4, 128, *a.shape[1:])[:, j]
    return np.ascontiguousarray(r.reshape(T // 4, *a.shape[1:]))


def make_in_maps(inputs):
    shared = {}
    for nm in WNAMES:
        a = np.asarray(inputs[nm], dtype=np.float32)[0]
        if a.ndim == 1:
            a = a.reshape(1, -1)
        if nm == "nsa_gate_b":
            a = a.reshape(1, 24)
        shared[nm] = np.ascontiguousarray(a)
    maps = []
    x = np.asarray(inputs["x"], dtype=np.float32)
    p = np.asarray(inputs["p"], dtype=np.float32)[0]
    positions = np.asarray(inputs["positions"]).astype(np.int32)
    for c in range(8):
        b, j = c // 4, c % 4
        m = dict(shared)
        m["x"] = shard_tokens(x[b], b, j)
        m["p"] = shard_tokens(p[b], b, j)
        m["pos"] = shard_tokens(positions[b], b, j).reshape(1, NT)
        maps.append(m)
    return maps


def unshard(outs):
    res = np.zeros((2, 4096, D), dtype=np.float32)
    for c in range(8):
        b, j = c // 4, c % 4
        res[b].reshape(8, 4, 128, D)[:, j] = np.asarray(outs[c]).reshape(8, 128, D)
    return res


BIGNEG = -30000.0
SCALE = 128 ** -0.5
RG = [[0, 1, 2, 3], [4, 5, 6, 7]]
GROWS = 12 * 128


def mixer(C):
    nc, P, W, sb, ps, AT, ident, identf = C.nc, C.P, C.W, C.sb, C.ps, C.AT, C.ident, C.identf
    contextlib = C.contextlib
    C.dbg_ops = []

    def cin(name, shape, dt=F32):
        return nc.dram_tensor(name, list(shape), dt, kind="ExternalInput").ap()

    c_rope = cin("c_rope", [128, 4])
    c_perm = cin("c_perm", [128, 128])
    c_oh = cin("c_oh", [128, 4])
    c_cmpm = cin("c_cmpm", [128, 2 * 8 * 128], BF16)
    c_selcm = cin("c_selcm", [128, 4 * 128], BF16)
    c_winm = cin("c_winm", [128, 8 * 128], BF16)
    c_selA = cin("c_selA", [128, 8 * 64])
    c_selB = cin("c_selB", [128, 8 * 64])
    c_selF = cin("c_selF", [128, 8 * 64])
    c_E = cin("c_E", [64, 4096], BF16)
    c_ovl = cin("c_ovl", [128, 2 * 64])
    c_selmat = cin("c_selmat", [24, 24 * 128])

    gin = nc.dram_tensor("gin", [GROWS, NT], BF16, kind="Internal").ap()
    gbuf = nc.dram_tensor("gbuf", [4 * GROWS, NT], BF16, kind="Internal").ap()
    hin = nc.dram_tensor("hin", [128, 192], F32, kind="Internal").ap()
    hbuf = nc.dram_tensor("hbuf", [4 * 128, 192], F32, kind="Internal").ap()
    sin_ = nc.dram_tensor("sin", [128, 128], F32, kind="Internal").ap()
    sbuf_ = nc.dram_tensor("sbuf", [4 * 128, 128], F32, kind="Internal").ap()

    def load(dst, src, key, q="sp", reads=()):
        return P.dma(lambda e: e.dma_start(out=dst, in_=src), reads=list(reads), writes=[key], q=q)

    with contextlib.ExitStack() as SM:
        qT = sb("qT", [128, 8, NT], BF16, SM)
        gT = sb("gT", [24, NT], F32, SM)
        orecT = sb("orecT", [128, 8, NT], BF16, SM)
        ones_bf = sb("ones_bf", [128, 128], BF16, SM)
        ones_f = sb("ones_f", [128, 128], F32, SM)
        onec = sb("onec", [128, 1], F32, SM)
        epsT = sb("m_eps", [128, 1], F32, SM)
        oh = sb("oh", [128, 4], F32, SM)
        P.pool(lambda e: e.memset(ones_bf[:], 1.0), writes=["ones_bf"])
        P.pool(lambda e: e.memset(ones_f[:], 1.0), writes=["ones_f"])
        P.pool(lambda e: e.memset(onec[:], 1.0), writes=["onec"])
        P.pool(lambda e: e.memset(epsT[:], EPS), writes=["m_eps"])
        load(oh[:], c_oh[:, :], "oh")
        C.ones_bf, C.ones_f, C.epsT = ones_bf, ones_f, epsT
        ATall = [("AT", t) for t in range(NTILE)]

        with contextlib.ExitStack() as S2:
            ZX = sb("ZX", [128, 8, NT], F32, S2)
            gy = sb("gy", [128, 8, NT], BF16, S2)
            with contextlib.ExitStack() as S2a:
                rope = sb("rope", [128, 4], F32, S2a)
                perm = sb("perm", [128, 128], F32, S2a)
                posi = sb("posi", [128, NT], I32, S2a)
                ang = sb("ang", [128, NT], F32, S2a)
                kf = sb("kf", [128, NT], F32, S2a)
                ki = sb("ki", [128, NT], I32, S2a)
                Ct = sb("Ct", [128, NT], F32, S2a)
                St = sb("St", [128, NT], F32, S2a)
                gbias = sb("gbias", [24, 1], F32, S2a)
                load(rope[:], c_rope[:, :], "rope")
                load(perm[:], c_perm[:, :], "perm")
                load(posi[:], C.bcast_rows(C.pos, NT), "posi")
                with nc.allow_non_contiguous_dma(reason="tiny param"):
                    P.dma(lambda e: e.dma_start(out=gbias[:], in_=W["nsa_gate_b"].rearrange("o n -> n o"), allow_slow_non_contiguous=True), writes=["gbias"])
                P.dve(lambda e: e.tensor_copy(out=ang[:], in_=posi[:]), reads=["posi"], writes=["ang"])
                P.dve(lambda e: e.tensor_scalar(out=ang[:], in0=ang[:], scalar1=rope[:, 0:1], scalar2=None, op0=ALU.mult), reads=["ang", "rope"], writes=["ang"])
                TWO_PI = 6.283185307179586
                C1 = 6.28125
                C2 = TWO_PI - C1
                P.dve(lambda e: e.tensor_scalar(out=kf[:], in0=ang[:], scalar1=1.0 / TWO_PI, scalar2=None, op0=ALU.mult), reads=["ang"], writes=["kf"])
                P.dve(lambda e: e.tensor_copy(out=ki[:], in_=kf[:]), reads=["kf"], writes=["ki"])
                P.dve(lambda e: e.tensor_copy(out=kf[:], in_=ki[:]), reads=["ki"], writes=["kf"])
                P.dve(lambda e: e.scalar_tensor_tensor(out=ang[:], in0=kf[:], scalar=-C1, in1=ang[:], op0=ALU.mult, op1=ALU.add), reads=["kf", "ang"], writes=["ang"])
                P.dve(lambda e: e.scalar_tensor_tensor(out=ang[:], in0=kf[:], scalar=-C2, in1=ang[:], op0=ALU.mult, op1=ALU.add), reads=["kf", "ang"], writes=["ang"])
                P.dve(lambda e: e.tensor_scalar(out=kf[:], in0=ang[:], scalar1=3.141592653589793, scalar2=-TWO_PI, op0=ALU.is_gt, op1=ALU.mult), reads=["ang"], writes=["kf"])
                P.dve(lambda e: e.tensor_tensor(out=ang[:], in0=ang[:], in1=kf[:], op=ALU.add), reads=["ang", "kf"], writes=["ang"])
                P.dve(lambda e: e.tensor_scalar(out=kf[:], in0=ang[:], scalar1=-3.141592653589793, scalar2=TWO_PI, op0=ALU.is_lt, op1=ALU.mult), reads=["ang"], writes=["kf"])
                P.dve(lambda e: e.tensor_tensor(out=ang[:], in0=ang[:], in1=kf[:], op=ALU.add), reads=["ang", "kf"], writes=["ang"])
                P.act(lambda e: e.activation(out=St[:], in_=ang[:], func=AF.Sin), reads=["ang"], writes=["St"])
                P.dve(lambda e: e.tensor_scalar(out=St[:], in0=St[:], scalar1=rope[:, 1:2], scalar2=None, op0=ALU.mult), reads=["St", "rope"], writes=["St"])
                P.act(lambda e: e.activation(out=kf[:], in_=ang[:], func=AF.Abs), reads=["ang"], writes=["kf"])
                P.dve(lambda e: e.tensor_scalar(out=kf[:], in0=kf[:], scalar1=-1.0, scalar2=1.5707963267948966, op0=ALU.mult, op1=ALU.add), reads=["kf"], writes=["kf"])
                P.act(lambda e: e.activation(out=Ct[:], in_=kf[:], func=AF.Sin), reads=["kf"], writes=["Ct"])

                wpan = [sb("wpan%d" % i, [128, KD, 512], BF16, S2a) for i in range(2)]
                zf = [sb("zf%d" % i, [128, NT], F32, S2a) for i in range(2)]
                t1 = [sb("t1%d" % i, [128, 512], F32, S2a) for i in range(2)]
                stg = [sb("stg%d" % i, [128, NT], BF16, S2a) for i in range(2)]
                pz = [[ps("pz%d%d" % (i, hf), [128, 512], F32, S2a) for hf in range(2)] for i in range(2)]
                psw = [ps("psw%d" % i, [128, 512], F32, S2a) for i in range(2)]
                pvt = [ps("pvt%d" % i, [128, 8, 128], BF16, S2a) for i in range(2)]
                wv = W["w_in"].rearrange("(kc p) m -> p kc m", p=128)
                panels = [(0, 512), (512, 512), (1024, 512), (1536, 512), (2048, 512), (2560, 24), (2584, 512), (3096, 512), (3608, 512), (4120, 512)]
                kinds = {}
                for h in range(8):
                    kinds[h * 128] = ("q", h)
                for g in range(2):
                    kinds[1024 + g * 128] = ("kf", 0 + g)
                    kinds[1280 + g * 128] = ("vf", 2 + g)
                    kinds[1536 + g * 128] = ("kf", 4 + g)
                    kinds[1792 + g * 128] = ("vt", 8 + g)
                    kinds[2048 + g * 128] = ("kf", 6 + g)
                    kinds[2304 + g * 128] = ("vt", 10 + g)
                kinds[2560] = ("g", 0)
                for h in range(8):
                    kinds[2584 + h * 128] = ("zx", h)
                    kinds[3608 + h * 128] = ("zy", h)
                cnt = 0
                for pi, (c0, wdt) in enumerate(panels):
                    b = pi % 2
                    P.dma(lambda e, b=b, c0=c0, wdt=wdt: e.dma_start(out=wpan[b][:, :, 0:wdt], in_=wv[:, :, c0:c0 + wdt]), writes=["wpan%d" % b], q="pool")
                    for ci in range(max(1, wdt // 128)):
                        col = c0 + ci * 128
                        kind, idx = kinds[col]
                        cw = 24 if kind == "g" else 128
                        r = cnt % 2
                        cnt += 1
                        for hf in range(2):
                            for k in range(KD):
                                P.pe(lambda e, k=k, hf=hf, r=r, b=b, ci=ci, cw=cw: e.matmul(
                                    pz[r][hf][0:cw, :], lhsT=wpan[b][:, k, ci * 128:ci * 128 + cw], rhs=AT[:, k, hf * 512:(hf + 1) * 512],
                                    start=(k == 0), stop=(k == KD - 1)), reads=["wpan%d" % b] + ATall[hf * 4:hf * 4 + 4], writes=["pz%d%d" % (r, hf)])
                        pzk = ["pz%d%d" % (r, 0), "pz%d%d" % (r, 1)]
                        if kind == "g":
                            for hf in range(2):
                                P.act(lambda e, hf=hf, r=r: e.activation(out=gT[:, hf * 512:(hf + 1) * 512], in_=pz[r][hf][0:24, :], func=AF.Sigmoid, bias=gbias[:, 0:1]),
                                      reads=[pzk[hf], "gbias"], writes=["gT"])
                        elif kind == "zx":
                            for hf in range(2):
                                P.act(lambda e, hf=hf, r=r, idx=idx: e.copy(out=ZX[:, idx, hf * 512:(hf + 1) * 512], in_=pz[r][hf][:]),
                                      reads=[pzk[hf]], writes=[("ZX", idx)])
                        elif kind == "zy":
                            for hf in range(2):
                                sl = slice(hf * 512, (hf + 1) * 512)
                                P.act(lambda e, hf=hf, r=r, sl=sl: e.activation(out=zf[r][:, sl], in_=pz[r][hf][:], func=AF.Square), reads=[pzk[hf]], writes=[("zf%d" % r, hf)])
                                P.dve(lambda e, r=r, sl=sl, hf=hf: e.tensor_scalar(out=zf[r][:, sl], in0=zf[r][:, sl], scalar1=0.044715, scalar2=1.0, op0=ALU.mult, op1=ALU.add), reads=[("zf%d" % r, hf)], writes=[("zf%d" % r, hf)])
                                P.dve(lambda e, hf=hf, r=r, sl=sl: e.tensor_tensor(out=zf[r][:, sl], in0=zf[r][:, sl], in1=pz[r][hf][:], op=ALU.mult), reads=[("zf%d" % r, hf), pzk[hf]], writes=[("zf%d" % r, hf)])
                                P.act(lambda e, r=r, sl=sl, hf=hf: e.activation(out=zf[r][:, sl], in_=zf[r][:, sl], func=AF.Sigmoid, scale=1.5957691216057308), reads=[("zf%d" % r, hf)], writes=[("zf%d" % r, hf)])
                                P.dve(lambda e, hf=hf, r=r, sl=sl, idx=idx: e.tensor_tensor(out=gy[:, idx, sl], in0=zf[r][:, sl], in1=pz[r][hf][:], op=ALU.mult), reads=[("zf%d" % r, hf), pzk[hf]], writes=[("gy", idx)])
                        else:
                            for hf in range(2):
                                P.act(lambda e, hf=hf, r=r: e.copy(out=zf[r][:, hf * 512:(hf + 1) * 512], in_=pz[r][hf][:]), reads=[pzk[hf]], writes=[("zf%d" % r, hf)])
                            if kind in ("q", "kf"):
                                dst = qT[:, idx, :] if kind == "q" else stg[r][:]
                                dkey = ("qT", idx) if kind == "q" else "stg%d" % r
                                for hf in range(2):
                                    sl = slice(hf * 512, (hf + 1) * 512)
                                    P.pe(lambda e, hf=hf, r=r, sl=sl: e.matmul(psw[hf][:], lhsT=perm[:], rhs=zf[r][:, sl], start=True, stop=True),
                                         reads=["perm", ("zf%d" % r, hf)], writes=["psw%d" % hf])
                                    P.dve(lambda e, hf=hf, r=r, sl=sl: e.tensor_tensor(out=t1[hf][:], in0=zf[r][:, sl], in1=Ct[:, sl], op=ALU.mult),
                                          reads=[("zf%d" % r, hf), "Ct"], writes=["t1%d" % hf])
                                    P.dve(lambda e, hf=hf, r=r, sl=sl: e.tensor_tensor(out=zf[r][:, sl], in0=psw[hf][:], in1=St[:, sl], op=ALU.mult),
                                          reads=["psw%d" % hf, "St", ("zf%d" % r, hf)], writes=[("zf%d" % r, hf)])
                                    P.dve(lambda e, hf=hf, r=r, sl=sl, dst=dst: e.tensor_tensor(out=dst[:, sl], in0=zf[r][:, sl], in1=t1[hf][:], op=ALU.add),
                                          reads=[("zf%d" % r, hf), "t1%d" % hf], writes=[dkey])
                                if kind == "kf":
                                    P.dma(lambda e, r=r, idx=idx: e.dma_start(out=gin[idx * 128:(idx + 1) * 128, :], in_=stg[r][:]), reads=["stg%d" % r], writes=[("gin", idx)])
                            elif kind == "vf":
                                P.dve(lambda e, r=r: e.tensor_copy(out=stg[r][:], in_=zf[r][:]), reads=[("zf%d" % r, 0), ("zf%d" % r, 1)], writes=["stg%d" % r])
                                P.dma(lambda e, r=r, idx=idx: e.dma_start(out=gin[idx * 128:(idx + 1) * 128, :], in_=stg[r][:]), reads=["stg%d" % r], writes=[("gin", idx)])
                            elif kind == "vt":
                                P.dve(lambda e, r=r: e.tensor_copy(out=stg[r][:], in_=zf[r][:]), reads=[("zf%d" % r, 0), ("zf%d" % r, 1)], writes=["stg%d" % r])
                                for l in range(8):
                                    P.pe(lambda e, r=r, l=l: e.transpose(out=pvt[r][:, l, :], in_=stg[r][:, l * 128:(l + 1) * 128], identity=ident[:]),
                                         reads=["stg%d" % r, "ident"], writes=["pvt%d" % r])
                                P.act(lambda e, r=r: e.copy(out=stg[r][:], in_=pvt[r][:].rearrange("p l d -> p (l d)")), reads=["pvt%d" % r], writes=["stg%d" % r])
                                P.dma(lambda e, r=r, idx=idx: e.dma_start(out=gin[idx * 128:(idx + 1) * 128, :], in_=stg[r][:]), reads=["stg%d" % r], writes=[("gin", idx)])
            P.barrier()
            with contextlib.ExitStack() as S2b:
                hst = sb("hst", [128, 8, 8, 3], F32, S2b)
                for blk in range(8):
                    P.dve(lambda e, blk=blk: e.tensor_copy(out=hst[:, blk, :, :], in_=ZX[:, blk, :].rearrange("p (l t) -> p l t", t=128)[:, :, 125:128]),
                          reads=[("ZX", blk)], writes=["hst"])
                P.dma(lambda e: e.dma_start(out=hin[:, :], in_=hst[:].rearrange("p b l t -> p (b l t)")), reads=["hst"], writes=["hin"])
                for it in range(12):
                    P.cc(lambda e, it=it: e.collective_compute("AllGather", ALU.bypass, replica_groups=RG, ins=[gin[it * 128:(it + 1) * 128, :]], outs=[gbuf[it * 512:(it + 1) * 512, :]]),
                           reads=[("gin", it)], writes=[("gbuf", it)])
                cc2 = P.cc(lambda e: e.collective_compute("AllGather", ALU.bypass, replica_groups=RG, ins=[hin[:, :]], outs=[hbuf[:, :]]), reads=["hin"], writes=["hbuf"])
            P.barrier()
            if C.stop == 21:
                C.dbg["qT"] = nc.dram_tensor("dbg_qT", [128, 8, NT], BF16, kind="ExternalOutput").ap()
                C.dbg["gT"] = nc.dram_tensor("dbg_gT", [24, NT], F32, kind="ExternalOutput").ap()
                C.dbg["gbuf"] = nc.dram_tensor("dbg_gbuf", [4 * GROWS, NT], BF16, kind="ExternalOutput").ap()
                C.dbg["ZX"] = nc.dram_tensor("dbg_ZX", [128, 8, NT], F32, kind="ExternalOutput").ap()
                C.dbg["gy"] = nc.dram_tensor("dbg_gy", [128, 8, NT], BF16, kind="ExternalOutput").ap()
                C.dbg_ops.append(P.dma(lambda e: e.dma_start(out=C.dbg["qT"][:, :, :], in_=qT[:]), reads=[("qT", h) for h in range(8)], writes=["d1"]))
                C.dbg_ops.append(P.dma(lambda e: e.dma_start(out=C.dbg["gT"][:, :], in_=gT[:]), reads=["gT"], writes=["d2"]))
                C.dbg_ops.append(P.dma(lambda e: e.dma_start(out=C.dbg["gbuf"][:, :], in_=gbuf[:, :]), reads=[("gbuf", it) for it in range(12)], writes=["d3"]))
                C.dbg_ops.append(P.dma(lambda e: e.dma_start(out=C.dbg["ZX"][:, :, :], in_=ZX[:]), reads=[("ZX", h) for h in range(8)], writes=["d4"]))
                C.dbg_ops.append(P.dma(lambda e: e.dma_start(out=C.dbg["gy"][:, :, :], in_=gy[:]), reads=[("gy", h) for h in range(8)], writes=["d5"]))
                P.emit(final_wait_ops=C.dbg_ops)
                return "stop"
            lru(C, SM, ZX, gy, orecT, hbuf, sin_, sbuf_, oh, onec)
        P.barrier()
        attention(C, SM, qT, gT, orecT, gbuf, ones_bf, ones_f, epsT,
                  dict(cmpm=c_cmpm, selcm=c_selcm, winm=c_winm, selA=c_selA, selB=c_selB, selF=c_selF, E=c_E, ovl=c_ovl, selmat=c_selmat))
    P.barrier()
    if C.stop == 22:
        C.dbg["cat"] = nc.dram_tensor("dbg_cat", [128, KD, NT], BF16, kind="ExternalOutput").ap()
        C.dbg_ops.append(P.dma(lambda e: e.dma_start(out=C.dbg["cat"][:, :, :], in_=AT[:]), reads=[("AT", t) for t in range(NTILE)], writes=["dcat"]))
        P.barrier()
    outproj(C)


def lru(C, SM, ZX, gy, orecT, hbuf, sin_, sbuf_, oh, onec):
    nc, P, W, sb, ps = C.nc, C.P, C.W, C.sb, C.ps
    contextlib = C.contextlib
    ones_bf, epsT = C.ones_bf, C.epsT
    with contextlib.ExitStack() as SL:
        Pc = sb("Pc", [128, 8, NT], F32, SL)
        cw = sb("cw", [128, 4, 8], F32, SL)
        cb = sb("cb", [128, 8], F32, SL)
        ba = sb("ba", [128, 8], F32, SL)
        bi = sb("bi", [128, 8], F32, SL)
        lam = sb("lam", [128, 8], F32, SL)
        rg = sb("rg", [128, 8], F32, SL)
        cch = sb("cch", [128, 8], F32, SL)
        wa = sb("wa", [128, 8, 128], BF16, SL)
        wi = sb("wi", [128, 8, 128], BF16, SL)
        G2 = sb("G2", [128, 4, 192], F32, SL)
        halo = sb("halo", [128, 8, 8, 3], F32, SL)
        zeros = sb("zeros", [128, 128], F32, SL)
        P.pool(lambda e: e.memset(zeros[:], 0.0), writes=["zeros"])
        with nc.allow_non_contiguous_dma(reason="tiny params"):
            for w_ in range(4):
                P.dma(lambda e, w_=w_: e.dma_start(out=cw[:, w_, :], in_=W["conv_w"][w_:w_ + 1, :].rearrange("o (b p) -> p (o b)", p=128), allow_slow_non_contiguous=True), writes=[("cw", w_)])
            for t_, nm in [(cb, "conv_b"), (ba, "rg_b_a"), (bi, "rg_b_i"), (lam, "rg_lambda"), (rg, "rec_out_g")]:
                P.dma(lambda e, t_=t_, nm=nm: e.dma_start(out=t_[:], in_=W[nm].rearrange("o (b p) -> p (o b)", p=128), allow_slow_non_contiguous=True), writes=[nm])
        P.dma(lambda e: e.dma_start(out=wa[:], in_=W["rg_w_a"].rearrange("b i j -> i b j")), writes=["wa"], q="pool")
        P.dma(lambda e: e.dma_start(out=wi[:], in_=W["rg_w_i"].rearrange("b i j -> i b j")), writes=["wi"], q="pool")
        P.dma(lambda e: e.dma_start(out=G2[:], in_=hbuf.rearrange("(r p) c -> p r c", p=128)), reads=["hbuf"], writes=["G2"])
        P.act(lambda e: e.activation(out=cch[:], in_=lam[:], func=AF.Sigmoid), reads=["rg_lambda"], writes=["cch"])
        P.act(lambda e: e.activation(out=cch[:], in_=cch[:], func=AF.Ln), reads=["cch"], writes=["cch"])
        P.dve(lambda e: e.tensor_scalar(out=cch[:], in0=cch[:], scalar1=8.0, scalar2=None, op0=ALU.mult), reads=["cch"], writes=["cch"])
        hv = halo[:].rearrange("p b l t -> p (b l t)")
        P.dve(lambda e: e.tensor_scalar(out=hv, in0=G2[:, 0, :], scalar1=oh[:, 1:2], scalar2=None, op0=ALU.mult), reads=["G2", "oh"], writes=["halo"])
        P.dve(lambda e: e.scalar_tensor_tensor(out=hv, in0=G2[:, 1, :], scalar=oh[:, 2:3], in1=hv, op0=ALU.mult, op1=ALU.add), reads=["G2", "oh", "halo"], writes=["halo"])
        P.dve(lambda e: e.scalar_tensor_tensor(out=hv, in0=G2[:, 2, :], scalar=oh[:, 3:4], in1=hv, op0=ALU.mult, op1=ALU.add), reads=["G2", "oh", "halo"], writes=["halo"])
        g3 = G2[:, 3, :].rearrange("p (b l t) -> p b l t", b=8, l=8)
        P.dve(lambda e: e.scalar_tensor_tensor(out=halo[:, :, 1:8, :], in0=g3[:, :, 0:7, :], scalar=oh[:, 0:1], in1=halo[:, :, 1:8, :], op0=ALU.mult, op1=ALU.add),
              reads=["G2", "oh", "halo"], writes=["halo"])
        with contextlib.ExitStack() as SB:
            NB = 1
            xpad = [sb("xpad%d" % i, [128, 8, 131], F32, SB) for i in range(NB)]
            xc = [sb("xc%d" % i, [128, 8, 128], F32, SB) for i in range(NB)]
            xcb = [sb("xcb%d" % i, [128, NT], BF16, SB) for i in range(NB)]
            rr = [sb("rr%d" % i, [128, NT], F32, SB) for i in range(NB)]
            ig = [sb("ig%d" % i, [128, NT], F32, SB) for i in range(NB)]
            aa = [sb("aa%d" % i, [128, NT], F32, SB) for i in range(NB)]
            uu = [sb("uu%d" % i, [128, NT], F32, SB) for i in range(NB)]
            pr = [[ps("pr%d%d" % (i, hf), [128, 512], F32, SB) for hf in range(2)] for i in range(NB)]
            pg = [[ps("pi%d%d" % (i, hf), [128, 512], F32, SB) for hf in range(2)] for i in range(NB)]
            for blk in range(8):
                i = blk % NB
                k = lambda s: "%s%d" % (s, i)
                P.pool(lambda e, i=i, blk=blk: e.tensor_copy(out=xpad[i][:, :, 0:3], in_=halo[:, blk, :, :]), reads=["halo"], writes=[k("xpad")])
                P.pool(lambda e, i=i, blk=blk: e.tensor_copy(out=xpad[i][:, :, 3:131], in_=ZX[:, blk, :].rearrange("p (l t) -> p l t", t=128)), reads=[("ZX", blk)], writes=[k("xpad")])
                P.dve(lambda e, i=i, blk=blk: e.tensor_scalar(out=xc[i][:], in0=xpad[i][:, :, 0:128], scalar1=cw[:, 0, blk:blk + 1], scalar2=cb[:, blk:blk + 1], op0=ALU.mult, op1=ALU.add),
                      reads=[k("xpad"), ("cw", 0), "conv_b"], writes=[k("xc")])
                for w in range(1, 4):
                    P.dve(lambda e, i=i, blk=blk, w=w: e.scalar_tensor_tensor(out=xc[i][:], in0=xpad[i][:, :, w:w + 128], scalar=cw[:, w, blk:blk + 1], in1=xc[i][:], op0=ALU.mult, op1=ALU.add),
                          reads=[k("xpad"), ("cw", w), k("xc")], writes=[k("xc")])
                xcf = xc[i][:].rearrange("p l t -> p (l t)")
                P.act(lambda e, i=i, xcf=xcf: e.copy(out=xcb[i][:], in_=xcf), reads=[k("xc")], writes=[k("xcb")])
                for hf in range(2):
                    sl = slice(hf * 512, (hf + 1) * 512)
                    P.pe(lambda e, i=i, blk=blk, hf=hf, sl=sl: e.matmul(pr[i][hf][:], lhsT=wa[:, blk, :], rhs=xcb[i][:, sl], start=True, stop=True), reads=["wa", k("xcb")], writes=["pr%d%d" % (i, hf)])
                    P.pe(lambda e, i=i, blk=blk, hf=hf, sl=sl: e.matmul(pg[i][hf][:], lhsT=wi[:, blk, :], rhs=xcb[i][:, sl], start=True, stop=True), reads=["wi", k("xcb")], writes=["pi%d%d" % (i, hf)])
                    P.act(lambda e, i=i, blk=blk, hf=hf, sl=sl: e.activation(out=rr[i][:, sl], in_=pr[i][hf][:], func=AF.Sigmoid, bias=ba[:, blk:blk + 1]), reads=["pr%d%d" % (i, hf), "rg_b_a"], writes=[k("rr")])
                    P.act(lambda e, i=i, blk=blk, hf=hf, sl=sl: e.activation(out=ig[i][:, sl], in_=pg[i][hf][:], func=AF.Sigmoid, bias=bi[:, blk:blk + 1]), reads=["pi%d%d" % (i, hf), "rg_b_i"], writes=[k("ig")])
                P.act(lambda e, i=i, blk=blk: e.activation(out=aa[i][:], in_=rr[i][:], func=AF.Exp, scale=cch[:, blk:blk + 1]), reads=[k("rr"), "cch"], writes=[k("aa")])
                P.dve(lambda e, i=i: e.tensor_tensor(out=uu[i][:], in0=aa[i][:], in1=aa[i][:], op=ALU.mult), reads=[k("aa")], writes=[k("uu")])
                P.act(lambda e, i=i: e.activation(out=uu[i][:], in_=uu[i][:], func=AF.Sqrt, scale=-1.0, bias=onec[:, 0:1]), reads=[k("uu"), "onec"], writes=[k("uu")])
                P.dve(lambda e, i=i: e.tensor_tensor(out=uu[i][:], in0=uu[i][:], in1=ig[i][:], op=ALU.mult), reads=[k("uu"), k("ig")], writes=[k("uu")])
                P.dve(lambda e, i=i, xcf=xcf: e.tensor_tensor(out=uu[i][:], in0=uu[i][:], in1=xcf, op=ALU.mult), reads=[k("uu"), k("xc")], writes=[k("uu")])
                for l in range(8):
                    sl = slice(l * 128, (l + 1) * 128)
                    P.dve(lambda e, i=i, blk=blk, sl=sl: e.tensor_tensor_scan(out=ZX[:, blk, sl], data0=aa[i][:, sl], data1=uu[i][:, sl], initial=0.0, op0=ALU.mult, op1=ALU.add),
                          reads=[k("aa"), k("uu"), k("xpad")], writes=[("ZX", blk)])
                    P.dve(lambda e, i=i, blk=blk, sl=sl: e.tensor_tensor_scan(out=Pc[:, blk, sl], data0=aa[i][:, sl], data1=zeros[:], initial=1.0, op0=ALU.mult, op1=ALU.add),
                          reads=[k("aa"), "zeros"], writes=[("Pc", blk)])
        P.barrier()
        with contextlib.ExitStack() as SC:
            sst = sb("sst", [128, 8, 8, 2], F32, SC)
            SG = sb("SG", [128, 4, 128], F32, SC)
            Aall = sb("Aall", [128, 8, 32], F32, SC)
            Hall = sb("Hall", [128, 8, 32], F32, SC)
            Spad = sb("Spad", [128, 8, 33], F32, SC)
            hinit = sb("hinit", [128, 8, 8], F32, SC)
            rstdb = sb("rstdb", [128, NT], F32, SC)
            sqb = [sb("sqb%d" % i, [128, NT], BF16, SC) for i in range(2)]
            pss = [ps("pss%d" % hf, [128, 512], F32, SC) for hf in range(2)]
            allZX = [("ZX", b) for b in range(8)]
            allPc = [("Pc", b) for b in range(8)]
            P.dve(lambda e: e.tensor_copy(out=sst[:, :, :, 0], in_=Pc[:].rearrange("p b (l t) -> p b l t", t=128)[:, :, :, 127]), reads=allPc, writes=["sst"])
            P.dve(lambda e: e.tensor_copy(out=sst[:, :, :, 1], in_=ZX[:].rearrange("p b (l t) -> p b l t", t=128)[:, :, :, 127]), reads=allZX, writes=["sst"])
            P.dma(lambda e: e.dma_start(out=sin_[:, :], in_=sst[:].rearrange("p b l t -> p (b l t)")), reads=["sst"], writes=["sin"])
            P.cc(lambda e: e.collective_compute("AllGather", ALU.bypass, replica_groups=RG, ins=[sin_[:, :]], outs=[sbuf_[:, :]]), reads=["sin"], writes=["sbufd"])
            P.dma(lambda e: e.dma_start(out=SG[:], in_=sbuf_.rearrange("(r p) c -> p r c", p=128)), reads=["sbufd"], writes=["SG"])
            for j in range(4):
                sgv = SG[:, j, :].rearrange("p (b l t) -> p b l t", b=8, l=8)
                P.dve(lambda e, j=j, sgv=sgv: e.tensor_copy(out=Aall[:, :, j:j + 29:4], in_=sgv[:, :, :, 0]), reads=["SG"], writes=["Aall"])
                P.dve(lambda e, j=j, sgv=sgv: e.tensor_copy(out=Hall[:, :, j:j + 29:4], in_=sgv[:, :, :, 1]), reads=["SG"], writes=["Hall"])
            P.pool(lambda e: e.memset(Spad[:], 0.0), writes=["Spad"])
            for blk in range(8):
                P.dve(lambda e, blk=blk: e.tensor_tensor_scan(out=Spad[:, blk, 1:33], data0=Aall[:, blk, :], data1=Hall[:, blk, :], initial=0.0, op0=ALU.mult, op1=ALU.add),
                      reads=["Aall", "Hall", "Spad"], writes=["Spad"])
            P.dve(lambda e: e.tensor_scalar(out=hinit[:], in0=Spad[:, :, 0:29:4], scalar1=oh[:, 0:1], scalar2=None, op0=ALU.mult), reads=["Spad", "oh"], writes=["hinit"])
            for m in range(1, 4):
                P.dve(lambda e, m=m: e.scalar_tensor_tensor(out=hinit[:], in0=Spad[:, :, m:m + 29:4], scalar=oh[:, m:m + 1], in1=hinit[:], op0=ALU.mult, op1=ALU.add),
                      reads=["Spad", "oh", "hinit"], writes=["hinit"])
            for blk in range(8):
                for l in range(8):
                    sl = slice(l * 128, (l + 1) * 128)
                    P.dve(lambda e, blk=blk, l=l, sl=sl: e.scalar_tensor_tensor(out=ZX[:, blk, sl], in0=Pc[:, blk, sl], scalar=hinit[:, blk, l:l + 1], in1=ZX[:, blk, sl], op0=ALU.mult, op1=ALU.add),
                          reads=[("Pc", blk), "hinit", ("ZX", blk)], writes=[("ZX", blk)])
                P.dve(lambda e, blk=blk: e.tensor_tensor(out=ZX[:, blk, :], in0=ZX[:, blk, :], in1=gy[:, blk, :], op=ALU.mult), reads=[("ZX", blk), ("gy", blk)], writes=[("ZX", blk)])
                i = blk % 2
                P.act(lambda e, blk=blk, i=i: e.activation(out=sqb[i][:], in_=ZX[:, blk, :], func=AF.Square), reads=[("ZX", blk)], writes=["sqb%d" % i])
                for hf in range(2):
                    P.pe(lambda e, blk=blk, i=i, hf=hf: e.matmul(pss[hf][:], lhsT=ones_bf[:], rhs=sqb[i][:, hf * 512:(hf + 1) * 512], start=(blk == 0), stop=(blk == 7)),
                         reads=["ones_bf", "sqb%d" % i], writes=["pss%d" % hf])
            for hf in range(2):
                sl = slice(hf * 512, (hf + 1) * 512)
                P.act(lambda e, hf=hf, sl=sl: e.activation(out=rstdb[:, sl], in_=pss[hf][:], func=AF.Sqrt, scale=1.0 / 1024, bias=epsT[:, 0:1]), reads=["pss%d" % hf, "m_eps"], writes=["rstdb"])
            P.dve(lambda e: e.reciprocal(out=rstdb[:], in_=rstdb[:]), reads=["rstdb"], writes=["rstdb"])
            for blk in range(8):
                P.dve(lambda e, blk=blk: e.scalar_tensor_tensor(out=orecT[:, blk, :], in0=ZX[:, blk, :], scalar=rg[:, blk:blk + 1], in1=rstdb[:], op0=ALU.mult, op1=ALU.mult),
                      reads=[("ZX", blk), "rec_out_g", "rstdb"], writes=[("orecT", blk)])
        P.barrier()


def attention(C, SM, qT, gT, orecT, gbuf, ones_bf, ones_f, epsT, K):
    nc, P, W, sb, ps, AT, ident = C.nc, C.P, C.W, C.sb, C.ps, C.AT, C.ident
    contextlib = C.contextlib

    def bc4(ap, n):
        return ap.unsqueeze(1).broadcast_to([n, 4, 128])

    with contextlib.ExitStack() as SA:
        cmpm = sb("cmpm", [128, 2, 8, 128], BF16, SA)
        selcm = sb("selcm", [128, 4, 128], BF16, SA)
        winm = sb("winm", [128, 8, 128], BF16, SA)
        selA = sb("selA", [128, 8, 64], F32, SA)
        selB = sb("selB", [128, 8, 64], F32, SA)
        selF = sb("selF", [128, 8, 64], F32, SA)
        E = sb("E", [64, 4096], BF16, SA)
        ovl = sb("ovl", [128, 2, 64], F32, SA)
        selmat = sb("selmat", [24, 24, 128], F32, SA)
        oaT = sb("oaT", [128, 8, NT], F32, SA)
        for t_, src, nm in [(cmpm, K["cmpm"], "cmpm"), (selcm, K["selcm"], "selcm"), (winm, K["winm"], "winm"), (selA, K["selA"], "selA"),
                            (selB, K["selB"], "selB"), (selF, K["selF"], "selF"), (E, K["E"], "E"), (ovl, K["ovl"], "ovl"), (selmat, K["selmat"], "selmat")]:
            fl = t_[:]
            if len(t_.shape) == 3:
                fl = t_[:].rearrange("p a b -> p (a b)")
            elif len(t_.shape) == 4:
                fl = t_[:].rearrange("p a b c -> p (a b c)")
            P.dma(lambda e, fl=fl, src=src: e.dma_start(out=fl, in_=src[:, :]), writes=[nm])

        gview = gbuf.rearrange("(i j d) (l p) -> d i l j p", j=4, i=12, d=128, l=8, p=128)
        for g in range(2):
            with contextlib.ExitStack() as SG_:
                KV = {}
                for nm, item in [("kc", 0 + g), ("vc", 2 + g), ("ks", 4 + g), ("kw", 6 + g), ("vs", 8 + g), ("vw", 10 + g)]:
                    t_ = sb("%s%d" % (nm, g), [128, 4096], BF16, SG_)
                    KV[nm] = t_
                    for l in range(8):
                        P.dma(lambda e, t_=t_, item=item, l=l: e.dma_start(out=t_[:, l * 512:(l + 1) * 512].rearrange("d (j p) -> d j p", j=4), in_=gview[:, item, l, :, :]),
                              reads=[("gbuf", item)], writes=[("kv", nm, l)])
                kvall = lambda nm: [("kv", nm, l) for l in range(8)]
                kcT = sb("kcT%d" % g, [128, 256], BF16, SG_)
                vcm = sb("vcm%d" % g, [128, 2, 128], BF16, SG_)
                with contextlib.ExitStack() as SCm:
                    w1t = sb("w1t", [128, 32, 256], BF16, SCm)
                    w2t = sb("w2t", [128, 2, 128], BF16, SCm)
                    posf = sb("posf", [128, 32], F32, SCm)
                    posb = sb("posb", [128, 32], BF16, SCm)
                    b1 = sb("b1", [128, 2], F32, SCm)
                    hx = sb("hx", [128, 256], F32, SCm)
                    hy = sb("hy", [128, 256], F32, SCm)
                    ghid = sb("ghid", [128, 2, 256], BF16, SCm)
                    ph = [ps("ph%d" % i, [128, 256], F32, SCm) for i in range(2)]
                    pb = [ps("pb%d" % i, [128, 2], F32, SCm) for i in range(2)]
                    pk = ps("pk", [128, 256], F32, SCm)
                    for kvn, src, w1n, w2n, posn in [("k", "kc", "cmp_k_w1", "cmp_k_w2", "cmp_pos_k"), ("v", "vc", "cmp_v_w1", "cmp_v_w2", "cmp_pos_v")]:
                        P.dma(lambda e, w1n=w1n: e.dma_start(out=w1t[:], in_=W[w1n].rearrange("(l d) m -> d l m", d=128)), writes=["w1t"], q="pool")
                        P.dma(lambda e, w2n=w2n: e.dma_start(out=w2t[:], in_=W[w2n].rearrange("(c h) d -> h c d", h=128)), writes=["w2t"], q="pool")
                        with nc.allow_non_contiguous_dma(reason="tiny"):
                            P.dma(lambda e, posn=posn: e.dma_start(out=posf[:], in_=W[posn].rearrange("l d -> d l"), allow_slow_non_contiguous=True), writes=["posf"])
                        P.dve(lambda e: e.tensor_copy(out=posb[:], in_=posf[:]), reads=["posf"], writes=["posb"])
                        xT = KV[src]
                        for hc in range(2):
                            for l in range(32):
                                P.pe(lambda e, hc=hc, l=l, xT=xT: e.matmul(ph[hc][:, 0:255], lhsT=w1t[:, l, hc * 128:(hc + 1) * 128], rhs=xT[:, l:l + 16 * 254 + 1:16],
                                                                   start=(l == 0), stop=(l == 31)), reads=["w1t"] + kvall(src), writes=["ph%d" % hc])
                            for l in range(32):
                                P.pe(lambda e, hc=hc, l=l: e.matmul(pb[hc][:, 0:1], lhsT=w1t[:, l, hc * 128:(hc + 1) * 128], rhs=posb[:, l:l + 1],
                                                             start=(l == 0), stop=(l == 31)), reads=["w1t", "posb"], writes=["pb%d" % hc])
                            P.act(lambda e, hc=hc: e.copy(out=b1[:, hc:hc + 1], in_=pb[hc][:, 0:1]), reads=["pb%d" % hc], writes=["b1"])
                            P.act(lambda e, hc=hc: e.activation(out=hx[:, 0:255], in_=ph[hc][:, 0:255], func=AF.Identity, bias=b1[:, hc:hc + 1]), reads=["ph%d" % hc, "b1"], writes=["hx"])
                            P.act(lambda e: e.activation(out=hy[:, 0:255], in_=hx[:, 0:255], func=AF.Square), reads=["hx"], writes=["hy"])
                            P.dve(lambda e: e.tensor_scalar(out=hy[:, 0:255], in0=hy[:, 0:255], scalar1=0.044715, scalar2=1.0, op0=ALU.mult, op1=ALU.add), reads=["hy"], writes=["hy"])
                            P.dve(lambda e: e.tensor_tensor(out=hy[:, 0:255], in0=hy[:, 0:255], in1=hx[:, 0:255], op=ALU.mult), reads=["hy", "hx"], writes=["hy"])
                            P.act(lambda e: e.activation(out=hy[:, 0:255], in_=hy[:, 0:255], func=AF.Sigmoid, scale=1.5957691216057308), reads=["hy"], writes=["hy"])
                            P.dve(lambda e, hc=hc: e.tensor_tensor(out=ghid[:, hc, 0:255], in0=hy[:, 0:255], in1=hx[:, 0:255], op=ALU.mult), reads=["hy", "hx"], writes=[("ghid", hc)])
                        if kvn == "k":
                            for hc in range(2):
                                P.pe(lambda e, hc=hc: e.matmul(pk[:, 0:255], lhsT=w2t[:, hc, :], rhs=ghid[:, hc, 0:255], start=(hc == 0), stop=(hc == 1)),
                                     reads=["w2t", ("ghid", 0), ("ghid", 1)], writes=["pk"])
                            P.act(lambda e: e.copy(out=kcT[:, 0:255], in_=pk[:, 0:255]), reads=["pk"], writes=["kcT"])
                        else:
                            for ct in range(2):
                                cn = 128 if ct == 0 else 127
                                for hc in range(2):
                                    P.pe(lambda e, hc=hc, ct=ct, cn=cn: e.matmul(pk[0:cn, ct * 128:(ct + 1) * 128], lhsT=ghid[:, hc, ct * 128:ct * 128 + cn], rhs=w2t[:, hc, :], start=(hc == 0), stop=(hc == 1)),
                                         reads=["w2t", ("ghid", 0), ("ghid", 1)], writes=["pk"])
                            P.act(lambda e: e.copy(out=vcm[:, 0, :], in_=pk[:, 0:128]), reads=["pk"], writes=["vcm"])
                            P.act(lambda e: e.copy(out=vcm[0:127, 1, :], in_=pk[0:127, 128:256]), reads=["pk"], writes=["vcm"])
                P.barrier()
                with contextlib.ExitStack() as SQ:
                    Pcf = [sb("Pcf%d" % ct, [128, 512], F32, SQ) for ct in range(2)]
                    pcb = [sb("pcb%d" % ct, [128, 512], BF16, SQ) for ct in range(2)]
                    rd = sb("rd", [128, 512], F32, SQ)
                    gS = sb("gS", [128, 512], F32, SQ)
                    wgt = sb("wgt", [128, 512], F32, SQ)
                    acc = sb("acc", [128, 512], F32, SQ)
                    tmpo = sb("tmpo", [128, 512], F32, SQ)
                    t0 = sb("t0", [128, 64], F32, SQ)
                    t1 = sb("t1s", [128, 64], F32, SQ)
                    m8a = sb("m8a", [128, 8], F32, SQ)
                    m8b = sb("m8b", [128, 8], F32, SQ)
                    selbb = sb("selbb", [128, 64], BF16, SQ)
                    selbT = sb("selbT", [64, 4, 128], BF16, SQ)
                    Pt = [sb("Pt%d" % i, [128, 512], BF16, SQ) for i in range(3)]
                    pS = [ps("pS%d" % i, [128, 512], F32, SQ) for i in range(2)]
                    pO = ps("pO", [128, 512], F32, SQ)
                    pD = ps("pD", [128, 512], F32, SQ)
                    pG = ps("pG", [128, 512], F32, SQ)
                    pI = ps("pI", [128, 64], F32, SQ)
                    pTt = ps("pTt", [64, 128], BF16, SQ)
                    scnt = [0]
                    pcnt = [0]

                    def gates(br, l):
                        for h in range(4):
                            n = (4 * g + h) * 3 + br
                            P.pe(lambda e, h=h, n=n, l=l: e.matmul(pG[:, h * 128:(h + 1) * 128], lhsT=selmat[:, n, :], rhs=gT[:, l * 128:(l + 1) * 128], start=True, stop=True),
                                 reads=["selmat", "gT"], writes=["pG"])
                        P.act(lambda e: e.copy(out=gS[:], in_=pG[:]), reads=["pG"], writes=["gS"])

                    def branch(br, l, keyT, vtok, kts, maskfn, use_sel):
                        qg = qT[:, 4 * g:4 * g + 4, l * 128:(l + 1) * 128]
                        qkeys = [("qT", 4 * g + h) for h in range(4)]
                        nk = len(kts)
                        for ii, kt in enumerate(kts):
                            si = scnt[0] % 2
                            scnt[0] += 1
                            pi_ = pcnt[0] % 3
                            pcnt[0] += 1
                            lk = ("kv", keyT[1], kt // 4)
                            P.pe(lambda e, si=si, kt=kt: e.matmul(pS[si][:], lhsT=keyT[0][:, kt * 128:(kt + 1) * 128], rhs=qg, start=True, stop=(not use_sel)),
                                 reads=[lk] + qkeys, writes=["pS%d" % si])
                            if use_sel:
                                P.pe(lambda e, si=si, kt=kt: e.matmul(pS[si][:], lhsT=E[:, kt * 128:(kt + 1) * 128], rhs=selbT[:].rearrange("s h q -> s (h q)"), start=False, stop=True),
                                     reads=["E", "selbT"], writes=["pS%d" % si])
                            P.act(lambda e, si=si, pi_=pi_: e.activation(out=Pt[pi_][:], in_=pS[si][:], func=AF.Exp, scale=SCALE), reads=["pS%d" % si], writes=["Pt%d" % pi_])
                            mk = maskfn(kt)
                            if mk is not None:
                                P.dve(lambda e, pi_=pi_, mk=mk: e.tensor_tensor(out=Pt[pi_][:].rearrange("k (h q) -> k h q", h=4), in0=Pt[pi_][:].rearrange("k (h q) -> k h q", h=4),
                                                                          in1=bc4(mk[0], 128), op=ALU.mult), reads=["Pt%d" % pi_, mk[1]], writes=["Pt%d" % pi_])
                            vk = ("kv", vtok[1], kt // 4)
                            P.pe(lambda e, pi_=pi_, kt=kt, ii=ii: e.matmul(pO[:], lhsT=vtok[0][:, kt * 128:(kt + 1) * 128], rhs=Pt[pi_][:], start=(ii == 0), stop=(ii == nk - 1)),
                                 reads=[vk, "Pt%d" % pi_], writes=["pO"])
                            P.pe(lambda e, pi_=pi_, ii=ii: e.matmul(pD[:], lhsT=ones_bf[:], rhs=Pt[pi_][:], start=(ii == 0), stop=(ii == nk - 1)),
                                 reads=["ones_bf", "Pt%d" % pi_], writes=["pD"])
                        gates(br, l)
                        P.dve(lambda e: e.tensor_scalar(out=rd[:], in0=pD[:], scalar1=1e-30, scalar2=None, op0=ALU.max), reads=["pD"], writes=["rd"])
                        P.dve(lambda e: e.reciprocal(out=rd[:], in_=rd[:]), reads=["rd"], writes=["rd"])
                        P.dve(lambda e: e.tensor_tensor(out=wgt[:], in0=rd[:], in1=gS[:], op=ALU.mult), reads=["rd", "gS"], writes=["wgt"])
                        P.dve(lambda e: e.tensor_tensor(out=tmpo[:], in0=pO[:], in1=wgt[:], op=ALU.mult), reads=["pO", "wgt"], writes=["tmpo"])
                        P.dve(lambda e: e.tensor_tensor(out=acc[:], in0=acc[:], in1=tmpo[:], op=ALU.add), reads=["acc", "tmpo"], writes=["acc"])

                    for l in range(8):
                        qg = qT[:, 4 * g:4 * g + 4, l * 128:(l + 1) * 128]
                        qkeys = [("qT", 4 * g + h) for h in range(4)]
                        for ct in range(2):
                            cn = 128 if ct == 0 else 127
                            si = scnt[0] % 2
                            scnt[0] += 1
                            P.pe(lambda e, si=si, ct=ct, cn=cn, qg=qg: e.matmul(pS[si][0:cn, :], lhsT=kcT[:, ct * 128:ct * 128 + cn], rhs=qg, start=True, stop=True),
                                 reads=["kcT"] + qkeys, writes=["pS%d" % si])
                            P.act(lambda e, si=si, ct=ct, cn=cn: e.activation(out=Pcf[ct][0:cn, :], in_=pS[si][0:cn, :], func=AF.Exp, scale=SCALE), reads=["pS%d" % si], writes=["Pcf%d" % ct])
                            P.dve(lambda e, ct=ct, cn=cn, l=l: e.tensor_tensor(out=Pcf[ct][0:cn, :].rearrange("k (h q) -> k h q", h=4), in0=Pcf[ct][0:cn, :].rearrange("k (h q) -> k h q", h=4),
                                                                     in1=bc4(cmpm[0:cn, ct, l, :], cn), op=ALU.mult), reads=["Pcf%d" % ct, "cmpm"], writes=["Pcf%d" % ct])
                        for ct in range(2):
                            cn = 128 if ct == 0 else 127
                            P.pe(lambda e, ct=ct, cn=cn: e.matmul(pD[:], lhsT=ones_f[0:cn, :], rhs=Pcf[ct][0:cn, :], start=(ct == 0), stop=(ct == 1)),
                                 reads=["ones_f", "Pcf%d" % ct], writes=["pD"])
                        P.dve(lambda e: e.tensor_scalar(out=rd[:], in0=pD[:], scalar1=1e-30, scalar2=None, op0=ALU.max), reads=["pD"], writes=["rd"])
                        P.dve(lambda e: e.reciprocal(out=rd[:], in_=rd[:]), reads=["rd"], writes=["rd"])
                        for ct in range(2):
                            cn = 128 if ct == 0 else 127
                            P.dve(lambda e, ct=ct, cn=cn: e.tensor_tensor(out=Pcf[ct][0:cn, :], in0=Pcf[ct][0:cn, :], in1=rd[0:cn, :], op=ALU.mult), reads=["Pcf%d" % ct, "rd"], writes=["Pcf%d" % ct])
                            P.act(lambda e, ct=ct, cn=cn: e.copy(out=pcb[ct][0:cn, :], in_=Pcf[ct][0:cn, :]), reads=["Pcf%d" % ct], writes=["pcb%d" % ct])
                        for ct in range(2):
                            cn = 128 if ct == 0 else 127
                            P.pe(lambda e, ct=ct, cn=cn: e.matmul(pO[:], lhsT=vcm[0:cn, ct, :], rhs=pcb[ct][0:cn, :], start=(ct == 0), stop=(ct == 1)),
                                 reads=["vcm", "pcb%d" % ct], writes=["pO"])
                        cnt8 = 0
                        for r in range(4):
                            for ct in range(2):
                                cn = 128 if ct == 0 else 127
                                P.pe(lambda e, ct=ct, cn=cn, r=r, cnt8=cnt8: e.matmul(pI[:], lhsT=Pcf[ct][0:cn, r * 128:(r + 1) * 128], rhs=ovl[0:cn, ct, :], start=(cnt8 == 0), stop=(cnt8 == 7)),
                                     reads=["Pcf%d" % ct, "ovl"], writes=["pI"])
                                cnt8 += 1
                        gates(0, l)
                        P.dve(lambda e: e.tensor_tensor(out=acc[:], in0=pO[:], in1=gS[:], op=ALU.mult), reads=["pO", "gS"], writes=["acc"])
                        dbg_here = (C.stop == 22 and g == 0 and l == 1)

                        def dump(nm, tile_, key, shape, dt=F32):
                            C.dbg[nm] = nc.dram_tensor("dbg_" + nm, list(shape), dt, kind="ExternalOutput").ap()
                            C.dbg_ops.append(P.dma(lambda e: e.dma_start(out=C.dbg[nm][:, :], in_=tile_), reads=[key], writes=["dd_" + nm]))
                        if dbg_here:
                            dump("acc0", acc[:], "acc", [128, 512])
                            dump("kcT", kcT[:], "kcT", [128, 256], BF16)
                            dump("vcm", vcm[:].rearrange("p a b -> p (a b)"), "vcm", [128, 256], BF16)
                            dump("gS0", gS[:], "gS", [128, 512])
                        P.dve(lambda e, l=l: e.tensor_tensor(out=t0[:], in0=pI[:], in1=selA[:, l, :], op=ALU.mult), reads=["pI", "selA"], writes=["t0"])
                        P.dve(lambda e, l=l: e.tensor_tensor(out=t0[:], in0=t0[:], in1=selB[:, l, :], op=ALU.add), reads=["t0", "selB"], writes=["t0"])
                        P.dve(lambda e, l=l: e.tensor_tensor(out=t0[:], in0=t0[:], in1=selF[:, l, :], op=ALU.max), reads=["t0", "selF"], writes=["t0"])
                        P.dve(lambda e: e.max(out=m8a[:], in_=t0[:]), reads=["t0"], writes=["m8a"])
                        P.dve(lambda e: e.match_replace(out=t1[:], in_to_replace=m8a[:], in_values=t0[:], imm_value=-1e30), reads=["t0", "m8a"], writes=["t1s"])
                        P.dve(lambda e: e.max(out=m8b[:], in_=t1[:]), reads=["t1s"], writes=["m8b"])
                        P.dve(lambda e: e.tensor_scalar(out=t1[:], in0=t0[:], scalar1=m8b[:, 7:8], scalar2=-1.0, op0=ALU.is_ge, op1=ALU.add), reads=["t0", "m8b"], writes=["t1s"])
                        P.dve(lambda e: e.tensor_scalar(out=selbb[:], in0=t1[:], scalar1=-BIGNEG, scalar2=None, op0=ALU.mult), reads=["t1s"], writes=["selbb"])
                        P.pe(lambda e: e.transpose(out=pTt[:], in_=selbb[:], identity=ident[:]), reads=["selbb", "ident"], writes=["pTt"])
                        for h in range(4):
                            P.act(lambda e, h=h: e.copy(out=selbT[:, h, :], in_=pTt[:]), reads=["pTt"], writes=["selbT"])
                        if C.stop == 22 and g == 0 and l == 1:
                            C.dbg["t0"] = nc.dram_tensor("dbg_t0", [128, 64], F32, kind="ExternalOutput").ap()
                            C.dbg["selb"] = nc.dram_tensor("dbg_selb", [128, 64], BF16, kind="ExternalOutput").ap()
                            C.dbg_ops.append(P.dma(lambda e: e.dma_start(out=C.dbg["t0"][:, :], in_=t0[:]), reads=["t0"], writes=["dd1"]))
                            C.dbg_ops.append(P.dma(lambda e: e.dma_start(out=C.dbg["selb"][:, :], in_=selbb[:]), reads=["selbb"], writes=["dd2"]))
                        branch(1, l, (KV["ks"], "ks"), (KV["vs"], "vs"), list(range(4 * l + 4)),
                               lambda kt, l=l: ((selcm[:, kt - 4 * l, :], "selcm") if kt >= 4 * l else None), True)
                        if dbg_here:
                            dump("acc1", acc[:], "acc", [128, 512])
                        branch(2, l, (KV["kw"], "kw"), (KV["vw"], "vw"), list(range(max(0, 4 * l - 4), 4 * l + 4)),
                               lambda kt, l=l: (winm[:, kt - 4 * l + 4, :], "winm"), False)
                        if dbg_here:
                            dump("acc2", acc[:], "acc", [128, 512])
                        for h in range(4):
                            P.act(lambda e, h=h, l=l, g=g: e.copy(out=oaT[:, 4 * g + h, l * 128:(l + 1) * 128], in_=acc[:, h * 128:(h + 1) * 128]), reads=["acc"], writes=[("oaT", 4 * g + h)])
                P.barrier()
        with contextlib.ExitStack() as SN:
            ag = sb("ag", [128, 8], F32, SN)
            rstdb = sb("a_rstdb", [128, NT], F32, SN)
            sqb = [sb("a_sqb%d" % i, [128, NT], BF16, SN) for i in range(2)]
            pss = [ps("a_pss%d" % hf, [128, 512], F32, SN) for hf in range(2)]
            with nc.allow_non_contiguous_dma(reason="tiny"):
                P.dma(lambda e: e.dma_start(out=ag[:], in_=W["attn_out_g"].rearrange("o (b p) -> p (o b)", p=128), allow_slow_non_contiguous=True), writes=["ag"])
            for h in range(8):
                i = h % 2
                P.act(lambda e, h=h, i=i: e.activation(out=sqb[i][:], in_=oaT[:, h, :], func=AF.Square), reads=[("oaT", h)], writes=["a_sqb%d" % i])
                for hf in range(2):
                    P.pe(lambda e, h=h, i=i, hf=hf: e.matmul(pss[hf][:], lhsT=ones_bf[:], rhs=sqb[i][:, hf * 512:(hf + 1) * 512], start=(h == 0), stop=(h == 7)),
                         reads=["ones_bf", "a_sqb%d" % i], writes=["a_pss%d" % hf])
            for hf in range(2):
                sl = slice(hf * 512, (hf + 1) * 512)
                P.act(lambda e, hf=hf, sl=sl: e.activation(out=rstdb[:, sl], in_=pss[hf][:], func=AF.Sqrt, scale=1.0 / 1024, bias=epsT[:, 0:1]), reads=["a_pss%d" % hf, "m_eps"], writes=["a_rstdb"])
            P.dve(lambda e: e.reciprocal(out=rstdb[:], in_=rstdb[:]), reads=["a_rstdb"], writes=["a_rstdb"])
            ATall = [("AT", t) for t in range(NTILE)]
            for h in range(8):
                P.dve(lambda e, h=h: e.scalar_tensor_tensor(out=AT[:, h, :], in0=oaT[:, h, :], scalar=ag[:, h:h + 1], in1=rstdb[:], op0=ALU.mult, op1=ALU.mult),
                      reads=[("oaT", h), "ag", "a_rstdb"], writes=ATall)
                P.pool(lambda e, h=h: e.tensor_copy(out=AT[:, 8 + h, :], in_=orecT[:, h, :]), reads=[("orecT", h)], writes=ATall)
    P.barrier()


def outproj(C):
    nc, P, W, sb, ps, AT = C.nc, C.P, C.W, C.sb, C.ps, C.AT
    contextlib = C.contextlib
    ATall = [("AT", t) for t in range(NTILE)]
    with contextlib.ExitStack() as SO:
        YT = sb("YT", [128, KD, NT], BF16, SO)
        with contextlib.ExitStack() as S1:
            wp = [sb("wo%d" % i, [128, KD, 512], BF16, S1) for i in range(2)]
            py = [[ps("po%d%d" % (i, hf), [128, 512], F32, S1) for hf in range(2)] for i in range(2)]
            wv = W["w_out"].rearrange("(kc p) m -> p kc m", p=128)
            cnt = 0
            for pi in range(4):
                b = pi % 2
                P.dma(lambda e, pi=pi, b=b: e.dma_start(out=wp[b][:], in_=wv[:, :, pi * 512:(pi + 1) * 512]), writes=["wo%d" % b], q="pool")
                for mc in range(4):
                    m = pi * 4 + mc
                    r = cnt % 2
                    cnt += 1
                    for hf in range(2):
                        for k in range(KD):
                            P.pe(lambda e, k=k, hf=hf, r=r, b=b, mc=mc: e.matmul(py[r][hf][:], lhsT=wp[b][:, k, mc * 128:(mc + 1) * 128], rhs=AT[:, k, hf * 512:(hf + 1) * 512],
                                                                         start=(k == 0), stop=(k == KD - 1)), reads=["wo%d" % b] + ATall[hf * 4:hf * 4 + 4], writes=["po%d%d" % (r, hf)])
                        P.act(lambda e, m=m, hf=hf, r=r: e.copy(out=YT[:, m, hf * 512:(hf + 1) * 512], in_=py[r][hf][:]), reads=["po%d%d" % (r, hf)],
                              writes=[("YT", hf * 4 + q) for q in range(4)])
        C.post("mx", YT, C.h1d, W["mix_post_g"], 1.0, C.h2d, W["ff2_pre_g"])


def ple(C):
    nc, P, W, sb, ps, AT, ident = C.nc, C.P, C.W, C.sb, C.ps, C.AT, C.ident
    contextlib = C.contextlib
    ATall = [("AT", t) for t in range(NTILE)]
    with contextlib.ExitStack() as SO:
        YT = sb("YTp", [128, KD, NT], BF16, SO)
        with contextlib.ExitStack() as S1:
            pT = sb("pTp", [128, 2, NT], BF16, S1)
            ptl = [sb("ptl%d" % i, [128, 256], F32, S1) for i in range(2)]
            ptb = [sb("ptb%d" % i, [128, 256], BF16, S1) for i in range(2)]
            wpj = sb("wpj", [128, 2, D], BF16, S1)
            sg = [sb("psg%d" % i, [128, 512], F32, S1) for i in range(2)]
            wp = [sb("wg%d" % i, [128, KD, 512], BF16, S1) for i in range(2)]
            ppt = [ps("ppt%d" % i, [128, 2, 128], BF16, S1) for i in range(2)]
            pa = [[ps("pa%d%d" % (i, hf), [128, 512], F32, S1) for hf in range(2)] for i in range(1)]
            pb = [[ps("pbp%d%d" % (i, hf), [128, 512], F32, S1) for hf in range(2)] for i in range(1)]
            P.dma(lambda e: e.dma_start(out=wpj[:], in_=W["w_ple_proj"].rearrange("(c p) m -> p c m", p=128)), writes=["wpj"], q="pool")
            for t in range(NTILE):
                i = t % 2
                P.dma(lambda e, t=t, i=i: e.dma_start(out=ptl[i][:], in_=C.pin[t * 128:(t + 1) * 128, :]), writes=["ptl%d" % i])
                P.dve(lambda e, i=i: e.tensor_copy(out=ptb[i][:], in_=ptl[i][:]), reads=["ptl%d" % i], writes=["ptb%d" % i])
                for c in range(2):
                    P.pe(lambda e, i=i, c=c: e.transpose(out=ppt[i][:, c, :], in_=ptb[i][:, c * 128:(c + 1) * 128], identity=ident[:]), reads=["ptb%d" % i, "ident"], writes=["ppt%d" % i])
                P.act(lambda e, i=i, t=t: e.copy(out=pT[:, :, t * 128:(t + 1) * 128], in_=ppt[i][:]), reads=["ppt%d" % i], writes=[("pTp", t)])
            wv = W["w_ple_gate"].rearrange("(kc p) m -> p kc m", p=128)
            pTall = [("pTp", t) for t in range(NTILE)]
            for pi in range(4):
                b = pi % 2
                P.dma(lambda e, pi=pi, b=b: e.dma_start(out=wp[b][:], in_=wv[:, :, pi * 512:(pi + 1) * 512]), writes=["wg%d" % b], q="pool")
                for mc in range(4):
                    m = pi * 4 + mc
                    for hf in range(2):
                        for k in range(KD):
                            P.pe(lambda e, k=k, hf=hf, b=b, mc=mc: e.matmul(pa[0][hf][:], lhsT=wp[b][:, k, mc * 128:(mc + 1) * 128], rhs=AT[:, k, hf * 512:(hf + 1) * 512],
                                                                    start=(k == 0), stop=(k == KD - 1)), reads=["wg%d" % b] + ATall[hf * 4:hf * 4 + 4], writes=["pa0%d" % hf])
                        for c in range(2):
                            P.pe(lambda e, c=c, hf=hf, m=m: e.matmul(pb[0][hf][:], lhsT=wpj[:, c, m * 128:(m + 1) * 128], rhs=pT[:, c, hf * 512:(hf + 1) * 512],
                                                              start=(c == 0), stop=(c == 1)), reads=["wpj"] + pTall[hf * 4:hf * 4 + 4], writes=["pbp0%d" % hf])
                        P.act(lambda e, hf=hf: e.activation(out=sg[hf][:], in_=pa[0][hf][:], func=AF.Sigmoid), reads=["pa0%d" % hf], writes=["psg%d" % hf])
                        P.dve(lambda e, hf=hf, m=m: e.tensor_tensor(out=YT[:, m, hf * 512:(hf + 1) * 512], in0=sg[hf][:], in1=pb[0][hf][:], op=ALU.mult),
                              reads=["psg%d" % hf, "pbp0%d" % hf], writes=[("YTp", hf * 4 + q) for q in range(4)])
        C.post("pl", YT, C.h3d, W["ple_post_g"], 1.0, C.out, None)


def host_consts(j):
    import ml_dtypes
    bf = ml_dtypes.bfloat16
    c = {}
    d = np.arange(128)
    half = 16
    inv_freq = (500000.0 ** (-np.arange(half, dtype=np.float32) / half)).astype(np.float32)
    rope = np.zeros((128, 4), np.float32)
    rope[:32, 0] = inv_freq[d[:32] % 16]
    rope[:16, 1] = -1.0
    rope[16:32, 1] = 1.0
    rope[:, 2] = 1.0
    c["c_rope"] = rope
    perm = np.zeros((128, 128), np.float32)
    for m in range(128):
        k = m + 16 if m < 16 else (m - 16 if m < 32 else m)
        perm[k, m] = 1.0
    c["c_perm"] = perm
    oh = np.zeros((128, 4), np.float32)
    oh[:, j] = 1.0
    c["c_oh"] = oh
    q = np.arange(128)
    kk = np.arange(128)
    cm = np.zeros((128, 2, 8, 128), np.float32)
    for ct in range(2):
        cg = ct * 128 + kk
        for l in range(8):
            t = (4 * l + j) * 128 + q
            cm[:, ct, l, :] = ((16 * cg[:, None] + 31 <= t[None, :]) & (cg[:, None] < 255))
    c["c_cmpm"] = cm.reshape(128, -1).astype(bf)
    tri = (kk[:, None] <= q[None, :]).astype(np.float32)
    triu = (kk[:, None] > q[None, :]).astype(np.float32)
    sc = np.zeros((128, 4, 128), np.float32)
    for m in range(4):
        sc[:, m, :] = 1.0 if m < j else (tri if m == j else 0.0)
    c["c_selcm"] = sc.reshape(128, -1).astype(bf)
    wm = np.zeros((128, 8, 128), np.float32)
    for m in range(8):
        dd = m - 4 - j
        if dd == 0:
            wm[:, m, :] = tri
        elif dd == -4:
            wm[:, m, :] = triu
        elif -4 < dd < 0:
            wm[:, m, :] = 1.0
    c["c_winm"] = wm.reshape(128, -1).astype(bf)
    sA = np.zeros((128, 8, 64), np.float32)
    sB = np.zeros((128, 8, 64), np.float32)
    sF = np.zeros((128, 8, 64), np.float32)
    s = np.arange(64)
    for l in range(8):
        t = (4 * l + j) * 128 + q
        valid = (64 * s[None, :] <= t[:, None])
        forced = (s[None, :] == (t // 64)[:, None]) | (s[None, :] == 0)
        sA[:, l, :] = valid
        sB[:, l, :] = (valid.astype(np.float32) - 1.0) * 1e4
        sF[:, l, :] = np.where(forced, 1e4, -3e4)
    c["c_selA"] = sA.reshape(128, -1)
    c["c_selB"] = sB.reshape(128, -1)
    c["c_selF"] = sF.reshape(128, -1)
    key = np.arange(4096)
    c["c_E"] = (key[None, :] // 64 == s[:, None]).astype(np.float32).astype(bf)
    ov = np.zeros((128, 2, 64), np.float32)
    for ct in range(2):
        cg = ct * 128 + kk
        st_ = 16 * cg
        en_ = st_ + 31
        ov[:, ct, :] = ((st_[:, None] < 64 * s[None, :] + 64) & (en_[:, None] >= 64 * s[None, :]) & (cg[:, None] < 255))
    c["c_ovl"] = ov.reshape(128, -1)
    sm = np.zeros((24, 24, 128), np.float32)
    for n in range(24):
        sm[n, n, :] = 1.0
    c["c_selmat"] = sm.reshape(24, -1)
    return c


_orig_make_in_maps = make_in_maps


def make_in_maps(inputs):
    maps = _orig_make_in_maps(inputs)
    for c in range(8):
        maps[c].update(host_consts(c % 4))
    return maps


_NC_CACHE = {}


def kernel(**inputs):
    maps = make_in_maps(inputs)
    if "nc" not in _NC_CACHE:
        _NC_CACHE["nc"] = build()
    nc = _NC_CACHE["nc"]
    res = run_bass_kernel_spmd(nc, maps, core_ids=list(range(8)))
    return unshard([r["out"] for r in res.results])
```

```python
import numpy as np
import concourse.bass as bass
import concourse.mybir as mybir
from concourse.bass_utils import run_bass_kernel_spmd

F32 = mybir.dt.float32
BF16 = mybir.dt.bfloat16
I32 = mybir.dt.int32
AF = mybir.ActivationFunctionType
ALU = mybir.AluOpType
AX = mybir.AxisListType

COMPUTE = ("pe", "act", "dve", "pool")
NRING = 12


class Op:
    __slots__ = ("eng", "fn", "deps", "is_dma", "signal", "idx", "ring", "ringval", "prev_ring", "is_cc", "seg", "dur", "fin", "bdeps")

    def __init__(self, eng, fn, is_dma):
        self.eng = eng
        self.fn = fn
        self.is_dma = is_dma
        self.deps = []
        self.bdeps = []
        self.signal = 0
        self.ring = None
        self.ringval = 0
        self.prev_ring = None
        self.idx = 0
        self.is_cc = False
        self.seg = 0
        self.dur = 0.5
        self.fin = 0.0


DEFAULT_DUR = {"pe": 0.28, "act": 0.7, "dve": 0.9, "pool": 1.2, "sp": 2.5}
SCHED = True
WINDOW = 48


class Prog:
    def __init__(self, nc):
        self.nc = nc
        self.ops = {k: [] for k in ("pe", "act", "dve", "pool", "sp")}
        self.last_w = {}
        self.readers = {}
        self.nops = 0
        self.seg = 0

    def barrier(self):
        self.seg += 1

    def _add(self, eng, fn, reads, writes, is_dma, d=None):
        op = Op(eng, fn, is_dma)
        op.idx = self.nops
        op.seg = self.seg
        op.dur = DEFAULT_DUR[eng] if d is None else d
        self.nops += 1
        deps = {}
        for r in reads:
            w = self.last_w.get(r)
            if w is not None:
                deps[id(w)] = w
        for wr in writes:
            w = self.last_w.get(wr)
            if w is not None:
                deps[id(w)] = w
            for rd in self.readers.get(wr, ()):
                deps[id(rd)] = rd
        for dd in deps.values():
            if dd is op:
                continue
            op.deps.append(dd)
        for r in reads:
            self.readers.setdefault(r, []).append(op)
        for wr in writes:
            self.last_w[wr] = op
            self.readers[wr] = []
        self.ops[eng].append(op)
        return op

    def pe(self, fn, reads=(), writes=(), d=None):
        return self._add("pe", fn, reads, writes, False, d)

    def act(self, fn, reads=(), writes=(), d=None):
        return self._add("act", fn, reads, writes, False, d)

    def dve(self, fn, reads=(), writes=(), d=None):
        return self._add("dve", fn, reads, writes, False, d)

    def pool(self, fn, reads=(), writes=(), d=None):
        return self._add("pool", fn, reads, writes, False, d)

    def dma(self, fn, reads=(), writes=(), q="sp", d=None):
        return self._add(q, fn, reads, writes, True, d)

    def cc(self, fn, reads=(), writes=()):
        op = self._add("pool", fn, reads, writes, True, 15.0)
        op.is_cc = True
        return op

    def _schedule(self):
        nseg = self.seg + 1
        byseg = [{e: [] for e in self.ops} for _ in range(nseg)]
        for e, lst in self.ops.items():
            for op in lst:
                byseg[op.seg][e].append(op)
        new = {e: [] for e in self.ops}
        LAT_X, LAT_S = 1.2, 0.25
        t_base = 0.0
        for sg in range(nseg):
            queues = byseg[sg]
            if not SCHED:
                for e in queues:
                    new[e].extend(queues[e])
                continue
            pos = {e: 0 for e in queues}
            done = set()
            sched_flag = {e: [False] * len(queues[e]) for e in queues}
            free_at = {e: t_base for e in queues}
            remaining = sum(len(q) for q in queues.values())
            tmax = t_base
            while remaining:
                best = None
                for e, q in queues.items():
                    n = len(q)
                    p = pos[e]
                    while p < n and sched_flag[e][p]:
                        p += 1
                    pos[e] = p
                    cnt = 0
                    i = p
                    while i < n and cnt < WINDOW:
                        if not sched_flag[e][i]:
                            cnt += 1
                            op = q[i]
                            ok = True
                            rdy = free_at[e]
                            for dd in op.deps:
                                if dd.seg == sg:
                                    if id(dd) not in done:
                                        ok = False
                                        break
                                    lat = LAT_S if (dd.eng == e and not dd.is_dma) else LAT_X
                                    if dd.eng == "pe" and e == "pe":
                                        lat = 0.0
                                    tt = dd.fin + lat
                                    if tt > rdy:
                                        rdy = tt
                            if ok:
                                key = (rdy, op.idx)
                                if best is None or key < best[0]:
                                    best = (key, e, i, op)
                                if rdy <= free_at[e]:
                                    break
                        i += 1
                (rdy, _), e, i, op = best
                if op.is_dma:
                    free_at[e] = rdy + 0.15
                    op.fin = rdy + op.dur
                else:
                    free_at[e] = rdy + op.dur
                    op.fin = free_at[e]
                tmax = max(tmax, op.fin)
                sched_flag[e][i] = True
                done.add(id(op))
                new[e].append(op)
                remaining -= 1
            t_base = tmax + 2.0
        self.ops = new

    def emit(self, final_wait_ops=()):
        nc = self.nc
        self._schedule()
        nseg = self.seg + 1
        last_compute = {}
        first_in_seg = {}
        dmas_in_seg = [[] for _ in range(nseg)]
        lastc_upto = [dict() for _ in range(nseg)]
        for e, lst in self.ops.items():
            for op in lst:
                if (op.seg, e) not in first_in_seg:
                    first_in_seg[(op.seg, e)] = op
                if op.is_dma:
                    dmas_in_seg[op.seg].append(op)
                else:
                    lastc_upto[op.seg][e] = op
        run_last = {}
        pend = {e: [] for e in self.ops}
        for sg in range(1, nseg):
            for e2, op2 in lastc_upto[sg - 1].items():
                run_last[e2] = op2
            bd = list(run_last.values()) + dmas_in_seg[sg - 1]
            for e in self.ops:
                pend[e] = pend[e] + bd
                f = first_in_seg.get((sg, e))
                if f is not None:
                    f.bdeps = [d for d in pend[e] if not (d.eng == e and not d.is_dma and not f.is_dma)]
                    pend[e] = []
        needed = set()
        for e, lst in self.ops.items():
            for op in lst:
                for d in op.deps:
                    if not (d.eng == "pe" and e == "pe" and not d.is_dma):
                        needed.add(id(d))
                for d in op.bdeps:
                    needed.add(id(d))
        for op in final_wait_ops:
            needed.add(id(op))
        ringcount = {}
        for e, lst in self.ops.items():
            n = 0
            k = 0
            last_on_ring = {}
            for op in lst:
                if id(op) not in needed:
                    continue
                if op.is_cc:
                    op.ring = ("cc", op.idx)
                    op.ringval = 1
                    op.prev_ring = None
                elif op.is_dma:
                    slot = k % NRING
                    k += 1
                    op.ring = (e, slot)
                    ringcount[(e, slot)] = ringcount.get((e, slot), 0) + 16
                    op.ringval = ringcount[(e, slot)]
                    op.prev_ring = last_on_ring.get(slot)
                    last_on_ring[slot] = op
                else:
                    n += 1
                    op.signal = n
        import contextlib
        with contextlib.ExitStack() as st:
            csem = {e: st.enter_context(nc.semaphore("s_" + e)) for e in ("pe", "act", "dve", "pool")}
            rsem = {}
            for e in ("sp", "pool"):
                for s_ in range(NRING):
                    rsem[(e, s_)] = st.enter_context(nc.semaphore("r_%s_%d" % (e, s_)))
            for e, lst in self.ops.items():
                for op in lst:
                    if op.is_cc and op.ring is not None:
                        rsem[op.ring] = st.enter_context(nc.semaphore("cc_%d" % op.idx))
            block = st.enter_context(nc.Block())

            def run(ename):
                def body(eng):
                    waited = {}
                    lst = self.ops[ename]
                    for op in lst:
                        deps = [d for d in op.deps if not (d.eng == "pe" and ename == "pe" and not d.is_dma)] + list(op.bdeps)
                        if op.is_dma and op.prev_ring is not None:
                            deps.append(op.prev_ring)
                        need = {}
                        for d in deps:
                            if d.is_dma:
                                key = ("r",) + d.ring
                                val = d.ringval
                                sem = rsem[d.ring]
                            else:
                                key = ("c", d.eng)
                                val = d.signal
                                sem = csem[d.eng]
                            assert val > 0, (ename, d.eng)
                            if need.get(key, (0, None))[0] < val:
                                need[key] = (val, sem)
                        for key, (val, sem) in need.items():
                            if waited.get(key, 0) >= val:
                                continue
                            waited[key] = val
                            eng.wait_ge(sem, val)
                        ins = op.fn(eng)
                        if op.is_cc:
                            if op.ring is not None:
                                ins.then_inc(rsem[op.ring], 1)
                        elif op.is_dma:
                            if op.ring is not None:
                                ins.then_inc(rsem[op.ring], 16)
                        elif op.signal:
                            ins.then_inc(csem[ename], 1)
                    if ename == "sp":
                        for d in final_wait_ops:
                            if d.is_dma:
                                eng.wait_ge(rsem[d.ring], d.ringval)
                            else:
                                eng.wait_ge(csem[d.eng], d.signal)
                return body

            block.tensor(run("pe"))
            block.scalar(run("act"))
            block.vector(run("dve"))
            block.gpsimd(run("pool"))
            block.sync(run("sp"))


D = 2048
NT = 1024
NTILE = 8
DFF = 5632
NF = DFF // 128
KD = D // 128
EPS = 1e-6
IN_WIDTH = 4632


def bcast_rows(ap, n, parts=128):
    return bass.AP(ap.tensor, ap.offset, [[0, parts], [1, n]])


class Ctx:
    pass


def build(stop=99, debug=False):
    import contextlib
    nc = bass.Bass("TRN2", target_bir_lowering=False)
    C = Ctx()
    C.nc = nc
    P = Prog(nc)
    C.P = P

    def din(name, shape, dt=F32):
        return nc.dram_tensor(name, list(shape), dt, kind="ExternalInput").ap()

    x = din("x", [NT, D])
    pin = din("p", [NT, 256])
    pos = din("pos", [1, NT], I32)
    WSHAPES = dict([("ff1_pre_g", [1, D]), ("ff1_post_g", [1, D]), ("ff1_w_gate", [D, DFF]), ("ff1_w_up", [D, DFF]),
                    ("ff1_w_down", [DFF, D]), ("mix_pre_g", [1, D]), ("mix_post_g", [1, D]), ("w_in", [D, IN_WIDTH]),
                    ("cmp_pos_k", [32, 128]), ("cmp_pos_v", [32, 128]), ("cmp_k_w1", [4096, 256]), ("cmp_k_w2", [256, 128]),
                    ("cmp_v_w1", [4096, 256]), ("cmp_v_w2", [256, 128]), ("nsa_gate_b", [1, 24]),
                    ("conv_w", [4, 1024]), ("conv_b", [1, 1024]), ("rg_w_a", [8, 128, 128]), ("rg_b_a", [1, 1024]),
                    ("rg_w_i", [8, 128, 128]), ("rg_b_i", [1, 1024]), ("rg_lambda", [1, 1024]),
                    ("attn_out_g", [1, 1024]), ("rec_out_g", [1, 1024]), ("w_out", [D, D]),
                    ("ff2_pre_g", [1, D]), ("ff2_post_g", [1, D]), ("ff2_w_gate", [D, DFF]), ("ff2_w_up", [D, DFF]),
                    ("ff2_w_down", [DFF, D]), ("ple_pre_g", [1, D]), ("ple_post_g", [1, D]),
                    ("w_ple_gate", [D, D]), ("w_ple_proj", [256, D])])

    class LazyW(dict):
        def __missing__(self, nm):
            v = din(nm, WSHAPES[nm])
            self[nm] = v
            return v
    W = LazyW()
    C.W = W
    out = nc.dram_tensor("out", [NT, D], F32, kind="ExternalOutput").ap()
    h1d = nc.dram_tensor("h1d", [NT, D], F32, kind="Internal").ap()
    h2d = nc.dram_tensor("h2d", [NT, D], F32, kind="Internal").ap()
    h3d = nc.dram_tensor("h3d", [NT, D], F32, kind="Internal").ap()
    dbg = {}
    if debug:
        dbg["uT"] = nc.dram_tensor("dbg_uT", [128, KD, NT], BF16, kind="ExternalOutput").ap()

    finals = []
    with contextlib.ExitStack() as st:
        used_names = {}

        def uniq(name):
            n = used_names.get(name, 0)
            used_names[name] = n + 1
            return name if n == 0 else "%s_v%d" % (name, n)

        def sb(name, shape, dt, stack=st):
            return stack.enter_context(nc.sbuf_tensor(uniq(name), list(shape), dt))

        def ps(name, shape, dt, stack=st):
            return stack.enter_context(nc.psum_tensor(uniq(name), list(shape), dt))

        identf = sb("identf", [128, 128], F32)
        ident = sb("ident", [128, 128], BF16)
        AT = sb("AT", [128, KD, NT], BF16)
        P.pool(lambda e: e.memset(identf[:], 1.0), writes=["identf"])
        P.pool(lambda e: e.affine_select(out=identf[:], in_=identf[:], pattern=[[-1, 128]], compare_op=ALU.is_equal,
                                         fill=0.0, base=0, channel_multiplier=1), reads=["identf"], writes=["identf"])
        P.dve(lambda e: e.tensor_copy(out=ident[:], in_=identf[:]), reads=["identf"], writes=["ident"])
        C.ident, C.identf, C.AT = ident, identf, AT
        C.sb, C.ps = sb, ps

        def norm_transpose(S, tg, ht, hkey, gb, gkey, t, scr):
            sq, ss, ub, pT = scr["sq"], scr["ss"], scr["ub"], scr["pT"]
            P.act(lambda e: e.activation(out=sq[:], in_=ht[:], func=AF.Square, accum_out=ss[:, 0:1]),
                  reads=[hkey], writes=[tg + "sq", tg + "ss"])
            P.act(lambda e: e.activation(out=ss[:, 1:2], in_=ss[:, 0:1], func=AF.Sqrt, scale=1.0 / D, bias=scr["eps"][:, 0:1]),
                  reads=[tg + "ss", scr["epskey"]], writes=[tg + "ss1"])
            P.dve(lambda e: e.reciprocal(out=ss[:, 2:3], in_=ss[:, 1:2]), reads=[tg + "ss1"], writes=[tg + "ss2"])
            P.dve(lambda e: e.scalar_tensor_tensor(out=ub[:], in0=ht[:], scalar=ss[:, 2:3], in1=gb[:], op0=ALU.mult, op1=ALU.mult),
                  reads=[hkey, tg + "ss2", gkey], writes=[tg + "ub"])
            for k in range(KD):
                P.pe(lambda e, k=k: e.transpose(out=pT[:, k, :], in_=ub[:, k * 128:(k + 1) * 128], identity=ident[:]),
                     reads=[tg + "ub", "ident"], writes=[tg + "pT"])
            P.act(lambda e: e.copy(out=AT[:, :, t * 128:(t + 1) * 128], in_=pT[:]), reads=[tg + "pT"], writes=[("AT", t)])

        C.norm_transpose = norm_transpose

        def ffn(tg, h_src, pre_g, post_g, wg, wu, wd, h_dst, next_g, first):
            with contextlib.ExitStack() as S:
                hid = sb(tg + "hid", [128, NF, NT], BF16, S)
                epsT = sb(tg + "eps", [128, 1], F32, S)
                P.pool(lambda e: e.memset(epsT[:], EPS), writes=[tg + "eps"])
                if first:
                    with contextlib.ExitStack() as S0:
                        gb = sb(tg + "gb", [128, D], F32, S0)
                        P.dma(lambda e: e.dma_start(out=gb[:], in_=bcast_rows(pre_g, D)), writes=[tg + "gb"])
                        scr = [dict(sq=sb(tg + "sq%d" % i, [128, D], BF16, S0), ss=sb(tg + "ss%d" % i, [128, 4], F32, S0),
                                    ub=sb(tg + "ub%d" % i, [128, D], BF16, S0), pT=ps(tg + "pT%d" % i, [128, KD, 128], BF16, S0),
                                    eps=epsT, epskey=tg + "eps") for i in range(2)]
                        hts = [sb(tg + "ht%d" % i, [128, D], F32, S0) for i in range(2)]
                        for t in range(NTILE):
                            i = t % 2
                            P.dma(lambda e, t=t, i=i: e.dma_start(out=hts[i][:], in_=h_src[t * 128:(t + 1) * 128, :]),
                                  writes=[tg + "ht%d" % i])
                            norm_transpose(S0, tg + "n%d" % i, hts[i], tg + "ht%d" % i, gb, tg + "gb", t, scr[i])
                P.barrier()
                with contextlib.ExitStack() as S1:
                    wgp = [sb(tg + "wgp%d" % i, [128, KD, 512], BF16, S1) for i in range(2)]
                    wup = [sb(tg + "wup%d" % i, [128, KD, 512], BF16, S1) for i in range(2)]
                    sgt = [sb(tg + "sg%d" % i, [128, 512], BF16, S1) for i in range(2)]
                    pg = [[ps(tg + "pg%d%d" % (i, hf), [128, 512], F32, S1) for hf in range(2)] for i in range(2)]
                    pu = [[ps(tg + "pu%d%d" % (i, hf), [128, 512], F32, S1) for hf in range(2)] for i in range(2)]
                    wgv = wg.rearrange("(kc p) m -> p kc m", p=128)
                    wuv = wu.rearrange("(kc p) m -> p kc m", p=128)
                    ATall = [("AT", t) for t in range(NTILE)]
                    for pi in range(NF // 4):
                        b = pi % 2
                        P.dma(lambda e, pi=pi, b=b: e.dma_start(out=wgp[b][:], in_=wgv[:, :, pi * 512:(pi + 1) * 512]),
                              writes=[tg + "wgp%d" % b], q="pool")
                        P.dma(lambda e, pi=pi, b=b: e.dma_start(out=wup[b][:], in_=wuv[:, :, pi * 512:(pi + 1) * 512]),
                              writes=[tg + "wup%d" % b], q="pool")
                        for fl in range(4):
                            f = pi * 4 + fl
                            r = f % 2
                            for hf in range(2):
                                for k in range(KD):
                                    P.pe(lambda e, k=k, hf=hf, r=r, b=b, fl=fl: e.matmul(
                                        pg[r][hf][:], lhsT=wgp[b][:, k, fl * 128:(fl + 1) * 128], rhs=AT[:, k, hf * 512:(hf + 1) * 512],
                                        start=(k == 0), stop=(k == KD - 1)),
                                        reads=[tg + "wgp%d" % b] + ATall[hf * 4:hf * 4 + 4], writes=[tg + "pg%d%d" % (r, hf)])
                                for k in range(KD):
                                    P.pe(lambda e, k=k, hf=hf, r=r, b=b, fl=fl: e.matmul(
                                        pu[r][hf][:], lhsT=wup[b][:, k, fl * 128:(fl + 1) * 128], rhs=AT[:, k, hf * 512:(hf + 1) * 512],
                                        start=(k == 0), stop=(k == KD - 1)),
                                        reads=[tg + "wup%d" % b] + ATall[hf * 4:hf * 4 + 4], writes=[tg + "pu%d%d" % (r, hf)])
                                P.act(lambda e, hf=hf, r=r: e.activation(out=sgt[hf][:], in_=pg[r][hf][:], func=AF.Silu),
                                      reads=[tg + "pg%d%d" % (r, hf)], writes=[tg + "sg%d" % hf])
                                P.dve(lambda e, hf=hf, r=r, f=f: e.tensor_tensor(out=hid[:, f, hf * 512:(hf + 1) * 512], in0=sgt[hf][:],
                                                                               in1=pu[r][hf][:], op=ALU.mult),
                                      reads=[tg + "sg%d" % hf, tg + "pu%d%d" % (r, hf)], writes=[(tg + "hid", f)])
                P.barrier()
                if debug and tg == "f1":
                    dbg["hid"] = nc.dram_tensor("dbg_hid", [128, NF, NT], BF16, kind="ExternalOutput").ap()
                    C.dbg_extra = [P.dma(lambda e: e.dma_start(out=dbg["hid"][:, :, :], in_=hid[:]), reads=[(tg + "hid", f) for f in range(NF)], writes=["dbg_hid"])]
                with contextlib.ExitStack() as S2:
                    NP = 4
                    FP = NF // NP
                    wdp = [sb(tg + "wdp%d" % i, [128, FP, 256], BF16, S2) for i in range(4)]
                    py = [[[ps(tg + "py%d%d%d" % (i, mc, hf), [128, 512], F32, S2) for hf in range(2)] for mc in range(2)] for i in range(2)]
                    wdv = wd.rearrange("(fc p) m -> p fc m", p=128)
                    cnt = 0
                    for cb in range(8):
                        r = cb % 2
                        for pc in range(NP):
                            bi = cnt % 4
                            cnt += 1
                            P.dma(lambda e, cb=cb, pc=pc, bi=bi: e.dma_start(out=wdp[bi][:], in_=wdv[:, pc * FP:(pc + 1) * FP, cb * 256:(cb + 1) * 256]),
                                  writes=[tg + "wdp%d" % bi], q="pool")
                            for mc in range(2):
                                for hf in range(2):
                                    for fi in range(FP):
                                        f = pc * FP + fi
                                        P.pe(lambda e, mc=mc, hf=hf, fi=fi, f=f, bi=bi, r=r: e.matmul(
                                            py[r][mc][hf][:], lhsT=wdp[bi][:, fi, mc * 128:(mc + 1) * 128], rhs=hid[:, f, hf * 512:(hf + 1) * 512],
                                            start=(f == 0), stop=(f == NF - 1)),
                                            reads=[tg + "wdp%d" % bi, (tg + "hid", f)], writes=[tg + "py%d%d%d" % (r, mc, hf)])
                        for mc in range(2):
                            for hf in range(2):
                                m = cb * 2 + mc
                                P.act(lambda e, m=m, mc=mc, hf=hf, r=r: e.copy(out=AT[:, m, hf * 512:(hf + 1) * 512], in_=py[r][mc][hf][:]),
                                      reads=[tg + "py%d%d%d" % (r, mc, hf)], writes=[("AT", hf * 4 + q) for q in range(4)])
            if debug and tg == "f1":
                P.barrier()
                dbg["yT"] = nc.dram_tensor("dbg_yT", [128, KD, NT], BF16, kind="ExternalOutput").ap()
                C.dbg_extra.append(P.dma(lambda e: e.dma_start(out=dbg["yT"][:, :, :], in_=AT[:]), reads=[("AT", t) for t in range(NTILE)], writes=["dbg_yT"]))
            post(tg, AT, h_src, post_g, 0.5, h_dst, next_g)

        def post(tg, YT, h_src, post_g, coef, h_dst, next_g):
            P.barrier()
            with contextlib.ExitStack() as S3:
                epsT = sb(tg + "eps3", [128, 1], F32, S3)
                P.pool(lambda e: e.memset(epsT[:], EPS), writes=[tg + "eps3"])
                gpo = sb(tg + "gpo", [128, D], F32, S3)
                P.dma(lambda e: e.dma_start(out=gpo[:], in_=bcast_rows(post_g, D)), writes=[tg + "gpo"])
                gnx = None
                if next_g is not None:
                    gnx = sb(tg + "gnx", [128, D], F32, S3)
                    P.dma(lambda e: e.dma_start(out=gnx[:], in_=bcast_rows(next_g, D)), writes=[tg + "gnx"])
                scr = [dict(sq=sb(tg + "psq%d" % i, [128, D], BF16, S3), ss=sb(tg + "pss%d" % i, [128, 4], F32, S3),
                            ub=sb(tg + "pub%d" % i, [128, D], BF16, S3), pT=ps(tg + "ppT%d" % i, [128, KD, 128], BF16, S3),
                            eps=epsT, epskey=tg + "eps3") for i in range(2)]
                xt = [sb(tg + "xt%d" % i, [128, D], F32, S3) for i in range(2)]
                tt = [sb(tg + "tt%d" % i, [128, D], F32, S3) for i in range(2)]
                s2 = [sb(tg + "s2%d" % i, [128, 4], F32, S3) for i in range(2)]
                pyT = [ps(tg + "pyT%d" % i, [128, KD, 128], BF16, S3) for i in range(2)]
                ykey = YT.name
                for t in range(NTILE):
                    i = t % 2
                    P.dma(lambda e, t=t, i=i: e.dma_start(out=xt[i][:], in_=h_src[t * 128:(t + 1) * 128, :]),
                          reads=[("hd", h_src.tensor.name, t)], writes=[tg + "xt%d" % i])
                    for m in range(KD):
                        P.pe(lambda e, m=m, t=t, i=i: e.transpose(out=pyT[i][:, m, :], in_=YT[:, m, t * 128:(t + 1) * 128], identity=ident[:]),
                             reads=[(ykey, t), "ident"], writes=[tg + "pyT%d" % i])
                    yv = pyT[i][:].rearrange("p k c -> p (k c)")
                    P.act(lambda e, i=i, yv=yv: e.activation(out=scr[i]["sq"][:], in_=yv, func=AF.Square, accum_out=s2[i][:, 0:1]),
                          reads=[tg + "pyT%d" % i], writes=[tg + "n%dsq" % i, tg + "s2a%d" % i])
                    P.act(lambda e, i=i: e.activation(out=s2[i][:, 1:2], in_=s2[i][:, 0:1], func=AF.Sqrt, scale=1.0 / D, bias=epsT[:, 0:1]),
                          reads=[tg + "s2a%d" % i, tg + "eps3"], writes=[tg + "s2b%d" % i])
                    P.dve(lambda e, i=i: e.reciprocal(out=s2[i][:, 2:3], in_=s2[i][:, 1:2]), reads=[tg + "s2b%d" % i], writes=[tg + "s2c%d" % i])
                    P.dve(lambda e, i=i, yv=yv: e.scalar_tensor_tensor(out=tt[i][:], in0=yv, scalar=s2[i][:, 2:3], in1=gpo[:], op0=ALU.mult, op1=ALU.mult),
                          reads=[tg + "pyT%d" % i, tg + "s2c%d" % i, tg + "gpo"], writes=[tg + "tt%d" % i])
                    P.dve(lambda e, i=i: e.scalar_tensor_tensor(out=xt[i][:], in0=tt[i][:], scalar=float(coef), in1=xt[i][:], op0=ALU.mult, op1=ALU.add),
                          reads=[tg + "tt%d" % i, tg + "xt%d" % i], writes=[tg + "xt%d" % i])
                    o = P.dma(lambda e, t=t, i=i: e.dma_start(out=h_dst[t * 128:(t + 1) * 128, :], in_=xt[i][:]), reads=[tg + "xt%d" % i],
                              writes=[("hd", h_dst.tensor.name, t)])
                    if next_g is not None:
                        norm_transpose(S3, tg + "n%d" % i, xt[i], tg + "xt%d" % i, gnx, tg + "gnx", t, scr[i])
                    else:
                        finals.append(o)
            P.barrier()

        C.post = post
        C.x, C.pin, C.pos, C.out, C.h1d, C.h2d, C.h3d, C.dbg, C.finals = x, pin, pos, out, h1d, h2d, h3d, dbg, finals
        C.bcast_rows = bcast_rows
        C.contextlib = contextlib
        C.stop = stop
        C.debug = debug

        if stop not in (20, 21, 22):
            ffn("f1", x, W["ff1_pre_g"], W["ff1_post_g"], W["ff1_w_gate"], W["ff1_w_up"], W["ff1_w_down"], h1d, W["mix_pre_g"], True)
        if stop <= 1:
            C.dbg_extra = getattr(C, "dbg_extra", [])
            o = P.dma(lambda e: e.dma_start(out=dbg["uT"][:, :, :], in_=AT[:]), reads=[("AT", t) for t in range(NTILE)], writes=["dbg_uT"])
            o2 = P.dma(lambda e: e.dma_start(out=out[:, :], in_=h1d[:, :]), reads=[("hd", "h1d", t) for t in range(NTILE)], writes=["out"])
            P.emit(final_wait_ops=[o, o2] + C.dbg_extra)
            return nc
        if stop in (20, 21, 22):
            with contextlib.ExitStack() as S0:
                epsT = sb("eps0", [128, 1], F32, S0)
                P.pool(lambda e: e.memset(epsT[:], EPS), writes=["eps0"])
                gb = sb("gb0", [128, D], F32, S0)
                P.dma(lambda e: e.dma_start(out=gb[:], in_=bcast_rows(W["mix_pre_g"], D)), writes=["gb0"])
                scr = [dict(sq=sb("sq0%d" % i, [128, D], BF16, S0), ss=sb("ss0%d" % i, [128, 4], F32, S0), ub=sb("ub0%d" % i, [128, D], BF16, S0),
                            pT=ps("pT0%d" % i, [128, KD, 128], BF16, S0), eps=epsT, epskey="eps0") for i in range(2)]
                hts = [sb("ht0%d" % i, [128, D], F32, S0) for i in range(2)]
                for t in range(NTILE):
                    i = t % 2
                    P.dma(lambda e, t=t, i=i: e.dma_start(out=hts[i][:], in_=x[t * 128:(t + 1) * 128, :]), writes=["ht0%d" % i])
                    P.dma(lambda e, t=t, i=i: e.dma_start(out=h1d[t * 128:(t + 1) * 128, :], in_=hts[i][:]), reads=["ht0%d" % i], writes=[("hd", "h1d", t)])
                    norm_transpose(S0, "n0%d" % i, hts[i], "ht0%d" % i, gb, "gb0", t, scr[i])
            P.barrier()
        if mixer(C) == "stop":
            return nc
        if stop <= 2 or stop in (20, 22):
            o2 = P.dma(lambda e: e.dma_start(out=out[:, :], in_=h2d[:, :]), reads=[("hd", "h2d", t) for t in range(NTILE)], writes=["out"])
            P.emit(final_wait_ops=[o2] + C.dbg_ops)
            return nc
        ffn("f2", h2d, None, W["ff2_post_g"], W["ff2_w_gate"], W["ff2_w_up"], W["ff2_w_down"], h3d, W["ple_pre_g"], False)
        ple(C)
        P.emit(final_wait_ops=finals)
    return nc


WNAMES = ["ff1_pre_g", "ff1_post_g", "ff1_w_gate", "ff1_w_up", "ff1_w_down", "mix_pre_g", "mix_post_g", "w_in",
          "cmp_pos_k", "cmp_pos_v", "cmp_k_w1", "cmp_k_w2", "cmp_v_w1", "cmp_v_w2", "nsa_gate_b",
          "conv_w", "conv_b", "rg_w_a", "rg_b_a", "rg_w_i", "rg_b_i", "rg_lambda",
          "attn_out_g", "rec_out_g", "w_out", "ff2_pre_g", "ff2_post_g", "ff2_w_gate", "ff2_w_up", "ff2_w_down",
          "ple_pre_g", "ple_post_g", "w_ple_gate", "w_ple_proj"]


def shard_tokens(a, b, j):
    T = a.shape[0]
    r = a.reshape(T // 512, 4, 128, *a.shape[1:])[:, j]
    return np.ascontiguousarray(r.reshape(T // 4, *a.shape[1:]))


def make_in_maps(inputs):
    shared = {}
    for nm in WNAMES:
        a = np.asarray(inputs[nm], dtype=np.float32)[0]
        if a.ndim == 1:
            a = a.reshape(1, -1)
        if nm == "nsa_gate_b":
            a = a.reshape(1, 24)
        shared[nm] = np.ascontiguousarray(a)
    maps = []
    x = np.asarray(inputs["x"], dtype=np.float32)
    p = np.asarray(inputs["p"], dtype=np.float32)[0]
    positions = np.asarray(inputs["positions"]).astype(np.int32)
    for c in range(8):
        b, j = c // 4, c % 4
        m = dict(shared)
        m["x"] = shard_tokens(x[b], b, j)
        m["p"] = shard_tokens(p[b], b, j)
        m["pos"] = shard_tokens(positions[b], b, j).reshape(1, NT)
        maps.append(m)
    return maps


def unshard(outs):
    res = np.zeros((2, 4096, D), dtype=np.float32)
    for c in range(8):
        b, j = c // 4, c % 4
        res[b].reshape(8, 4, 128, D)[:, j] = np.asarray(outs[c]).reshape(8, 128, D)
    return res


BIGNEG = -30000.0
SCALE = 128 ** -0.5
RG = [[0, 1, 2, 3], [4, 5, 6, 7]]
GROWS = 12 * 128


def mixer(C):
    nc, P, W, sb, ps, AT, ident, identf = C.nc, C.P, C.W, C.sb, C.ps, C.AT, C.ident, C.identf
    contextlib = C.contextlib
    C.dbg_ops = []

    def cin(name, shape, dt=F32):
        return nc.dram_tensor(name, list(shape), dt, kind="ExternalInput").ap()

    c_rope = cin("c_rope", [128, 4])
    c_perm = cin("c_perm", [128, 128])
    c_oh = cin("c_oh", [128, 4])
    c_cmpm = cin("c_cmpm", [128, 2 * 8 * 128], BF16)
    c_selcm = cin("c_selcm", [128, 4 * 128], BF16)
    c_winm = cin("c_winm", [128, 8 * 128], BF16)
    c_selA = cin("c_selA", [128, 8 * 64])
    c_selB = cin("c_selB", [128, 8 * 64])
    c_selF = cin("c_selF", [128, 8 * 64])
    c_E = cin("c_E", [64, 4096], BF16)
    c_ovl = cin("c_ovl", [128, 2 * 64])
    c_selmat = cin("c_selmat", [24, 24 * 128])

    gin = nc.dram_tensor("gin", [GROWS, NT], BF16, kind="Internal").ap()
    gbuf = nc.dram_tensor("gbuf", [4 * GROWS, NT], BF16, kind="Internal").ap()
    hin = nc.dram_tensor("hin", [128, 192], F32, kind="Internal").ap()
    hbuf = nc.dram_tensor("hbuf", [4 * 128, 192], F32, kind="Internal").ap()
    sin_ = nc.dram_tensor("sin", [128, 128], F32, kind="Internal").ap()
    sbuf_ = nc.dram_tensor("sbuf", [4 * 128, 128], F32, kind="Internal").ap()

    def load(dst, src, key, q="sp", reads=()):
        return P.dma(lambda e: e.dma_start(out=dst, in_=src), reads=list(reads), writes=[key], q=q)

    with contextlib.ExitStack() as SM:
        qT = sb("qT", [128, 8, NT], BF16, SM)
        gT = sb("gT", [24, NT], F32, SM)
        orecT = sb("orecT", [128, 8, NT], BF16, SM)
        ones_bf = sb("ones_bf", [128, 128], BF16, SM)
        ones_f = sb("ones_f", [128, 128], F32, SM)
        onec = sb("onec", [128, 1], F32, SM)
        epsT = sb("m_eps", [128, 1], F32, SM)
        oh = sb("oh", [128, 4], F32, SM)
        P.pool(lambda e: e.memset(ones_bf[:], 1.0), writes=["ones_bf"])
        P.pool(lambda e: e.memset(ones_f[:], 1.0), writes=["ones_f"])
        P.pool(lambda e: e.memset(onec[:], 1.0), writes=["onec"])
        P.pool(lambda e: e.memset(epsT[:], EPS), writes=["m_eps"])
        load(oh[:], c_oh[:, :], "oh")
        C.ones_bf, C.ones_f, C.epsT = ones_bf, ones_f, epsT
        ATall = [("AT", t) for t in range(NTILE)]

        with contextlib.ExitStack() as S2:
            ZX = sb("ZX", [128, 8, NT], F32, S2)
            gy = sb("gy", [128, 8, NT], BF16, S2)
            with contextlib.ExitStack() as S2a:
                rope = sb("rope", [128, 4], F32, S2a)
                perm = sb("perm", [128, 128], F32, S2a)
                posi = sb("posi", [128, NT], I32, S2a)
                ang = sb("ang", [128, NT], F32, S2a)
                kf = sb("kf", [128, NT], F32, S2a)
                ki = sb("ki", [128, NT], I32, S2a)
                Ct = sb("Ct", [128, NT], F32, S2a)
                St = sb("St", [128, NT], F32, S2a)
                gbias = sb("gbias", [24, 1], F32, S2a)
                load(rope[:], c_rope[:, :], "rope")
                load(perm[:], c_perm[:, :], "perm")
                load(posi[:], C.bcast_rows(C.pos, NT), "posi")
                with nc.allow_non_contiguous_dma(reason="tiny param"):
                    P.dma(lambda e: e.dma_start(out=gbias[:], in_=W["nsa_gate_b"].rearrange("o n -> n o"), allow_slow_non_contiguous=True), writes=["gbias"])
                P.dve(lambda e: e.tensor_copy(out=ang[:], in_=posi[:]), reads=["posi"], writes=["ang"])
                P.dve(lambda e: e.tensor_scalar(out=ang[:], in0=ang[:], scalar1=rope[:, 0:1], scalar2=None, op0=ALU.mult), reads=["ang", "rope"], writes=["ang"])
                TWO_PI = 6.283185307179586
                C1 = 6.28125
                C2 = TWO_PI - C1
                P.dve(lambda e: e.tensor_scalar(out=kf[:], in0=ang[:], scalar1=1.0 / TWO_PI, scalar2=None, op0=ALU.mult), reads=["ang"], writes=["kf"])
                P.dve(lambda e: e.tensor_copy(out=ki[:], in_=kf[:]), reads=["kf"], writes=["ki"])
                P.dve(lambda e: e.tensor_copy(out=kf[:], in_=ki[:]), reads=["ki"], writes=["kf"])
                P.dve(lambda e: e.scalar_tensor_tensor(out=ang[:], in0=kf[:], scalar=-C1, in1=ang[:], op0=ALU.mult, op1=ALU.add), reads=["kf", "ang"], writes=["ang"])
                P.dve(lambda e: e.scalar_tensor_tensor(out=ang[:], in0=kf[:], scalar=-C2, in1=ang[:], op0=ALU.mult, op1=ALU.add), reads=["kf", "ang"], writes=["ang"])
                P.dve(lambda e: e.tensor_scalar(out=kf[:], in0=ang[:], scalar1=3.141592653589793, scalar2=-TWO_PI, op0=ALU.is_gt, op1=ALU.mult), reads=["ang"], writes=["kf"])
                P.dve(lambda e: e.tensor_tensor(out=ang[:], in0=ang[:], in1=kf[:], op=ALU.add), reads=["ang", "kf"], writes=["ang"])
                P.dve(lambda e: e.tensor_scalar(out=kf[:], in0=ang[:], scalar1=-3.141592653589793, scalar2=TWO_PI, op0=ALU.is_lt, op1=ALU.mult), reads=["ang"], writes=["kf"])
                P.dve(lambda e: e.tensor_tensor(out=ang[:], in0=ang[:], in1=kf[:], op=ALU.add), reads=["ang", "kf"], writes=["ang"])
                P.act(lambda e: e.activation(out=St[:], in_=ang[:], func=AF.Sin), reads=["ang"], writes=["St"])
                P.dve(lambda e: e.tensor_scalar(out=St[:], in0=St[:], scalar1=rope[:, 1:2], scalar2=None, op0=ALU.mult), reads=["St", "rope"], writes=["St"])
                P.act(lambda e: e.activation(out=kf[:], in_=ang[:], func=AF.Abs), reads=["ang"], writes=["kf"])
                P.dve(lambda e: e.tensor_scalar(out=kf[:], in0=kf[:], scalar1=-1.0, scalar2=1.5707963267948966, op0=ALU.mult, op1=ALU.add), reads=["kf"], writes=["kf"])
                P.act(lambda e: e.activation(out=Ct[:], in_=kf[:], func=AF.Sin), reads=["kf"], writes=["Ct"])

                wpan = [sb("wpan%d" % i, [128, KD, 512], BF16, S2a) for i in range(2)]
                zf = [sb("zf%d" % i, [128, NT], F32, S2a) for i in range(2)]
                t1 = [sb("t1%d" % i, [128, 512], F32, S2a) for i in range(2)]
                stg = [sb("stg%d" % i, [128, NT], BF16, S2a) for i in range(2)]
                pz = [[ps("pz%d%d" % (i, hf), [128, 512], F32, S2a) for hf in range(2)] for i in range(2)]
                psw = [ps("psw%d" % i, [128, 512], F32, S2a) for i in range(2)]
                pvt = [ps("pvt%d" % i, [128, 8, 128], BF16, S2a) for i in range(2)]
                wv = W["w_in"].rearrange("(kc p) m -> p kc m", p=128)
                panels = [(0, 512), (512, 512), (1024, 512), (1536, 512), (2048, 512), (2560, 24), (2584, 512), (3096, 512), (3608, 512), (4120, 512)]
                kinds = {}
                for h in range(8):
                    kinds[h * 128] = ("q", h)
                for g in range(2):
                    kinds[1024 + g * 128] = ("kf", 0 + g)
                    kinds[1280 + g * 128] = ("vf", 2 + g)
                    kinds[1536 + g * 128] = ("kf", 4 + g)
                    kinds[1792 + g * 128] = ("vt", 8 + g)
                    kinds[2048 + g * 128] = ("kf", 6 + g)
                    kinds[2304 + g * 128] = ("vt", 10 + g)
                kinds[2560] = ("g", 0)
                for h in range(8):
                    kinds[2584 + h * 128] = ("zx", h)
                    kinds[3608 + h * 128] = ("zy", h)
                cnt = 0
                for pi, (c0, wdt) in enumerate(panels):
                    b = pi % 2
                    P.dma(lambda e, b=b, c0=c0, wdt=wdt: e.dma_start(out=wpan[b][:, :, 0:wdt], in_=wv[:, :, c0:c0 + wdt]), writes=["wpan%d" % b], q="pool")
                    for ci in range(max(1, wdt // 128)):
                        col = c0 + ci * 128
                        kind, idx = kinds[col]
                        cw = 24 if kind == "g" else 128
                        r = cnt % 2
                        cnt += 1
                        for hf in range(2):
                            for k in range(KD):
                                P.pe(lambda e, k=k, hf=hf, r=r, b=b, ci=ci, cw=cw: e.matmul(
                                    pz[r][hf][0:cw, :], lhsT=wpan[b][:, k, ci * 128:ci * 128 + cw], rhs=AT[:, k, hf * 512:(hf + 1) * 512],
                                    start=(k == 0), stop=(k == KD - 1)), reads=["wpan%d" % b] + ATall[hf * 4:hf * 4 + 4], writes=["pz%d%d" % (r, hf)])
                        pzk = ["pz%d%d" % (r, 0), "pz%d%d" % (r, 1)]
                        if kind == "g":
                            for hf in range(2):
                                P.act(lambda e, hf=hf, r=r: e.activation(out=gT[:, hf * 512:(hf + 1) * 512], in_=pz[r][hf][0:24, :], func=AF.Sigmoid, bias=gbias[:, 0:1]),
                                      reads=[pzk[hf], "gbias"], writes=["gT"])
                        elif kind == "zx":
                            for hf in range(2):
                                P.act(lambda e, hf=hf, r=r, idx=idx: e.copy(out=ZX[:, idx, hf * 512:(hf + 1) * 512], in_=pz[r][hf][:]),
                                      reads=[pzk[hf]], writes=[("ZX", idx)])
                        elif kind == "zy":
                            for hf in range(2):
                                sl = slice(hf * 512, (hf + 1) * 512)
                                P.act(lambda e, hf=hf, r=r, sl=sl: e.activation(out=zf[r][:, sl], in_=pz[r][hf][:], func=AF.Square), reads=[pzk[hf]], writes=[("zf%d" % r, hf)])
                                P.dve(lambda e, r=r, sl=sl, hf=hf: e.tensor_scalar(out=zf[r][:, sl], in0=zf[r][:, sl], scalar1=0.044715, scalar2=1.0, op0=ALU.mult, op1=ALU.add), reads=[("zf%d" % r, hf)], writes=[("zf%d" % r, hf)])
                                P.dve(lambda e, hf=hf, r=r, sl=sl: e.tensor_tensor(out=zf[r][:, sl], in0=zf[r][:, sl], in1=pz[r][hf][:], op=ALU.mult), reads=[("zf%d" % r, hf), pzk[hf]], writes=[("zf%d" % r, hf)])
                                P.act(lambda e, r=r, sl=sl, hf=hf: e.activation(out=zf[r][:, sl], in_=zf[r][:, sl], func=AF.Sigmoid, scale=1.5957691216057308), reads=[("zf%d" % r, hf)], writes=[("zf%d" % r, hf)])
                                P.dve(lambda e, hf=hf, r=r, sl=sl, idx=idx: e.tensor_tensor(out=gy[:, idx, sl], in0=zf[r][:, sl], in1=pz[r][hf][:], op=ALU.mult), reads=[("zf%d" % r, hf), pzk[hf]], writes=[("gy", idx)])
                        else:
                            for hf in range(2):
                                P.act(lambda e, hf=hf, r=r: e.copy(out=zf[r][:, hf * 512:(hf + 1) * 512], in_=pz[r][hf][:]), reads=[pzk[hf]], writes=[("zf%d" % r, hf)])
                            if kind in ("q", "kf"):
                                dst = qT[:, idx, :] if kind == "q" else stg[r][:]
                                dkey = ("qT", idx) if kind == "q" else "stg%d" % r
                                for hf in range(2):
                                    sl = slice(hf * 512, (hf + 1) * 512)
                                    P.pe(lambda e, hf=hf, r=r, sl=sl: e.matmul(psw[hf][:], lhsT=perm[:], rhs=zf[r][:, sl], start=True, stop=True),
                                         reads=["perm", ("zf%d" % r, hf)], writes=["psw%d" % hf])
                                    P.dve(lambda e, hf=hf, r=r, sl=sl: e.tensor_tensor(out=t1[hf][:], in0=zf[r][:, sl], in1=Ct[:, sl], op=ALU.mult),
                                          reads=[("zf%d" % r, hf), "Ct"], writes=["t1%d" % hf])
                                    P.dve(lambda e, hf=hf, r=r, sl=sl: e.tensor_tensor(out=zf[r][:, sl], in0=psw[hf][:], in1=St[:, sl], op=ALU.mult),
                                          reads=["psw%d" % hf, "St", ("zf%d" % r, hf)], writes=[("zf%d" % r, hf)])
                                    P.dve(lambda e, hf=hf, r=r, sl=sl, dst=dst: e.tensor_tensor(out=dst[:, sl], in0=zf[r][:, sl], in1=t1[hf][:], op=ALU.add),
                                          reads=[("zf%d" % r, hf), "t1%d" % hf], writes=[dkey])
                                if kind == "kf":
                                    P.dma(lambda e, r=r, idx=idx: e.dma_start(out=gin[idx * 128:(idx + 1) * 128, :], in_=stg[r][:]), reads=["stg%d" % r], writes=[("gin", idx)])
                            elif kind == "vf":
                                P.dve(lambda e, r=r: e.tensor_copy(out=stg[r][:], in_=zf[r][:]), reads=[("zf%d" % r, 0), ("zf%d" % r, 1)], writes=["stg%d" % r])
                                P.dma(lambda e, r=r, idx=idx: e.dma_start(out=gin[idx * 128:(idx + 1) * 128, :], in_=stg[r][:]), reads=["stg%d" % r], writes=[("gin", idx)])
                            elif kind == "vt":
                                P.dve(lambda e, r=r: e.tensor_copy(out=stg[r][:], in_=zf[r][:]), reads=[("zf%d" % r, 0), ("zf%d" % r, 1)], writes=["stg%d" % r])
                                for l in range(8):
                                    P.pe(lambda e, r=r, l=l: e.transpose(out=pvt[r][:, l, :], in_=stg[r][:, l * 128:(l + 1) * 128], identity=ident[:]),
                                         reads=["stg%d" % r, "ident"], writes=["pvt%d" % r])
                                P.act(lambda e, r=r: e.copy(out=stg[r][:], in_=pvt[r][:].rearrange("p l d -> p (l d)")), reads=["pvt%d" % r], writes=["stg%d" % r])
                                P.dma(lambda e, r=r, idx=idx: e.dma_start(out=gin[idx * 128:(idx + 1) * 128, :], in_=stg[r][:]), reads=["stg%d" % r], writes=[("gin", idx)])
            P.barrier()
            with contextlib.ExitStack() as S2b:
                hst = sb("hst", [128, 8, 8, 3], F32, S2b)
                for blk in range(8):
                    P.dve(lambda e, blk=blk: e.tensor_copy(out=hst[:, blk, :, :], in_=ZX[:, blk, :].rearrange("p (l t) -> p l t", t=128)[:, :, 125:128]),
                          reads=[("ZX", blk)], writes=["hst"])
                P.dma(lambda e: e.dma_start(out=hin[:, :], in_=hst[:].rearrange("p b l t -> p (b l t)")), reads=["hst"], writes=["hin"])
                for it in range(12):
                    P.cc(lambda e, it=it: e.collective_compute("AllGather", ALU.bypass, replica_groups=RG, ins=[gin[it * 128:(it + 1) * 128, :]], outs=[gbuf[it * 512:(it + 1) * 512, :]]),
                           reads=[("gin", it)], writes=[("gbuf", it)])
                cc2 = P.cc(lambda e: e.collective_compute("AllGather", ALU.bypass, replica_groups=RG, ins=[hin[:, :]], outs=[hbuf[:, :]]), reads=["hin"], writes=["hbuf"])
            P.barrier()
            if C.stop == 21:
                C.dbg["qT"] = nc.dram_tensor("dbg_qT", [128, 8, NT], BF16, kind="ExternalOutput").ap()
                C.dbg["gT"] = nc.dram_tensor("dbg_gT", [24, NT], F32, kind="ExternalOutput").ap()
                C.dbg["gbuf"] = nc.dram_tensor("dbg_gbuf", [4 * GROWS, NT], BF16, kind="ExternalOutput").ap()
                C.dbg["ZX"] = nc.dram_tensor("dbg_ZX", [128, 8, NT], F32, kind="ExternalOutput").ap()
                C.dbg["gy"] = nc.dram_tensor("dbg_gy", [128, 8, NT], BF16, kind="ExternalOutput").ap()
                C.dbg_ops.append(P.dma(lambda e: e.dma_start(out=C.dbg["qT"][:, :, :], in_=qT[:]), reads=[("qT", h) for h in range(8)], writes=["d1"]))
                C.dbg_ops.append(P.dma(lambda e: e.dma_start(out=C.dbg["gT"][:, :], in_=gT[:]), reads=["gT"], writes=["d2"]))
                C.dbg_ops.append(P.dma(lambda e: e.dma_start(out=C.dbg["gbuf"][:, :], in_=gbuf[:, :]), reads=[("gbuf", it) for it in range(12)], writes=["d3"]))
                C.dbg_ops.append(P.dma(lambda e: e.dma_start(out=C.dbg["ZX"][:, :, :], in_=ZX[:]), reads=[("ZX", h) for h in range(8)], writes=["d4"]))
                C.dbg_ops.append(P.dma(lambda e: e.dma_start(out=C.dbg["gy"][:, :, :], in_=gy[:]), reads=[("gy", h) for h in range(8)], writes=["d5"]))
                P.emit(final_wait_ops=C.dbg_ops)
                return "stop"
            lru(C, SM, ZX, gy, orecT, hbuf, sin_, sbuf_, oh, onec)
        P.barrier()
        attention(C, SM, qT, gT, orecT, gbuf, ones_bf, ones_f, epsT,
                  dict(cmpm=c_cmpm, selcm=c_selcm, winm=c_winm, selA=c_selA, selB=c_selB, selF=c_selF, E=c_E, ovl=c_ovl, selmat=c_selmat))
    P.barrier()
    if C.stop == 22:
        C.dbg["cat"] = nc.dram_tensor("dbg_cat", [128, KD, NT], BF16, kind="ExternalOutput").ap()
        C.dbg_ops.append(P.dma(lambda e: e.dma_start(out=C.dbg["cat"][:, :, :], in_=AT[:]), reads=[("AT", t) for t in range(NTILE)], writes=["dcat"]))
        P.barrier()
    outproj(C)


def lru(C, SM, ZX, gy, orecT, hbuf, sin_, sbuf_, oh, onec):
    nc, P, W, sb, ps = C.nc, C.P, C.W, C.sb, C.ps
    contextlib = C.contextlib
    ones_bf, epsT = C.ones_bf, C.epsT
    with contextlib.ExitStack() as SL:
        Pc = sb("Pc", [128, 8, NT], F32, SL)
        cw = sb("cw", [128, 4, 8], F32, SL)
        cb = sb("cb", [128, 8], F32, SL)
        ba = sb("ba", [128, 8], F32, SL)
        bi = sb("bi", [128, 8], F32, SL)
        lam = sb("lam", [128, 8], F32, SL)
        rg = sb("rg", [128, 8], F32, SL)
        cch = sb("cch", [128, 8], F32, SL)
        wa = sb("wa", [128, 8, 128], BF16, SL)
        wi = sb("wi", [128, 8, 128], BF16, SL)
        G2 = sb("G2", [128, 4, 192], F32, SL)
        halo = sb("halo", [128, 8, 8, 3], F32, SL)
        zeros = sb("zeros", [128, 128], F32, SL)
        P.pool(lambda e: e.memset(zeros[:], 0.0), writes=["zeros"])
        with nc.allow_non_contiguous_dma(reason="tiny params"):
            for w_ in range(4):
                P.dma(lambda e, w_=w_: e.dma_start(out=cw[:, w_, :], in_=W["conv_w"][w_:w_ + 1, :].rearrange("o (b p) -> p (o b)", p=128), allow_slow_non_contiguous=True), writes=[("cw", w_)])
            for t_, nm in [(cb, "conv_b"), (ba, "rg_b_a"), (bi, "rg_b_i"), (lam, "rg_lambda"), (rg, "rec_out_g")]:
                P.dma(lambda e, t_=t_, nm=nm: e.dma_start(out=t_[:], in_=W[nm].rearrange("o (b p) -> p (o b)", p=128), allow_slow_non_contiguous=True), writes=[nm])
        P.dma(lambda e: e.dma_start(out=wa[:], in_=W["rg_w_a"].rearrange("b i j -> i b j")), writes=["wa"], q="pool")
        P.dma(lambda e: e.dma_start(out=wi[:], in_=W["rg_w_i"].rearrange("b i j -> i b j")), writes=["wi"], q="pool")
        P.dma(lambda e: e.dma_start(out=G2[:], in_=hbuf.rearrange("(r p) c -> p r c", p=128)), reads=["hbuf"], writes=["G2"])
        P.act(lambda e: e.activation(out=cch[:], in_=lam[:], func=AF.Sigmoid), reads=["rg_lambda"], writes=["cch"])
        P.act(lambda e: e.activation(out=cch[:], in_=cch[:], func=AF.Ln), reads=["cch"], writes=["cch"])
        P.dve(lambda e: e.tensor_scalar(out=cch[:], in0=cch[:], scalar1=8.0, scalar2=None, op0=ALU.mult), reads=["cch"], writes=["cch"])
        hv = halo[:].rearrange("p b l t -> p (b l t)")
        P.dve(lambda e: e.tensor_scalar(out=hv, in0=G2[:, 0, :], scalar1=oh[:, 1:2], scalar2=None, op0=ALU.mult), reads=["G2", "oh"], writes=["halo"])
        P.dve(lambda e: e.scalar_tensor_tensor(out=hv, in0=G2[:, 1, :], scalar=oh[:, 2:3], in1=hv, op0=ALU.mult, op1=ALU.add), reads=["G2", "oh", "halo"], writes=["halo"])
        P.dve(lambda e: e.scalar_tensor_tensor(out=hv, in0=G2[:, 2, :], scalar=oh[:, 3:4], in1=hv, op0=ALU.mult, op1=ALU.add), reads=["G2", "oh", "halo"], writes=["halo"])
        g3 = G2[:, 3, :].rearrange("p (b l t) -> p b l t", b=8, l=8)
        P.dve(lambda e: e.scalar_tensor_tensor(out=halo[:, :, 1:8, :], in0=g3[:, :, 0:7, :], scalar=oh[:, 0:1], in1=halo[:, :, 1:8, :], op0=ALU.mult, op1=ALU.add),
              reads=["G2", "oh", "halo"], writes=["halo"])
        with contextlib.ExitStack() as SB:
            NB = 1
            xpad = [sb("xpad%d" % i, [128, 8, 131], F32, SB) for i in range(NB)]
            xc = [sb("xc%d" % i, [128, 8, 128], F32, SB) for i in range(NB)]
            xcb = [sb("xcb%d" % i, [128, NT], BF16, SB) for i in range(NB)]
            rr = [sb("rr%d" % i, [128, NT], F32, SB) for i in range(NB)]
            ig = [sb("ig%d" % i, [128, NT], F32, SB) for i in range(NB)]
            aa = [sb("aa%d" % i, [128, NT], F32, SB) for i in range(NB)]
            uu = [sb("uu%d" % i, [128, NT], F32, SB) for i in range(NB)]
            pr = [[ps("pr%d%d" % (i, hf), [128, 512], F32, SB) for hf in range(2)] for i in range(NB)]
            pg = [[ps("pi%d%d" % (i, hf), [128, 512], F32, SB) for hf in range(2)] for i in range(NB)]
            for blk in range(8):
                i = blk % NB
                k = lambda s: "%s%d" % (s, i)
                P.pool(lambda e, i=i, blk=blk: e.tensor_copy(out=xpad[i][:, :, 0:3], in_=halo[:, blk, :, :]), reads=["halo"], writes=[k("xpad")])
                P.pool(lambda e, i=i, blk=blk: e.tensor_copy(out=xpad[i][:, :, 3:131], in_=ZX[:, blk, :].rearrange("p (l t) -> p l t", t=128)), reads=[("ZX", blk)], writes=[k("xpad")])
                P.dve(lambda e, i=i, blk=blk: e.tensor_scalar(out=xc[i][:], in0=xpad[i][:, :, 0:128], scalar1=cw[:, 0, blk:blk + 1], scalar2=cb[:, blk:blk + 1], op0=ALU.mult, op1=ALU.add),
                      reads=[k("xpad"), ("cw", 0), "conv_b"], writes=[k("xc")])
                for w in range(1, 4):
                    P.dve(lambda e, i=i, blk=blk, w=w: e.scalar_tensor_tensor(out=xc[i][:], in0=xpad[i][:, :, w:w + 128], scalar=cw[:, w, blk:blk + 1], in1=xc[i][:], op0=ALU.mult, op1=ALU.add),
                          reads=[k("xpad"), ("cw", w), k("xc")], writes=[k("xc")])
                xcf = xc[i][:].rearrange("p l t -> p (l t)")
                P.act(lambda e, i=i, xcf=xcf: e.copy(out=xcb[i][:], in_=xcf), reads=[k("xc")], writes=[k("xcb")])
                for hf in range(2):
                    sl = slice(hf * 512, (hf + 1) * 512)
                    P.pe(lambda e, i=i, blk=blk, hf=hf, sl=sl: e.matmul(pr[i][hf][:], lhsT=wa[:, blk, :], rhs=xcb[i][:, sl], start=True, stop=True), reads=["wa", k("xcb")], writes=["pr%d%d" % (i, hf)])
                    P.pe(lambda e, i=i, blk=blk, hf=hf, sl=sl: e.matmul(pg[i][hf][:], lhsT=wi[:, blk, :], rhs=xcb[i][:, sl], start=True, stop=True), reads=["wi", k("xcb")], writes=["pi%d%d" % (i, hf)])
                    P.act(lambda e, i=i, blk=blk, hf=hf, sl=sl: e.activation(out=rr[i][:, sl], in_=pr[i][hf][:], func=AF.Sigmoid, bias=ba[:, blk:blk + 1]), reads=["pr%d%d" % (i, hf), "rg_b_a"], writes=[k("rr")])
                    P.act(lambda e, i=i, blk=blk, hf=hf, sl=sl: e.activation(out=ig[i][:, sl], in_=pg[i][hf][:], func=AF.Sigmoid, bias=bi[:, blk:blk + 1]), reads=["pi%d%d" % (i, hf), "rg_b_i"], writes=[k("ig")])
                P.act(lambda e, i=i, blk=blk: e.activation(out=aa[i][:], in_=rr[i][:], func=AF.Exp, scale=cch[:, blk:blk + 1]), reads=[k("rr"), "cch"], writes=[k("aa")])
                P.dve(lambda e, i=i: e.tensor_tensor(out=uu[i][:], in0=aa[i][:], in1=aa[i][:], op=ALU.mult), reads=[k("aa")], writes=[k("uu")])
                P.act(lambda e, i=i: e.activation(out=uu[i][:], in_=uu[i][:], func=AF.Sqrt, scale=-1.0, bias=onec[:, 0:1]), reads=[k("uu"), "onec"], writes=[k("uu")])
                P.dve(lambda e, i=i: e.tensor_tensor(out=uu[i][:], in0=uu[i][:], in1=ig[i][:], op=ALU.mult), reads=[k("uu"), k("ig")], writes=[k("uu")])
                P.dve(lambda e, i=i, xcf=xcf: e.tensor_tensor(out=uu[i][:], in0=uu[i][:], in1=xcf, op=ALU.mult), reads=[k("uu"), k("xc")], writes=[k("uu")])
                for l in range(8):
                    sl = slice(l * 128, (l + 1) * 128)
                    P.dve(lambda e, i=i, blk=blk, sl=sl: e.tensor_tensor_scan(out=ZX[:, blk, sl], data0=aa[i][:, sl], data1=uu[i][:, sl], initial=0.0, op0=ALU.mult, op1=ALU.add),
                          reads=[k("aa"), k("uu"), k("xpad")], writes=[("ZX", blk)])
                    P.dve(lambda e, i=i, blk=blk, sl=sl: e.tensor_tensor_scan(out=Pc[:, blk, sl], data0=aa[i][:, sl], data1=zeros[:], initial=1.0, op0=ALU.mult, op1=ALU.add),
                          reads=[k("aa"), "zeros"], writes=[("Pc", blk)])
        P.barrier()
        with contextlib.ExitStack() as SC:
            sst = sb("sst", [128, 8, 8, 2], F32, SC)
            SG = sb("SG", [128, 4, 128], F32, SC)
            Aall = sb("Aall", [128, 8, 32], F32, SC)
            Hall = sb("Hall", [128, 8, 32], F32, SC)
            Spad = sb("Spad", [128, 8, 33], F32, SC)
            hinit = sb("hinit", [128, 8, 8], F32, SC)
            rstdb = sb("rstdb", [128, NT], F32, SC)
            sqb = [sb("sqb%d" % i, [128, NT], BF16, SC) for i in range(2)]
            pss = [ps("pss%d" % hf, [128, 512], F32, SC) for hf in range(2)]
            allZX = [("ZX", b) for b in range(8)]
            allPc = [("Pc", b) for b in range(8)]
            P.dve(lambda e: e.tensor_copy(out=sst[:, :, :, 0], in_=Pc[:].rearrange("p b (l t) -> p b l t", t=128)[:, :, :, 127]), reads=allPc, writes=["sst"])
            P.dve(lambda e: e.tensor_copy(out=sst[:, :, :, 1], in_=ZX[:].rearrange("p b (l t) -> p b l t", t=128)[:, :, :, 127]), reads=allZX, writes=["sst"])
            P.dma(lambda e: e.dma_start(out=sin_[:, :], in_=sst[:].rearrange("p b l t -> p (b l t)")), reads=["sst"], writes=["sin"])
            P.cc(lambda e: e.collective_compute("AllGather", ALU.bypass, replica_groups=RG, ins=[sin_[:, :]], outs=[sbuf_[:, :]]), reads=["sin"], writes=["sbufd"])
            P.dma(lambda e: e.dma_start(out=SG[:], in_=sbuf_.rearrange("(r p) c -> p r c", p=128)), reads=["sbufd"], writes=["SG"])
            for j in range(4):
                sgv = SG[:, j, :].rearrange("p (b l t) -> p b l t", b=8, l=8)
                P.dve(lambda e, j=j, sgv=sgv: e.tensor_copy(out=Aall[:, :, j:j + 29:4], in_=sgv[:, :, :, 0]), reads=["SG"], writes=["Aall"])
                P.dve(lambda e, j=j, sgv=sgv: e.tensor_copy(out=Hall[:, :, j:j + 29:4], in_=sgv[:, :, :, 1]), reads=["SG"], writes=["Hall"])
            P.pool(lambda e: e.memset(Spad[:], 0.0), writes=["Spad"])
            for blk in range(8):
                P.dve(lambda e, blk=blk: e.tensor_tensor_scan(out=Spad[:, blk, 1:33], data0=Aall[:, blk, :], data1=Hall[:, blk, :], initial=0.0, op0=ALU.mult, op1=ALU.add),
                      reads=["Aall", "Hall", "Spad"], writes=["Spad"])
            P.dve(lambda e: e.tensor_scalar(out=hinit[:], in0=Spad[:, :, 0:29:4], scalar1=oh[:, 0:1], scalar2=None, op0=ALU.mult), reads=["Spad", "oh"], writes=["hinit"])
            for m in range(1, 4):
                P.dve(lambda e, m=m: e.scalar_tensor_tensor(out=hinit[:], in0=Spad[:, :, m:m + 29:4], scalar=oh[:, m:m + 1], in1=hinit[:], op0=ALU.mult, op1=ALU.add),
                      reads=["Spad", "oh", "hinit"], writes=["hinit"])
            for blk in range(8):
                for l in range(8):
                    sl = slice(l * 128, (l + 1) * 128)
                    P.dve(lambda e, blk=blk, l=l, sl=sl: e.scalar_tensor_tensor(out=ZX[:, blk, sl], in0=Pc[:, blk, sl], scalar=hinit[:, blk, l:l + 1], in1=ZX[:, blk, sl], op0=ALU.mult, op1=ALU.add),
                          reads=[("Pc", blk), "hinit", ("ZX", blk)], writes=[("ZX", blk)])
                P.dve(lambda e, blk=blk: e.tensor_tensor(out=ZX[:, blk, :], in0=ZX[:, blk, :], in1=gy[:, blk, :], op=ALU.mult), reads=[("ZX", blk), ("gy", blk)], writes=[("ZX", blk)])
                i = blk % 2
                P.act(lambda e, blk=blk, i=i: e.activation(out=sqb[i][:], in_=ZX[:, blk, :], func=AF.Square), reads=[("ZX", blk)], writes=["sqb%d" % i])
                for hf in range(2):
                    P.pe(lambda e, blk=blk, i=i, hf=hf: e.matmul(pss[hf][:], lhsT=ones_bf[:], rhs=sqb[i][:, hf * 512:(hf + 1) * 512], start=(blk == 0), stop=(blk == 7)),
                         reads=["ones_bf", "sqb%d" % i], writes=["pss%d" % hf])
            for hf in range(2):
                sl = slice(hf * 512, (hf + 1) * 512)
                P.act(lambda e, hf=hf, sl=sl: e.activation(out=rstdb[:, sl], in_=pss[hf][:], func=AF.Sqrt, scale=1.0 / 1024, bias=epsT[:, 0:1]), reads=["pss%d" % hf, "m_eps"], writes=["rstdb"])
            P.dve(lambda e: e.reciprocal(out=rstdb[:], in_=rstdb[:]), reads=["rstdb"], writes=["rstdb"])
            for blk in range(8):
                P.dve(lambda e, blk=blk: e.scalar_tensor_tensor(out=orecT[:, blk, :], in0=ZX[:, blk, :], scalar=rg[:, blk:blk + 1], in1=rstdb[:], op0=ALU.mult, op1=ALU.mult),
                      reads=[("ZX", blk), "rec_out_g", "rstdb"], writes=[("orecT", blk)])
        P.barrier()


def attention(C, SM, qT, gT, orecT, gbuf, ones_bf, ones_f, epsT, K):
    nc, P, W, sb, ps, AT, ident = C.nc, C.P, C.W, C.sb, C.ps, C.AT, C.ident
    contextlib = C.contextlib

    def bc4(ap, n):
        return ap.unsqueeze(1).broadcast_to([n, 4, 128])

    with contextlib.ExitStack() as SA:
        cmpm = sb("cmpm", [128, 2, 8, 128], BF16, SA)
        selcm = sb("selcm", [128, 4, 128], BF16, SA)
        winm = sb("winm", [128, 8, 128], BF16, SA)
        selA = sb("selA", [128, 8, 64], F32, SA)
        selB = sb("selB", [128, 8, 64], F32, SA)
        selF = sb("selF", [128, 8, 64], F32, SA)
        E = sb("E", [64, 4096], BF16, SA)
        ovl = sb("ovl", [128, 2, 64], F32, SA)
        selmat = sb("selmat", [24, 24, 128], F32, SA)
        oaT = sb("oaT", [128, 8, NT], F32, SA)
        for t_, src, nm in [(cmpm, K["cmpm"], "cmpm"), (selcm, K["selcm"], "selcm"), (winm, K["winm"], "winm"), (selA, K["selA"], "selA"),
                            (selB, K["selB"], "selB"), (selF, K["selF"], "selF"), (E, K["E"], "E"), (ovl, K["ovl"], "ovl"), (selmat, K["selmat"], "selmat")]:
            fl = t_[:]
            if len(t_.shape) == 3:
                fl = t_[:].rearrange("p a b -> p (a b)")
            elif len(t_.shape) == 4:
                fl = t_[:].rearrange("p a b c -> p (a b c)")
            P.dma(lambda e, fl=fl, src=src: e.dma_start(out=fl, in_=src[:, :]), writes=[nm])

        gview = gbuf.rearrange("(i j d) (l p) -> d i l j p", j=4, i=12, d=128, l=8, p=128)
        for g in range(2):
            with contextlib.ExitStack() as SG_:
                KV = {}
                for nm, item in [("kc", 0 + g), ("vc", 2 + g), ("ks", 4 + g), ("kw", 6 + g), ("vs", 8 + g), ("vw", 10 + g)]:
                    t_ = sb("%s%d" % (nm, g), [128, 4096], BF16, SG_)
                    KV[nm] = t_
                    for l in range(8):
                        P.dma(lambda e, t_=t_, item=item, l=l: e.dma_start(out=t_[:, l * 512:(l + 1) * 512].rearrange("d (j p) -> d j p", j=4), in_=gview[:, item, l, :, :]),
                              reads=[("gbuf", item)], writes=[("kv", nm, l)])
                kvall = lambda nm: [("kv", nm, l) for l in range(8)]
                kcT = sb("kcT%d" % g, [128, 256], BF16, SG_)
                vcm = sb("vcm%d" % g, [128, 2, 128], BF16, SG_)
                with contextlib.ExitStack() as SCm:
                    w1t = sb("w1t", [128, 32, 256], BF16, SCm)
                    w2t = sb("w2t", [128, 2, 128], BF16, SCm)
                    posf = sb("posf", [128, 32], F32, SCm)
                    posb = sb("posb", [128, 32], BF16, SCm)
                    b1 = sb("b1", [128, 2], F32, SCm)
                    hx = sb("hx", [128, 256], F32, SCm)
                    hy = sb("hy", [128, 256], F32, SCm)
                    ghid = sb("ghid", [128, 2, 256], BF16, SCm)
                    ph = [ps("ph%d" % i, [128, 256], F32, SCm) for i in range(2)]
                    pb = [ps("pb%d" % i, [128, 2], F32, SCm) for i in range(2)]
                    pk = ps("pk", [128, 256], F32, SCm)
                    for kvn, src, w1n, w2n, posn in [("k", "kc", "cmp_k_w1", "cmp_k_w2", "cmp_pos_k"), ("v", "vc", "cmp_v_w1", "cmp_v_w2", "cmp_pos_v")]:
                        P.dma(lambda e, w1n=w1n: e.dma_start(out=w1t[:], in_=W[w1n].rearrange("(l d) m -> d l m", d=128)), writes=["w1t"], q="pool")
                        P.dma(lambda e, w2n=w2n: e.dma_start(out=w2t[:], in_=W[w2n].rearrange("(c h) d -> h c d", h=128)), writes=["w2t"], q="pool")
                        with nc.allow_non_contiguous_dma(reason="tiny"):
                            P.dma(lambda e, posn=posn: e.dma_start(out=posf[:], in_=W[posn].rearrange("l d -> d l"), allow_slow_non_contiguous=True), writes=["posf"])
                        P.dve(lambda e: e.tensor_copy(out=posb[:], in_=posf[:]), reads=["posf"], writes=["posb"])
                        xT = KV[src]
                        for hc in range(2):
                            for l in range(32):
                                P.pe(lambda e, hc=hc, l=l, xT=xT: e.matmul(ph[hc][:, 0:255], lhsT=w1t[:, l, hc * 128:(hc + 1) * 128], rhs=xT[:, l:l + 16 * 254 + 1:16],
                                                                   start=(l == 0), stop=(l == 31)), reads=["w1t"] + kvall(src), writes=["ph%d" % hc])
                            for l in range(32):
                                P.pe(lambda e, hc=hc, l=l: e.matmul(pb[hc][:, 0:1], lhsT=w1t[:, l, hc * 128:(hc + 1) * 128], rhs=posb[:, l:l + 1],
                                                             start=(l == 0), stop=(l == 31)), reads=["w1t", "posb"], writes=["pb%d" % hc])
                            P.act(lambda e, hc=hc: e.copy(out=b1[:, hc:hc + 1], in_=pb[hc][:, 0:1]), reads=["pb%d" % hc], writes=["b1"])
                            P.act(lambda e, hc=hc: e.activation(out=hx[:, 0:255], in_=ph[hc][:, 0:255], func=AF.Identity, bias=b1[:, hc:hc + 1]), reads=["ph%d" % hc, "b1"], writes=["hx"])
                            P.act(lambda e: e.activation(out=hy[:, 0:255], in_=hx[:, 0:255], func=AF.Square), reads=["hx"], writes=["hy"])
                            P.dve(lambda e: e.tensor_scalar(out=hy[:, 0:255], in0=hy[:, 0:255], scalar1=0.044715, scalar2=1.0, op0=ALU.mult, op1=ALU.add), reads=["hy"], writes=["hy"])
                            P.dve(lambda e: e.tensor_tensor(out=hy[:, 0:255], in0=hy[:, 0:255], in1=hx[:, 0:255], op=ALU.mult), reads=["hy", "hx"], writes=["hy"])
                            P.act(lambda e: e.activation(out=hy[:, 0:255], in_=hy[:, 0:255], func=AF.Sigmoid, scale=1.5957691216057308), reads=["hy"], writes=["hy"])
                            P.dve(lambda e, hc=hc: e.tensor_tensor(out=ghid[:, hc, 0:255], in0=hy[:, 0:255], in1=hx[:, 0:255], op=ALU.mult), reads=["hy", "hx"], writes=[("ghid", hc)])
                        if kvn == "k":
                            for hc in range(2):
                                P.pe(lambda e, hc=hc: e.matmul(pk[:, 0:255], lhsT=w2t[:, hc, :], rhs=ghid[:, hc, 0:255], start=(hc == 0), stop=(hc == 1)),
                                     reads=["w2t", ("ghid", 0), ("ghid", 1)], writes=["pk"])
                            P.act(lambda e: e.copy(out=kcT[:, 0:255], in_=pk[:, 0:255]), reads=["pk"], writes=["kcT"])
                        else:
                            for ct in range(2):
                                cn = 128 if ct == 0 else 127
                                for hc in range(2):
                                    P.pe(lambda e, hc=hc, ct=ct, cn=cn: e.matmul(pk[0:cn, ct * 128:(ct + 1) * 128], lhsT=ghid[:, hc, ct * 128:ct * 128 + cn], rhs=w2t[:, hc, :], start=(hc == 0), stop=(hc == 1)),
                                         reads=["w2t", ("ghid", 0), ("ghid", 1)], writes=["pk"])
                            P.act(lambda e: e.copy(out=vcm[:, 0, :], in_=pk[:, 0:128]), reads=["pk"], writes=["vcm"])
                            P.act(lambda e: e.copy(out=vcm[0:127, 1, :], in_=pk[0:127, 128:256]), reads=["pk"], writes=["vcm"])
                P.barrier()
                with contextlib.ExitStack() as SQ:
                    Pcf = [sb("Pcf%d" % ct, [128, 512], F32, SQ) for ct in range(2)]
                    pcb = [sb("pcb%d" % ct, [128, 512], BF16, SQ) for ct in range(2)]
                    rd = sb("rd", [128, 512], F32, SQ)
                    gS = sb("gS", [128, 512], F32, SQ)
                    wgt = sb("wgt", [128, 512], F32, SQ)
                    acc = sb("acc", [128, 512], F32, SQ)
                    tmpo = sb("tmpo", [128, 512], F32, SQ)
                    t0 = sb("t0", [128, 64], F32, SQ)
                    t1 = sb("t1s", [128, 64], F32, SQ)
                    m8a = sb("m8a", [128, 8], F32, SQ)
                    m8b = sb("m8b", [128, 8], F32, SQ)
                    selbb = sb("selbb", [128, 64], BF16, SQ)
                    selbT = sb("selbT", [64, 4, 128], BF16, SQ)
                    Pt = [sb("Pt%d" % i, [128, 512], BF16, SQ) for i in range(3)]
                    pS = [ps("pS%d" % i, [128, 512], F32, SQ) for i in range(2)]
                    pO = ps("pO", [128, 512], F32, SQ)
                    pD = ps("pD", [128, 512], F32, SQ)
                    pG = ps("pG", [128, 512], F32, SQ)
                    pI = ps("pI", [128, 64], F32, SQ)
                    pTt = ps("pTt", [64, 128], BF16, SQ)
                    scnt = [0]
                    pcnt = [0]

                    def gates(br, l):
                        for h in range(4):
                            n = (4 * g + h) * 3 + br
                            P.pe(lambda e, h=h, n=n, l=l: e.matmul(pG[:, h * 128:(h + 1) * 128], lhsT=selmat[:, n, :], rhs=gT[:, l * 128:(l + 1) * 128], start=True, stop=True),
                                 reads=["selmat", "gT"], writes=["pG"])
                        P.act(lambda e: e.copy(out=gS[:], in_=pG[:]), reads=["pG"], writes=["gS"])

                    def branch(br, l, keyT, vtok, kts, maskfn, use_sel):
                        qg = qT[:, 4 * g:4 * g + 4, l * 128:(l + 1) * 128]
                        qkeys = [("qT", 4 * g + h) for h in range(4)]
                        nk = len(kts)
                        for ii, kt in enumerate(kts):
                            si = scnt[0] % 2
                            scnt[0] += 1
                            pi_ = pcnt[0] % 3
                            pcnt[0] += 1
                            lk = ("kv", keyT[1], kt // 4)
                            P.pe(lambda e, si=si, kt=kt: e.matmul(pS[si][:], lhsT=keyT[0][:, kt * 128:(kt + 1) * 128], rhs=qg, start=True, stop=(not use_sel)),
                                 reads=[lk] + qkeys, writes=["pS%d" % si])
                            if use_sel:
                                P.pe(lambda e, si=si, kt=kt: e.matmul(pS[si][:], lhsT=E[:, kt * 128:(kt + 1) * 128], rhs=selbT[:].rearrange("s h q -> s (h q)"), start=False, stop=True),
                                     reads=["E", "selbT"], writes=["pS%d" % si])
                            P.act(lambda e, si=si, pi_=pi_: e.activation(out=Pt[pi_][:], in_=pS[si][:], func=AF.Exp, scale=SCALE), reads=["pS%d" % si], writes=["Pt%d" % pi_])
                            mk = maskfn(kt)
                            if mk is not None:
                                P.dve(lambda e, pi_=pi_, mk=mk: e.tensor_tensor(out=Pt[pi_][:].rearrange("k (h q) -> k h q", h=4), in0=Pt[pi_][:].rearrange("k (h q) -> k h q", h=4),
                                                                          in1=bc4(mk[0], 128), op=ALU.mult), reads=["Pt%d" % pi_, mk[1]], writes=["Pt%d" % pi_])
                            vk = ("kv", vtok[1], kt // 4)
                            P.pe(lambda e, pi_=pi_, kt=kt, ii=ii: e.matmul(pO[:], lhsT=vtok[0][:, kt * 128:(kt + 1) * 128], rhs=Pt[pi_][:], start=(ii == 0), stop=(ii == nk - 1)),
                                 reads=[vk, "Pt%d" % pi_], writes=["pO"])
                            P.pe(lambda e, pi_=pi_, ii=ii: e.matmul(pD[:], lhsT=ones_bf[:], rhs=Pt[pi_][:], start=(ii == 0), stop=(ii == nk - 1)),
                                 reads=["ones_bf", "Pt%d" % pi_], writes=["pD"])
                        gates(br, l)
                        P.dve(lambda e: e.tensor_scalar(out=rd[:], in0=pD[:], scalar1=1e-30, scalar2=None, op0=ALU.max), reads=["pD"], writes=["rd"])
                        P.dve(lambda e: e.reciprocal(out=rd[:], in_=rd[:]), reads=["rd"], writes=["rd"])
                        P.dve(lambda e: e.tensor_tensor(out=wgt[:], in0=rd[:], in1=gS[:], op=ALU.mult), reads=["rd", "gS"], writes=["wgt"])
                        P.dve(lambda e: e.tensor_tensor(out=tmpo[:], in0=pO[:], in1=wgt[:], op=ALU.mult), reads=["pO", "wgt"], writes=["tmpo"])
                        P.dve(lambda e: e.tensor_tensor(out=acc[:], in0=acc[:], in1=tmpo[:], op=ALU.add), reads=["acc", "tmpo"], writes=["acc"])

                    for l in range(8):
                        qg = qT[:, 4 * g:4 * g + 4, l * 128:(l + 1) * 128]
                        qkeys = [("qT", 4 * g + h) for h in range(4)]
                        for ct in range(2):
                            cn = 128 if ct == 0 else 127
                            si = scnt[0] % 2
                            scnt[0] += 1
                            P.pe(lambda e, si=si, ct=ct, cn=cn, qg=qg: e.matmul(pS[si][0:cn, :], lhsT=kcT[:, ct * 128:ct * 128 + cn], rhs=qg, start=True, stop=True),
                                 reads=["kcT"] + qkeys, writes=["pS%d" % si])
                            P.act(lambda e, si=si, ct=ct, cn=cn: e.activation(out=Pcf[ct][0:cn, :], in_=pS[si][0:cn, :], func=AF.Exp, scale=SCALE), reads=["pS%d" % si], writes=["Pcf%d" % ct])
                            P.dve(lambda e, ct=ct, cn=cn, l=l: e.tensor_tensor(out=Pcf[ct][0:cn, :].rearrange("k (h q) -> k h q", h=4), in0=Pcf[ct][0:cn, :].rearrange("k (h q) -> k h q", h=4),
                                                                     in1=bc4(cmpm[0:cn, ct, l, :], cn), op=ALU.mult), reads=["Pcf%d" % ct, "cmpm"], writes=["Pcf%d" % ct])
                        for ct in range(2):
                            cn = 128 if ct == 0 else 127
                            P.pe(lambda e, ct=ct, cn=cn: e.matmul(pD[:], lhsT=ones_f[0:cn, :], rhs=Pcf[ct][0:cn, :], start=(ct == 0), stop=(ct == 1)),
                                 reads=["ones_f", "Pcf%d" % ct], writes=["pD"])
                        P.dve(lambda e: e.tensor_scalar(out=rd[:], in0=pD[:], scalar1=1e-30, scalar2=None, op0=ALU.max), reads=["pD"], writes=["rd"])
                        P.dve(lambda e: e.reciprocal(out=rd[:], in_=rd[:]), reads=["rd"], writes=["rd"])
                        for ct in range(2):
                            cn = 128 if ct == 0 else 127
                            P.dve(lambda e, ct=ct, cn=cn: e.tensor_tensor(out=Pcf[ct][0:cn, :], in0=Pcf[ct][0:cn, :], in1=rd[0:cn, :], op=ALU.mult), reads=["Pcf%d" % ct, "rd"], writes=["Pcf%d" % ct])
                            P.act(lambda e, ct=ct, cn=cn: e.copy(out=pcb[ct][0:cn, :], in_=Pcf[ct][0:cn, :]), reads=["Pcf%d" % ct], writes=["pcb%d" % ct])
                        for ct in range(2):
                            cn = 128 if ct == 0 else 127
                            P.pe(lambda e, ct=ct, cn=cn: e.matmul(pO[:], lhsT=vcm[0:cn, ct, :], rhs=pcb[ct][0:cn, :], start=(ct == 0), stop=(ct == 1)),
                                 reads=["vcm", "pcb%d" % ct], writes=["pO"])
                        cnt8 = 0
                        for r in range(4):
                            for ct in range(2):
                                cn = 128 if ct == 0 else 127
                                P.pe(lambda e, ct=ct, cn=cn, r=r, cnt8=cnt8: e.matmul(pI[:], lhsT=Pcf[ct][0:cn, r * 128:(r + 1) * 128], rhs=ovl[0:cn, ct, :], start=(cnt8 == 0), stop=(cnt8 == 7)),
                                     reads=["Pcf%d" % ct, "ovl"], writes=["pI"])
                                cnt8 += 1
                        gates(0, l)
                        P.dve(lambda e: e.tensor_tensor(out=acc[:], in0=pO[:], in1=gS[:], op=ALU.mult), reads=["pO", "gS"], writes=["acc"])
                        dbg_here = (C.stop == 22 and g == 0 and l == 1)

                        def dump(nm, tile_, key, shape, dt=F32):
                            C.dbg[nm] = nc.dram_tensor("dbg_" + nm, list(shape), dt, kind="ExternalOutput").ap()
                            C.dbg_ops.append(P.dma(lambda e: e.dma_start(out=C.dbg[nm][:, :], in_=tile_), reads=[key], writes=["dd_" + nm]))
                        if dbg_here:
                            dump("acc0", acc[:], "acc", [128, 512])
                            dump("kcT", kcT[:], "kcT", [128, 256], BF16)
                            dump("vcm", vcm[:].rearrange("p a b -> p (a b)"), "vcm", [128, 256], BF16)
                            dump("gS0", gS[:], "gS", [128, 512])
                        P.dve(lambda e, l=l: e.tensor_tensor(out=t0[:], in0=pI[:], in1=selA[:, l, :], op=ALU.mult), reads=["pI", "selA"], writes=["t0"])
                        P.dve(lambda e, l=l: e.tensor_tensor(out=t0[:], in0=t0[:], in1=selB[:, l, :], op=ALU.add), reads=["t0", "selB"], writes=["t0"])
                        P.dve(lambda e, l=l: e.tensor_tensor(out=t0[:], in0=t0[:], in1=selF[:, l, :], op=ALU.max), reads=["t0", "selF"], writes=["t0"])
                        P.dve(lambda e: e.max(out=m8a[:], in_=t0[:]), reads=["t0"], writes=["m8a"])
                        P.dve(lambda e: e.match_replace(out=t1[:], in_to_replace=m8a[:], in_values=t0[:], imm_value=-1e30), reads=["t0", "m8a"], writes=["t1s"])
                        P.dve(lambda e: e.max(out=m8b[:], in_=t1[:]), reads=["t1s"], writes=["m8b"])
                        P.dve(lambda e: e.tensor_scalar(out=t1[:], in0=t0[:], scalar1=m8b[:, 7:8], scalar2=-1.0, op0=ALU.is_ge, op1=ALU.add), reads=["t0", "m8b"], writes=["t1s"])
                        P.dve(lambda e: e.tensor_scalar(out=selbb[:], in0=t1[:], scalar1=-BIGNEG, scalar2=None, op0=ALU.mult), reads=["t1s"], writes=["selbb"])
                        P.pe(lambda e: e.transpose(out=pTt[:], in_=selbb[:], identity=ident[:]), reads=["selbb", "ident"], writes=["pTt"])
                        for h in range(4):
                            P.act(lambda e, h=h: e.copy(out=selbT[:, h, :], in_=pTt[:]), reads=["pTt"], writes=["selbT"])
                        if C.stop == 22 and g == 0 and l == 1:
                            C.dbg["t0"] = nc.dram_tensor("dbg_t0", [128, 64], F32, kind="ExternalOutput").ap()
                            C.dbg["selb"] = nc.dram_tensor("dbg_selb", [128, 64], BF16, kind="ExternalOutput").ap()
                            C.dbg_ops.append(P.dma(lambda e: e.dma_start(out=C.dbg["t0"][:, :], in_=t0[:]), reads=["t0"], writes=["dd1"]))
                            C.dbg_ops.append(P.dma(lambda e: e.dma_start(out=C.dbg["selb"][:, :], in_=selbb[:]), reads=["selbb"], writes=["dd2"]))
                        branch(1, l, (KV["ks"], "ks"), (KV["vs"], "vs"), list(range(4 * l + 4)),
                               lambda kt, l=l: ((selcm[:, kt - 4 * l, :], "selcm") if kt >= 4 * l else None), True)
                        if dbg_here:
                            dump("acc1", acc[:], "acc", [128, 512])
                        branch(2, l, (KV["kw"], "kw"), (KV["vw"], "vw"), list(range(max(0, 4 * l - 4), 4 * l + 4)),
                               lambda kt, l=l: (winm[:, kt - 4 * l + 4, :], "winm"), False)
                        if dbg_here:
                            dump("acc2", acc[:], "acc", [128, 512])
                        for h in range(4):
                            P.act(lambda e, h=h, l=l, g=g: e.copy(out=oaT[:, 4 * g + h, l * 128:(l + 1) * 128], in_=acc[:, h * 128:(h + 1) * 128]), reads=["acc"], writes=[("oaT", 4 * g + h)])
                P.barrier()
        with contextlib.ExitStack() as SN:
            ag = sb("ag", [128, 8], F32, SN)
            rstdb = sb("a_rstdb", [128, NT], F32, SN)
            sqb = [sb("a_sqb%d" % i, [128, NT], BF16, SN) for i in range(2)]
            pss = [ps("a_pss%d" % hf, [128, 512], F32, SN) for hf in range(2)]
            with nc.allow_non_contiguous_dma(reason="tiny"):
                P.dma(lambda e: e.dma_start(out=ag[:], in_=W["attn_out_g"].rearrange("o (b p) -> p (o b)", p=128), allow_slow_non_contiguous=True), writes=["ag"])
            for h in range(8):
                i = h % 2
                P.act(lambda e, h=h, i=i: e.activation(out=sqb[i][:], in_=oaT[:, h, :], func=AF.Square), reads=[("oaT", h)], writes=["a_sqb%d" % i])
                for hf in range(2):
                    P.pe(lambda e, h=h, i=i, hf=hf: e.matmul(pss[hf][:], lhsT=ones_bf[:], rhs=sqb[i][:, hf * 512:(hf + 1) * 512], start=(h == 0), stop=(h == 7)),
                         reads=["ones_bf", "a_sqb%d" % i], writes=["a_pss%d" % hf])
            for hf in range(2):
                sl = slice(hf * 512, (hf + 1) * 512)
                P.act(lambda e, hf=hf, sl=sl: e.activation(out=rstdb[:, sl], in_=pss[hf][:], func=AF.Sqrt, scale=1.0 / 1024, bias=epsT[:, 0:1]), reads=["a_pss%d" % hf, "m_eps"], writes=["a_rstdb"])
            P.dve(lambda e: e.reciprocal(out=rstdb[:], in_=rstdb[:]), reads=["a_rstdb"], writes=["a_rstdb"])
            ATall = [("AT", t) for t in range(NTILE)]
            for h in range(8):
                P.dve(lambda e, h=h: e.scalar_tensor_tensor(out=AT[:, h, :], in0=oaT[:, h, :], scalar=ag[:, h:h + 1], in1=rstdb[:], op0=ALU.mult, op1=ALU.mult),
                      reads=[("oaT", h), "ag", "a_rstdb"], writes=ATall)
                P.pool(lambda e, h=h: e.tensor_copy(out=AT[:, 8 + h, :], in_=orecT[:, h, :]), reads=[("orecT", h)], writes=ATall)
    P.barrier()


def outproj(C):
    nc, P, W, sb, ps, AT = C.nc, C.P, C.W, C.sb, C.ps, C.AT
    contextlib = C.contextlib
    ATall = [("AT", t) for t in range(NTILE)]
    with contextlib.ExitStack() as SO:
        YT = sb("YT", [128, KD, NT], BF16, SO)
        with contextlib.ExitStack() as S1:
            wp = [sb("wo%d" % i, [128, KD, 512], BF16, S1) for i in range(2)]
            py = [[ps("po%d%d" % (i, hf), [128, 512], F32, S1) for hf in range(2)] for i in range(2)]
            wv = W["w_out"].rearrange("(kc p) m -> p kc m", p=128)
            cnt = 0
            for pi in range(4):
                b = pi % 2
                P.dma(lambda e, pi=pi, b=b: e.dma_start(out=wp[b][:], in_=wv[:, :, pi * 512:(pi + 1) * 512]), writes=["wo%d" % b], q="pool")
                for mc in range(4):
                    m = pi * 4 + mc
                    r = cnt % 2
                    cnt += 1
                    for hf in range(2):
                        for k in range(KD):
                            P.pe(lambda e, k=k, hf=hf, r=r, b=b, mc=mc: e.matmul(py[r][hf][:], lhsT=wp[b][:, k, mc * 128:(mc + 1) * 128], rhs=AT[:, k, hf * 512:(hf + 1) * 512],
                                                                         start=(k == 0), stop=(k == KD - 1)), reads=["wo%d" % b] + ATall[hf * 4:hf * 4 + 4], writes=["po%d%d" % (r, hf)])
                        P.act(lambda e, m=m, hf=hf, r=r: e.copy(out=YT[:, m, hf * 512:(hf + 1) * 512], in_=py[r][hf][:]), reads=["po%d%d" % (r, hf)],
                              writes=[("YT", hf * 4 + q) for q in range(4)])
        C.post("mx", YT, C.h1d, W["mix_post_g"], 1.0, C.h2d, W["ff2_pre_g"])


def ple(C):
    nc, P, W, sb, ps, AT, ident = C.nc, C.P, C.W, C.sb, C.ps, C.AT, C.ident
    contextlib = C.contextlib
    ATall = [("AT", t) for t in range(NTILE)]
    with contextlib.ExitStack() as SO:
        YT = sb("YTp", [128, KD, NT], BF16, SO)
        with contextlib.ExitStack() as S1:
            pT = sb("pTp", [128, 2, NT], BF16, S1)
            ptl = [sb("ptl%d" % i, [128, 256], F32, S1) for i in range(2)]
            ptb = [sb("ptb%d" % i, [128, 256], BF16, S1) for i in range(2)]
            wpj = sb("wpj", [128, 2, D], BF16, S1)
            sg = [sb("psg%d" % i, [128, 512], F32, S1) for i in range(2)]
            wp = [sb("wg%d" % i, [128, KD, 512], BF16, S1) for i in range(2)]
            ppt = [ps("ppt%d" % i, [128, 2, 128], BF16, S1) for i in range(2)]
            pa = [[ps("pa%d%d" % (i, hf), [128, 512], F32, S1) for hf in range(2)] for i in range(1)]
            pb = [[ps("pbp%d%d" % (i, hf), [128, 512], F32, S1) for hf in range(2)] for i in range(1)]
            P.dma(lambda e: e.dma_start(out=wpj[:], in_=W["w_ple_proj"].rearrange("(c p) m -> p c m", p=128)), writes=["wpj"], q="pool")
            for t in range(NTILE):
                i = t % 2
                P.dma(lambda e, t=t, i=i: e.dma_start(out=ptl[i][:], in_=C.pin[t * 128:(t + 1) * 128, :]), writes=["ptl%d" % i])
                P.dve(lambda e, i=i: e.tensor_copy(out=ptb[i][:], in_=ptl[i][:]), reads=["ptl%d" % i], writes=["ptb%d" % i])
                for c in range(2):
                    P.pe(lambda e, i=i, c=c: e.transpose(out=ppt[i][:, c, :], in_=ptb[i][:, c * 128:(c + 1) * 128], identity=ident[:]), reads=["ptb%d" % i, "ident"], writes=["ppt%d" % i])
                P.act(lambda e, i=i, t=t: e.copy(out=pT[:, :, t * 128:(t + 1) * 128], in_=ppt[i][:]), reads=["ppt%d" % i], writes=[("pTp", t)])
            wv = W["w_ple_gate"].rearrange("(kc p) m -> p kc m", p=128)
            pTall = [("pTp", t) for t in range(NTILE)]
            for pi in range(4):
                b = pi % 2
                P.dma(lambda e, pi=pi, b=b: e.dma_start(out=wp[b][:], in_=wv[:, :, pi * 512:(pi + 1) * 512]), writes=["wg%d" % b], q="pool")
                for mc in range(4):
                    m = pi * 4 + mc
                    for hf in range(2):
                        for k in range(KD):
                            P.pe(lambda e, k=k, hf=hf, b=b, mc=mc: e.matmul(pa[0][hf][:], lhsT=wp[b][:, k, mc * 128:(mc + 1) * 128], rhs=AT[:, k, hf * 512:(hf + 1) * 512],
                                                                    start=(k == 0), stop=(k == KD - 1)), reads=["wg%d" % b] + ATall[hf * 4:hf * 4 + 4], writes=["pa0%d" % hf])
                        for c in range(2):
                            P.pe(lambda e, c=c, hf=hf, m=m: e.matmul(pb[0][hf][:], lhsT=wpj[:, c, m * 128:(m + 1) * 128], rhs=pT[:, c, hf * 512:(hf + 1) * 512],
                                                              start=(c == 0), stop=(c == 1)), reads=["wpj"] + pTall[hf * 4:hf * 4 + 4], writes=["pbp0%d" % hf])
                        P.act(lambda e, hf=hf: e.activation(out=sg[hf][:], in_=pa[0][hf][:], func=AF.Sigmoid), reads=["pa0%d" % hf], writes=["psg%d" % hf])
                        P.dve(lambda e, hf=hf, m=m: e.tensor_tensor(out=YT[:, m, hf * 512:(hf + 1) * 512], in0=sg[hf][:], in1=pb[0][hf][:], op=ALU.mult),
                              reads=["psg%d" % hf, "pbp0%d" % hf], writes=[("YTp", hf * 4 + q) for q in range(4)])
        C.post("pl", YT, C.h3d, W["ple_post_g"], 1.0, C.out, None)


def host_consts(j):
    import ml_dtypes
    bf = ml_dtypes.bfloat16
    c = {}
    d = np.arange(128)
    half = 16
    inv_freq = (500000.0 ** (-np.arange(half, dtype=np.float32) / half)).astype(np.float32)
    rope = np.zeros((128, 4), np.float32)
    rope[:32, 0] = inv_freq[d[:32] % 16]
    rope[:16, 1] = -1.0
    rope[16:32, 1] = 1.0
    rope[:, 2] = 1.0
    c["c_rope"] = rope
    perm = np.zeros((128, 128), np.float32)
    for m in range(128):
        k = m + 16 if m < 16 else (m - 16 if m < 32 else m)
        perm[k, m] = 1.0
    c["c_perm"] = perm
    oh = np.zeros((128, 4), np.float32)
    oh[:, j] = 1.0
    c["c_oh"] = oh
    q = np.arange(128)
    kk = np.arange(128)
    cm = np.zeros((128, 2, 8, 128), np.float32)
    for ct in range(2):
        cg = ct * 128 + kk
        for l in range(8):
            t = (4 * l + j) * 128 + q
            cm[:, ct, l, :] = ((16 * cg[:, None] + 31 <= t[None, :]) & (cg[:, None] < 255))
    c["c_cmpm"] = cm.reshape(128, -1).astype(bf)
    tri = (kk[:, None] <= q[None, :]).astype(np.float32)
    triu = (kk[:, None] > q[None, :]).astype(np.float32)
    sc = np.zeros((128, 4, 128), np.float32)
    for m in range(4):
        sc[:, m, :] = 1.0 if m < j else (tri if m == j else 0.0)
    c["c_selcm"] = sc.reshape(128, -1).astype(bf)
    wm = np.zeros((128, 8, 128), np.float32)
    for m in range(8):
        dd = m - 4 - j
        if dd == 0:
            wm[:, m, :] = tri
        elif dd == -4:
            wm[:, m, :] = triu
        elif -4 < dd < 0:
            wm[:, m, :] = 1.0
    c["c_winm"] = wm.reshape(128, -1).astype(bf)
    sA = np.zeros((128, 8, 64), np.float32)
    sB = np.zeros((128, 8, 64), np.float32)
    sF = np.zeros((128, 8, 64), np.float32)
    s = np.arange(64)
    for l in range(8):
        t = (4 * l + j) * 128 + q
        valid = (64 * s[None, :] <= t[:, None])
        forced = (s[None, :] == (t // 64)[:, None]) | (s[None, :] == 0)
        sA[:, l, :] = valid
        sB[:, l, :] = (valid.astype(np.float32) - 1.0) * 1e4
        sF[:, l, :] = np.where(forced, 1e4, -3e4)
    c["c_selA"] = sA.reshape(128, -1)
    c["c_selB"] = sB.reshape(128, -1)
    c["c_selF"] = sF.reshape(128, -1)
    key = np.arange(4096)
    c["c_E"] = (key[None, :] // 64 == s[:, None]).astype(np.float32).astype(bf)
    ov = np.zeros((128, 2, 64), np.float32)
    for ct in range(2):
        cg = ct * 128 + kk
        st_ = 16 * cg
        en_ = st_ + 31
        ov[:, ct, :] = ((st_[:, None] < 64 * s[None, :] + 64) & (en_[:, None] >= 64 * s[None, :]) & (cg[:, None] < 255))
    c["c_ovl"] = ov.reshape(128, -1)
    sm = np.zeros((24, 24, 128), np.float32)
    for n in range(24):
        sm[n, n, :] = 1.0
    c["c_selmat"] = sm.reshape(24, -1)
    return c


_orig_make_in_maps = make_in_maps


def make_in_maps(inputs):
    maps = _orig_make_in_maps(inputs)
    for c in range(8):
        maps[c].update(host_consts(c % 4))
    return maps


_NC_CACHE = {}


def kernel(**inputs):
    maps = make_in_maps(inputs)
    if "nc" not in _NC_CACHE:
        _NC_CACHE["nc"] = build()
    nc = _NC_CACHE["nc"]
    res = run_bass_kernel_spmd(nc, maps, core_ids=list(range(8)))
    return unshard([r["out"] for r in res.results])
```

```python
import numpy as np
import concourse.bass as bass
import concourse.mybir as mybir
from concourse.bass_utils import run_bass_kernel_spmd

F32 = mybir.dt.float32
BF16 = mybir.dt.bfloat16
I32 = mybir.dt.int32
AF = mybir.ActivationFunctionType
ALU = mybir.AluOpType
AX = mybir.AxisListType

COMPUTE = ("pe", "act", "dve", "pool")
NRING = 12


class Op:
    __slots__ = ("eng", "fn", "deps", "is_dma", "signal", "idx", "ring", "ringval", "prev_ring", "is_cc", "seg", "dur", "fin", "bdeps")

    def __init__(self, eng, fn, is_dma):
        self.eng = eng
        self.fn = fn
        self.is_dma = is_dma
        self.deps = []
        self.bdeps = []
        self.signal = 0
        self.ring = None
        self.ringval = 0
        self.prev_ring = None
        self.idx = 0
        self.is_cc = False
        self.seg = 0
        self.dur = 0.5
        self.fin = 0.0


DEFAULT_DUR = {"pe": 0.28, "act": 0.7, "dve": 0.9, "pool": 1.2, "sp": 2.5}
SCHED = True
WINDOW = 48


class Prog:
    def __init__(self, nc):
        self.nc = nc
        self.ops = {k: [] for k in ("pe", "act", "dve", "pool", "sp")}
        self.last_w = {}
        self.readers = {}
        self.nops = 0
        self.seg = 0

    def barrier(self):
        self.seg += 1

    def _add(self, eng, fn, reads, writes, is_dma, d=None):
        op = Op(eng, fn, is_dma)
        op.idx = self.nops
        op.seg = self.seg
        op.dur = DEFAULT_DUR[eng] if d is None else d
        self.nops += 1
        deps = {}
        for r in reads:
            w = self.last_w.get(r)
            if w is not None:
                deps[id(w)] = w
        for wr in writes:
            w = self.last_w.get(wr)
            if w is not None:
                deps[id(w)] = w
            for rd in self.readers.get(wr, ()):
                deps[id(rd)] = rd
        for dd in deps.values():
            if dd is op:
                continue
            op.deps.append(dd)
        for r in reads:
            self.readers.setdefault(r, []).append(op)
        for wr in writes:
            self.last_w[wr] = op
            self.readers[wr] = []
        self.ops[eng].append(op)
        return op

    def pe(self, fn, reads=(), writes=(), d=None):
        return self._add("pe", fn, reads, writes, False, d)

    def act(self, fn, reads=(), writes=(), d=None):
        return self._add("act", fn, reads, writes, False, d)

    def dve(self, fn, reads=(), writes=(), d=None):
        return self._add("dve", fn, reads, writes, False, d)

    def pool(self, fn, reads=(), writes=(), d=None):
        return self._add("pool", fn, reads, writes, False, d)

    def dma(self, fn, reads=(), writes=(), q="sp", d=None):
        return self._add(q, fn, reads, writes, True, d)

    def cc(self, fn, reads=(), writes=()):
        op = self._add("pool", fn, reads, writes, True, 15.0)
        op.is_cc = True
        return op

    def _schedule(self):
        nseg = self.seg + 1
        byseg = [{e: [] for e in self.ops} for _ in range(nseg)]
        for e, lst in self.ops.items():
            for op in lst:
                byseg[op.seg][e].append(op)
        new = {e: [] for e in self.ops}
        LAT_X, LAT_S = 1.2, 0.25
        t_base = 0.0
        for sg in range(nseg):
            queues = byseg[sg]
            if not SCHED:
                for e in queues:
                    new[e].extend(queues[e])
                continue
            pos = {e: 0 for e in queues}
            done = set()
            sched_flag = {e: [False] * len(queues[e]) for e in queues}
            free_at = {e: t_base for e in queues}
            remaining = sum(len(q) for q in queues.values())
            tmax = t_base
            while remaining:
                best = None
                for e, q in queues.items():
                    n = len(q)
                    p = pos[e]
                    while p < n and sched_flag[e][p]:
                        p += 1
                    pos[e] = p
                    cnt = 0
                    i = p
                    while i < n and cnt < WINDOW:
                        if not sched_flag[e][i]:
                            cnt += 1
                            op = q[i]
                            ok = True
                            rdy = free_at[e]
                            for dd in op.deps:
                                if dd.seg == sg:
                                    if id(dd) not in done:
                                        ok = False
                                        break
                                    lat = LAT_S if (dd.eng == e and not dd.is_dma) else LAT_X
                                    if dd.eng == "pe" and e == "pe":
                                        lat = 0.0
                                    tt = dd.fin + lat
                                    if tt > rdy:
                                        rdy = tt
                            if ok:
                                key = (rdy, op.idx)
                                if best is None or key < best[0]:
                                    best = (key, e, i, op)
                                if rdy <= free_at[e]:
                                    break
                        i += 1
                (rdy, _), e, i, op = best
                if op.is_dma:
                    free_at[e] = rdy + 0.15
                    op.fin = rdy + op.dur
                else:
                    free_at[e] = rdy + op.dur
                    op.fin = free_at[e]
                tmax = max(tmax, op.fin)
                sched_flag[e][i] = True
                done.add(id(op))
                new[e].append(op)
                remaining -= 1
            t_base = tmax + 2.0
        self.ops = new

    def emit(self, final_wait_ops=()):
        nc = self.nc
        self._schedule()
        nseg = self.seg + 1
        last_compute = {}
        first_in_seg = {}
        dmas_in_seg = [[] for _ in range(nseg)]
        lastc_upto = [dict() for _ in range(nseg)]
        for e, lst in self.ops.items():
            for op in lst:
                if (op.seg, e) not in first_in_seg:
                    first_in_seg[(op.seg, e)] = op
                if op.is_dma:
                    dmas_in_seg[op.seg].append(op)
                else:
                    lastc_upto[op.seg][e] = op
        run_last = {}
        pend = {e: [] for e in self.ops}
        for sg in range(1, nseg):
            for e2, op2 in lastc_upto[sg - 1].items():
                run_last[e2] = op2
            bd = list(run_last.values()) + dmas_in_seg[sg - 1]
            for e in self.ops:
                pend[e] = pend[e] + bd
                f = first_in_seg.get((sg, e))
                if f is not None:
                    f.bdeps = [d for d in pend[e] if not (d.eng == e and not d.is_dma and not f.is_dma)]
                    pend[e] = []
        needed = set()
        for e, lst in self.ops.items():
            for op in lst:
                for d in op.deps:
                    if not (d.eng == "pe" and e == "pe" and not d.is_dma):
                        needed.add(id(d))
                for d in op.bdeps:
                    needed.add(id(d))
        for op in final_wait_ops:
            needed.add(id(op))
        ringcount = {}
        for e, lst in self.ops.items():
            n = 0
            k = 0
            last_on_ring = {}
            for op in lst:
                if id(op) not in needed:
                    continue
                if op.is_cc:
                    op.ring = ("cc", op.idx)
                    op.ringval = 1
                    op.prev_ring = None
                elif op.is_dma:
                    slot = k % NRING
                    k += 1
                    op.ring = (e, slot)
                    ringcount[(e, slot)] = ringcount.get((e, slot), 0) + 16
                    op.ringval = ringcount[(e, slot)]
                    op.prev_ring = last_on_ring.get(slot)
                    last_on_ring[slot] = op
                else:
                    n += 1
                    op.signal = n
        import contextlib
        with contextlib.ExitStack() as st:
            csem = {e: st.enter_context(nc.semaphore("s_" + e)) for e in ("pe", "act", "dve", "pool")}
            rsem = {}
            for e in ("sp", "pool"):
                for s_ in range(NRING):
                    rsem[(e, s_)] = st.enter_context(nc.semaphore("r_%s_%d" % (e, s_)))
            for e, lst in self.ops.items():
                for op in lst:
                    if op.is_cc and op.ring is not None:
                        rsem[op.ring] = st.enter_context(nc.semaphore("cc_%d" % op.idx))
            block = st.enter_context(nc.Block())

            def run(ename):
                def body(eng):
                    waited = {}
                    lst = self.ops[ename]
                    for op in lst:
                        deps = [d for d in op.deps if not (d.eng == "pe" and ename == "pe" and not d.is_dma)] + list(op.bdeps)
                        if op.is_dma and op.prev_ring is not None:
                            deps.append(op.prev_ring)
                        need = {}
                        for d in deps:
                            if d.is_dma:
                                key = ("r",) + d.ring
                                val = d.ringval
                                sem = rsem[d.ring]
                            else:
                                key = ("c", d.eng)
                                val = d.signal
                                sem = csem[d.eng]
                            assert val > 0, (ename, d.eng)
                            if need.get(key, (0, None))[0] < val:
                                need[key] = (val, sem)
                        for key, (val, sem) in need.items():
                            if waited.get(key, 0) >= val:
                                continue
                            waited[key] = val
                            eng.wait_ge(sem, val)
                        ins = op.fn(eng)
                        if op.is_cc:
                            if op.ring is not None:
                                ins.then_inc(rsem[op.ring], 1)
                        elif op.is_dma:
                            if op.ring is not None:
                                ins.then_inc(rsem[op.ring], 16)
                        elif op.signal:
                            ins.then_inc(csem[ename], 1)
                    if ename == "sp":
                        for d in final_wait_ops:
                            if d.is_dma:
                                eng.wait_ge(rsem[d.ring], d.ringval)
                            else:
                                eng.wait_ge(csem[d.eng], d.signal)
                return body

            block.tensor(run("pe"))
            block.scalar(run("act"))
            block.vector(run("dve"))
            block.gpsimd(run("pool"))
            block.sync(run("sp"))


D = 2048
NT = 1024
NTILE = 8
DFF = 5632
NF = DFF // 128
KD = D // 128
EPS = 1e-6
IN_WIDTH = 4632


def bcast_rows(ap, n, parts=128):
    return bass.AP(ap.tensor, ap.offset, [[0, parts], [1, n]])


class Ctx:
    pass


def build(stop=99, debug=False):
    import contextlib
    nc = bass.Bass("TRN2", target_bir_lowering=False)
    C = Ctx()
    C.nc = nc
    P = Prog(nc)
    C.P = P

    def din(name, shape, dt=F32):
        return nc.dram_tensor(name, list(shape), dt, kind="ExternalInput").ap()

    x = din("x", [NT, D])
    pin = din("p", [NT, 256])
    pos = din("pos", [1, NT], I32)
    WSHAPES = dict([("ff1_pre_g", [1, D]), ("ff1_post_g", [1, D]), ("ff1_w_gate", [D, DFF]), ("ff1_w_up", [D, DFF]),
                    ("ff1_w_down", [DFF, D]), ("mix_pre_g", [1, D]), ("mix_post_g", [1, D]), ("w_in", [D, IN_WIDTH]),
                    ("cmp_pos_k", [32, 128]), ("cmp_pos_v", [32, 128]), ("cmp_k_w1", [4096, 256]), ("cmp_k_w2", [256, 128]),
                    ("cmp_v_w1", [4096, 256]), ("cmp_v_w2", [256, 128]), ("nsa_gate_b", [1, 24]),
                    ("conv_w", [4, 1024]), ("conv_b", [1, 1024]), ("rg_w_a", [8, 128, 128]), ("rg_b_a", [1, 1024]),
                    ("rg_w_i", [8, 128, 128]), ("rg_b_i", [1, 1024]), ("rg_lambda", [1, 1024]),
                    ("attn_out_g", [1, 1024]), ("rec_out_g", [1, 1024]), ("w_out", [D, D]),
                    ("ff2_pre_g", [1, D]), ("ff2_post_g", [1, D]), ("ff2_w_gate", [D, DFF]), ("ff2_w_up", [D, DFF]),
                    ("ff2_w_down", [DFF, D]), ("ple_pre_g", [1, D]), ("ple_post_g", [1, D]),
                    ("w_ple_gate", [D, D]), ("w_ple_proj", [256, D])])

    class LazyW(dict):
        def __missing__(self, nm):
            v = din(nm, WSHAPES[nm])
            self[nm] = v
            return v
    W = LazyW()
    C.W = W
    out = nc.dram_tensor("out", [NT, D], F32, kind="ExternalOutput").ap()
    h1d = nc.dram_tensor("h1d", [NT, D], F32, kind="Internal").ap()
    h2d = nc.dram_tensor("h2d", [NT, D], F32, kind="Internal").ap()
    h3d = nc.dram_tensor("h3d", [NT, D], F32, kind="Internal").ap()
    dbg = {}
    if debug:
        dbg["uT"] = nc.dram_tensor("dbg_uT", [128, KD, NT], BF16, kind="ExternalOutput").ap()

    finals = []
    with contextlib.ExitStack() as st:
        used_names = {}

        def uniq(name):
            n = used_names.get(name, 0)
            used_names[name] = n + 1
            return name if n == 0 else "%s_v%d" % (name, n)

        def sb(name, shape, dt, stack=st):
            return stack.enter_context(nc.sbuf_tensor(uniq(name), list(shape), dt))

        def ps(name, shape, dt, stack=st):
            return stack.enter_context(nc.psum_tensor(uniq(name), list(shape), dt))

        identf = sb("identf", [128, 128], F32)
        ident = sb("ident", [128, 128], BF16)
        AT = sb("AT", [128, KD, NT], BF16)
        P.pool(lambda e: e.memset(identf[:], 1.0), writes=["identf"])
        P.pool(lambda e: e.affine_select(out=identf[:], in_=identf[:], pattern=[[-1, 128]], compare_op=ALU.is_equal,
                                         fill=0.0, base=0, channel_multiplier=1), reads=["identf"], writes=["identf"])
        P.dve(lambda e: e.tensor_copy(out=ident[:], in_=identf[:]), reads=["identf"], writes=["ident"])
        C.ident, C.identf, C.AT = ident, identf, AT
        C.sb, C.ps = sb, ps

        def norm_transpose(S, tg, ht, hkey, gb, gkey, t, scr):
            sq, ss, ub, pT = scr["sq"], scr["ss"], scr["ub"], scr["pT"]
            P.act(lambda e: e.activation(out=sq[:], in_=ht[:], func=AF.Square, accum_out=ss[:, 0:1]),
                  reads=[hkey], writes=[tg + "sq", tg + "ss"])
            P.act(lambda e: e.activation(out=ss[:, 1:2], in_=ss[:, 0:1], func=AF.Sqrt, scale=1.0 / D, bias=scr["eps"][:, 0:1]),
                  reads=[tg + "ss", scr["epskey"]], writes=[tg + "ss1"])
            P.dve(lambda e: e.reciprocal(out=ss[:, 2:3], in_=ss[:, 1:2]), reads=[tg + "ss1"], writes=[tg + "ss2"])
            P.dve(lambda e: e.scalar_tensor_tensor(out=ub[:], in0=ht[:], scalar=ss[:, 2:3], in1=gb[:], op0=ALU.mult, op1=ALU.mult),
                  reads=[hkey, tg + "ss2", gkey], writes=[tg + "ub"])
            for k in range(KD):
                P.pe(lambda e, k=k: e.transpose(out=pT[:, k, :], in_=ub[:, k * 128:(k + 1) * 128], identity=ident[:]),
                     reads=[tg + "ub", "ident"], writes=[tg + "pT"])
            P.act(lambda e: e.copy(out=AT[:, :, t * 128:(t + 1) * 128], in_=pT[:]), reads=[tg + "pT"], writes=[("AT", t)])

        C.norm_transpose = norm_transpose

        def ffn(tg, h_src, pre_g, post_g, wg, wu, wd, h_dst, next_g, first):
            with contextlib.ExitStack() as S:
                hid = sb(tg + "hid", [128, NF, NT], BF16, S)
                epsT = sb(tg + "eps", [128, 1], F32, S)
                P.pool(lambda e: e.memset(epsT[:], EPS), writes=[tg + "eps"])
                if first:
                    with contextlib.ExitStack() as S0:
                        gb = sb(tg + "gb", [128, D], F32, S0)
                        P.dma(lambda e: e.dma_start(out=gb[:], in_=bcast_rows(pre_g, D)), writes=[tg + "gb"])
                        scr = [dict(sq=sb(tg + "sq%d" % i, [128, D], BF16, S0), ss=sb(tg + "ss%d" % i, [128, 4], F32, S0),
                                    ub=sb(tg + "ub%d" % i, [128, D], BF16, S0), pT=ps(tg + "pT%d" % i, [128, KD, 128], BF16, S0),
                                    eps=epsT, epskey=tg + "eps") for i in range(2)]
                        hts = [sb(tg + "ht%d" % i, [128, D], F32, S0) for i in range(2)]
                        for t in range(NTILE):
                            i = t % 2
                            P.dma(lambda e, t=t, i=i: e.dma_start(out=hts[i][:], in_=h_src[t * 128:(t + 1) * 128, :]),
                                  writes=[tg + "ht%d" % i])
                            norm_transpose(S0, tg + "n%d" % i, hts[i], tg + "ht%d" % i, gb, tg + "gb", t, scr[i])
                P.barrier()
                with contextlib.ExitStack() as S1:
                    wgp = [sb(tg + "wgp%d" % i, [128, KD, 512], BF16, S1) for i in range(2)]
                    wup = [sb(tg + "wup%d" % i, [128, KD, 512], BF16, S1) for i in range(2)]
                    sgt = [sb(tg + "sg%d" % i, [128, 512], BF16, S1) for i in range(2)]
                    pg = [[ps(tg + "pg%d%d" % (i, hf), [128, 512], F32, S1) for hf in range(2)] for i in range(2)]
                    pu = [[ps(tg + "pu%d%d" % (i, hf), [128, 512], F32, S1) for hf in range(2)] for i in range(2)]
                    wgv = wg.rearrange("(kc p) m -> p kc m", p=128)
                    wuv = wu.rearrange("(kc p) m -> p kc m", p=128)
                    ATall = [("AT", t) for t in range(NTILE)]
                    for pi in range(NF // 4):
                        b = pi % 2
                        P.dma(lambda e, pi=pi, b=b: e.dma_start(out=wgp[b][:], in_=wgv[:, :, pi * 512:(pi + 1) * 512]),
                              writes=[tg + "wgp%d" % b], q="pool")
                        P.dma(lambda e, pi=pi, b=b: e.dma_start(out=wup[b][:], in_=wuv[:, :, pi * 512:(pi + 1) * 512]),
                              writes=[tg + "wup%d" % b], q="pool")
                        for fl in range(4):
                            f = pi * 4 + fl
                            r = f % 2
                            for hf in range(2):
                                for k in range(KD):
                                    P.pe(lambda e, k=k, hf=hf, r=r, b=b, fl=fl: e.matmul(
                                        pg[r][hf][:], lhsT=wgp[b][:, k, fl * 128:(fl + 1) * 128], rhs=AT[:, k, hf * 512:(hf + 1) * 512],
                                        start=(k == 0), stop=(k == KD - 1)),
                                        reads=[tg + "wgp%d" % b] + ATall[hf * 4:hf * 4 + 4], writes=[tg + "pg%d%d" % (r, hf)])
                                for k in range(KD):
                                    P.pe(lambda e, k=k, hf=hf, r=r, b=b, fl=fl: e.matmul(
                                        pu[r][hf][:], lhsT=wup[b][:, k, fl * 128:(fl + 1) * 128], rhs=AT[:, k, hf * 512:(hf + 1) * 512],
                                        start=(k == 0), stop=(k == KD - 1)),
                                        reads=[tg + "wup%d" % b] + ATall[hf * 4:hf * 4 + 4], writes=[tg + "pu%d%d" % (r, hf)])
                                P.act(lambda e, hf=hf, r=r: e.activation(out=sgt[hf][:], in_=pg[r][hf][:], func=AF.Silu),
                                      reads=[tg + "pg%d%d" % (r, hf)], writes=[tg + "sg%d" % hf])
                                P.dve(lambda e, hf=hf, r=r, f=f: e.tensor_tensor(out=hid[:, f, hf * 512:(hf + 1) * 512], in0=sgt[hf][:],
                                                                               in1=pu[r][hf][:], op=ALU.mult),
                                      reads=[tg + "sg%d" % hf, tg + "pu%d%d" % (r, hf)], writes=[(tg + "hid", f)])
                P.barrier()
                if debug and tg == "f1":
                    dbg["hid"] = nc.dram_tensor("dbg_hid", [128, NF, NT], BF16, kind="ExternalOutput").ap()
                    C.dbg_extra = [P.dma(lambda e: e.dma_start(out=dbg["hid"][:, :, :], in_=hid[:]), reads=[(tg + "hid", f) for f in range(NF)], writes=["dbg_hid"])]
                with contextlib.ExitStack() as S2:
                    NP = 4
                    FP = NF // NP
                    wdp = [sb(tg + "wdp%d" % i, [128, FP, 256], BF16, S2) for i in range(4)]
                    py = [[[ps(tg + "py%d%d%d" % (i, mc, hf), [128, 512], F32, S2) for hf in range(2)] for mc in range(2)] for i in range(2)]
                    wdv = wd.rearrange("(fc p) m -> p fc m", p=128)
                    cnt = 0
                    for cb in range(8):
                        r = cb % 2
                        for pc in range(NP):
                            bi = cnt % 4
                            cnt += 1
                            P.dma(lambda e, cb=cb, pc=pc, bi=bi: e.dma_start(out=wdp[bi][:], in_=wdv[:, pc * FP:(pc + 1) * FP, cb * 256:(cb + 1) * 256]),
                                  writes=[tg + "wdp%d" % bi], q="pool")
                            for mc in range(2):
                                for hf in range(2):
                                    for fi in range(FP):
                                        f = pc * FP + fi
                                        P.pe(lambda e, mc=mc, hf=hf, fi=fi, f=f, bi=bi, r=r: e.matmul(
                                            py[r][mc][hf][:], lhsT=wdp[bi][:, fi, mc * 128:(mc + 1) * 128], rhs=hid[:, f, hf * 512:(hf + 1) * 512],
                                            start=(f == 0), stop=(f == NF - 1)),
                                            reads=[tg + "wdp%d" % bi, (tg + "hid", f)], writes=[tg + "py%d%d%d" % (r, mc, hf)])
                        for mc in range(2):
                            for hf in range(2):
                                m = cb * 2 + mc
                                P.act(lambda e, m=m, mc=mc, hf=hf, r=r: e.copy(out=AT[:, m, hf * 512:(hf + 1) * 512], in_=py[r][mc][hf][:]),
                                      reads=[tg + "py%d%d%d" % (r, mc, hf)], writes=[("AT", hf * 4 + q) for q in range(4)])
            if debug and tg == "f1":
                P.barrier()
                dbg["yT"] = nc.dram_tensor("dbg_yT", [128, KD, NT], BF16, kind="ExternalOutput").ap()
                C.dbg_extra.append(P.dma(lambda e: e.dma_start(out=dbg["yT"][:, :, :], in_=AT[:]), reads=[("AT", t) for t in range(NTILE)], writes=["dbg_yT"]))
            post(tg, AT, h_src, post_g, 0.5, h_dst, next_g)

        def post(tg, YT, h_src, post_g, coef, h_dst, next_g):
            P.barrier()
            with contextlib.ExitStack() as S3:
                epsT = sb(tg + "eps3", [128, 1], F32, S3)
                P.pool(lambda e: e.memset(epsT[:], EPS), writes=[tg + "eps3"])
                gpo = sb(tg + "gpo", [128, D], F32, S3)
                P.dma(lambda e: e.dma_start(out=gpo[:], in_=bcast_rows(post_g, D)), writes=[tg + "gpo"])
                gnx = None
                if next_g is not None:
                    gnx = sb(tg + "gnx", [128, D], F32, S3)
                    P.dma(lambda e: e.dma_start(out=gnx[:], in_=bcast_rows(next_g, D)), writes=[tg + "gnx"])
                scr = [dict(sq=sb(tg + "psq%d" % i, [128, D], BF16, S3), ss=sb(tg + "pss%d" % i, [128, 4], F32, S3),
                            ub=sb(tg + "pub%d" % i, [128, D], BF16, S3), pT=ps(tg + "ppT%d" % i, [128, KD, 128], BF16, S3),
                            eps=epsT, epskey=tg + "eps3") for i in range(2)]
                xt = [sb(tg + "xt%d" % i, [128, D], F32, S3) for i in range(2)]
                tt = [sb(tg + "tt%d" % i, [128, D], F32, S3) for i in range(2)]
                s2 = [sb(tg + "s2%d" % i, [128, 4], F32, S3) for i in range(2)]
                pyT = [ps(tg + "pyT%d" % i, [128, KD, 128], BF16, S3) for i in range(2)]
                ykey = YT.name
                for t in range(NTILE):
                    i = t % 2
                    P.dma(lambda e, t=t, i=i: e.dma_start(out=xt[i][:], in_=h_src[t * 128:(t + 1) * 128, :]),
                          reads=[("hd", h_src.tensor.name, t)], writes=[tg + "xt%d" % i])
                    for m in range(KD):
                        P.pe(lambda e, m=m, t=t, i=i: e.transpose(out=pyT[i][:, m, :], in_=YT[:, m, t * 128:(t + 1) * 128], identity=ident[:]),
                             reads=[(ykey, t), "ident"], writes=[tg + "pyT%d" % i])
                    yv = pyT[i][:].rearrange("p k c -> p (k c)")
                    P.act(lambda e, i=i, yv=yv: e.activation(out=scr[i]["sq"][:], in_=yv, func=AF.Square, accum_out=s2[i][:, 0:1]),
                          reads=[tg + "pyT%d" % i], writes=[tg + "n%dsq" % i, tg + "s2a%d" % i])
                    P.act(lambda e, i=i: e.activation(out=s2[i][:, 1:2], in_=s2[i][:, 0:1], func=AF.Sqrt, scale=1.0 / D, bias=epsT[:, 0:1]),
                          reads=[tg + "s2a%d" % i, tg + "eps3"], writes=[tg + "s2b%d" % i])
                    P.dve(lambda e, i=i: e.reciprocal(out=s2[i][:, 2:3], in_=s2[i][:, 1:2]), reads=[tg + "s2b%d" % i], writes=[tg + "s2c%d" % i])
                    P.dve(lambda e, i=i, yv=yv: e.scalar_tensor_tensor(out=tt[i][:], in0=yv, scalar=s2[i][:, 2:3], in1=gpo[:], op0=ALU.mult, op1=ALU.mult),
                          reads=[tg + "pyT%d" % i, tg + "s2c%d" % i, tg + "gpo"], writes=[tg + "tt%d" % i])
                    P.dve(lambda e, i=i: e.scalar_tensor_tensor(out=xt[i][:], in0=tt[i][:], scalar=float(coef), in1=xt[i][:], op0=ALU.mult, op1=ALU.add),
                          reads=[tg + "tt%d" % i, tg + "xt%d" % i], writes=[tg + "xt%d" % i])
                    o = P.dma(lambda e, t=t, i=i: e.dma_start(out=h_dst[t * 128:(t + 1) * 128, :], in_=xt[i][:]), reads=[tg + "xt%d" % i],
                              writes=[("hd", h_dst.tensor.name, t)])
                    if next_g is not None:
                        norm_transpose(S3, tg + "n%d" % i, xt[i], tg + "xt%d" % i, gnx, tg + "gnx", t, scr[i])
                    else:
                        finals.append(o)
            P.barrier()

        C.post = post
        C.x, C.pin, C.pos, C.out, C.h1d, C.h2d, C.h3d, C.dbg, C.finals = x, pin, pos, out, h1d, h2d, h3d, dbg, finals
        C.bcast_rows = bcast_rows
        C.contextlib = contextlib
        C.stop = stop
        C.debug = debug

        if stop not in (20, 21, 22):
            ffn("f1", x, W["ff1_pre_g"], W["ff1_post_g"], W["ff1_w_gate"], W["ff1_w_up"], W["ff1_w_down"], h1d, W["mix_pre_g"], True)
        if stop <= 1:
            C.dbg_extra = getattr(C, "dbg_extra", [])
            o = P.dma(lambda e: e.dma_start(out=dbg["uT"][:, :, :], in_=AT[:]), reads=[("AT", t) for t in range(NTILE)], writes=["dbg_uT"])
            o2 = P.dma(lambda e: e.dma_start(out=out[:, :], in_=h1d[:, :]), reads=[("hd", "h1d", t) for t in range(NTILE)], writes=["out"])
            P.emit(final_wait_ops=[o, o2] + C.dbg_extra)
            return nc
        if stop in (20, 21, 22):
            with contextlib.ExitStack() as S0:
                epsT = sb("eps0", [128, 1], F32, S0)
                P.pool(lambda e: e.memset(epsT[:], EPS), writes=["eps0"])
                gb = sb("gb0", [128, D], F32, S0)
                P.dma(lambda e: e.dma_start(out=gb[:], in_=bcast_rows(W["mix_pre_g"], D)), writes=["gb0"])
                scr = [dict(sq=sb("sq0%d" % i, [128, D], BF16, S0), ss=sb("ss0%d" % i, [128, 4], F32, S0), ub=sb("ub0%d" % i, [128, D], BF16, S0),
                            pT=ps("pT0%d" % i, [128, KD, 128], BF16, S0), eps=epsT, epskey="eps0") for i in range(2)]
                hts = [sb("ht0%d" % i, [128, D], F32, S0) for i in range(2)]
                for t in range(NTILE):
                    i = t % 2
                    P.dma(lambda e, t=t, i=i: e.dma_start(out=hts[i][:], in_=x[t * 128:(t + 1) * 128, :]), writes=["ht0%d" % i])
                    P.dma(lambda e, t=t, i=i: e.dma_start(out=h1d[t * 128:(t + 1) * 128, :], in_=hts[i][:]), reads=["ht0%d" % i], writes=[("hd", "h1d", t)])
                    norm_transpose(S0, "n0%d" % i, hts[i], "ht0%d" % i, gb, "gb0", t, scr[i])
            P.barrier()
        if mixer(C) == "stop":
            return nc
        if stop <= 2 or stop in (20, 22):
            o2 = P.dma(lambda e: e.dma_start(out=out[:, :], in_=h2d[:, :]), reads=[("hd", "h2d", t) for t in range(NTILE)], writes=["out"])
            P.emit(final_wait_ops=[o2] + C.dbg_ops)
            return nc
        ffn("f2", h2d, None, W["ff2_post_g"], W["ff2_w_gate"], W["ff2_w_up"], W["ff2_w_down"], h3d, W["ple_pre_g"], False)
        ple(C)
        P.emit(final_wait_ops=finals)
    return nc


WNAMES = ["ff1_pre_g", "ff1_post_g", "ff1_w_gate", "ff1_w_up", "ff1_w_down", "mix_pre_g", "mix_post_g", "w_in",
          "cmp_pos_k", "cmp_pos_v", "cmp_k_w1", "cmp_k_w2", "cmp_v_w1", "cmp_v_w2", "nsa_gate_b",
          "conv_w", "conv_b", "rg_w_a", "rg_b_a", "rg_w_i", "rg_b_i", "rg_lambda",
          "attn_out_g", "rec_out_g", "w_out", "ff2_pre_g", "ff2_post_g", "ff2_w_gate", "ff2_w_up", "ff2_w_down",
          "ple_pre_g", "ple_post_g", "w_ple_gate", "w_ple_proj"]


def shard_tokens(a, b, j):
    T = a.shape[0]
    r = a.reshape(T // 512, 4, 128, *a.shape[1:])[:, j]
    return np.ascontiguousarray(r.reshape(T // 4, *a.shape[1:]))


def make_in_maps(inputs):
    shared = {}
    for nm in WNAMES:
        a = np.asarray(inputs[nm], dtype=np.float32)[0]
        if a.ndim == 1:
            a = a.reshape(1, -1)
        if nm == "nsa_gate_b":
            a = a.reshape(1, 24)
        shared[nm] = np.ascontiguousarray(a)
    maps = []
    x = np.asarray(inputs["x"], dtype=np.float32)
    p = np.asarray(inputs["p"], dtype=np.float32)[0]
    positions = np.asarray(inputs["positions"]).astype(np.int32)
    for c in range(8):
        b, j = c // 4, c % 4
        m = dict(shared)
        m["x"] = shard_tokens(x[b], b, j)
        m["p"] = shard_tokens(p[b], b, j)
        m["pos"] = shard_tokens(positions[b], b, j).reshape(1, NT)
        maps.append(m)
    return maps


def unshard(outs):
    res = np.zeros((2, 4096, D), dtype=np.float32)
    for c in range(8):
        b, j = c // 4, c % 4
        res[b].reshape(8, 4, 128, D)[:, j] = np.asarray(outs[c]).reshape(8, 128, D)
    return res


BIGNEG = -30000.0
SCALE = 128 ** -0.5
RG = [[0, 1, 2, 3], [4, 5, 6, 7]]
GROWS = 12 * 128


def mixer(C):
    nc, P, W, sb, ps, AT, ident, identf = C.nc, C.P, C.W, C.sb, C.ps, C.AT, C.ident, C.identf
    contextlib = C.contextlib
    C.dbg_ops = []

    def cin(name, shape, dt=F32):
        return nc.dram_tensor(name, list(shape), dt, kind="ExternalInput").ap()

    c_rope = cin("c_rope", [128, 4])
    c_perm = cin("c_perm", [128, 128])
    c_oh = cin("c_oh", [128, 4])
    c_cmpm = cin("c_cmpm", [128, 2 * 8 * 128], BF16)
    c_selcm = cin("c_selcm", [128, 4 * 128], BF16)
    c_winm = cin("c_winm", [128, 8 * 128], BF16)
    c_selA = cin("c_selA", [128, 8 * 64])
    c_selB = cin("c_selB", [128, 8 * 64])
    c_selF = cin("c_selF", [128, 8 * 64])
    c_E = cin("c_E", [64, 4096], BF16)
    c_ovl = cin("c_ovl", [128, 2 * 64])
    c_selmat = cin("c_selmat", [24, 24 * 128])

    gin = nc.dram_tensor("gin", [GROWS, NT], BF16, kind="Internal").ap()
    gbuf = nc.dram_tensor("gbuf", [4 * GROWS, NT], BF16, kind="Internal").ap()
    hin = nc.dram_tensor("hin", [128, 192], F32, kind="Internal").ap()
    hbuf = nc.dram_tensor("hbuf", [4 * 128, 192], F32, kind="Internal").ap()
    sin_ = nc.dram_tensor("sin", [128, 128], F32, kind="Internal").ap()
    sbuf_ = nc.dram_tensor("sbuf", [4 * 128, 128], F32, kind="Internal").ap()

    def load(dst, src, key, q="sp", reads=()):
        return P.dma(lambda e: e.dma_start(out=dst, in_=src), reads=list(reads), writes=[key], q=q)

    with contextlib.ExitStack() as SM:
        qT = sb("qT", [128, 8, NT], BF16, SM)
        gT = sb("gT", [24, NT], F32, SM)
        orecT = sb("orecT", [128, 8, NT], BF16, SM)
        ones_bf = sb("ones_bf", [128, 128], BF16, SM)
        ones_f = sb("ones_f", [128, 128], F32, SM)
        onec = sb("onec", [128, 1], F32, SM)
        epsT = sb("m_eps", [128, 1], F32, SM)
        oh = sb("oh", [128, 4], F32, SM)
        P.pool(lambda e: e.memset(ones_bf[:], 1.0), writes=["ones_bf"])
        P.pool(lambda e: e.memset(ones_f[:], 1.0), writes=["ones_f"])
        P.pool(lambda e: e.memset(onec[:], 1.0), writes=["onec"])
        P.pool(lambda e: e.memset(epsT[:], EPS), writes=["m_eps"])
        load(oh[:], c_oh[:, :], "oh")
        C.ones_bf, C.ones_f, C.epsT = ones_bf, ones_f, epsT
        ATall = [("AT", t) for t in range(NTILE)]

        with contextlib.ExitStack() as S2:
            ZX = sb("ZX", [128, 8, NT], F32, S2)
            gy = sb("gy", [128, 8, NT], BF16, S2)
            with contextlib.ExitStack() as S2a:
                rope = sb("rope", [128, 4], F32, S2a)
                perm = sb("perm", [128, 128], F32, S2a)
                posi = sb("posi", [128, NT], I32, S2a)
                ang = sb("ang", [128, NT], F32, S2a)
                kf = sb("kf", [128, NT], F32, S2a)
                ki = sb("ki", [128, NT], I32, S2a)
                Ct = sb("Ct", [128, NT], F32, S2a)
                St = sb("St", [128, NT], F32, S2a)
                gbias = sb("gbias", [24, 1], F32, S2a)
                load(rope[:], c_rope[:, :], "rope")
                load(perm[:], c_perm[:, :], "perm")
                load(posi[:], C.bcast_rows(C.pos, NT), "posi")
                with nc.allow_non_contiguous_dma(reason="tiny param"):
                    P.dma(lambda e: e.dma_start(out=gbias[:], in_=W["nsa_gate_b"].rearrange("o n -> n o"), allow_slow_non_contiguous=True), writes=["gbias"])
                P.dve(lambda e: e.tensor_copy(out=ang[:], in_=posi[:]), reads=["posi"], writes=["ang"])
                P.dve(lambda e: e.tensor_scalar(out=ang[:], in0=ang[:], scalar1=rope[:, 0:1], scalar2=None, op0=ALU.mult), reads=["ang", "rope"], writes=["ang"])
                TWO_PI = 6.283185307179586
                C1 = 6.28125
                C2 = TWO_PI - C1
                P.dve(lambda e: e.tensor_scalar(out=kf[:], in0=ang[:], scalar1=1.0 / TWO_PI, scalar2=None, op0=ALU.mult), reads=["ang"], writes=["kf"])
                P.dve(lambda e: e.tensor_copy(out=ki[:], in_=kf[:]), reads=["kf"], writes=["ki"])
                P.dve(lambda e: e.tensor_copy(out=kf[:], in_=ki[:]), reads=["ki"], writes=["kf"])
                P.dve(lambda e: e.scalar_tensor_tensor(out=ang[:], in0=kf[:], scalar=-C1, in1=ang[:], op0=ALU.mult, op1=ALU.add), reads=["kf", "ang"], writes=["ang"])
                P.dve(lambda e: e.scalar_tensor_tensor(out=ang[:], in0=kf[:], scalar=-C2, in1=ang[:], op0=ALU.mult, op1=ALU.add), reads=["kf", "ang"], writes=["ang"])
                P.dve(lambda e: e.tensor_scalar(out=kf[:], in0=ang[:], scalar1=3.141592653589793, scalar2=-TWO_PI, op0=ALU.is_gt, op1=ALU.mult), reads=["ang"], writes=["kf"])
                P.dve(lambda e: e.tensor_tensor(out=ang[:], in0=ang[:], in1=kf[:], op=ALU.add), reads=["ang", "kf"], writes=["ang"])
                P.dve(lambda e: e.tensor_scalar(out=kf[:], in0=ang[:], scalar1=-3.141592653589793, scalar2=TWO_PI, op0=ALU.is_lt, op1=ALU.mult), reads=["ang"], writes=["kf"])
                P.dve(lambda e: e.tensor_tensor(out=ang[:], in0=ang[:], in1=kf[:], op=ALU.add), reads=["ang", "kf"], writes=["ang"])
                P.act(lambda e: e.activation(out=St[:], in_=ang[:], func=AF.Sin), reads=["ang"], writes=["St"])
                P.dve(lambda e: e.tensor_scalar(out=St[:], in0=St[:], scalar1=rope[:, 1:2], scalar2=None, op0=ALU.mult), reads=["St", "rope"], writes=["St"])
                P.act(lambda e: e.activation(out=kf[:], in_=ang[:], func=AF.Abs), reads=["ang"], writes=["kf"])
                P.dve(lambda e: e.tensor_scalar(out=kf[:], in0=kf[:], scalar1=-1.0, scalar2=1.5707963267948966, op0=ALU.mult, op1=ALU.add), reads=["kf"], writes=["kf"])
                P.act(lambda e: e.activation(out=Ct[:], in_=kf[:], func=AF.Sin), reads=["kf"], writes=["Ct"])

                wpan = [sb("wpan%d" % i, [128, KD, 512], BF16, S2a) for i in range(2)]
                zf = [sb("zf%d" % i, [128, NT], F32, S2a) for i in range(2)]
                t1 = [sb("t1%d" % i, [128, 512], F32, S2a) for i in range(2)]
                stg = [sb("stg%d" % i, [128, NT], BF16, S2a) for i in range(2)]
                pz = [[ps("pz%d%d" % (i, hf), [128, 512], F32, S2a) for hf in range(2)] for i in range(2)]
                psw = [ps("psw%d" % i, [128, 512], F32, S2a) for i in range(2)]
                pvt = [ps("pvt%d" % i, [128, 8, 128], BF16, S2a) for i in range(2)]
                wv = W["w_in"].rearrange("(kc p) m -> p kc m", p=128)
                panels = [(0, 512), (512, 512), (1024, 512), (1536, 512), (2048, 512), (2560, 24), (2584, 512), (3096, 512), (3608, 512), (4120, 512)]
                kinds = {}
                for h in range(8):
                    kinds[h * 128] = ("q", h)
                for g in range(2):
                    kinds[1024 + g * 128] = ("kf", 0 + g)
                    kinds[1280 + g * 128] = ("vf", 2 + g)
                    kinds[1536 + g * 128] = ("kf", 4 + g)
                    kinds[1792 + g * 128] = ("vt", 8 + g)
                    kinds[2048 + g * 128] = ("kf", 6 + g)
                    kinds[2304 + g * 128] = ("vt", 10 + g)
                kinds[2560] = ("g", 0)
                for h in range(8):
                    kinds[2584 + h * 128] = ("zx", h)
                    kinds[3608 + h * 128] = ("zy", h)
                cnt = 0
                for pi, (c0, wdt) in enumerate(panels):
                    b = pi % 2
                    P.dma(lambda e, b=b, c0=c0, wdt=wdt: e.dma_start(out=wpan[b][:, :, 0:wdt], in_=wv[:, :, c0:c0 + wdt]), writes=["wpan%d" % b], q="pool")
                    for ci in range(max(1, wdt // 128)):
                        col = c0 + ci * 128
                        kind, idx = kinds[col]
                        cw = 24 if kind == "g" else 128
                        r = cnt % 2
                        cnt += 1
                        for hf in range(2):
                            for k in range(KD):
                                P.pe(lambda e, k=k, hf=hf, r=r, b=b, ci=ci, cw=cw: e.matmul(
                                    pz[r][hf][0:cw, :], lhsT=wpan[b][:, k, ci * 128:ci * 128 + cw], rhs=AT[:, k, hf * 512:(hf + 1) * 512],
                                    start=(k == 0), stop=(k == KD - 1)), reads=["wpan%d" % b] + ATall[hf * 4:hf * 4 + 4], writes=["pz%d%d" % (r, hf)])
                        pzk = ["pz%d%d" % (r, 0), "pz%d%d" % (r, 1)]
                        if kind == "g":
                            for hf in range(2):
                                P.act(lambda e, hf=hf, r=r: e.activation(out=gT[:, hf * 512:(hf + 1) * 512], in_=pz[r][hf][0:24, :], func=AF.Sigmoid, bias=gbias[:, 0:1]),
                                      reads=[pzk[hf], "gbias"], writes=["gT"])
                        elif kind == "zx":
                            for hf in range(2):
                                P.act(lambda e, hf=hf, r=r, idx=idx: e.copy(out=ZX[:, idx, hf * 512:(hf + 1) * 512], in_=pz[r][hf][:]),
                                      reads=[pzk[hf]], writes=[("ZX", idx)])
                        elif kind == "zy":
                            for hf in range(2):
                                sl = slice(hf * 512, (hf + 1) * 512)
                                P.act(lambda e, hf=hf, r=r, sl=sl: e.activation(out=zf[r][:, sl], in_=pz[r][hf][:], func=AF.Square), reads=[pzk[hf]], writes=[("zf%d" % r, hf)])
                                P.dve(lambda e, r=r, sl=sl, hf=hf: e.tensor_scalar(out=zf[r][:, sl], in0=zf[r][:, sl], scalar1=0.044715, scalar2=1.0, op0=ALU.mult, op1=ALU.add), reads=[("zf%d" % r, hf)], writes=[("zf%d" % r, hf)])
                                P.dve(lambda e, hf=hf, r=r, sl=sl: e.tensor_tensor(out=zf[r][:, sl], in0=zf[r][:, sl], in1=pz[r][hf][:], op=ALU.mult), reads=[("zf%d" % r, hf), pzk[hf]], writes=[("zf%d" % r, hf)])
                                P.act(lambda e, r=r, sl=sl, hf=hf: e.activation(out=zf[r][:, sl], in_=zf[r][:, sl], func=AF.Sigmoid, scale=1.5957691216057308), reads=[("zf%d" % r, hf)], writes=[("zf%d" % r, hf)])
                                P.dve(lambda e, hf=hf, r=r, sl=sl, idx=idx: e.tensor_tensor(out=gy[:, idx, sl], in0=zf[r][:, sl], in1=pz[r][hf][:], op=ALU.mult), reads=[("zf%d" % r, hf), pzk[hf]], writes=[("gy", idx)])
                        else:
                            for hf in range(2):
                                P.act(lambda e, hf=hf, r=r: e.copy(out=zf[r][:, hf * 512:(hf + 1) * 512], in_=pz[r][hf][:]), reads=[pzk[hf]], writes=[("zf%d" % r, hf)])
                            if kind in ("q", "kf"):
                                dst = qT[:, idx, :] if kind == "q" else stg[r][:]
                                dkey = ("qT", idx) if kind == "q" else "stg%d" % r
                                for hf in range(2):
                                    sl = slice(hf * 512, (hf + 1) * 512)
                                    P.pe(lambda e, hf=hf, r=r, sl=sl: e.matmul(psw[hf][:], lhsT=perm[:], rhs=zf[r][:, sl], start=True, stop=True),
                                         reads=["perm", ("zf%d" % r, hf)], writes=["psw%d" % hf])
                                    P.dve(lambda e, hf=hf, r=r, sl=sl: e.tensor_tensor(out=t1[hf][:], in0=zf[r][:, sl], in1=Ct[:, sl], op=ALU.mult),
                                          reads=[("zf%d" % r, hf), "Ct"], writes=["t1%d" % hf])
                                    P.dve(lambda e, hf=hf, r=r, sl=sl: e.tensor_tensor(out=zf[r][:, sl], in0=psw[hf][:], in1=St[:, sl], op=ALU.mult),
                                          reads=["psw%d" % hf, "St", ("zf%d" % r, hf)], writes=[("zf%d" % r, hf)])
                                    P.dve(lambda e, hf=hf, r=r, sl=sl, dst=dst: e.tensor_tensor(out=dst[:, sl], in0=zf[r][:, sl], in1=t1[hf][:], op=ALU.add),
                                          reads=[("zf%d" % r, hf), "t1%d" % hf], writes=[dkey])
                                if kind == "kf":
                                    P.dma(lambda e, r=r, idx=idx: e.dma_start(out=gin[idx * 128:(idx + 1) * 128, :], in_=stg[r][:]), reads=["stg%d" % r], writes=[("gin", idx)])
                            elif kind == "vf":
                                P.dve(lambda e, r=r: e.tensor_copy(out=stg[r][:], in_=zf[r][:]), reads=[("zf%d" % r, 0), ("zf%d" % r, 1)], writes=["stg%d" % r])
                                P.dma(lambda e, r=r, idx=idx: e.dma_start(out=gin[idx * 128:(idx + 1) * 128, :], in_=stg[r][:]), reads=["stg%d" % r], writes=[("gin", idx)])
                            elif kind == "vt":
                                P.dve(lambda e, r=r: e.tensor_copy(out=stg[r][:], in_=zf[r][:]), reads=[("zf%d" % r, 0), ("zf%d" % r, 1)], writes=["stg%d" % r])
                                for l in range(8):
                                    P.pe(lambda e, r=r, l=l: e.transpose(out=pvt[r][:, l, :], in_=stg[r][:, l * 128:(l + 1) * 128], identity=ident[:]),
                                         reads=["stg%d" % r, "ident"], writes=["pvt%d" % r])
                                P.act(lambda e, r=r: e.copy(out=stg[r][:], in_=pvt[r][:].rearrange("p l d -> p (l d)")), reads=["pvt%d" % r], writes=["stg%d" % r])
                                P.dma(lambda e, r=r, idx=idx: e.dma_start(out=gin[idx * 128:(idx + 1) * 128, :], in_=stg[r][:]), reads=["stg%d" % r], writes=[("gin", idx)])
            P.barrier()
            with contextlib.ExitStack() as S2b:
                hst = sb("hst", [128, 8, 8, 3], F32, S2b)
                for blk in range(8):
                    P.dve(lambda e, blk=blk: e.tensor_copy(out=hst[:, blk, :, :], in_=ZX[:, blk, :].rearrange("p (l t) -> p l t", t=128)[:, :, 125:128]),
                          reads=[("ZX", blk)], writes=["hst"])
                P.dma(lambda e: e.dma_start(out=hin[:, :], in_=hst[:].rearrange("p b l t -> p (b l t)")), reads=["hst"], writes=["hin"])
                for it in range(12):
                    P.cc(lambda e, it=it: e.collective_compute("AllGather", ALU.bypass, replica_groups=RG, ins=[gin[it * 128:(it + 1) * 128, :]], outs=[gbuf[it * 512:(it + 1) * 512, :]]),
                           reads=[("gin", it)], writes=[("gbuf", it)])
                cc2 = P.cc(lambda e: e.collective_compute("AllGather", ALU.bypass, replica_groups=RG, ins=[hin[:, :]], outs=[hbuf[:, :]]), reads=["hin"], writes=["hbuf"])
            P.barrier()
            if C.stop == 21:
                C.dbg["qT"] = nc.dram_tensor("dbg_qT", [128, 8, NT], BF16, kind="ExternalOutput").ap()
                C.dbg["gT"] = nc.dram_tensor("dbg_gT", [24, NT], F32, kind="ExternalOutput").ap()
                C.dbg["gbuf"] = nc.dram_tensor("dbg_gbuf", [4 * GROWS, NT], BF16, kind="ExternalOutput").ap()
                C.dbg["ZX"] = nc.dram_tensor("dbg_ZX", [128, 8, NT], F32, kind="ExternalOutput").ap()
                C.dbg["gy"] = nc.dram_tensor("dbg_gy", [128, 8, NT], BF16, kind="ExternalOutput").ap()
                C.dbg_ops.append(P.dma(lambda e: e.dma_start(out=C.dbg["qT"][:, :, :], in_=qT[:]), reads=[("qT", h) for h in range(8)], writes=["d1"]))
                C.dbg_ops.append(P.dma(lambda e: e.dma_start(out=C.dbg["gT"][:, :], in_=gT[:]), reads=["gT"], writes=["d2"]))
                C.dbg_ops.append(P.dma(lambda e: e.dma_start(out=C.dbg["gbuf"][:, :], in_=gbuf[:, :]), reads=[("gbuf", it) for it in range(12)], writes=["d3"]))
                C.dbg_ops.append(P.dma(lambda e: e.dma_start(out=C.dbg["ZX"][:, :, :], in_=ZX[:]), reads=[("ZX", h) for h in range(8)], writes=["d4"]))
                C.dbg_ops.append(P.dma(lambda e: e.dma_start(out=C.dbg["gy"][:, :, :], in_=gy[:]), reads=[("gy", h) for h in range(8)], writes=["d5"]))
                P.emit(final_wait_ops=C.dbg_ops)
                return "stop"
            lru(C, SM, ZX, gy, orecT, hbuf, sin_, sbuf_, oh, onec)
        P.barrier()
        attention(C, SM, qT, gT, orecT, gbuf, ones_bf, ones_f, epsT,
                  dict(cmpm=c_cmpm, selcm=c_selcm, winm=c_winm, selA=c_selA, selB=c_selB, selF=c_selF, E=c_E, ovl=c_ovl, selmat=c_selmat))
    P.barrier()
    if C.stop == 22:
        C.dbg["cat"] = nc.dram_tensor("dbg_cat", [128, KD, NT], BF16, kind="ExternalOutput").ap()
        C.dbg_ops.append(P.dma(lambda e: e.dma_start(out=C.dbg["cat"][:, :, :], in_=AT[:]), reads=[("AT", t) for t in range(NTILE)], writes=["dcat"]))
        P.barrier()
    outproj(C)


def lru(C, SM, ZX, gy, orecT, hbuf, sin_, sbuf_, oh, onec):
    nc, P, W, sb, ps = C.nc, C.P, C.W, C.sb, C.ps
    contextlib = C.contextlib
    ones_bf, epsT = C.ones_bf, C.epsT
    with contextlib.ExitStack() as SL:
        Pc = sb("Pc", [128, 8, NT], F32, SL)
        cw = sb("cw", [128, 4, 8], F32, SL)
        cb = sb("cb", [128, 8], F32, SL)
        ba = sb("ba", [128, 8], F32, SL)
        bi = sb("bi", [128, 8], F32, SL)
        lam = sb("lam", [128, 8], F32, SL)
        rg = sb("rg", [128, 8], F32, SL)
        cch = sb("cch", [128, 8], F32, SL)
        wa = sb("wa", [128, 8, 128], BF16, SL)
        wi = sb("wi", [128, 8, 128], BF16, SL)
        G2 = sb("G2", [128, 4, 192], F32, SL)
        halo = sb("halo", [128, 8, 8, 3], F32, SL)
        zeros = sb("zeros", [128, 128], F32, SL)
        P.pool(lambda e: e.memset(zeros[:], 0.0), writes=["zeros"])
        with nc.allow_non_contiguous_dma(reason="tiny params"):
            for w_ in range(4):
                P.dma(lambda e, w_=w_: e.dma_start(out=cw[:, w_, :], in_=W["conv_w"][w_:w_ + 1, :].rearrange("o (b p) -> p (o b)", p=128), allow_slow_non_contiguous=True), writes=[("cw", w_)])
            for t_, nm in [(cb, "conv_b"), (ba, "rg_b_a"), (bi, "rg_b_i"), (lam, "rg_lambda"), (rg, "rec_out_g")]:
                P.dma(lambda e, t_=t_, nm=nm: e.dma_start(out=t_[:], in_=W[nm].rearrange("o (b p) -> p (o b)", p=128), allow_slow_non_contiguous=True), writes=[nm])
        P.dma(lambda e: e.dma_start(out=wa[:], in_=W["rg_w_a"].rearrange("b i j -> i b j")), writes=["wa"], q="pool")
        P.dma(lambda e: e.dma_start(out=wi[:], in_=W["rg_w_i"].rearrange("b i j -> i b j")), writes=["wi"], q="pool")
        P.dma(lambda e: e.dma_start(out=G2[:], in_=hbuf.rearrange("(r p) c -> p r c", p=128)), reads=["hbuf"], writes=["G2"])
        P.act(lambda e: e.activation(out=cch[:], in_=lam[:], func=AF.Sigmoid), reads=["rg_lambda"], writes=["cch"])
        P.act(lambda e: e.activation(out=cch[:], in_=cch[:], func=AF.Ln), reads=["cch"], writes=["cch"])
        P.dve(lambda e: e.tensor_scalar(out=cch[:], in0=cch[:], scalar1=8.0, scalar2=None, op0=ALU.mult), reads=["cch"], writes=["cch"])
        hv = halo[:].rearrange("p b l t -> p (b l t)")
        P.dve(lambda e: e.tensor_scalar(out=hv, in0=G2[:, 0, :], scalar1=oh[:, 1:2], scalar2=None, op0=ALU.mult), reads=["G2", "oh"], writes=["halo"])
        P.dve(lambda e: e.scalar_tensor_tensor(out=hv, in0=G2[:, 1, :], scalar=oh[:, 2:3], in1=hv, op0=ALU.mult, op1=ALU.add), reads=["G2", "oh", "halo"], writes=["halo"])
        P.dve(lambda e: e.scalar_tensor_tensor(out=hv, in0=G2[:, 2, :], scalar=oh[:, 3:4], in1=hv, op0=ALU.mult, op1=ALU.add), reads=["G2", "oh", "halo"], writes=["halo"])
        g3 = G2[:, 3, :].rearrange("p (b l t) -> p b l t", b=8, l=8)
        P.dve(lambda e: e.scalar_tensor_tensor(out=halo[:, :, 1:8, :], in0=g3[:, :, 0:7, :], scalar=oh[:, 0:1], in1=halo[:, :, 1:8, :], op0=ALU.mult, op1=ALU.add),
              reads=["G2", "oh", "halo"], writes=["halo"])
        with contextlib.ExitStack() as SB:
            NB = 2
            xpad = [sb("xpad%d" % i, [128, 8, 131], F32, SB) for i in range(NB)]
            xc = [sb("xc%d" % i, [128, 8, 128], F32, SB) for i in range(NB)]
            xcb = [sb("xcb%d" % i, [128, NT], BF16, SB) for i in range(NB)]
            rr = [sb("rr%d" % i, [128, NT], F32, SB) for i in range(NB)]
            ig0 = sb("ig0", [128, NT], F32, SB)
            ig = [ig0, ig0]
            aa = [sb("aa%d" % i, [128, NT], F32, SB) for i in range(NB)]
            uu0 = sb("uu0", [128, NT], F32, SB)
            uu = [uu0, uu0]
            pr = [[ps("pr%d%d" % (i, hf), [128, 512], F32, SB) for hf in range(2)] for i in range(NB)]
            pg = [[ps("pi%d%d" % (i, hf), [128, 512], F32, SB) for hf in range(2)] for i in range(NB)]
            for blk in range(8):
                i = blk % NB
                k = lambda s: "%s%d" % (s, i)
                P.pool(lambda e, i=i, blk=blk: e.tensor_copy(out=xpad[i][:, :, 0:3], in_=halo[:, blk, :, :]), reads=["halo"], writes=[k("xpad")])
                P.pool(lambda e, i=i, blk=blk: e.tensor_copy(out=xpad[i][:, :, 3:131], in_=ZX[:, blk, :].rearrange("p (l t) -> p l t", t=128)), reads=[("ZX", blk)], writes=[k("xpad")])
                P.dve(lambda e, i=i, blk=blk: e.tensor_scalar(out=xc[i][:], in0=xpad[i][:, :, 0:128], scalar1=cw[:, 0, blk:blk + 1], scalar2=cb[:, blk:blk + 1], op0=ALU.mult, op1=ALU.add),
                      reads=[k("xpad"), ("cw", 0), "conv_b"], writes=[k("xc")])
                for w in range(1, 4):
                    P.dve(lambda e, i=i, blk=blk, w=w: e.scalar_tensor_tensor(out=xc[i][:], in0=xpad[i][:, :, w:w + 128], scalar=cw[:, w, blk:blk + 1], in1=xc[i][:], op0=ALU.mult, op1=ALU.add),
                          reads=[k("xpad"), ("cw", w), k("xc")], writes=[k("xc")])
                xcf = xc[i][:].rearrange("p l t -> p (l t)")
                P.act(lambda e, i=i, xcf=xcf: e.copy(out=xcb[i][:], in_=xcf), reads=[k("xc")], writes=[k("xcb")])
                for hf in range(2):
                    sl = slice(hf * 512, (hf + 1) * 512)
                    P.pe(lambda e, i=i, blk=blk, hf=hf, sl=sl: e.matmul(pr[i][hf][:], lhsT=wa[:, blk, :], rhs=xcb[i][:, sl], start=True, stop=True), reads=["wa", k("xcb")], writes=["pr%d%d" % (i, hf)])
                    P.pe(lambda e, i=i, blk=blk, hf=hf, sl=sl: e.matmul(pg[i][hf][:], lhsT=wi[:, blk, :], rhs=xcb[i][:, sl], start=True, stop=True), reads=["wi", k("xcb")], writes=["pi%d%d" % (i, hf)])
                    P.act(lambda e, i=i, blk=blk, hf=hf, sl=sl: e.activation(out=rr[i][:, sl], in_=pr[i][hf][:], func=AF.Sigmoid, bias=ba[:, blk:blk + 1]), reads=["pr%d%d" % (i, hf), "rg_b_a"], writes=[k("rr")])
                    P.act(lambda e, i=i, blk=blk, hf=hf, sl=sl: e.activation(out=ig[i][:, sl], in_=pg[i][hf][:], func=AF.Sigmoid, bias=bi[:, blk:blk + 1]), reads=["pi%d%d" % (i, hf), "rg_b_i"], writes=["ig0"])
                P.act(lambda e, i=i, blk=blk: e.activation(out=aa[i][:], in_=rr[i][:], func=AF.Exp, scale=cch[:, blk:blk + 1]), reads=[k("rr"), "cch"], writes=[k("aa")])
                P.dve(lambda e, i=i: e.tensor_tensor(out=uu[i][:], in0=aa[i][:], in1=aa[i][:], op=ALU.mult), reads=[k("aa")], writes=["uu0"])
                P.act(lambda e, i=i: e.activation(out=uu[i][:], in_=uu[i][:], func=AF.Sqrt, scale=-1.0, bias=onec[:, 0:1]), reads=["uu0", "onec"], writes=["uu0"])
                P.dve(lambda e, i=i: e.tensor_tensor(out=uu[i][:], in0=uu[i][:], in1=ig[i][:], op=ALU.mult), reads=["uu0", "ig0"], writes=["uu0"])
                P.dve(lambda e, i=i, xcf=xcf: e.tensor_tensor(out=uu[i][:], in0=uu[i][:], in1=xcf, op=ALU.mult), reads=["uu0", k("xc")], writes=["uu0"])
                for l in range(8):
                    sl = slice(l * 128, (l + 1) * 128)
                    P.dve(lambda e, i=i, blk=blk, sl=sl: e.tensor_tensor_scan(out=ZX[:, blk, sl], data0=aa[i][:, sl], data1=uu[i][:, sl], initial=0.0, op0=ALU.mult, op1=ALU.add),
                          reads=[k("aa"), "uu0", k("xpad")], writes=[("ZX", blk)])
                    P.dve(lambda e, i=i, blk=blk, sl=sl: e.tensor_tensor_scan(out=Pc[:, blk, sl], data0=aa[i][:, sl], data1=zeros[:], initial=1.0, op0=ALU.mult, op1=ALU.add),
                          reads=[k("aa"), "zeros"], writes=[("Pc", blk)])
        P.barrier()
        with contextlib.ExitStack() as SC:
            sst = sb("sst", [128, 8, 8, 2], F32, SC)
            SG = sb("SG", [128, 4, 128], F32, SC)
            Aall = sb("Aall", [128, 8, 32], F32, SC)
            Hall = sb("Hall", [128, 8, 32], F32, SC)
            Spad = sb("Spad", [128, 8, 33], F32, SC)
            hinit = sb("hinit", [128, 8, 8], F32, SC)
            rstdb = sb("rstdb", [128, NT], F32, SC)
            sqb = [sb("sqb%d" % i, [128, NT], BF16, SC) for i in range(2)]
            pss = [ps("pss%d" % hf, [128, 512], F32, SC) for hf in range(2)]
            allZX = [("ZX", b) for b in range(8)]
            allPc = [("Pc", b) for b in range(8)]
            P.dve(lambda e: e.tensor_copy(out=sst[:, :, :, 0], in_=Pc[:].rearrange("p b (l t) -> p b l t", t=128)[:, :, :, 127]), reads=allPc, writes=["sst"])
            P.dve(lambda e: e.tensor_copy(out=sst[:, :, :, 1], in_=ZX[:].rearrange("p b (l t) -> p b l t", t=128)[:, :, :, 127]), reads=allZX, writes=["sst"])
            P.dma(lambda e: e.dma_start(out=sin_[:, :], in_=sst[:].rearrange("p b l t -> p (b l t)")), reads=["sst"], writes=["sin"])
            P.cc(lambda e: e.collective_compute("AllGather", ALU.bypass, replica_groups=RG, ins=[sin_[:, :]], outs=[sbuf_[:, :]]), reads=["sin"], writes=["sbufd"])
            P.dma(lambda e: e.dma_start(out=SG[:], in_=sbuf_.rearrange("(r p) c -> p r c", p=128)), reads=["sbufd"], writes=["SG"])
            for j in range(4):
                sgv = SG[:, j, :].rearrange("p (b l t) -> p b l t", b=8, l=8)
                P.dve(lambda e, j=j, sgv=sgv: e.tensor_copy(out=Aall[:, :, j:j + 29:4], in_=sgv[:, :, :, 0]), reads=["SG"], writes=["Aall"])
                P.dve(lambda e, j=j, sgv=sgv: e.tensor_copy(out=Hall[:, :, j:j + 29:4], in_=sgv[:, :, :, 1]), reads=["SG"], writes=["Hall"])
            P.pool(lambda e: e.memset(Spad[:], 0.0), writes=["Spad"])
            for blk in range(8):
                P.dve(lambda e, blk=blk: e.tensor_tensor_scan(out=Spad[:, blk, 1:33], data0=Aall[:, blk, :], data1=Hall[:, blk, :], initial=0.0, op0=ALU.mult, op1=ALU.add),
                      reads=["Aall", "Hall", "Spad"], writes=["Spad"])
            P.dve(lambda e: e.tensor_scalar(out=hinit[:], in0=Spad[:, :, 0:29:4], scalar1=oh[:, 0:1], scalar2=None, op0=ALU.mult), reads=["Spad", "oh"], writes=["hinit"])
            for m in range(1, 4):
                P.dve(lambda e, m=m: e.scalar_tensor_tensor(out=hinit[:], in0=Spad[:, :, m:m + 29:4], scalar=oh[:, m:m + 1], in1=hinit[:], op0=ALU.mult, op1=ALU.add),
                      reads=["Spad", "oh", "hinit"], writes=["hinit"])
            for blk in range(8):
                for l in range(8):
                    sl = slice(l * 128, (l + 1) * 128)
                    P.dve(lambda e, blk=blk, l=l, sl=sl: e.scalar_tensor_tensor(out=ZX[:, blk, sl], in0=Pc[:, blk, sl], scalar=hinit[:, blk, l:l + 1], in1=ZX[:, blk, sl], op0=ALU.mult, op1=ALU.add),
                          reads=[("Pc", blk), "hinit", ("ZX", blk)], writes=[("ZX", blk)])
                P.dve(lambda e, blk=blk: e.tensor_tensor(out=ZX[:, blk, :], in0=ZX[:, blk, :], in1=gy[:, blk, :], op=ALU.mult), reads=[("ZX", blk), ("gy", blk)], writes=[("ZX", blk)])
                i = blk % 2
                P.act(lambda e, blk=blk, i=i: e.activation(out=sqb[i][:], in_=ZX[:, blk, :], func=AF.Square), reads=[("ZX", blk)], writes=["sqb%d" % i])
                for hf in range(2):
                    P.pe(lambda e, blk=blk, i=i, hf=hf: e.matmul(pss[hf][:], lhsT=ones_bf[:], rhs=sqb[i][:, hf * 512:(hf + 1) * 512], start=(blk == 0), stop=(blk == 7)),
                         reads=["ones_bf", "sqb%d" % i], writes=["pss%d" % hf])
            for hf in range(2):
                sl = slice(hf * 512, (hf + 1) * 512)
                P.act(lambda e, hf=hf, sl=sl: e.activation(out=rstdb[:, sl], in_=pss[hf][:], func=AF.Sqrt, scale=1.0 / 1024, bias=epsT[:, 0:1]), reads=["pss%d" % hf, "m_eps"], writes=["rstdb"])
            P.dve(lambda e: e.reciprocal(out=rstdb[:], in_=rstdb[:]), reads=["rstdb"], writes=["rstdb"])
            for blk in range(8):
                P.dve(lambda e, blk=blk: e.scalar_tensor_tensor(out=orecT[:, blk, :], in0=ZX[:, blk, :], scalar=rg[:, blk:blk + 1], in1=rstdb[:], op0=ALU.mult, op1=ALU.mult),
                      reads=[("ZX", blk), "rec_out_g", "rstdb"], writes=[("orecT", blk)])
        P.barrier()


def attention(C, SM, qT, gT, orecT, gbuf, ones_bf, ones_f, epsT, K):
    nc, P, W, sb, ps, AT, ident = C.nc, C.P, C.W, C.sb, C.ps, C.AT, C.ident
    contextlib = C.contextlib

    def bc4(ap, n):
        return ap.unsqueeze(1).broadcast_to([n, 4, 128])

    with contextlib.ExitStack() as SA:
        cmpm = sb("cmpm", [128, 2, 8, 128], BF16, SA)
        selcm = sb("selcm", [128, 4, 128], BF16, SA)
        winm = sb("winm", [128, 8, 128], BF16, SA)
        selA = sb("selA", [128, 8, 64], F32, SA)
        selB = sb("selB", [128, 8, 64], F32, SA)
        selF = sb("selF", [128, 8, 64], F32, SA)
        E = sb("E", [64, 4096], BF16, SA)
        ovl = sb("ovl", [128, 2, 64], F32, SA)
        selmat = sb("selmat", [24, 24, 128], F32, SA)
        oaT = sb("oaT", [128, 8, NT], BF16, SA)
        for t_, src, nm in [(cmpm, K["cmpm"], "cmpm"), (selcm, K["selcm"], "selcm"), (winm, K["winm"], "winm"), (selA, K["selA"], "selA"),
                            (selB, K["selB"], "selB"), (selF, K["selF"], "selF"), (E, K["E"], "E"), (ovl, K["ovl"], "ovl"), (selmat, K["selmat"], "selmat")]:
            fl = t_[:]
            if len(t_.shape) == 3:
                fl = t_[:].rearrange("p a b -> p (a b)")
            elif len(t_.shape) == 4:
                fl = t_[:].rearrange("p a b c -> p (a b c)")
            P.dma(lambda e, fl=fl, src=src: e.dma_start(out=fl, in_=src[:, :]), writes=[nm])

        gview = gbuf.rearrange("(i j d) (l p) -> d i l j p", j=4, i=12, d=128, l=8, p=128)
        for g in range(2):
            with contextlib.ExitStack() as SG_:
                KV = {}
                for nm, item in [("kc", 0 + g), ("vc", 2 + g), ("ks", 4 + g), ("kw", 6 + g), ("vs", 8 + g), ("vw", 10 + g)]:
                    t_ = sb("%s%d" % (nm, g), [128, 4096], BF16, SG_)
                    KV[nm] = t_
                    for l in range(8):
                        P.dma(lambda e, t_=t_, item=item, l=l: e.dma_start(out=t_[:, l * 512:(l + 1) * 512].rearrange("d (j p) -> d j p", j=4), in_=gview[:, item, l, :, :]),
                              reads=[("gbuf", item)], writes=[("kv", nm, l)])
                kvall = lambda nm: [("kv", nm, l) for l in range(8)]
                kcT = sb("kcT%d" % g, [128, 256], BF16, SG_)
                vcm = sb("vcm%d" % g, [128, 2, 128], BF16, SG_)
                with contextlib.ExitStack() as SCm:
                    w1t = sb("w1t", [128, 32, 256], BF16, SCm)
                    w2t = sb("w2t", [128, 2, 128], BF16, SCm)
                    posf = sb("posf", [128, 32], F32, SCm)
                    posb = sb("posb", [128, 32], BF16, SCm)
                    b1 = sb("b1", [128, 2], F32, SCm)
                    hx = sb("hx", [128, 256], F32, SCm)
                    hy = sb("hy", [128, 256], F32, SCm)
                    ghid = sb("ghid", [128, 2, 256], BF16, SCm)
                    ph = [ps("ph%d" % i, [128, 256], F32, SCm) for i in range(2)]
                    pb = [ps("pb%d" % i, [128, 2], F32, SCm) for i in range(2)]
                    pk = ps("pk", [128, 256], F32, SCm)
                    for kvn, src, w1n, w2n, posn in [("k", "kc", "cmp_k_w1", "cmp_k_w2", "cmp_pos_k"), ("v", "vc", "cmp_v_w1", "cmp_v_w2", "cmp_pos_v")]:
                        P.dma(lambda e, w1n=w1n: e.dma_start(out=w1t[:], in_=W[w1n].rearrange("(l d) m -> d l m", d=128)), writes=["w1t"], q="pool")
                        P.dma(lambda e, w2n=w2n: e.dma_start(out=w2t[:], in_=W[w2n].rearrange("(c h) d -> h c d", h=128)), writes=["w2t"], q="pool")
                        with nc.allow_non_contiguous_dma(reason="tiny"):
                            P.dma(lambda e, posn=posn: e.dma_start(out=posf[:], in_=W[posn].rearrange("l d -> d l"), allow_slow_non_contiguous=True), writes=["posf"])
                        P.dve(lambda e: e.tensor_copy(out=posb[:], in_=posf[:]), reads=["posf"], writes=["posb"])
                        xT = KV[src]
                        for hc in range(2):
                            for l in range(32):
                                P.pe(lambda e, hc=hc, l=l, xT=xT: e.matmul(ph[hc][:, 0:255], lhsT=w1t[:, l, hc * 128:(hc + 1) * 128], rhs=xT[:, l:l + 16 * 254 + 1:16],
                                                                   start=(l == 0), stop=(l == 31)), reads=["w1t"] + kvall(src), writes=["ph%d" % hc])
                            for l in range(32):
                                P.pe(lambda e, hc=hc, l=l: e.matmul(pb[hc][:, 0:1], lhsT=w1t[:, l, hc * 128:(hc + 1) * 128], rhs=posb[:, l:l + 1],
                                                             start=(l == 0), stop=(l == 31)), reads=["w1t", "posb"], writes=["pb%d" % hc])
                            P.act(lambda e, hc=hc: e.copy(out=b1[:, hc:hc + 1], in_=pb[hc][:, 0:1]), reads=["pb%d" % hc], writes=["b1"])
                            P.act(lambda e, hc=hc: e.activation(out=hx[:, 0:255], in_=ph[hc][:, 0:255], func=AF.Identity, bias=b1[:, hc:hc + 1]), reads=["ph%d" % hc, "b1"], writes=["hx"])
                            P.act(lambda e: e.activation(out=hy[:, 0:255], in_=hx[:, 0:255], func=AF.Square), reads=["hx"], writes=["hy"])
                            P.dve(lambda e: e.tensor_scalar(out=hy[:, 0:255], in0=hy[:, 0:255], scalar1=0.044715, scalar2=1.0, op0=ALU.mult, op1=ALU.add), reads=["hy"], writes=["hy"])
                            P.dve(lambda e: e.tensor_tensor(out=hy[:, 0:255], in0=hy[:, 0:255], in1=hx[:, 0:255], op=ALU.mult), reads=["hy", "hx"], writes=["hy"])
                            P.act(lambda e: e.activation(out=hy[:, 0:255], in_=hy[:, 0:255], func=AF.Sigmoid, scale=1.5957691216057308), reads=["hy"], writes=["hy"])
                            P.dve(lambda e, hc=hc: e.tensor_tensor(out=ghid[:, hc, 0:255], in0=hy[:, 0:255], in1=hx[:, 0:255], op=ALU.mult), reads=["hy", "hx"], writes=[("ghid", hc)])
                        if kvn == "k":
                            for hc in range(2):
                                P.pe(lambda e, hc=hc: e.matmul(pk[:, 0:255], lhsT=w2t[:, hc, :], rhs=ghid[:, hc, 0:255], start=(hc == 0), stop=(hc == 1)),
                                     reads=["w2t", ("ghid", 0), ("ghid", 1)], writes=["pk"])
                            P.act(lambda e: e.copy(out=kcT[:, 0:255], in_=pk[:, 0:255]), reads=["pk"], writes=["kcT"])
                        else:
                            for ct in range(2):
                                cn = 128 if ct == 0 else 127
                                for hc in range(2):
                                    P.pe(lambda e, hc=hc, ct=ct, cn=cn: e.matmul(pk[0:cn, ct * 128:(ct + 1) * 128], lhsT=ghid[:, hc, ct * 128:ct * 128 + cn], rhs=w2t[:, hc, :], start=(hc == 0), stop=(hc == 1)),
                                         reads=["w2t", ("ghid", 0), ("ghid", 1)], writes=["pk"])
                            P.act(lambda e: e.copy(out=vcm[:, 0, :], in_=pk[:, 0:128]), reads=["pk"], writes=["vcm"])
                            P.act(lambda e: e.copy(out=vcm[0:127, 1, :], in_=pk[0:127, 128:256]), reads=["pk"], writes=["vcm"])
                P.barrier()
                with contextlib.ExitStack() as SQ:
                    Pcf = [sb("Pcf%d" % ct, [128, 512], F32, SQ) for ct in range(2)]
                    pcb = [sb("pcb%d" % ct, [128, 512], BF16, SQ) for ct in range(2)]
                    rd = sb("rd", [128, 512], F32, SQ)
                    gS = sb("gS", [128, 512], F32, SQ)
                    wgt = sb("wgt", [128, 512], F32, SQ)
                    accs = [sb("acc%d" % i, [128, 512], F32, SQ) for i in range(2)]
                    rdc = sb("rdc", [128, 512], F32, SQ)
                    gSc = sb("gSc", [128, 512], F32, SQ)
                    t0 = sb("t0", [128, 64], F32, SQ)
                    t1 = sb("t1s", [128, 64], F32, SQ)
                    m8a = sb("m8a", [128, 8], F32, SQ)
                    m8b = sb("m8b", [128, 8], F32, SQ)
                    selbb = sb("selbb", [128, 64], BF16, SQ)
                    selbTs = [sb("selbT%d" % i, [64, 4, 128], BF16, SQ) for i in range(2)]
                    Pt = [sb("Pt%d" % i, [128, 512], BF16, SQ) for i in range(3)]
                    pS = [ps("pS%d" % i, [128, 512], F32, SQ) for i in range(2)]
                    pO = ps("pO", [128, 512], F32, SQ)
                    pD = ps("pD", [128, 512], F32, SQ)
                    pG = ps("pG", [128, 512], F32, SQ)
                    pI = ps("pI", [128, 64], F32, SQ)
                    pTt = ps("pTt", [64, 128], BF16, SQ)
                    pC = ps("pC", [128, 512], F32, SQ)
                    scnt = [0]
                    pcnt = [0]

                    def dump(nm, tile_, key, shape, dt=F32):
                        C.dbg[nm] = nc.dram_tensor("dbg_" + nm, list(shape), dt, kind="ExternalOutput").ap()
                        C.dbg_ops.append(P.dma(lambda e: e.dma_start(out=C.dbg[nm][:, :], in_=tile_), reads=[key], writes=["dd_" + nm]))

                    def gates(br, l, dst=None, dkey="gS"):
                        dst = gS if dst is None else dst
                        for h in range(4):
                            n = (4 * g + h) * 3 + br
                            P.pe(lambda e, h=h, n=n, l=l: e.matmul(pG[:, h * 128:(h + 1) * 128], lhsT=selmat[:, n, :], rhs=gT[:, l * 128:(l + 1) * 128], start=True, stop=True),
                                 reads=["selmat", "gT"], writes=["pG"])
                        P.act(lambda e, dst=dst: e.copy(out=dst[:], in_=pG[:]), reads=["pG"], writes=[dkey])

                    def branch(br, l, keyT, vtok, kts, maskfn, use_sel):
                        acc = accs[l % 2]
                        akey = "acc%d" % (l % 2)
                        selbT = selbTs[l % 2]
                        skey = "selbT%d" % (l % 2)
                        qg = qT[:, 4 * g:4 * g + 4, l * 128:(l + 1) * 128]
                        qkeys = [("qT", 4 * g + h) for h in range(4)]
                        nk = len(kts)
                        for ii, kt in enumerate(kts):
                            si = scnt[0] % 2
                            scnt[0] += 1
                            pi_ = pcnt[0] % 3
                            pcnt[0] += 1
                            lk = ("kv", keyT[1], kt // 4)
                            P.pe(lambda e, si=si, kt=kt: e.matmul(pS[si][:], lhsT=keyT[0][:, kt * 128:(kt + 1) * 128], rhs=qg, start=True, stop=(not use_sel)),
                                 reads=[lk] + qkeys, writes=["pS%d" % si])
                            if use_sel:
                                P.pe(lambda e, si=si, kt=kt, selbT=selbT: e.matmul(pS[si][:], lhsT=E[:, kt * 128:(kt + 1) * 128], rhs=selbT[:].rearrange("s h q -> s (h q)"), start=False, stop=True),
                                     reads=["E", skey], writes=["pS%d" % si])
                            P.act(lambda e, si=si, pi_=pi_: e.activation(out=Pt[pi_][:], in_=pS[si][:], func=AF.Exp, scale=SCALE), reads=["pS%d" % si], writes=["Pt%d" % pi_])
                            mk = maskfn(kt)
                            if mk is not None:
                                P.dve(lambda e, pi_=pi_, mk=mk: e.tensor_tensor(out=Pt[pi_][:].rearrange("k (h q) -> k h q", h=4), in0=Pt[pi_][:].rearrange("k (h q) -> k h q", h=4),
                                                                          in1=bc4(mk[0], 128), op=ALU.mult), reads=["Pt%d" % pi_, mk[1]], writes=["Pt%d" % pi_])
                            vk = ("kv", vtok[1], kt // 4)
                            P.pe(lambda e, pi_=pi_, kt=kt, ii=ii: e.matmul(pO[:], lhsT=vtok[0][:, kt * 128:(kt + 1) * 128], rhs=Pt[pi_][:], start=(ii == 0), stop=(ii == nk - 1)),
                                 reads=[vk, "Pt%d" % pi_], writes=["pO"])
                            P.pe(lambda e, pi_=pi_, ii=ii: e.matmul(pD[:], lhsT=ones_bf[:], rhs=Pt[pi_][:], start=(ii == 0), stop=(ii == nk - 1)),
                                 reads=["ones_bf", "Pt%d" % pi_], writes=["pD"])
                        gates(br, l)
                        P.dve(lambda e: e.tensor_scalar(out=rd[:], in0=pD[:], scalar1=1e-30, scalar2=None, op0=ALU.max), reads=["pD"], writes=["rd"])
                        P.dve(lambda e: e.reciprocal(out=rd[:], in_=rd[:]), reads=["rd"], writes=["rd"])
                        P.dve(lambda e: e.tensor_tensor(out=wgt[:], in0=rd[:], in1=gS[:], op=ALU.mult), reads=["rd", "gS"], writes=["wgt"])
                        P.dve(lambda e: e.tensor_tensor(out=wgt[:], in0=pO[:], in1=wgt[:], op=ALU.mult), reads=["pO", "wgt"], writes=["wgt"])
                        P.dve(lambda e, acc=acc: e.tensor_tensor(out=acc[:], in0=acc[:], in1=wgt[:], op=ALU.add), reads=[akey, "wgt"], writes=[akey])

                    def cmp_stage(l):
                        acc = accs[l % 2]
                        akey = "acc%d" % (l % 2)
                        selbT = selbTs[l % 2]
                        skey = "selbT%d" % (l % 2)
                        qg = qT[:, 4 * g:4 * g + 4, l * 128:(l + 1) * 128]
                        qkeys = [("qT", 4 * g + h) for h in range(4)]
                        for ct in range(2):
                            cn = 128 if ct == 0 else 127
                            si = scnt[0] % 2
                            scnt[0] += 1
                            P.pe(lambda e, si=si, ct=ct, cn=cn, qg=qg: e.matmul(pS[si][0:cn, :], lhsT=kcT[:, ct * 128:ct * 128 + cn], rhs=qg, start=True, stop=True),
                                 reads=["kcT"] + qkeys, writes=["pS%d" % si])
                            P.act(lambda e, si=si, ct=ct, cn=cn: e.activation(out=Pcf[ct][0:cn, :], in_=pS[si][0:cn, :], func=AF.Exp, scale=SCALE), reads=["pS%d" % si], writes=["Pcf%d" % ct])
                            P.dve(lambda e, ct=ct, cn=cn, l=l: e.tensor_tensor(out=Pcf[ct][0:cn, :].rearrange("k (h q) -> k h q", h=4), in0=Pcf[ct][0:cn, :].rearrange("k (h q) -> k h q", h=4),
                                                                     in1=bc4(cmpm[0:cn, ct, l, :], cn), op=ALU.mult), reads=["Pcf%d" % ct, "cmpm"], writes=["Pcf%d" % ct])
                        for ct in range(2):
                            cn = 128 if ct == 0 else 127
                            P.pe(lambda e, ct=ct, cn=cn: e.matmul(pC[:], lhsT=ones_f[0:cn, :], rhs=Pcf[ct][0:cn, :], start=(ct == 0), stop=(ct == 1)),
                                 reads=["ones_f", "Pcf%d" % ct], writes=["pC"])
                        P.dve(lambda e: e.tensor_scalar(out=rdc[:], in0=pC[:], scalar1=1e-30, scalar2=None, op0=ALU.max), reads=["pC"], writes=["rdc"])
                        P.dve(lambda e: e.reciprocal(out=rdc[:], in_=rdc[:]), reads=["rdc"], writes=["rdc"])
                        for ct in range(2):
                            cn = 128 if ct == 0 else 127
                            P.dve(lambda e, ct=ct, cn=cn: e.tensor_tensor(out=Pcf[ct][0:cn, :], in0=Pcf[ct][0:cn, :], in1=rdc[0:cn, :], op=ALU.mult), reads=["Pcf%d" % ct, "rdc"], writes=["Pcf%d" % ct])
                            P.act(lambda e, ct=ct, cn=cn: e.copy(out=pcb[ct][0:cn, :], in_=Pcf[ct][0:cn, :]), reads=["Pcf%d" % ct], writes=["pcb%d" % ct])
                        for ct in range(2):
                            cn = 128 if ct == 0 else 127
                            P.pe(lambda e, ct=ct, cn=cn: e.matmul(pC[:], lhsT=vcm[0:cn, ct, :], rhs=pcb[ct][0:cn, :], start=(ct == 0), stop=(ct == 1)),
                                 reads=["vcm", "pcb%d" % ct, "rdc"], writes=["pC"])
                        cnt8 = 0
                        for r in range(4):
                            for ct in range(2):
                                cn = 128 if ct == 0 else 127
                                P.pe(lambda e, ct=ct, cn=cn, r=r, cnt8=cnt8: e.matmul(pI[:], lhsT=Pcf[ct][0:cn, r * 128:(r + 1) * 128], rhs=ovl[0:cn, ct, :], start=(cnt8 == 0), stop=(cnt8 == 7)),
                                     reads=["Pcf%d" % ct, "ovl"], writes=["pI"])
                                cnt8 += 1
                        gates(0, l, gSc, "gSc")
                        P.dve(lambda e, acc=acc: e.tensor_tensor(out=acc[:], in0=pC[:], in1=gSc[:], op=ALU.mult), reads=["pC", "gSc"], writes=[akey])
                        dbg_here = (C.stop == 22 and g == 0 and l == 1)
                        if dbg_here:
                            dump("acc0", acc[:], akey, [128, 512])
                            dump("kcT", kcT[:], "kcT", [128, 256], BF16)
                            dump("vcm", vcm[:].rearrange("p a b -> p (a b)"), "vcm", [128, 256], BF16)
                            dump("gS0", gSc[:], "gSc", [128, 512])
                        P.dve(lambda e, l=l: e.tensor_tensor(out=t0[:], in0=pI[:], in1=selA[:, l, :], op=ALU.mult), reads=["pI", "selA"], writes=["t0"])
                        P.dve(lambda e, l=l: e.tensor_tensor(out=t0[:], in0=t0[:], in1=selB[:, l, :], op=ALU.add), reads=["t0", "selB"], writes=["t0"])
                        P.dve(lambda e, l=l: e.tensor_tensor(out=t0[:], in0=t0[:], in1=selF[:, l, :], op=ALU.max), reads=["t0", "selF"], writes=["t0"])
                        P.dve(lambda e: e.max(out=m8a[:], in_=t0[:]), reads=["t0"], writes=["m8a"])
                        P.dve(lambda e: e.match_replace(out=t1[:], in_to_replace=m8a[:], in_values=t0[:], imm_value=-1e30), reads=["t0", "m8a"], writes=["t1s"])
                        P.dve(lambda e: e.max(out=m8b[:], in_=t1[:]), reads=["t1s"], writes=["m8b"])
                        P.dve(lambda e: e.tensor_scalar(out=t1[:], in0=t0[:], scalar1=m8b[:, 7:8], scalar2=-1.0, op0=ALU.is_ge, op1=ALU.add), reads=["t0", "m8b"], writes=["t1s"])
                        P.dve(lambda e: e.tensor_scalar(out=selbb[:], in0=t1[:], scalar1=-BIGNEG, scalar2=None, op0=ALU.mult), reads=["t1s"], writes=["selbb"])
                        P.pe(lambda e: e.transpose(out=pTt[:], in_=selbb[:], identity=ident[:]), reads=["selbb", "ident"], writes=["pTt"])
                        for h in range(4):
                            P.act(lambda e, h=h, selbT=selbT: e.copy(out=selbT[:, h, :], in_=pTt[:]), reads=["pTt"], writes=[skey])
                        if C.stop == 22 and g == 0 and l == 1:
                            C.dbg["t0"] = nc.dram_tensor("dbg_t0", [128, 64], F32, kind="ExternalOutput").ap()
                            C.dbg["selb"] = nc.dram_tensor("dbg_selb", [128, 64], BF16, kind="ExternalOutput").ap()
                            C.dbg_ops.append(P.dma(lambda e: e.dma_start(out=C.dbg["t0"][:, :], in_=t0[:]), reads=["t0"], writes=["dd1"]))
                            C.dbg_ops.append(P.dma(lambda e: e.dma_start(out=C.dbg["selb"][:, :], in_=selbb[:]), reads=["selbb"], writes=["dd2"]))
                    def rest_stage(l):
                        acc = accs[l % 2]
                        akey = "acc%d" % (l % 2)
                        dbg_here = (C.stop == 22 and g == 0 and l == 1)
                        branch(1, l, (KV["ks"], "ks"), (KV["vs"], "vs"), list(range(4 * l + 4)),
                               lambda kt, l=l: ((selcm[:, kt - 4 * l, :], "selcm") if kt >= 4 * l else None), True)
                        if dbg_here:
                            dump("acc1", acc[:], akey, [128, 512])
                        branch(2, l, (KV["kw"], "kw"), (KV["vw"], "vw"), list(range(max(0, 4 * l - 4), 4 * l + 4)),
                               lambda kt, l=l: (winm[:, kt - 4 * l + 4, :], "winm"), False)
                        if dbg_here:
                            dump("acc2", acc[:], akey, [128, 512])
                        for h in range(4):
                            P.act(lambda e, h=h, l=l, g=g, acc=acc: e.copy(out=oaT[:, 4 * g + h, l * 128:(l + 1) * 128], in_=acc[:, h * 128:(h + 1) * 128]), reads=[akey], writes=[("oaT", 4 * g + h)])
                    cmp_stage(0)
                    for l in range(8):
                        if l + 1 < 8:
                            cmp_stage(l + 1)
                        rest_stage(l)
                P.barrier()
        with contextlib.ExitStack() as SN:
            ag = sb("ag", [128, 8], F32, SN)
            rstdb = sb("a_rstdb", [128, NT], F32, SN)
            sqb = [sb("a_sqb%d" % i, [128, NT], BF16, SN) for i in range(2)]
            pss = [ps("a_pss%d" % hf, [128, 512], F32, SN) for hf in range(2)]
            with nc.allow_non_contiguous_dma(reason="tiny"):
                P.dma(lambda e: e.dma_start(out=ag[:], in_=W["attn_out_g"].rearrange("o (b p) -> p (o b)", p=128), allow_slow_non_contiguous=True), writes=["ag"])
            for h in range(8):
                i = h % 2
                P.act(lambda e, h=h, i=i: e.activation(out=sqb[i][:], in_=oaT[:, h, :], func=AF.Square), reads=[("oaT", h)], writes=["a_sqb%d" % i])
                for hf in range(2):
                    P.pe(lambda e, h=h, i=i, hf=hf: e.matmul(pss[hf][:], lhsT=ones_bf[:], rhs=sqb[i][:, hf * 512:(hf + 1) * 512], start=(h == 0), stop=(h == 7)),
                         reads=["ones_bf", "a_sqb%d" % i], writes=["a_pss%d" % hf])
            for hf in range(2):
                sl = slice(hf * 512, (hf + 1) * 512)
                P.act(lambda e, hf=hf, sl=sl: e.activation(out=rstdb[:, sl], in_=pss[hf][:], func=AF.Sqrt, scale=1.0 / 1024, bias=epsT[:, 0:1]), reads=["a_pss%d" % hf, "m_eps"], writes=["a_rstdb"])
            P.dve(lambda e: e.reciprocal(out=rstdb[:], in_=rstdb[:]), reads=["a_rstdb"], writes=["a_rstdb"])
            ATall = [("AT", t) for t in range(NTILE)]
            for h in range(8):
                P.dve(lambda e, h=h: e.scalar_tensor_tensor(out=AT[:, h, :], in0=oaT[:, h, :], scalar=ag[:, h:h + 1], in1=rstdb[:], op0=ALU.mult, op1=ALU.mult),
                      reads=[("oaT", h), "ag", "a_rstdb"], writes=ATall)
                P.pool(lambda e, h=h: e.tensor_copy(out=AT[:, 8 + h, :], in_=orecT[:, h, :]), reads=[("orecT", h)], writes=ATall)
    P.barrier()


def outproj(C):
    nc, P, W, sb, ps, AT = C.nc, C.P, C.W, C.sb, C.ps, C.AT
    contextlib = C.contextlib
    ATall = [("AT", t) for t in range(NTILE)]
    with contextlib.ExitStack() as SO:
        YT = sb("YT", [128, KD, NT], BF16, SO)
        with contextlib.ExitStack() as S1:
            wp = [sb("wo%d" % i, [128, KD, 512], BF16, S1) for i in range(2)]
            py = [[ps("po%d%d" % (i, hf), [128, 512], F32, S1) for hf in range(2)] for i in range(2)]
            wv = W["w_out"].rearrange("(kc p) m -> p kc m", p=128)
            cnt = 0
            for pi in range(4):
                b = pi % 2
                P.dma(lambda e, pi=pi, b=b: e.dma_start(out=wp[b][:], in_=wv[:, :, pi * 512:(pi + 1) * 512]), writes=["wo%d" % b], q="pool")
                for mc in range(4):
                    m = pi * 4 + mc
                    r = cnt % 2
                    cnt += 1
                    for hf in range(2):
                        for k in range(KD):
                            P.pe(lambda e, k=k, hf=hf, r=r, b=b, mc=mc: e.matmul(py[r][hf][:], lhsT=wp[b][:, k, mc * 128:(mc + 1) * 128], rhs=AT[:, k, hf * 512:(hf + 1) * 512],
                                                                         start=(k == 0), stop=(k == KD - 1)), reads=["wo%d" % b] + ATall[hf * 4:hf * 4 + 4], writes=["po%d%d" % (r, hf)])
                        P.act(lambda e, m=m, hf=hf, r=r: e.copy(out=YT[:, m, hf * 512:(hf + 1) * 512], in_=py[r][hf][:]), reads=["po%d%d" % (r, hf)],
                              writes=[("YT", hf * 4 + q) for q in range(4)])
        C.post("mx", YT, C.h1d, W["mix_post_g"], 1.0, C.h2d, W["ff2_pre_g"])


def ple(C):
    nc, P, W, sb, ps, AT, ident = C.nc, C.P, C.W, C.sb, C.ps, C.AT, C.ident
    contextlib = C.contextlib
    ATall = [("AT", t) for t in range(NTILE)]
    with contextlib.ExitStack() as SO:
        YT = sb("YTp", [128, KD, NT], BF16, SO)
        with contextlib.ExitStack() as S1:
            pT = sb("pTp", [128, 2, NT], BF16, S1)
            ptl = [sb("ptl%d" % i, [128, 256], F32, S1) for i in range(2)]
            ptb = [sb("ptb%d" % i, [128, 256], BF16, S1) for i in range(2)]
            wpj = sb("wpj", [128, 2, D], BF16, S1)
            sg = [sb("psg%d" % i, [128, 512], F32, S1) for i in range(2)]
            wp = [sb("wg%d" % i, [128, KD, 512], BF16, S1) for i in range(2)]
            ppt = [ps("ppt%d" % i, [128, 2, 128], BF16, S1) for i in range(2)]
            pa = [[ps("pa%d%d" % (i, hf), [128, 512], F32, S1) for hf in range(2)] for i in range(1)]
            pb = [[ps("pbp%d%d" % (i, hf), [128, 512], F32, S1) for hf in range(2)] for i in range(1)]
            P.dma(lambda e: e.dma_start(out=wpj[:], in_=W["w_ple_proj"].rearrange("(c p) m -> p c m", p=128)), writes=["wpj"], q="pool")
            for t in range(NTILE):
                i = t % 2
                P.dma(lambda e, t=t, i=i: e.dma_start(out=ptl[i][:], in_=C.pin[t * 128:(t + 1) * 128, :]), writes=["ptl%d" % i])
                P.dve(lambda e, i=i: e.tensor_copy(out=ptb[i][:], in_=ptl[i][:]), reads=["ptl%d" % i], writes=["ptb%d" % i])
                for c in range(2):
                    P.pe(lambda e, i=i, c=c: e.transpose(out=ppt[i][:, c, :], in_=ptb[i][:, c * 128:(c + 1) * 128], identity=ident[:]), reads=["ptb%d" % i, "ident"], writes=["ppt%d" % i])
                P.act(lambda e, i=i, t=t: e.copy(out=pT[:, :, t * 128:(t + 1) * 128], in_=ppt[i][:]), reads=["ppt%d" % i], writes=[("pTp", t)])
            wv = W["w_ple_gate"].rearrange("(kc p) m -> p kc m", p=128)
            pTall = [("pTp", t) for t in range(NTILE)]
            for pi in range(4):
                b = pi % 2
                P.dma(lambda e, pi=pi, b=b: e.dma_start(out=wp[b][:], in_=wv[:, :, pi * 512:(pi + 1) * 512]), writes=["wg%d" % b], q="pool")
                for mc in range(4):
                    m = pi * 4 + mc
                    for hf in range(2):
                        for k in range(KD):
                            P.pe(lambda e, k=k, hf=hf, b=b, mc=mc: e.matmul(pa[0][hf][:], lhsT=wp[b][:, k, mc * 128:(mc + 1) * 128], rhs=AT[:, k, hf * 512:(hf + 1) * 512],
                                                                    start=(k == 0), stop=(k == KD - 1)), reads=["wg%d" % b] + ATall[hf * 4:hf * 4 + 4], writes=["pa0%d" % hf])
                        for c in range(2):
                            P.pe(lambda e, c=c, hf=hf, m=m: e.matmul(pb[0][hf][:], lhsT=wpj[:, c, m * 128:(m + 1) * 128], rhs=pT[:, c, hf * 512:(hf + 1) * 512],
                                                              start=(c == 0), stop=(c == 1)), reads=["wpj"] + pTall[hf * 4:hf * 4 + 4], writes=["pbp0%d" % hf])
                        P.act(lambda e, hf=hf: e.activation(out=sg[hf][:], in_=pa[0][hf][:], func=AF.Sigmoid), reads=["pa0%d" % hf], writes=["psg%d" % hf])
                        P.dve(lambda e, hf=hf, m=m: e.tensor_tensor(out=YT[:, m, hf * 512:(hf + 1) * 512], in0=sg[hf][:], in1=pb[0][hf][:], op=ALU.mult),
                              reads=["psg%d" % hf, "pbp0%d" % hf], writes=[("YTp", hf * 4 + q) for q in range(4)])
        C.post("pl", YT, C.h3d, W["ple_post_g"], 1.0, C.out, None)


def host_consts(j):
    import ml_dtypes
    bf = ml_dtypes.bfloat16
    c = {}
    d = np.arange(128)
    half = 16
    inv_freq = (500000.0 ** (-np.arange(half, dtype=np.float32) / half)).astype(np.float32)
    rope = np.zeros((128, 4), np.float32)
    rope[:32, 0] = inv_freq[d[:32] % 16]
    rope[:16, 1] = -1.0
    rope[16:32, 1] = 1.0
    rope[:, 2] = 1.0
    c["c_rope"] = rope
    perm = np.zeros((128, 128), np.float32)
    for m in range(128):
        k = m + 16 if m < 16 else (m - 16 if m < 32 else m)
        perm[k, m] = 1.0
    c["c_perm"] = perm
    oh = np.zeros((128, 4), np.float32)
    oh[:, j] = 1.0
    c["c_oh"] = oh
    q = np.arange(128)
    kk = np.arange(128)
    cm = np.zeros((128, 2, 8, 128), np.float32)
    for ct in range(2):
        cg = ct * 128 + kk
        for l in range(8):
            t = (4 * l + j) * 128 + q
            cm[:, ct, l, :] = ((16 * cg[:, None] + 31 <= t[None, :]) & (cg[:, None] < 255))
    c["c_cmpm"] = cm.reshape(128, -1).astype(bf)
    tri = (kk[:, None] <= q[None, :]).astype(np.float32)
    triu = (kk[:, None] > q[None, :]).astype(np.float32)
    sc = np.zeros((128, 4, 128), np.float32)
    for m in range(4):
        sc[:, m, :] = 1.0 if m < j else (tri if m == j else 0.0)
    c["c_selcm"] = sc.reshape(128, -1).astype(bf)
    wm = np.zeros((128, 8, 128), np.float32)
    for m in range(8):
        dd = m - 4 - j
        if dd == 0:
            wm[:, m, :] = tri
        elif dd == -4:
            wm[:, m, :] = triu
        elif -4 < dd < 0:
            wm[:, m, :] = 1.0
    c["c_winm"] = wm.reshape(128, -1).astype(bf)
    sA = np.zeros((128, 8, 64), np.float32)
    sB = np.zeros((128, 8, 64), np.float32)
    sF = np.zeros((128, 8, 64), np.float32)
    s = np.arange(64)
    for l in range(8):
        t = (4 * l + j) * 128 + q
        valid = (64 * s[None, :] <= t[:, None])
        forced = (s[None, :] == (t // 64)[:, None]) | (s[None, :] == 0)
        sA[:, l, :] = valid
        sB[:, l, :] = (valid.astype(np.float32) - 1.0) * 1e4
        sF[:, l, :] = np.where(forced, 1e4, -3e4)
    c["c_selA"] = sA.reshape(128, -1)
    c["c_selB"] = sB.reshape(128, -1)
    c["c_selF"] = sF.reshape(128, -1)
    key = np.arange(4096)
    c["c_E"] = (key[None, :] // 64 == s[:, None]).astype(np.float32).astype(bf)
    ov = np.zeros((128, 2, 64), np.float32)
    for ct in range(2):
        cg = ct * 128 + kk
        st_ = 16 * cg
        en_ = st_ + 31
        ov[:, ct, :] = ((st_[:, None] < 64 * s[None, :] + 64) & (en_[:, None] >= 64 * s[None, :]) & (cg[:, None] < 255))
    c["c_ovl"] = ov.reshape(128, -1)
    sm = np.zeros((24, 24, 128), np.float32)
    for n in range(24):
        sm[n, n, :] = 1.0
    c["c_selmat"] = sm.reshape(24, -1)
    return c


_orig_make_in_maps = make_in_maps


def make_in_maps(inputs):
    maps = _orig_make_in_maps(inputs)
    for c in range(8):
        maps[c].update(host_consts(c % 4))
    return maps


_NC_CACHE = {}


def kernel(**inputs):
    maps = make_in_maps(inputs)
    if "nc" not in _NC_CACHE:
        _NC_CACHE["nc"] = build()
    nc = _NC_CACHE["nc"]
    res = run_bass_kernel_spmd(nc, maps, core_ids=list(range(8)))
    return unshard([r["out"] for r in res.results])
```

```python
import numpy as np
import concourse.bass as bass
import concourse.mybir as mybir
from concourse.bass_utils import run_bass_kernel_spmd

F32 = mybir.dt.float32
BF16 = mybir.dt.bfloat16
I32 = mybir.dt.int32
AF = mybir.ActivationFunctionType
ALU = mybir.AluOpType
AX = mybir.AxisListType

COMPUTE = ("pe", "act", "dve", "pool")
NRING = 12


class Op:
    __slots__ = ("eng", "fn", "deps", "is_dma", "signal", "idx", "ring", "ringval", "prev_ring", "is_cc", "seg", "dur", "fin", "bdeps")

    def __init__(self, eng, fn, is_dma):
        self.eng = eng
        self.fn = fn
        self.is_dma = is_dma
        self.deps = []
        self.bdeps = []
        self.signal = 0
        self.ring = None
        self.ringval = 0
        self.prev_ring = None
        self.idx = 0
        self.is_cc = False
        self.seg = 0
        self.dur = 0.5
        self.fin = 0.0


DEFAULT_DUR = {"pe": 0.28, "act": 0.7, "dve": 0.9, "pool": 1.2, "sp": 2.5}
SCHED = True
WINDOW = 48


class Prog:
    def __init__(self, nc):
        self.nc = nc
        self.ops = {k: [] for k in ("pe", "act", "dve", "pool", "sp")}
        self.last_w = {}
        self.readers = {}
        self.nops = 0
        self.seg = 0

    def barrier(self):
        self.seg += 1

    def _add(self, eng, fn, reads, writes, is_dma, d=None):
        op = Op(eng, fn, is_dma)
        op.idx = self.nops
        op.seg = self.seg
        op.dur = DEFAULT_DUR[eng] if d is None else d
        self.nops += 1
        deps = {}
        for r in reads:
            w = self.last_w.get(r)
            if w is not None:
                deps[id(w)] = w
        for wr in writes:
            w = self.last_w.get(wr)
            if w is not None:
                deps[id(w)] = w
            for rd in self.readers.get(wr, ()):
                deps[id(rd)] = rd
        for dd in deps.values():
            if dd is op:
                continue
            op.deps.append(dd)
        for r in reads:
            self.readers.setdefault(r, []).append(op)
        for wr in writes:
            self.last_w[wr] = op
            self.readers[wr] = []
        self.ops[eng].append(op)
        return op

    def pe(self, fn, reads=(), writes=(), d=None):
        return self._add("pe", fn, reads, writes, False, d)

    def act(self, fn, reads=(), writes=(), d=None):
        return self._add("act", fn, reads, writes, False, d)

    def dve(self, fn, reads=(), writes=(), d=None):
        return self._add("dve", fn, reads, writes, False, d)

    def pool(self, fn, reads=(), writes=(), d=None):
        return self._add("pool", fn, reads, writes, False, d)

    def dma(self, fn, reads=(), writes=(), q="sp", d=None):
        return self._add(q, fn, reads, writes, True, d)

    def cc(self, fn, reads=(), writes=()):
        op = self._add("pool", fn, reads, writes, True, 15.0)
        op.is_cc = True
        return op

    def _schedule(self):
        nseg = self.seg + 1
        byseg = [{e: [] for e in self.ops} for _ in range(nseg)]
        for e, lst in self.ops.items():
            for op in lst:
                byseg[op.seg][e].append(op)
        new = {e: [] for e in self.ops}
        LAT_X, LAT_S = 1.2, 0.25
        t_base = 0.0
        for sg in range(nseg):
            queues = byseg[sg]
            if not SCHED:
                for e in queues:
                    new[e].extend(queues[e])
                continue
            pos = {e: 0 for e in queues}
            done = set()
            sched_flag = {e: [False] * len(queues[e]) for e in queues}
            free_at = {e: t_base for e in queues}
            remaining = sum(len(q) for q in queues.values())
            tmax = t_base
            while remaining:
                best = None
                for e, q in queues.items():
                    n = len(q)
                    p = pos[e]
                    while p < n and sched_flag[e][p]:
                        p += 1
                    pos[e] = p
                    cnt = 0
                    i = p
                    while i < n and cnt < WINDOW:
                        if not sched_flag[e][i]:
                            cnt += 1
                            op = q[i]
                            ok = True
                            rdy = free_at[e]
                            for dd in op.deps:
                                if dd.seg == sg:
                                    if id(dd) not in done:
                                        ok = False
                                        break
                                    lat = LAT_S if (dd.eng == e and not dd.is_dma) else LAT_X
                                    if dd.eng == "pe" and e == "pe":
                                        lat = 0.0
                                    tt = dd.fin + lat
                                    if tt > rdy:
                                        rdy = tt
                            if ok:
                                key = (rdy, op.idx)
                                if best is None or key < best[0]:
                                    best = (key, e, i, op)
                                if rdy <= free_at[e]:
                                    break
                        i += 1
                (rdy, _), e, i, op = best
                if op.is_dma:
                    free_at[e] = rdy + 0.15
                    op.fin = rdy + op.dur
                else:
                    free_at[e] = rdy + op.dur
                    op.fin = free_at[e]
                tmax = max(tmax, op.fin)
                sched_flag[e][i] = True
                done.add(id(op))
                new[e].append(op)
                remaining -= 1
            t_base = tmax + 2.0
        self.ops = new

    def emit(self, final_wait_ops=()):
        nc = self.nc
        self._schedule()
        nseg = self.seg + 1
        last_compute = {}
        first_in_seg = {}
        dmas_in_seg = [[] for _ in range(nseg)]
        lastc_upto = [dict() for _ in range(nseg)]
        for e, lst in self.ops.items():
            for op in lst:
                if (op.seg, e) not in first_in_seg:
                    first_in_seg[(op.seg, e)] = op
                if op.is_dma:
                    dmas_in_seg[op.seg].append(op)
                else:
                    lastc_upto[op.seg][e] = op
        run_last = {}
        pend = {e: [] for e in self.ops}
        for sg in range(1, nseg):
            for e2, op2 in lastc_upto[sg - 1].items():
                run_last[e2] = op2
            bd = list(run_last.values()) + dmas_in_seg[sg - 1]
            for e in self.ops:
                pend[e] = pend[e] + bd
                f = first_in_seg.get((sg, e))
                if f is not None:
                    f.bdeps = [d for d in pend[e] if not (d.eng == e and not d.is_dma and not f.is_dma)]
                    pend[e] = []
        needed = set()
        for e, lst in self.ops.items():
            for op in lst:
                for d in op.deps:
                    if not (d.eng == "pe" and e == "pe" and not d.is_dma):
                        needed.add(id(d))
                for d in op.bdeps:
                    needed.add(id(d))
        for op in final_wait_ops:
            needed.add(id(op))
        ringcount = {}
        for e, lst in self.ops.items():
            n = 0
            k = 0
            last_on_ring = {}
            for op in lst:
                if id(op) not in needed:
                    continue
                if op.is_cc:
                    op.ring = ("cc", op.idx)
                    op.ringval = 1
                    op.prev_ring = None
                elif op.is_dma:
                    slot = k % NRING
                    k += 1
                    op.ring = (e, slot)
                    ringcount[(e, slot)] = ringcount.get((e, slot), 0) + 16
                    op.ringval = ringcount[(e, slot)]
                    op.prev_ring = last_on_ring.get(slot)
                    last_on_ring[slot] = op
                else:
                    n += 1
                    op.signal = n
        import contextlib
        with contextlib.ExitStack() as st:
            csem = {e: st.enter_context(nc.semaphore("s_" + e)) for e in ("pe", "act", "dve", "pool")}
            rsem = {}
            for e in ("sp", "pool"):
                for s_ in range(NRING):
                    rsem[(e, s_)] = st.enter_context(nc.semaphore("r_%s_%d" % (e, s_)))
            for e, lst in self.ops.items():
                for op in lst:
                    if op.is_cc and op.ring is not None:
                        rsem[op.ring] = st.enter_context(nc.semaphore("cc_%d" % op.idx))
            block = st.enter_context(nc.Block())

            def run(ename):
                def body(eng):
                    waited = {}
                    lst = self.ops[ename]
                    for op in lst:
                        deps = [d for d in op.deps if not (d.eng == "pe" and ename == "pe" and not d.is_dma)] + list(op.bdeps)
                        if op.is_dma and op.prev_ring is not None:
                            deps.append(op.prev_ring)
                        need = {}
                        for d in deps:
                            if d.is_dma:
                                key = ("r",) + d.ring
                                val = d.ringval
                                sem = rsem[d.ring]
                            else:
                                key = ("c", d.eng)
                                val = d.signal
                                sem = csem[d.eng]
                            assert val > 0, (ename, d.eng)
                            if need.get(key, (0, None))[0] < val:
                                need[key] = (val, sem)
                        for key, (val, sem) in need.items():
                            if waited.get(key, 0) >= val:
                                continue
                            waited[key] = val
                            eng.wait_ge(sem, val)
                        ins = op.fn(eng)
                        if op.is_cc:
                            if op.ring is not None:
                                ins.then_inc(rsem[op.ring], 1)
                        elif op.is_dma:
                            if op.ring is not None:
                                ins.then_inc(rsem[op.ring], 16)
                        elif op.signal:
                            ins.then_inc(csem[ename], 1)
                    if ename == "sp":
                        for d in final_wait_ops:
                            if d.is_dma:
                                eng.wait_ge(rsem[d.ring], d.ringval)
                            else:
                                eng.wait_ge(csem[d.eng], d.signal)
                return body

            block.tensor(run("pe"))
            block.scalar(run("act"))
            block.vector(run("dve"))
            block.gpsimd(run("pool"))
            block.sync(run("sp"))


D = 2048
NT = 1024
NTILE = 8
DFF = 5632
NF = DFF // 128
KD = D // 128
EPS = 1e-6
IN_WIDTH = 4632


def bcast_rows(ap, n, parts=128):
    return bass.AP(ap.tensor, ap.offset, [[0, parts], [1, n]])


class Ctx:
    pass


def build(stop=99, debug=False):
    import contextlib
    nc = bass.Bass("TRN2", target_bir_lowering=False)
    C = Ctx()
    C.nc = nc
    P = Prog(nc)
    C.P = P

    def din(name, shape, dt=F32):
        return nc.dram_tensor(name, list(shape), dt, kind="ExternalInput").ap()

    x = din("x", [NT, D])
    pin = din("p", [NT, 256])
    pos = din("pos", [1, NT], I32)
    WSHAPES = dict([("ff1_pre_g", [1, D]), ("ff1_post_g", [1, D]), ("ff1_w_gate", [D, DFF]), ("ff1_w_up", [D, DFF]),
                    ("ff1_w_down", [DFF, D]), ("mix_pre_g", [1, D]), ("mix_post_g", [1, D]), ("w_in", [D, IN_WIDTH]),
                    ("cmp_pos_k", [32, 128]), ("cmp_pos_v", [32, 128]), ("cmp_k_w1", [4096, 256]), ("cmp_k_w2", [256, 128]),
                    ("cmp_v_w1", [4096, 256]), ("cmp_v_w2", [256, 128]), ("nsa_gate_b", [1, 24]),
                    ("conv_w", [4, 1024]), ("conv_b", [1, 1024]), ("rg_w_a", [8, 128, 128]), ("rg_b_a", [1, 1024]),
                    ("rg_w_i", [8, 128, 128]), ("rg_b_i", [1, 1024]), ("rg_lambda", [1, 1024]),
                    ("attn_out_g", [1, 1024]), ("rec_out_g", [1, 1024]), ("w_out", [D, D]),
                    ("ff2_pre_g", [1, D]), ("ff2_post_g", [1, D]), ("ff2_w_gate", [D, DFF]), ("ff2_w_up", [D, DFF]),
                    ("ff2_w_down", [DFF, D]), ("ple_pre_g", [1, D]), ("ple_post_g", [1, D]),
                    ("w_ple_gate", [D, D]), ("w_ple_proj", [256, D])])

    class LazyW(dict):
        def __missing__(self, nm):
            v = din(nm, WSHAPES[nm])
            self[nm] = v
            return v
    W = LazyW()
    C.W = W
    out = nc.dram_tensor("out", [NT, D], F32, kind="ExternalOutput").ap()
    h1d = nc.dram_tensor("h1d", [NT, D], F32, kind="Internal").ap()
    h2d = nc.dram_tensor("h2d", [NT, D], F32, kind="Internal").ap()
    h3d = nc.dram_tensor("h3d", [NT, D], F32, kind="Internal").ap()
    dbg = {}
    if debug:
        dbg["uT"] = nc.dram_tensor("dbg_uT", [128, KD, NT], BF16, kind="ExternalOutput").ap()

    finals = []
    with contextlib.ExitStack() as st:
        used_names = {}

        def uniq(name):
            n = used_names.get(name, 0)
            used_names[name] = n + 1
            return name if n == 0 else "%s_v%d" % (name, n)

        def sb(name, shape, dt, stack=st):
            return stack.enter_context(nc.sbuf_tensor(uniq(name), list(shape), dt))

        def ps(name, shape, dt, stack=st):
            return stack.enter_context(nc.psum_tensor(uniq(name), list(shape), dt))

        identf = sb("identf", [128, 128], F32)
        ident = sb("ident", [128, 128], BF16)
        AT = sb("AT", [128, KD, NT], BF16)
        P.pool(lambda e: e.memset(identf[:], 1.0), writes=["identf"])
        P.pool(lambda e: e.affine_select(out=identf[:], in_=identf[:], pattern=[[-1, 128]], compare_op=ALU.is_equal,
                                         fill=0.0, base=0, channel_multiplier=1), reads=["identf"], writes=["identf"])
        P.dve(lambda e: e.tensor_copy(out=ident[:], in_=identf[:]), reads=["identf"], writes=["ident"])
        C.ident, C.identf, C.AT = ident, identf, AT
        C.sb, C.ps = sb, ps

        def norm_transpose(S, tg, ht, hkey, gb, gkey, t, scr):
            sq, ss, ub, pT = scr["sq"], scr["ss"], scr["ub"], scr["pT"]
            P.act(lambda e: e.activation(out=sq[:], in_=ht[:], func=AF.Square, accum_out=ss[:, 0:1]),
                  reads=[hkey], writes=[tg + "sq", tg + "ss"])
            P.act(lambda e: e.activation(out=ss[:, 1:2], in_=ss[:, 0:1], func=AF.Sqrt, scale=1.0 / D, bias=scr["eps"][:, 0:1]),
                  reads=[tg + "ss", scr["epskey"]], writes=[tg + "ss1"])
            P.dve(lambda e: e.reciprocal(out=ss[:, 2:3], in_=ss[:, 1:2]), reads=[tg + "ss1"], writes=[tg + "ss2"])
            P.dve(lambda e: e.scalar_tensor_tensor(out=ub[:], in0=ht[:], scalar=ss[:, 2:3], in1=gb[:], op0=ALU.mult, op1=ALU.mult),
                  reads=[hkey, tg + "ss2", gkey], writes=[tg + "ub"])
            for k in range(KD):
                P.pe(lambda e, k=k: e.transpose(out=pT[:, k, :], in_=ub[:, k * 128:(k + 1) * 128], identity=ident[:]),
                     reads=[tg + "ub", "ident"], writes=[tg + "pT"])
            P.act(lambda e: e.copy(out=AT[:, :, t * 128:(t + 1) * 128], in_=pT[:]), reads=[tg + "pT"], writes=[("AT", t)])

        C.norm_transpose = norm_transpose

        def ffn(tg, h_src, pre_g, post_g, wg, wu, wd, h_dst, next_g, first):
            with contextlib.ExitStack() as S:
                hid = sb(tg + "hid", [128, NF, NT], BF16, S)
                epsT = sb(tg + "eps", [128, 1], F32, S)
                P.pool(lambda e: e.memset(epsT[:], EPS), writes=[tg + "eps"])
                if first:
                    with contextlib.ExitStack() as S0:
                        gb = sb(tg + "gb", [128, D], F32, S0)
                        P.dma(lambda e: e.dma_start(out=gb[:], in_=bcast_rows(pre_g, D)), writes=[tg + "gb"])
                        scr = [dict(sq=sb(tg + "sq%d" % i, [128, D], BF16, S0), ss=sb(tg + "ss%d" % i, [128, 4], F32, S0),
                                    ub=sb(tg + "ub%d" % i, [128, D], BF16, S0), pT=ps(tg + "pT%d" % i, [128, KD, 128], BF16, S0),
                                    eps=epsT, epskey=tg + "eps") for i in range(2)]
                        hts = [sb(tg + "ht%d" % i, [128, D], F32, S0) for i in range(2)]
                        for t in range(NTILE):
                            i = t % 2
                            P.dma(lambda e, t=t, i=i: e.dma_start(out=hts[i][:], in_=h_src[t * 128:(t + 1) * 128, :]),
                                  writes=[tg + "ht%d" % i])
                            norm_transpose(S0, tg + "n%d" % i, hts[i], tg + "ht%d" % i, gb, tg + "gb", t, scr[i])
                P.barrier()
                with contextlib.ExitStack() as S1:
                    wgp = [sb(tg + "wgp%d" % i, [128, KD, 512], BF16, S1) for i in range(2)]
                    wup = [sb(tg + "wup%d" % i, [128, KD, 512], BF16, S1) for i in range(2)]
                    sgt = [sb(tg + "sg%d" % i, [128, 512], BF16, S1) for i in range(2)]
                    pg = [[ps(tg + "pg%d%d" % (i, hf), [128, 512], F32, S1) for hf in range(2)] for i in range(2)]
                    pu = [[ps(tg + "pu%d%d" % (i, hf), [128, 512], F32, S1) for hf in range(2)] for i in range(2)]
                    wgv = wg.rearrange("(kc p) m -> p kc m", p=128)
                    wuv = wu.rearrange("(kc p) m -> p kc m", p=128)
                    ATall = [("AT", t) for t in range(NTILE)]
                    for pi in range(NF // 4):
                        b = pi % 2
                        P.dma(lambda e, pi=pi, b=b: e.dma_start(out=wgp[b][:], in_=wgv[:, :, pi * 512:(pi + 1) * 512]),
                              writes=[tg + "wgp%d" % b], q="pool")
                        P.dma(lambda e, pi=pi, b=b: e.dma_start(out=wup[b][:], in_=wuv[:, :, pi * 512:(pi + 1) * 512]),
                              writes=[tg + "wup%d" % b], q="pool")
                        for fl in range(4):
                            f = pi * 4 + fl
                            r = f % 2
                            for hf in range(2):
                                for k in range(KD):
                                    P.pe(lambda e, k=k, hf=hf, r=r, b=b, fl=fl: e.matmul(
                                        pg[r][hf][:], lhsT=wgp[b][:, k, fl * 128:(fl + 1) * 128], rhs=AT[:, k, hf * 512:(hf + 1) * 512],
                                        start=(k == 0), stop=(k == KD - 1)),
                                        reads=[tg + "wgp%d" % b] + ATall[hf * 4:hf * 4 + 4], writes=[tg + "pg%d%d" % (r, hf)])
                                for k in range(KD):
                                    P.pe(lambda e, k=k, hf=hf, r=r, b=b, fl=fl: e.matmul(
                                        pu[r][hf][:], lhsT=wup[b][:, k, fl * 128:(fl + 1) * 128], rhs=AT[:, k, hf * 512:(hf + 1) * 512],
                                        start=(k == 0), stop=(k == KD - 1)),
                                        reads=[tg + "wup%d" % b] + ATall[hf * 4:hf * 4 + 4], writes=[tg + "pu%d%d" % (r, hf)])
                                P.act(lambda e, hf=hf, r=r: e.activation(out=sgt[hf][:], in_=pg[r][hf][:], func=AF.Silu),
                                      reads=[tg + "pg%d%d" % (r, hf)], writes=[tg + "sg%d" % hf])
                                P.dve(lambda e, hf=hf, r=r, f=f: e.tensor_tensor(out=hid[:, f, hf * 512:(hf + 1) * 512], in0=sgt[hf][:],
                                                                               in1=pu[r][hf][:], op=ALU.mult),
                                      reads=[tg + "sg%d" % hf, tg + "pu%d%d" % (r, hf)], writes=[(tg + "hid", f)])
                P.barrier()
                if debug and tg == "f1":
                    dbg["hid"] = nc.dram_tensor("dbg_hid", [128, NF, NT], BF16, kind="ExternalOutput").ap()
                    C.dbg_extra = [P.dma(lambda e: e.dma_start(out=dbg["hid"][:, :, :], in_=hid[:]), reads=[(tg + "hid", f) for f in range(NF)], writes=["dbg_hid"])]
                with contextlib.ExitStack() as S2:
                    NP = 4
                    FP = NF // NP
                    wdp = [sb(tg + "wdp%d" % i, [128, FP, 256], BF16, S2) for i in range(4)]
                    py = [[[ps(tg + "py%d%d%d" % (i, mc, hf), [128, 512], F32, S2) for hf in range(2)] for mc in range(2)] for i in range(2)]
                    wdv = wd.rearrange("(fc p) m -> p fc m", p=128)
                    cnt = 0
                    for cb in range(8):
                        r = cb % 2
                        for pc in range(NP):
                            bi = cnt % 4
                            cnt += 1
                            P.dma(lambda e, cb=cb, pc=pc, bi=bi: e.dma_start(out=wdp[bi][:], in_=wdv[:, pc * FP:(pc + 1) * FP, cb * 256:(cb + 1) * 256]),
                                  writes=[tg + "wdp%d" % bi], q="pool")
                            for mc in range(2):
                                for hf in range(2):
                                    for fi in range(FP):
                                        f = pc * FP + fi
                                        P.pe(lambda e, mc=mc, hf=hf, fi=fi, f=f, bi=bi, r=r: e.matmul(
                                            py[r][mc][hf][:], lhsT=wdp[bi][:, fi, mc * 128:(mc + 1) * 128], rhs=hid[:, f, hf * 512:(hf + 1) * 512],
                                            start=(f == 0), stop=(f == NF - 1)),
                                            reads=[tg + "wdp%d" % bi, (tg + "hid", f)], writes=[tg + "py%d%d%d" % (r, mc, hf)])
                        for mc in range(2):
                            for hf in range(2):
                                m = cb * 2 + mc
                                P.act(lambda e, m=m, mc=mc, hf=hf, r=r: e.copy(out=AT[:, m, hf * 512:(hf + 1) * 512], in_=py[r][mc][hf][:]),
                                      reads=[tg + "py%d%d%d" % (r, mc, hf)], writes=[("AT", hf * 4 + q) for q in range(4)])
            if debug and tg == "f1":
                P.barrier()
                dbg["yT"] = nc.dram_tensor("dbg_yT", [128, KD, NT], BF16, kind="ExternalOutput").ap()
                C.dbg_extra.append(P.dma(lambda e: e.dma_start(out=dbg["yT"][:, :, :], in_=AT[:]), reads=[("AT", t) for t in range(NTILE)], writes=["dbg_yT"]))
            post(tg, AT, h_src, post_g, 0.5, h_dst, next_g)

        def post(tg, YT, h_src, post_g, coef, h_dst, next_g):
            P.barrier()
            with contextlib.ExitStack() as S3:
                epsT = sb(tg + "eps3", [128, 1], F32, S3)
                P.pool(lambda e: e.memset(epsT[:], EPS), writes=[tg + "eps3"])
                gpo = sb(tg + "gpo", [128, D], F32, S3)
                P.dma(lambda e: e.dma_start(out=gpo[:], in_=bcast_rows(post_g, D)), writes=[tg + "gpo"])
                gnx = None
                if next_g is not None:
                    gnx = sb(tg + "gnx", [128, D], F32, S3)
                    P.dma(lambda e: e.dma_start(out=gnx[:], in_=bcast_rows(next_g, D)), writes=[tg + "gnx"])
                scr = [dict(sq=sb(tg + "psq%d" % i, [128, D], BF16, S3), ss=sb(tg + "pss%d" % i, [128, 4], F32, S3),
                            ub=sb(tg + "pub%d" % i, [128, D], BF16, S3), pT=ps(tg + "ppT%d" % i, [128, KD, 128], BF16, S3),
                            eps=epsT, epskey=tg + "eps3") for i in range(2)]
                xt = [sb(tg + "xt%d" % i, [128, D], F32, S3) for i in range(2)]
                tt = [sb(tg + "tt%d" % i, [128, D], F32, S3) for i in range(2)]
                s2 = [sb(tg + "s2%d" % i, [128, 4], F32, S3) for i in range(2)]
                pyT = [ps(tg + "pyT%d" % i, [128, KD, 128], BF16, S3) for i in range(2)]
                ykey = YT.name
                for t in range(NTILE):
                    i = t % 2
                    P.dma(lambda e, t=t, i=i: e.dma_start(out=xt[i][:], in_=h_src[t * 128:(t + 1) * 128, :]),
                          reads=[("hd", h_src.tensor.name, t)], writes=[tg + "xt%d" % i])
                    for m in range(KD):
                        P.pe(lambda e, m=m, t=t, i=i: e.transpose(out=pyT[i][:, m, :], in_=YT[:, m, t * 128:(t + 1) * 128], identity=ident[:]),
                             reads=[(ykey, t), "ident"], writes=[tg + "pyT%d" % i])
                    yv = pyT[i][:].rearrange("p k c -> p (k c)")
                    P.act(lambda e, i=i, yv=yv: e.activation(out=scr[i]["sq"][:], in_=yv, func=AF.Square, accum_out=s2[i][:, 0:1]),
                          reads=[tg + "pyT%d" % i], writes=[tg + "n%dsq" % i, tg + "s2a%d" % i])
                    P.act(lambda e, i=i: e.activation(out=s2[i][:, 1:2], in_=s2[i][:, 0:1], func=AF.Sqrt, scale=1.0 / D, bias=epsT[:, 0:1]),
                          reads=[tg + "s2a%d" % i, tg + "eps3"], writes=[tg + "s2b%d" % i])
                    P.dve(lambda e, i=i: e.reciprocal(out=s2[i][:, 2:3], in_=s2[i][:, 1:2]), reads=[tg + "s2b%d" % i], writes=[tg + "s2c%d" % i])
                    P.dve(lambda e, i=i, yv=yv: e.scalar_tensor_tensor(out=tt[i][:], in0=yv, scalar=s2[i][:, 2:3], in1=gpo[:], op0=ALU.mult, op1=ALU.mult),
                          reads=[tg + "pyT%d" % i, tg + "s2c%d" % i, tg + "gpo"], writes=[tg + "tt%d" % i])
                    P.dve(lambda e, i=i: e.scalar_tensor_tensor(out=xt[i][:], in0=tt[i][:], scalar=float(coef), in1=xt[i][:], op0=ALU.mult, op1=ALU.add),
                          reads=[tg + "tt%d" % i, tg + "xt%d" % i], writes=[tg + "xt%d" % i])
                    o = P.dma(lambda e, t=t, i=i: e.dma_start(out=h_dst[t * 128:(t + 1) * 128, :], in_=xt[i][:]), reads=[tg + "xt%d" % i],
                              writes=[("hd", h_dst.tensor.name, t)])
                    if next_g is not None:
                        norm_transpose(S3, tg + "n%d" % i, xt[i], tg + "xt%d" % i, gnx, tg + "gnx", t, scr[i])
                    else:
                        finals.append(o)
            P.barrier()

        C.post = post
        C.x, C.pin, C.pos, C.out, C.h1d, C.h2d, C.h3d, C.dbg, C.finals = x, pin, pos, out, h1d, h2d, h3d, dbg, finals
        C.bcast_rows = bcast_rows
        C.contextlib = contextlib
        C.stop = stop
        C.debug = debug

        if stop not in (20, 21, 22):
            ffn("f1", x, W["ff1_pre_g"], W["ff1_post_g"], W["ff1_w_gate"], W["ff1_w_up"], W["ff1_w_down"], h1d, W["mix_pre_g"], True)
        if stop <= 1:
            C.dbg_extra = getattr(C, "dbg_extra", [])
            o = P.dma(lambda e: e.dma_start(out=dbg["uT"][:, :, :], in_=AT[:]), reads=[("AT", t) for t in range(NTILE)], writes=["dbg_uT"])
            o2 = P.dma(lambda e: e.dma_start(out=out[:, :], in_=h1d[:, :]), reads=[("hd", "h1d", t) for t in range(NTILE)], writes=["out"])
            P.emit(final_wait_ops=[o, o2] + C.dbg_extra)
            return nc
        if stop in (20, 21, 22):
            with contextlib.ExitStack() as S0:
                epsT = sb("eps0", [128, 1], F32, S0)
                P.pool(lambda e: e.memset(epsT[:], EPS), writes=["eps0"])
                gb = sb("gb0", [128, D], F32, S0)
                P.dma(lambda e: e.dma_start(out=gb[:], in_=bcast_rows(W["mix_pre_g"], D)), writes=["gb0"])
                scr = [dict(sq=sb("sq0%d" % i, [128, D], BF16, S0), ss=sb("ss0%d" % i, [128, 4], F32, S0), ub=sb("ub0%d" % i, [128, D], BF16, S0),
                            pT=ps("pT0%d" % i, [128, KD, 128], BF16, S0), eps=epsT, epskey="eps0") for i in range(2)]
                hts = [sb("ht0%d" % i, [128, D], F32, S0) for i in range(2)]
                for t in range(NTILE):
                    i = t % 2
                    P.dma(lambda e, t=t, i=i: e.dma_start(out=hts[i][:], in_=x[t * 128:(t + 1) * 128, :]), writes=["ht0%d" % i])
                    P.dma(lambda e, t=t, i=i: e.dma_start(out=h1d[t * 128:(t + 1) * 128, :], in_=hts[i][:]), reads=["ht0%d" % i], writes=[("hd", "h1d", t)])
                    norm_transpose(S0, "n0%d" % i, hts[i], "ht0%d" % i, gb, "gb0", t, scr[i])
            P.barrier()
        if mixer(C) == "stop":
            return nc
        if stop <= 2 or stop in (20, 22):
            o2 = P.dma(lambda e: e.dma_start(out=out[:, :], in_=h2d[:, :]), reads=[("hd", "h2d", t) for t in range(NTILE)], writes=["out"])
            P.emit(final_wait_ops=[o2] + C.dbg_ops)
            return nc
        ffn("f2", h2d, None, W["ff2_post_g"], W["ff2_w_gate"], W["ff2_w_up"], W["ff2_w_down"], h3d, W["ple_pre_g"], False)
        ple(C)
        P.emit(final_wait_ops=finals)
    return nc


WNAMES = ["ff1_pre_g", "ff1_post_g", "ff1_w_gate", "ff1_w_up", "ff1_w_down", "mix_pre_g", "mix_post_g", "w_in",
          "cmp_pos_k", "cmp_pos_v", "cmp_k_w1", "cmp_k_w2", "cmp_v_w1", "cmp_v_w2", "nsa_gate_b",
          "conv_w", "conv_b", "rg_w_a", "rg_b_a", "rg_w_i", "rg_b_i", "rg_lambda",
          "attn_out_g", "rec_out_g", "w_out", "ff2_pre_g", "ff2_post_g", "ff2_w_gate", "ff2_w_up", "ff2_w_down",
          "ple_pre_g", "ple_post_g", "w_ple_gate", "w_ple_proj"]


def shard_tokens(a, b, j):
    T = a.shape[0]
    r = a.reshape(T // 512, 4, 128, *a.shape[1:])[:, j]
    return np.ascontiguousarray(r.reshape(T // 4, *a.shape[1:]))


def make_in_maps(inputs):
    shared = {}
    for nm in WNAMES:
        a = np.asarray(inputs[nm], dtype=np.float32)[0]
        if a.ndim == 1:
            a = a.reshape(1, -1)
        if nm == "nsa_gate_b":
            a = a.reshape(1, 24)
        shared[nm] = np.ascontiguousarray(a)
    maps = []
    x = np.asarray(inputs["x"], dtype=np.float32)
    p = np.asarray(inputs["p"], dtype=np.float32)[0]
    positions = np.asarray(inputs["positions"]).astype(np.int32)
    for c in range(8):
        b, j = c // 4, c % 4
        m = dict(shared)
        m["x"] = shard_tokens(x[b], b, j)
        m["p"] = shard_tokens(p[b], b, j)
        m["pos"] = shard_tokens(positions[b], b, j).reshape(1, NT)
        maps.append(m)
    return maps


def unshard(outs):
    res = np.zeros((2, 4096, D), dtype=np.float32)
    for c in range(8):
        b, j = c // 4, c % 4
        res[b].reshape(8, 4, 128, D)[:, j] = np.asarray(outs[c]).reshape(8, 128, D)
    return res


BIGNEG = -30000.0
SCALE = 128 ** -0.5
RG = [[0, 1, 2, 3], [4, 5, 6, 7]]
GROWS = 12 * 128


def mixer(C):
    nc, P, W, sb, ps, AT, ident, identf = C.nc, C.P, C.W, C.sb, C.ps, C.AT, C.ident, C.identf
    contextlib = C.contextlib
    C.dbg_ops = []

    def cin(name, shape, dt=F32):
        return nc.dram_tensor(name, list(shape), dt, kind="ExternalInput").ap()

    c_rope = cin("c_rope", [128, 4])
    c_perm = cin("c_perm", [128, 128])
    c_oh = cin("c_oh", [128, 4])
    c_cmpm = cin("c_cmpm", [128, 2 * 8 * 128], BF16)
    c_selcm = cin("c_selcm", [128, 4 * 128], BF16)
    c_winm = cin("c_winm", [128, 8 * 128], BF16)
    c_selA = cin("c_selA", [128, 8 * 64])
    c_selB = cin("c_selB", [128, 8 * 64])
    c_selF = cin("c_selF", [128, 8 * 64])
    c_E = cin("c_E", [64, 4096], BF16)
    c_ovl = cin("c_ovl", [128, 2 * 64])
    c_selmat = cin("c_selmat", [24, 24 * 128])

    gin = nc.dram_tensor("gin", [GROWS, NT], BF16, kind="Internal").ap()
    gbuf = nc.dram_tensor("gbuf", [4 * GROWS, NT], BF16, kind="Internal").ap()
    hin = nc.dram_tensor("hin", [128, 192], F32, kind="Internal").ap()
    hbuf = nc.dram_tensor("hbuf", [4 * 128, 192], F32, kind="Internal").ap()
    sin_ = nc.dram_tensor("sin", [128, 128], F32, kind="Internal").ap()
    sbuf_ = nc.dram_tensor("sbuf", [4 * 128, 128], F32, kind="Internal").ap()

    def load(dst, src, key, q="sp", reads=()):
        return P.dma(lambda e: e.dma_start(out=dst, in_=src), reads=list(reads), writes=[key], q=q)

    with contextlib.ExitStack() as SM:
        qT = sb("qT", [128, 8, NT], BF16, SM)
        gT = sb("gT", [24, NT], F32, SM)
        orecT = sb("orecT", [128, 8, NT], BF16, SM)
        ones_bf = sb("ones_bf", [128, 128], BF16, SM)
        ones_f = sb("ones_f", [128, 128], F32, SM)
        onec = sb("onec", [128, 1], F32, SM)
        epsT = sb("m_eps", [128, 1], F32, SM)
        oh = sb("oh", [128, 4], F32, SM)
        P.pool(lambda e: e.memset(ones_bf[:], 1.0), writes=["ones_bf"])
        P.pool(lambda e: e.memset(ones_f[:], 1.0), writes=["ones_f"])
        P.pool(lambda e: e.memset(onec[:], 1.0), writes=["onec"])
        P.pool(lambda e: e.memset(epsT[:], EPS), writes=["m_eps"])
        load(oh[:], c_oh[:, :], "oh")
        C.ones_bf, C.ones_f, C.epsT = ones_bf, ones_f, epsT
        ATall = [("AT", t) for t in range(NTILE)]

        with contextlib.ExitStack() as S2:
            ZX = sb("ZX", [128, 8, NT], F32, S2)
            gy = sb("gy", [128, 8, NT], BF16, S2)
            with contextlib.ExitStack() as S2a:
                rope = sb("rope", [128, 4], F32, S2a)
                perm = sb("perm", [128, 128], F32, S2a)
                posi = sb("posi", [128, NT], I32, S2a)
                ang = sb("ang", [128, NT], F32, S2a)
                kf = sb("kf", [128, NT], F32, S2a)
                ki = sb("ki", [128, NT], I32, S2a)
                Ct = sb("Ct", [128, NT], F32, S2a)
                St = sb("St", [128, NT], F32, S2a)
                gbias = sb("gbias", [24, 1], F32, S2a)
                load(rope[:], c_rope[:, :], "rope")
                load(perm[:], c_perm[:, :], "perm")
                load(posi[:], C.bcast_rows(C.pos, NT), "posi")
                with nc.allow_non_contiguous_dma(reason="tiny param"):
                    P.dma(lambda e: e.dma_start(out=gbias[:], in_=W["nsa_gate_b"].rearrange("o n -> n o"), allow_slow_non_contiguous=True), writes=["gbias"])
                P.dve(lambda e: e.tensor_copy(out=ang[:], in_=posi[:]), reads=["posi"], writes=["ang"])
                P.dve(lambda e: e.tensor_scalar(out=ang[:], in0=ang[:], scalar1=rope[:, 0:1], scalar2=None, op0=ALU.mult), reads=["ang", "rope"], writes=["ang"])
                TWO_PI = 6.283185307179586
                C1 = 6.28125
                C2 = TWO_PI - C1
                P.dve(lambda e: e.tensor_scalar(out=kf[:], in0=ang[:], scalar1=1.0 / TWO_PI, scalar2=None, op0=ALU.mult), reads=["ang"], writes=["kf"])
                P.dve(lambda e: e.tensor_copy(out=ki[:], in_=kf[:]), reads=["kf"], writes=["ki"])
                P.dve(lambda e: e.tensor_copy(out=kf[:], in_=ki[:]), reads=["ki"], writes=["kf"])
                P.dve(lambda e: e.scalar_tensor_tensor(out=ang[:], in0=kf[:], scalar=-C1, in1=ang[:], op0=ALU.mult, op1=ALU.add), reads=["kf", "ang"], writes=["ang"])
                P.dve(lambda e: e.scalar_tensor_tensor(out=ang[:], in0=kf[:], scalar=-C2, in1=ang[:], op0=ALU.mult, op1=ALU.add), reads=["kf", "ang"], writes=["ang"])
                P.dve(lambda e: e.tensor_scalar(out=kf[:], in0=ang[:], scalar1=3.141592653589793, scalar2=-TWO_PI, op0=ALU.is_gt, op1=ALU.mult), reads=["ang"], writes=["kf"])
                P.dve(lambda e: e.tensor_tensor(out=ang[:], in0=ang[:], in1=kf[:], op=ALU.add), reads=["ang", "kf"], writes=["ang"])
                P.dve(lambda e: e.tensor_scalar(out=kf[:], in0=ang[:], scalar1=-3.141592653589793, scalar2=TWO_PI, op0=ALU.is_lt, op1=ALU.mult), reads=["ang"], writes=["kf"])
                P.dve(lambda e: e.tensor_tensor(out=ang[:], in0=ang[:], in1=kf[:], op=ALU.add), reads=["ang", "kf"], writes=["ang"])
                P.act(lambda e: e.activation(out=St[:], in_=ang[:], func=AF.Sin), reads=["ang"], writes=["St"])
                P.dve(lambda e: e.tensor_scalar(out=St[:], in0=St[:], scalar1=rope[:, 1:2], scalar2=None, op0=ALU.mult), reads=["St", "rope"], writes=["St"])
                P.act(lambda e: e.activation(out=kf[:], in_=ang[:], func=AF.Abs), reads=["ang"], writes=["kf"])
                P.dve(lambda e: e.tensor_scalar(out=kf[:], in0=kf[:], scalar1=-1.0, scalar2=1.5707963267948966, op0=ALU.mult, op1=ALU.add), reads=["kf"], writes=["kf"])
                P.act(lambda e: e.activation(out=Ct[:], in_=kf[:], func=AF.Sin), reads=["kf"], writes=["Ct"])

                wpan = [sb("wpan%d" % i, [128, KD, 512], BF16, S2a) for i in range(2)]
                zf = [sb("zf%d" % i, [128, NT], F32, S2a) for i in range(2)]
                t1 = [sb("t1%d" % i, [128, 512], F32, S2a) for i in range(2)]
                stg = [sb("stg%d" % i, [128, NT], BF16, S2a) for i in range(2)]
                pz = [[ps("pz%d%d" % (i, hf), [128, 512], F32, S2a) for hf in range(2)] for i in range(2)]
                psw = [ps("psw%d" % i, [128, 512], F32, S2a) for i in range(2)]
                pvt = [ps("pvt%d" % i, [128, 8, 128], BF16, S2a) for i in range(2)]
                wv = W["w_in"].rearrange("(kc p) m -> p kc m", p=128)
                panels = [(0, 512), (512, 512), (1024, 512), (1536, 512), (2048, 512), (2560, 24), (2584, 512), (3096, 512), (3608, 512), (4120, 512)]
                kinds = {}
                for h in range(8):
                    kinds[h * 128] = ("q", h)
                for g in range(2):
                    kinds[1024 + g * 128] = ("kf", 0 + g)
                    kinds[1280 + g * 128] = ("vf", 2 + g)
                    kinds[1536 + g * 128] = ("kf", 4 + g)
                    kinds[1792 + g * 128] = ("vt", 8 + g)
                    kinds[2048 + g * 128] = ("kf", 6 + g)
                    kinds[2304 + g * 128] = ("vt", 10 + g)
                kinds[2560] = ("g", 0)
                for h in range(8):
                    kinds[2584 + h * 128] = ("zx", h)
                    kinds[3608 + h * 128] = ("zy", h)
                cnt = 0
                for pi, (c0, wdt) in enumerate(panels):
                    b = pi % 2
                    P.dma(lambda e, b=b, c0=c0, wdt=wdt: e.dma_start(out=wpan[b][:, :, 0:wdt], in_=wv[:, :, c0:c0 + wdt]), writes=["wpan%d" % b], q="pool")
                    for ci in range(max(1, wdt // 128)):
                        col = c0 + ci * 128
                        kind, idx = kinds[col]
                        cw = 24 if kind == "g" else 128
                        r = cnt % 2
                        cnt += 1
                        for hf in range(2):
                            for k in range(KD):
                                P.pe(lambda e, k=k, hf=hf, r=r, b=b, ci=ci, cw=cw: e.matmul(
                                    pz[r][hf][0:cw, :], lhsT=wpan[b][:, k, ci * 128:ci * 128 + cw], rhs=AT[:, k, hf * 512:(hf + 1) * 512],
                                    start=(k == 0), stop=(k == KD - 1)), reads=["wpan%d" % b] + ATall[hf * 4:hf * 4 + 4], writes=["pz%d%d" % (r, hf)])
                        pzk = ["pz%d%d" % (r, 0), "pz%d%d" % (r, 1)]
                        if kind == "g":
                            for hf in range(2):
                                P.act(lambda e, hf=hf, r=r: e.activation(out=gT[:, hf * 512:(hf + 1) * 512], in_=pz[r][hf][0:24, :], func=AF.Sigmoid, bias=gbias[:, 0:1]),
                                      reads=[pzk[hf], "gbias"], writes=["gT"])
                        elif kind == "zx":
                            for hf in range(2):
                                P.act(lambda e, hf=hf, r=r, idx=idx: e.copy(out=ZX[:, idx, hf * 512:(hf + 1) * 512], in_=pz[r][hf][:]),
                                      reads=[pzk[hf]], writes=[("ZX", idx)])
                        elif kind == "zy":
                            for hf in range(2):
                                sl = slice(hf * 512, (hf + 1) * 512)
                                P.act(lambda e, hf=hf, r=r, sl=sl: e.activation(out=zf[r][:, sl], in_=pz[r][hf][:], func=AF.Square), reads=[pzk[hf]], writes=[("zf%d" % r, hf)])
                                P.dve(lambda e, r=r, sl=sl, hf=hf: e.tensor_scalar(out=zf[r][:, sl], in0=zf[r][:, sl], scalar1=0.044715, scalar2=1.0, op0=ALU.mult, op1=ALU.add), reads=[("zf%d" % r, hf)], writes=[("zf%d" % r, hf)])
                                P.dve(lambda e, hf=hf, r=r, sl=sl: e.tensor_tensor(out=zf[r][:, sl], in0=zf[r][:, sl], in1=pz[r][hf][:], op=ALU.mult), reads=[("zf%d" % r, hf), pzk[hf]], writes=[("zf%d" % r, hf)])
                                P.act(lambda e, r=r, sl=sl, hf=hf: e.activation(out=zf[r][:, sl], in_=zf[r][:, sl], func=AF.Sigmoid, scale=1.5957691216057308), reads=[("zf%d" % r, hf)], writes=[("zf%d" % r, hf)])
                                P.dve(lambda e, hf=hf, r=r, sl=sl, idx=idx: e.tensor_tensor(out=gy[:, idx, sl], in0=zf[r][:, sl], in1=pz[r][hf][:], op=ALU.mult), reads=[("zf%d" % r, hf), pzk[hf]], writes=[("gy", idx)])
                        else:
                            for hf in range(2):
                                P.act(lambda e, hf=hf, r=r: e.copy(out=zf[r][:, hf * 512:(hf + 1) * 512], in_=pz[r][hf][:]), reads=[pzk[hf]], writes=[("zf%d" % r, hf)])
                            if kind in ("q", "kf"):
                                dst = qT[:, idx, :] if kind == "q" else stg[r][:]
                                dkey = ("qT", idx) if kind == "q" else "stg%d" % r
                                for hf in range(2):
                                    sl = slice(hf * 512, (hf + 1) * 512)
                                    P.pe(lambda e, hf=hf, r=r, sl=sl: e.matmul(psw[hf][:], lhsT=perm[:], rhs=zf[r][:, sl], start=True, stop=True),
                                         reads=["perm", ("zf%d" % r, hf)], writes=["psw%d" % hf])
                                    P.dve(lambda e, hf=hf, r=r, sl=sl: e.tensor_tensor(out=t1[hf][:], in0=zf[r][:, sl], in1=Ct[:, sl], op=ALU.mult),
                                          reads=[("zf%d" % r, hf), "Ct"], writes=["t1%d" % hf])
                                    P.dve(lambda e, hf=hf, r=r, sl=sl: e.tensor_tensor(out=zf[r][:, sl], in0=psw[hf][:], in1=St[:, sl], op=ALU.mult),
                                          reads=["psw%d" % hf, "St", ("zf%d" % r, hf)], writes=[("zf%d" % r, hf)])
                                    P.dve(lambda e, hf=hf, r=r, sl=sl, dst=dst: e.tensor_tensor(out=dst[:, sl], in0=zf[r][:, sl], in1=t1[hf][:], op=ALU.add),
                                          reads=[("zf%d" % r, hf), "t1%d" % hf], writes=[dkey])
                                if kind == "kf":
                                    P.dma(lambda e, r=r, idx=idx: e.dma_start(out=gin[idx * 128:(idx + 1) * 128, :], in_=stg[r][:]), reads=["stg%d" % r], writes=[("gin", idx)])
                            elif kind == "vf":
                                P.dve(lambda e, r=r: e.tensor_copy(out=stg[r][:], in_=zf[r][:]), reads=[("zf%d" % r, 0), ("zf%d" % r, 1)], writes=["stg%d" % r])
                                P.dma(lambda e, r=r, idx=idx: e.dma_start(out=gin[idx * 128:(idx + 1) * 128, :], in_=stg[r][:]), reads=["stg%d" % r], writes=[("gin", idx)])
                            elif kind == "vt":
                                P.dve(lambda e, r=r: e.tensor_copy(out=stg[r][:], in_=zf[r][:]), reads=[("zf%d" % r, 0), ("zf%d" % r, 1)], writes=["stg%d" % r])
                                for l in range(8):
                                    P.pe(lambda e, r=r, l=l: e.transpose(out=pvt[r][:, l, :], in_=stg[r][:, l * 128:(l + 1) * 128], identity=ident[:]),
                                         reads=["stg%d" % r, "ident"], writes=["pvt%d" % r])
                                P.act(lambda e, r=r: e.copy(out=stg[r][:], in_=pvt[r][:].rearrange("p l d -> p (l d)")), reads=["pvt%d" % r], writes=["stg%d" % r])
                                P.dma(lambda e, r=r, idx=idx: e.dma_start(out=gin[idx * 128:(idx + 1) * 128, :], in_=stg[r][:]), reads=["stg%d" % r], writes=[("gin", idx)])
            P.barrier()
            with contextlib.ExitStack() as S2b:
                hst = sb("hst", [128, 8, 8, 3], F32, S2b)
                for blk in range(8):
                    P.dve(lambda e, blk=blk: e.tensor_copy(out=hst[:, blk, :, :], in_=ZX[:, blk, :].rearrange("p (l t) -> p l t", t=128)[:, :, 125:128]),
                          reads=[("ZX", blk)], writes=["hst"])
                P.dma(lambda e: e.dma_start(out=hin[:, :], in_=hst[:].rearrange("p b l t -> p (b l t)")), reads=["hst"], writes=["hin"])
                for kq in range(4):
                    P.cc(lambda e, kq=kq: e.collective_compute("AllGather", ALU.bypass, replica_groups=RG, ins=[gin[kq * 384:(kq + 1) * 384, :]], outs=[gbuf[kq * 1536:(kq + 1) * 1536, :]]),
                         reads=[("gin", it) for it in range(3 * kq, 3 * kq + 3)], writes=[("gbuf", kq)])
                cc2 = P.cc(lambda e: e.collective_compute("AllGather", ALU.bypass, replica_groups=RG, ins=[hin[:, :]], outs=[hbuf[:, :]]), reads=["hin"], writes=["hbuf"])
            P.barrier()
            if C.stop == 21:
                C.dbg["qT"] = nc.dram_tensor("dbg_qT", [128, 8, NT], BF16, kind="ExternalOutput").ap()
                C.dbg["gT"] = nc.dram_tensor("dbg_gT", [24, NT], F32, kind="ExternalOutput").ap()
                C.dbg["gbuf"] = nc.dram_tensor("dbg_gbuf", [4 * GROWS, NT], BF16, kind="ExternalOutput").ap()
                C.dbg["ZX"] = nc.dram_tensor("dbg_ZX", [128, 8, NT], F32, kind="ExternalOutput").ap()
                C.dbg["gy"] = nc.dram_tensor("dbg_gy", [128, 8, NT], BF16, kind="ExternalOutput").ap()
                C.dbg_ops.append(P.dma(lambda e: e.dma_start(out=C.dbg["qT"][:, :, :], in_=qT[:]), reads=[("qT", h) for h in range(8)], writes=["d1"]))
                C.dbg_ops.append(P.dma(lambda e: e.dma_start(out=C.dbg["gT"][:, :], in_=gT[:]), reads=["gT"], writes=["d2"]))
                C.dbg_ops.append(P.dma(lambda e: e.dma_start(out=C.dbg["gbuf"][:, :], in_=gbuf[:, :]), reads=[("gbuf", it) for it in range(4)], writes=["d3"]))
                C.dbg_ops.append(P.dma(lambda e: e.dma_start(out=C.dbg["ZX"][:, :, :], in_=ZX[:]), reads=[("ZX", h) for h in range(8)], writes=["d4"]))
                C.dbg_ops.append(P.dma(lambda e: e.dma_start(out=C.dbg["gy"][:, :, :], in_=gy[:]), reads=[("gy", h) for h in range(8)], writes=["d5"]))
                P.emit(final_wait_ops=C.dbg_ops)
                return "stop"
            lru(C, SM, ZX, gy, orecT, hbuf, sin_, sbuf_, oh, onec)
        P.barrier()
        attention(C, SM, qT, gT, orecT, gbuf, ones_bf, ones_f, epsT,
                  dict(cmpm=c_cmpm, selcm=c_selcm, winm=c_winm, selA=c_selA, selB=c_selB, selF=c_selF, E=c_E, ovl=c_ovl, selmat=c_selmat))
    P.barrier()
    if C.stop == 22:
        C.dbg["cat"] = nc.dram_tensor("dbg_cat", [128, KD, NT], BF16, kind="ExternalOutput").ap()
        C.dbg_ops.append(P.dma(lambda e: e.dma_start(out=C.dbg["cat"][:, :, :], in_=AT[:]), reads=[("AT", t) for t in range(NTILE)], writes=["dcat"]))
        P.barrier()
    outproj(C)


def lru(C, SM, ZX, gy, orecT, hbuf, sin_, sbuf_, oh, onec):
    nc, P, W, sb, ps = C.nc, C.P, C.W, C.sb, C.ps
    contextlib = C.contextlib
    ones_bf, epsT = C.ones_bf, C.epsT
    with contextlib.ExitStack() as SL:
        Pc = sb("Pc", [128, 8, NT], F32, SL)
        cw = sb("cw", [128, 4, 8], F32, SL)
        cb = sb("cb", [128, 8], F32, SL)
        ba = sb("ba", [128, 8], F32, SL)
        bi = sb("bi", [128, 8], F32, SL)
        lam = sb("lam", [128, 8], F32, SL)
        rg = sb("rg", [128, 8], F32, SL)
        cch = sb("cch", [128, 8], F32, SL)
        wa = sb("wa", [128, 8, 128], BF16, SL)
        wi = sb("wi", [128, 8, 128], BF16, SL)
        G2 = sb("G2", [128, 4, 192], F32, SL)
        halo = sb("halo", [128, 8, 8, 3], F32, SL)
        zeros = sb("zeros", [128, 128], F32, SL)
        P.pool(lambda e: e.memset(zeros[:], 0.0), writes=["zeros"])
        with nc.allow_non_contiguous_dma(reason="tiny params"):
            for w_ in range(4):
                P.dma(lambda e, w_=w_: e.dma_start(out=cw[:, w_, :], in_=W["conv_w"][w_:w_ + 1, :].rearrange("o (b p) -> p (o b)", p=128), allow_slow_non_contiguous=True), writes=[("cw", w_)])
            for t_, nm in [(cb, "conv_b"), (ba, "rg_b_a"), (bi, "rg_b_i"), (lam, "rg_lambda"), (rg, "rec_out_g")]:
                P.dma(lambda e, t_=t_, nm=nm: e.dma_start(out=t_[:], in_=W[nm].rearrange("o (b p) -> p (o b)", p=128), allow_slow_non_contiguous=True), writes=[nm])
        P.dma(lambda e: e.dma_start(out=wa[:], in_=W["rg_w_a"].rearrange("b i j -> i b j")), writes=["wa"], q="pool")
        P.dma(lambda e: e.dma_start(out=wi[:], in_=W["rg_w_i"].rearrange("b i j -> i b j")), writes=["wi"], q="pool")
        P.dma(lambda e: e.dma_start(out=G2[:], in_=hbuf.rearrange("(r p) c -> p r c", p=128)), reads=["hbuf"], writes=["G2"])
        P.act(lambda e: e.activation(out=cch[:], in_=lam[:], func=AF.Sigmoid), reads=["rg_lambda"], writes=["cch"])
        P.act(lambda e: e.activation(out=cch[:], in_=cch[:], func=AF.Ln), reads=["cch"], writes=["cch"])
        P.dve(lambda e: e.tensor_scalar(out=cch[:], in0=cch[:], scalar1=8.0, scalar2=None, op0=ALU.mult), reads=["cch"], writes=["cch"])
        hv = halo[:].rearrange("p b l t -> p (b l t)")
        P.dve(lambda e: e.tensor_scalar(out=hv, in0=G2[:, 0, :], scalar1=oh[:, 1:2], scalar2=None, op0=ALU.mult), reads=["G2", "oh"], writes=["halo"])
        P.dve(lambda e: e.scalar_tensor_tensor(out=hv, in0=G2[:, 1, :], scalar=oh[:, 2:3], in1=hv, op0=ALU.mult, op1=ALU.add), reads=["G2", "oh", "halo"], writes=["halo"])
        P.dve(lambda e: e.scalar_tensor_tensor(out=hv, in0=G2[:, 2, :], scalar=oh[:, 3:4], in1=hv, op0=ALU.mult, op1=ALU.add), reads=["G2", "oh", "halo"], writes=["halo"])
        g3 = G2[:, 3, :].rearrange("p (b l t) -> p b l t", b=8, l=8)
        P.dve(lambda e: e.scalar_tensor_tensor(out=halo[:, :, 1:8, :], in0=g3[:, :, 0:7, :], scalar=oh[:, 0:1], in1=halo[:, :, 1:8, :], op0=ALU.mult, op1=ALU.add),
              reads=["G2", "oh", "halo"], writes=["halo"])
        with contextlib.ExitStack() as SB:
            NB = 2
            xpad = [sb("xpad%d" % i, [128, 8, 131], F32, SB) for i in range(NB)]
            xc = [sb("xc%d" % i, [128, 8, 128], F32, SB) for i in range(NB)]
            xcb = [sb("xcb%d" % i, [128, NT], BF16, SB) for i in range(NB)]
            rr = [sb("rr%d" % i, [128, NT], F32, SB) for i in range(NB)]
            ig0 = sb("ig0", [128, NT], F32, SB)
            ig = [ig0, ig0]
            aa = [sb("aa%d" % i, [128, NT], F32, SB) for i in range(NB)]
            uu0 = sb("uu0", [128, NT], F32, SB)
            uu = [uu0, uu0]
            pr = [[ps("pr%d%d" % (i, hf), [128, 512], F32, SB) for hf in range(2)] for i in range(NB)]
            pg = [[ps("pi%d%d" % (i, hf), [128, 512], F32, SB) for hf in range(2)] for i in range(NB)]
            for blk in range(8):
                i = blk % NB
                k = lambda s: "%s%d" % (s, i)
                P.pool(lambda e, i=i, blk=blk: e.tensor_copy(out=xpad[i][:, :, 0:3], in_=halo[:, blk, :, :]), reads=["halo"], writes=[k("xpad")])
                P.pool(lambda e, i=i, blk=blk: e.tensor_copy(out=xpad[i][:, :, 3:131], in_=ZX[:, blk, :].rearrange("p (l t) -> p l t", t=128)), reads=[("ZX", blk)], writes=[k("xpad")])
                P.dve(lambda e, i=i, blk=blk: e.tensor_scalar(out=xc[i][:], in0=xpad[i][:, :, 0:128], scalar1=cw[:, 0, blk:blk + 1], scalar2=cb[:, blk:blk + 1], op0=ALU.mult, op1=ALU.add),
                      reads=[k("xpad"), ("cw", 0), "conv_b"], writes=[k("xc")])
                for w in range(1, 4):
                    P.dve(lambda e, i=i, blk=blk, w=w: e.scalar_tensor_tensor(out=xc[i][:], in0=xpad[i][:, :, w:w + 128], scalar=cw[:, w, blk:blk + 1], in1=xc[i][:], op0=ALU.mult, op1=ALU.add),
                          reads=[k("xpad"), ("cw", w), k("xc")], writes=[k("xc")])
                xcf = xc[i][:].rearrange("p l t -> p (l t)")
                P.act(lambda e, i=i, xcf=xcf: e.copy(out=xcb[i][:], in_=xcf), reads=[k("xc")], writes=[k("xcb")])
                for hf in range(2):
                    sl = slice(hf * 512, (hf + 1) * 512)
                    P.pe(lambda e, i=i, blk=blk, hf=hf, sl=sl: e.matmul(pr[i][hf][:], lhsT=wa[:, blk, :], rhs=xcb[i][:, sl], start=True, stop=True), reads=["wa", k("xcb")], writes=["pr%d%d" % (i, hf)])
                    P.pe(lambda e, i=i, blk=blk, hf=hf, sl=sl: e.matmul(pg[i][hf][:], lhsT=wi[:, blk, :], rhs=xcb[i][:, sl], start=True, stop=True), reads=["wi", k("xcb")], writes=["pi%d%d" % (i, hf)])
                    P.act(lambda e, i=i, blk=blk, hf=hf, sl=sl: e.activation(out=rr[i][:, sl], in_=pr[i][hf][:], func=AF.Sigmoid, bias=ba[:, blk:blk + 1]), reads=["pr%d%d" % (i, hf), "rg_b_a"], writes=[k("rr")])
                    P.act(lambda e, i=i, blk=blk, hf=hf, sl=sl: e.activation(out=ig[i][:, sl], in_=pg[i][hf][:], func=AF.Sigmoid, bias=bi[:, blk:blk + 1]), reads=["pi%d%d" % (i, hf), "rg_b_i"], writes=["ig0"])
                P.act(lambda e, i=i, blk=blk: e.activation(out=aa[i][:], in_=rr[i][:], func=AF.Exp, scale=cch[:, blk:blk + 1]), reads=[k("rr"), "cch"], writes=[k("aa")])
                P.dve(lambda e, i=i: e.tensor_tensor(out=uu[i][:], in0=aa[i][:], in1=aa[i][:], op=ALU.mult), reads=[k("aa")], writes=["uu0"])
                P.act(lambda e, i=i: e.activation(out=uu[i][:], in_=uu[i][:], func=AF.Sqrt, scale=-1.0, bias=onec[:, 0:1]), reads=["uu0", "onec"], writes=["uu0"])
                P.dve(lambda e, i=i: e.tensor_tensor(out=uu[i][:], in0=uu[i][:], in1=ig[i][:], op=ALU.mult), reads=["uu0", "ig0"], writes=["uu0"])
                P.dve(lambda e, i=i, xcf=xcf: e.tensor_tensor(out=uu[i][:], in0=uu[i][:], in1=xcf, op=ALU.mult), reads=["uu0", k("xc")], writes=["uu0"])
                for l in range(8):
                    sl = slice(l * 128, (l + 1) * 128)
                    P.dve(lambda e, i=i, blk=blk, sl=sl: e.tensor_tensor_scan(out=ZX[:, blk, sl], data0=aa[i][:, sl], data1=uu[i][:, sl], initial=0.0, op0=ALU.mult, op1=ALU.add),
                          reads=[k("aa"), "uu0", k("xpad")], writes=[("ZX", blk)])
                    P.dve(lambda e, i=i, blk=blk, sl=sl: e.tensor_tensor_scan(out=Pc[:, blk, sl], data0=aa[i][:, sl], data1=zeros[:], initial=1.0, op0=ALU.mult, op1=ALU.add),
                          reads=[k("aa"), "zeros"], writes=[("Pc", blk)])
        P.barrier()
        with contextlib.ExitStack() as SC:
            sst = sb("sst", [128, 8, 8, 2], F32, SC)
            SG = sb("SG", [128, 4, 128], F32, SC)
            Aall = sb("Aall", [128, 8, 32], F32, SC)
            Hall = sb("Hall", [128, 8, 32], F32, SC)
            Spad = sb("Spad", [128, 8, 33], F32, SC)
            hinit = sb("hinit", [128, 8, 8], F32, SC)
            rstdb = sb("rstdb", [128, NT], F32, SC)
            sqb = [sb("sqb%d" % i, [128, NT], BF16, SC) for i in range(2)]
            pss = [ps("pss%d" % hf, [128, 512], F32, SC) for hf in range(2)]
            allZX = [("ZX", b) for b in range(8)]
            allPc = [("Pc", b) for b in range(8)]
            P.dve(lambda e: e.tensor_copy(out=sst[:, :, :, 0], in_=Pc[:].rearrange("p b (l t) -> p b l t", t=128)[:, :, :, 127]), reads=allPc, writes=["sst"])
            P.dve(lambda e: e.tensor_copy(out=sst[:, :, :, 1], in_=ZX[:].rearrange("p b (l t) -> p b l t", t=128)[:, :, :, 127]), reads=allZX, writes=["sst"])
            P.dma(lambda e: e.dma_start(out=sin_[:, :], in_=sst[:].rearrange("p b l t -> p (b l t)")), reads=["sst"], writes=["sin"])
            P.cc(lambda e: e.collective_compute("AllGather", ALU.bypass, replica_groups=RG, ins=[sin_[:, :]], outs=[sbuf_[:, :]]), reads=["sin"], writes=["sbufd"])
            P.dma(lambda e: e.dma_start(out=SG[:], in_=sbuf_.rearrange("(r p) c -> p r c", p=128)), reads=["sbufd"], writes=["SG"])
            for j in range(4):
                sgv = SG[:, j, :].rearrange("p (b l t) -> p b l t", b=8, l=8)
                P.dve(lambda e, j=j, sgv=sgv: e.tensor_copy(out=Aall[:, :, j:j + 29:4], in_=sgv[:, :, :, 0]), reads=["SG"], writes=["Aall"])
                P.dve(lambda e, j=j, sgv=sgv: e.tensor_copy(out=Hall[:, :, j:j + 29:4], in_=sgv[:, :, :, 1]), reads=["SG"], writes=["Hall"])
            P.pool(lambda e: e.memset(Spad[:], 0.0), writes=["Spad"])
            for blk in range(8):
                P.dve(lambda e, blk=blk: e.tensor_tensor_scan(out=Spad[:, blk, 1:33], data0=Aall[:, blk, :], data1=Hall[:, blk, :], initial=0.0, op0=ALU.mult, op1=ALU.add),
                      reads=["Aall", "Hall", "Spad"], writes=["Spad"])
            P.dve(lambda e: e.tensor_scalar(out=hinit[:], in0=Spad[:, :, 0:29:4], scalar1=oh[:, 0:1], scalar2=None, op0=ALU.mult), reads=["Spad", "oh"], writes=["hinit"])
            for m in range(1, 4):
                P.dve(lambda e, m=m: e.scalar_tensor_tensor(out=hinit[:], in0=Spad[:, :, m:m + 29:4], scalar=oh[:, m:m + 1], in1=hinit[:], op0=ALU.mult, op1=ALU.add),
                      reads=["Spad", "oh", "hinit"], writes=["hinit"])
            for blk in range(8):
                for l in range(8):
                    sl = slice(l * 128, (l + 1) * 128)
                    P.dve(lambda e, blk=blk, l=l, sl=sl: e.scalar_tensor_tensor(out=ZX[:, blk, sl], in0=Pc[:, blk, sl], scalar=hinit[:, blk, l:l + 1], in1=ZX[:, blk, sl], op0=ALU.mult, op1=ALU.add),
                          reads=[("Pc", blk), "hinit", ("ZX", blk)], writes=[("ZX", blk)])
                P.dve(lambda e, blk=blk: e.tensor_tensor(out=ZX[:, blk, :], in0=ZX[:, blk, :], in1=gy[:, blk, :], op=ALU.mult), reads=[("ZX", blk), ("gy", blk)], writes=[("ZX", blk)])
                i = blk % 2
                P.act(lambda e, blk=blk, i=i: e.activation(out=sqb[i][:], in_=ZX[:, blk, :], func=AF.Square), reads=[("ZX", blk)], writes=["sqb%d" % i])
                for hf in range(2):
                    P.pe(lambda e, blk=blk, i=i, hf=hf: e.matmul(pss[hf][:], lhsT=ones_bf[:], rhs=sqb[i][:, hf * 512:(hf + 1) * 512], start=(blk == 0), stop=(blk == 7)),
                         reads=["ones_bf", "sqb%d" % i], writes=["pss%d" % hf])
            for hf in range(2):
                sl = slice(hf * 512, (hf + 1) * 512)
                P.act(lambda e, hf=hf, sl=sl: e.activation(out=rstdb[:, sl], in_=pss[hf][:], func=AF.Sqrt, scale=1.0 / 1024, bias=epsT[:, 0:1]), reads=["pss%d" % hf, "m_eps"], writes=["rstdb"])
            P.dve(lambda e: e.reciprocal(out=rstdb[:], in_=rstdb[:]), reads=["rstdb"], writes=["rstdb"])
            for blk in range(8):
                P.dve(lambda e, blk=blk: e.scalar_tensor_tensor(out=orecT[:, blk, :], in0=ZX[:, blk, :], scalar=rg[:, blk:blk + 1], in1=rstdb[:], op0=ALU.mult, op1=ALU.mult),
                      reads=[("ZX", blk), "rec_out_g", "rstdb"], writes=[("orecT", blk)])
        P.barrier()


def attention(C, SM, qT, gT, orecT, gbuf, ones_bf, ones_f, epsT, K):
    nc, P, W, sb, ps, AT, ident = C.nc, C.P, C.W, C.sb, C.ps, C.AT, C.ident
    contextlib = C.contextlib

    def bc4(ap, n):
        return ap.unsqueeze(1).broadcast_to([n, 4, 128])

    with contextlib.ExitStack() as SA:
        cmpm = sb("cmpm", [128, 2, 8, 128], BF16, SA)
        selcm = sb("selcm", [128, 4, 128], BF16, SA)
        winm = sb("winm", [128, 8, 128], BF16, SA)
        selA = sb("selA", [128, 8, 64], F32, SA)
        selB = sb("selB", [128, 8, 64], F32, SA)
        selF = sb("selF", [128, 8, 64], F32, SA)
        E = sb("E", [64, 4096], BF16, SA)
        ovl = sb("ovl", [128, 2, 64], F32, SA)
        selmat = sb("selmat", [24, 24, 128], F32, SA)
        oaT = sb("oaT", [128, 8, NT], BF16, SA)
        for t_, src, nm in [(cmpm, K["cmpm"], "cmpm"), (selcm, K["selcm"], "selcm"), (winm, K["winm"], "winm"), (selA, K["selA"], "selA"),
                            (selB, K["selB"], "selB"), (selF, K["selF"], "selF"), (E, K["E"], "E"), (ovl, K["ovl"], "ovl"), (selmat, K["selmat"], "selmat")]:
            fl = t_[:]
            if len(t_.shape) == 3:
                fl = t_[:].rearrange("p a b -> p (a b)")
            elif len(t_.shape) == 4:
                fl = t_[:].rearrange("p a b c -> p (a b c)")
            P.dma(lambda e, fl=fl, src=src: e.dma_start(out=fl, in_=src[:, :]), writes=[nm])

        gview = gbuf.rearrange("(k j il d) (l p) -> d k il l j p", k=4, j=4, il=3, d=128, l=8, p=128)
        for g in range(2):
            with contextlib.ExitStack() as SG_:
                KV = {}
                for nm, item in [("kc", 0 + g), ("vc", 2 + g), ("ks", 4 + g), ("kw", 6 + g), ("vs", 8 + g), ("vw", 10 + g)]:
                    t_ = sb("%s%d" % (nm, g), [128, 4096], BF16, SG_)
                    KV[nm] = t_
                    for l in range(8):
                        P.dma(lambda e, t_=t_, item=item, l=l: e.dma_start(out=t_[:, l * 512:(l + 1) * 512].rearrange("d (j p) -> d j p", j=4), in_=gview[:, item // 3, item % 3, l, :, :]),
                              reads=[("gbuf", item // 3)], writes=[("kv", nm, l)])
                kvall = lambda nm: [("kv", nm, l) for l in range(8)]
                kcT = sb("kcT%d" % g, [128, 256], BF16, SG_)
                vcm = sb("vcm%d" % g, [128, 2, 128], BF16, SG_)
                with contextlib.ExitStack() as SCm:
                    w1t = sb("w1t", [128, 32, 256], BF16, SCm)
                    w2t = sb("w2t", [128, 2, 128], BF16, SCm)
                    posf = sb("posf", [128, 32], F32, SCm)
                    posb = sb("posb", [128, 32], BF16, SCm)
                    b1 = sb("b1", [128, 2], F32, SCm)
                    hx = sb("hx", [128, 256], F32, SCm)
                    hy = sb("hy", [128, 256], F32, SCm)
                    ghid = sb("ghid", [128, 2, 256], BF16, SCm)
                    ph = [ps("ph%d" % i, [128, 256], F32, SCm) for i in range(2)]
                    pb = [ps("pb%d" % i, [128, 2], F32, SCm) for i in range(2)]
                    pk = ps("pk", [128, 256], F32, SCm)
                    for kvn, src, w1n, w2n, posn in [("k", "kc", "cmp_k_w1", "cmp_k_w2", "cmp_pos_k"), ("v", "vc", "cmp_v_w1", "cmp_v_w2", "cmp_pos_v")]:
                        P.dma(lambda e, w1n=w1n: e.dma_start(out=w1t[:], in_=W[w1n].rearrange("(l d) m -> d l m", d=128)), writes=["w1t"], q="pool")
                        P.dma(lambda e, w2n=w2n: e.dma_start(out=w2t[:], in_=W[w2n].rearrange("(c h) d -> h c d", h=128)), writes=["w2t"], q="pool")
                        with nc.allow_non_contiguous_dma(reason="tiny"):
                            P.dma(lambda e, posn=posn: e.dma_start(out=posf[:], in_=W[posn].rearrange("l d -> d l"), allow_slow_non_contiguous=True), writes=["posf"])
                        P.dve(lambda e: e.tensor_copy(out=posb[:], in_=posf[:]), reads=["posf"], writes=["posb"])
                        xT = KV[src]
                        for hc in range(2):
                            for l in range(32):
                                P.pe(lambda e, hc=hc, l=l, xT=xT: e.matmul(ph[hc][:, 0:255], lhsT=w1t[:, l, hc * 128:(hc + 1) * 128], rhs=xT[:, l:l + 16 * 254 + 1:16],
                                                                   start=(l == 0), stop=(l == 31)), reads=["w1t"] + kvall(src), writes=["ph%d" % hc])
                            for l in range(32):
                                P.pe(lambda e, hc=hc, l=l: e.matmul(pb[hc][:, 0:1], lhsT=w1t[:, l, hc * 128:(hc + 1) * 128], rhs=posb[:, l:l + 1],
                                                             start=(l == 0), stop=(l == 31)), reads=["w1t", "posb"], writes=["pb%d" % hc])
                            P.act(lambda e, hc=hc: e.copy(out=b1[:, hc:hc + 1], in_=pb[hc][:, 0:1]), reads=["pb%d" % hc], writes=["b1"])
                            P.act(lambda e, hc=hc: e.activation(out=hx[:, 0:255], in_=ph[hc][:, 0:255], func=AF.Identity, bias=b1[:, hc:hc + 1]), reads=["ph%d" % hc, "b1"], writes=["hx"])
                            P.act(lambda e: e.activation(out=hy[:, 0:255], in_=hx[:, 0:255], func=AF.Square), reads=["hx"], writes=["hy"])
                            P.dve(lambda e: e.tensor_scalar(out=hy[:, 0:255], in0=hy[:, 0:255], scalar1=0.044715, scalar2=1.0, op0=ALU.mult, op1=ALU.add), reads=["hy"], writes=["hy"])
                            P.dve(lambda e: e.tensor_tensor(out=hy[:, 0:255], in0=hy[:, 0:255], in1=hx[:, 0:255], op=ALU.mult), reads=["hy", "hx"], writes=["hy"])
                            P.act(lambda e: e.activation(out=hy[:, 0:255], in_=hy[:, 0:255], func=AF.Sigmoid, scale=1.5957691216057308), reads=["hy"], writes=["hy"])
                            P.dve(lambda e, hc=hc: e.tensor_tensor(out=ghid[:, hc, 0:255], in0=hy[:, 0:255], in1=hx[:, 0:255], op=ALU.mult), reads=["hy", "hx"], writes=[("ghid", hc)])
                        if kvn == "k":
                            for hc in range(2):
                                P.pe(lambda e, hc=hc: e.matmul(pk[:, 0:255], lhsT=w2t[:, hc, :], rhs=ghid[:, hc, 0:255], start=(hc == 0), stop=(hc == 1)),
                                     reads=["w2t", ("ghid", 0), ("ghid", 1)], writes=["pk"])
                            P.act(lambda e: e.copy(out=kcT[:, 0:255], in_=pk[:, 0:255]), reads=["pk"], writes=["kcT"])
                        else:
                            for ct in range(2):
                                cn = 128 if ct == 0 else 127
                                for hc in range(2):
                                    P.pe(lambda e, hc=hc, ct=ct, cn=cn: e.matmul(pk[0:cn, ct * 128:(ct + 1) * 128], lhsT=ghid[:, hc, ct * 128:ct * 128 + cn], rhs=w2t[:, hc, :], start=(hc == 0), stop=(hc == 1)),
                                         reads=["w2t", ("ghid", 0), ("ghid", 1)], writes=["pk"])
                            P.act(lambda e: e.copy(out=vcm[:, 0, :], in_=pk[:, 0:128]), reads=["pk"], writes=["vcm"])
                            P.act(lambda e: e.copy(out=vcm[0:127, 1, :], in_=pk[0:127, 128:256]), reads=["pk"], writes=["vcm"])
                P.barrier()
                with contextlib.ExitStack() as SQ:
                    Pcf = [sb("Pcf%d" % ct, [128, 512], F32, SQ) for ct in range(2)]
                    pcb = [sb("pcb%d" % ct, [128, 512], BF16, SQ) for ct in range(2)]
                    rd = sb("rd", [128, 512], F32, SQ)
                    gS = sb("gS", [128, 512], F32, SQ)
                    wgt = sb("wgt", [128, 512], F32, SQ)
                    accs = [sb("acc%d" % i, [128, 512], F32, SQ) for i in range(2)]
                    rdc = sb("rdc", [128, 512], F32, SQ)
                    gSc = sb("gSc", [128, 512], F32, SQ)
                    t0 = sb("t0", [128, 64], F32, SQ)
                    t1 = sb("t1s", [128, 64], F32, SQ)
                    m8a = sb("m8a", [128, 8], F32, SQ)
                    m8b = sb("m8b", [128, 8], F32, SQ)
                    selbb = sb("selbb", [128, 64], BF16, SQ)
                    selbTs = [sb("selbT%d" % i, [64, 4, 128], BF16, SQ) for i in range(2)]
                    Pt = [sb("Pt%d" % i, [128, 512], BF16, SQ) for i in range(3)]
                    pS = [ps("pS%d" % i, [128, 512], F32, SQ) for i in range(2)]
                    pO = ps("pO", [128, 512], F32, SQ)
                    pD = ps("pD", [128, 512], F32, SQ)
                    pG = ps("pG", [128, 512], F32, SQ)
                    pI = ps("pI", [128, 64], F32, SQ)
                    pTt = ps("pTt", [64, 128], BF16, SQ)
                    pC = ps("pC", [128, 512], F32, SQ)
                    scnt = [0]
                    pcnt = [0]

                    def dump(nm, tile_, key, shape, dt=F32):
                        C.dbg[nm] = nc.dram_tensor("dbg_" + nm, list(shape), dt, kind="ExternalOutput").ap()
                        C.dbg_ops.append(P.dma(lambda e: e.dma_start(out=C.dbg[nm][:, :], in_=tile_), reads=[key], writes=["dd_" + nm]))

                    def gates(br, l, dst=None, dkey="gS"):
                        dst = gS if dst is None else dst
                        for h in range(4):
                            n = (4 * g + h) * 3 + br
                            P.pe(lambda e, h=h, n=n, l=l: e.matmul(pG[:, h * 128:(h + 1) * 128], lhsT=selmat[:, n, :], rhs=gT[:, l * 128:(l + 1) * 128], start=True, stop=True),
                                 reads=["selmat", "gT"], writes=["pG"])
                        P.act(lambda e, dst=dst: e.copy(out=dst[:], in_=pG[:]), reads=["pG"], writes=[dkey])

                    def branch(br, l, keyT, vtok, kts, maskfn, use_sel):
                        acc = accs[l % 2]
                        akey = "acc%d" % (l % 2)
                        selbT = selbTs[l % 2]
                        skey = "selbT%d" % (l % 2)
                        qg = qT[:, 4 * g:4 * g + 4, l * 128:(l + 1) * 128]
                        qkeys = [("qT", 4 * g + h) for h in range(4)]
                        nk = len(kts)
                        for ii, kt in enumerate(kts):
                            si = scnt[0] % 2
                            scnt[0] += 1
                            pi_ = pcnt[0] % 3
                            pcnt[0] += 1
                            lk = ("kv", keyT[1], kt // 4)
                            P.pe(lambda e, si=si, kt=kt: e.matmul(pS[si][:], lhsT=keyT[0][:, kt * 128:(kt + 1) * 128], rhs=qg, start=True, stop=(not use_sel)),
                                 reads=[lk] + qkeys, writes=["pS%d" % si])
                            if use_sel:
                                P.pe(lambda e, si=si, kt=kt, selbT=selbT: e.matmul(pS[si][:], lhsT=E[:, kt * 128:(kt + 1) * 128], rhs=selbT[:].rearrange("s h q -> s (h q)"), start=False, stop=True),
                                     reads=["E", skey], writes=["pS%d" % si])
                            P.act(lambda e, si=si, pi_=pi_: e.activation(out=Pt[pi_][:], in_=pS[si][:], func=AF.Exp, scale=SCALE), reads=["pS%d" % si], writes=["Pt%d" % pi_])
                            mk = maskfn(kt)
                            if mk is not None:
                                P.dve(lambda e, pi_=pi_, mk=mk: e.tensor_tensor(out=Pt[pi_][:].rearrange("k (h q) -> k h q", h=4), in0=Pt[pi_][:].rearrange("k (h q) -> k h q", h=4),
                                                                          in1=bc4(mk[0], 128), op=ALU.mult), reads=["Pt%d" % pi_, mk[1]], writes=["Pt%d" % pi_])
                            vk = ("kv", vtok[1], kt // 4)
                            P.pe(lambda e, pi_=pi_, kt=kt, ii=ii: e.matmul(pO[:], lhsT=vtok[0][:, kt * 128:(kt + 1) * 128], rhs=Pt[pi_][:], start=(ii == 0), stop=(ii == nk - 1)),
                                 reads=[vk, "Pt%d" % pi_], writes=["pO"])
                            P.pe(lambda e, pi_=pi_, ii=ii: e.matmul(pD[:], lhsT=ones_bf[:], rhs=Pt[pi_][:], start=(ii == 0), stop=(ii == nk - 1)),
                                 reads=["ones_bf", "Pt%d" % pi_], writes=["pD"])
                        gates(br, l)
                        P.dve(lambda e: e.tensor_scalar(out=rd[:], in0=pD[:], scalar1=1e-30, scalar2=None, op0=ALU.max), reads=["pD"], writes=["rd"])
                        P.dve(lambda e: e.reciprocal(out=rd[:], in_=rd[:]), reads=["rd"], writes=["rd"])
                        P.dve(lambda e: e.tensor_tensor(out=wgt[:], in0=rd[:], in1=gS[:], op=ALU.mult), reads=["rd", "gS"], writes=["wgt"])
                        P.dve(lambda e: e.tensor_tensor(out=wgt[:], in0=pO[:], in1=wgt[:], op=ALU.mult), reads=["pO", "wgt"], writes=["wgt"])
                        P.dve(lambda e, acc=acc: e.tensor_tensor(out=acc[:], in0=acc[:], in1=wgt[:], op=ALU.add), reads=[akey, "wgt"], writes=[akey])

                    def cmp_stage(l):
                        acc = accs[l % 2]
                        akey = "acc%d" % (l % 2)
                        selbT = selbTs[l % 2]
                        skey = "selbT%d" % (l % 2)
                        qg = qT[:, 4 * g:4 * g + 4, l * 128:(l + 1) * 128]
                        qkeys = [("qT", 4 * g + h) for h in range(4)]
                        for ct in range(2):
                            cn = 128 if ct == 0 else 127
                            si = scnt[0] % 2
                            scnt[0] += 1
                            P.pe(lambda e, si=si, ct=ct, cn=cn, qg=qg: e.matmul(pS[si][0:cn, :], lhsT=kcT[:, ct * 128:ct * 128 + cn], rhs=qg, start=True, stop=True),
                                 reads=["kcT"] + qkeys, writes=["pS%d" % si])
                            P.act(lambda e, si=si, ct=ct, cn=cn: e.activation(out=Pcf[ct][0:cn, :], in_=pS[si][0:cn, :], func=AF.Exp, scale=SCALE), reads=["pS%d" % si], writes=["Pcf%d" % ct])
                            P.dve(lambda e, ct=ct, cn=cn, l=l: e.tensor_tensor(out=Pcf[ct][0:cn, :].rearrange("k (h q) -> k h q", h=4), in0=Pcf[ct][0:cn, :].rearrange("k (h q) -> k h q", h=4),
                                                                     in1=bc4(cmpm[0:cn, ct, l, :], cn), op=ALU.mult), reads=["Pcf%d" % ct, "cmpm"], writes=["Pcf%d" % ct])
                        for ct in range(2):
                            cn = 128 if ct == 0 else 127
                            P.pe(lambda e, ct=ct, cn=cn: e.matmul(pC[:], lhsT=ones_f[0:cn, :], rhs=Pcf[ct][0:cn, :], start=(ct == 0), stop=(ct == 1)),
                                 reads=["ones_f", "Pcf%d" % ct], writes=["pC"])
                        P.dve(lambda e: e.tensor_scalar(out=rdc[:], in0=pC[:], scalar1=1e-30, scalar2=None, op0=ALU.max), reads=["pC"], writes=["rdc"])
                        P.dve(lambda e: e.reciprocal(out=rdc[:], in_=rdc[:]), reads=["rdc"], writes=["rdc"])
                        for ct in range(2):
                            cn = 128 if ct == 0 else 127
                            P.dve(lambda e, ct=ct, cn=cn: e.tensor_tensor(out=Pcf[ct][0:cn, :], in0=Pcf[ct][0:cn, :], in1=rdc[0:cn, :], op=ALU.mult), reads=["Pcf%d" % ct, "rdc"], writes=["Pcf%d" % ct])
                            P.act(lambda e, ct=ct, cn=cn: e.copy(out=pcb[ct][0:cn, :], in_=Pcf[ct][0:cn, :]), reads=["Pcf%d" % ct], writes=["pcb%d" % ct])
                        for ct in range(2):
                            cn = 128 if ct == 0 else 127
                            P.pe(lambda e, ct=ct, cn=cn: e.matmul(pC[:], lhsT=vcm[0:cn, ct, :], rhs=pcb[ct][0:cn, :], start=(ct == 0), stop=(ct == 1)),
                                 reads=["vcm", "pcb%d" % ct, "rdc"], writes=["pC"])
                        cnt8 = 0
                        for r in range(4):
                            for ct in range(2):
                                cn = 128 if ct == 0 else 127
                                P.pe(lambda e, ct=ct, cn=cn, r=r, cnt8=cnt8: e.matmul(pI[:], lhsT=Pcf[ct][0:cn, r * 128:(r + 1) * 128], rhs=ovl[0:cn, ct, :], start=(cnt8 == 0), stop=(cnt8 == 7)),
                                     reads=["Pcf%d" % ct, "ovl"], writes=["pI"])
                                cnt8 += 1
                        gates(0, l, gSc, "gSc")
                        P.dve(lambda e, acc=acc: e.tensor_tensor(out=acc[:], in0=pC[:], in1=gSc[:], op=ALU.mult), reads=["pC", "gSc"], writes=[akey])
                        dbg_here = (C.stop == 22 and g == 0 and l == 1)
                        if dbg_here:
                            dump("acc0", acc[:], akey, [128, 512])
                            dump("kcT", kcT[:], "kcT", [128, 256], BF16)
                            dump("vcm", vcm[:].rearrange("p a b -> p (a b)"), "vcm", [128, 256], BF16)
                            dump("gS0", gSc[:], "gSc", [128, 512])
                        P.dve(lambda e, l=l: e.tensor_tensor(out=t0[:], in0=pI[:], in1=selA[:, l, :], op=ALU.mult), reads=["pI", "selA"], writes=["t0"])
                        P.dve(lambda e, l=l: e.tensor_tensor(out=t0[:], in0=t0[:], in1=selB[:, l, :], op=ALU.add), reads=["t0", "selB"], writes=["t0"])
                        P.dve(lambda e, l=l: e.tensor_tensor(out=t0[:], in0=t0[:], in1=selF[:, l, :], op=ALU.max), reads=["t0", "selF"], writes=["t0"])
                        P.dve(lambda e: e.max(out=m8a[:], in_=t0[:]), reads=["t0"], writes=["m8a"])
                        P.dve(lambda e: e.match_replace(out=t1[:], in_to_replace=m8a[:], in_values=t0[:], imm_value=-1e30), reads=["t0", "m8a"], writes=["t1s"])
                        P.dve(lambda e: e.max(out=m8b[:], in_=t1[:]), reads=["t1s"], writes=["m8b"])
                        P.dve(lambda e: e.tensor_scalar(out=t1[:], in0=t0[:], scalar1=m8b[:, 7:8], scalar2=-1.0, op0=ALU.is_ge, op1=ALU.add), reads=["t0", "m8b"], writes=["t1s"])
                        P.dve(lambda e: e.tensor_scalar(out=selbb[:], in0=t1[:], scalar1=-BIGNEG, scalar2=None, op0=ALU.mult), reads=["t1s"], writes=["selbb"])
                        P.pe(lambda e: e.transpose(out=pTt[:], in_=selbb[:], identity=ident[:]), reads=["selbb", "ident"], writes=["pTt"])
                        for h in range(4):
                            P.act(lambda e, h=h, selbT=selbT: e.copy(out=selbT[:, h, :], in_=pTt[:]), reads=["pTt"], writes=[skey])
                        if C.stop == 22 and g == 0 and l == 1:
                            C.dbg["t0"] = nc.dram_tensor("dbg_t0", [128, 64], F32, kind="ExternalOutput").ap()
                            C.dbg["selb"] = nc.dram_tensor("dbg_selb", [128, 64], BF16, kind="ExternalOutput").ap()
                            C.dbg_ops.append(P.dma(lambda e: e.dma_start(out=C.dbg["t0"][:, :], in_=t0[:]), reads=["t0"], writes=["dd1"]))
                            C.dbg_ops.append(P.dma(lambda e: e.dma_start(out=C.dbg["selb"][:, :], in_=selbb[:]), reads=["selbb"], writes=["dd2"]))
                    def rest_stage(l):
                        acc = accs[l % 2]
                        akey = "acc%d" % (l % 2)
                        dbg_here = (C.stop == 22 and g == 0 and l == 1)
                        branch(1, l, (KV["ks"], "ks"), (KV["vs"], "vs"), list(range(4 * l + 4)),
                               lambda kt, l=l: ((selcm[:, kt - 4 * l, :], "selcm") if kt >= 4 * l else None), True)
                        if dbg_here:
                            dump("acc1", acc[:], akey, [128, 512])
                        branch(2, l, (KV["kw"], "kw"), (KV["vw"], "vw"), list(range(max(0, 4 * l - 4), 4 * l + 4)),
                               lambda kt, l=l: (winm[:, kt - 4 * l + 4, :], "winm"), False)
                        if dbg_here:
                            dump("acc2", acc[:], akey, [128, 512])
                        for h in range(4):
                            P.act(lambda e, h=h, l=l, g=g, acc=acc: e.copy(out=oaT[:, 4 * g + h, l * 128:(l + 1) * 128], in_=acc[:, h * 128:(h + 1) * 128]), reads=[akey], writes=[("oaT", 4 * g + h)])
                    cmp_stage(0)
                    for l in range(8):
                        if l + 1 < 8:
                            cmp_stage(l + 1)
                        rest_stage(l)
                P.barrier()
        with contextlib.ExitStack() as SN:
            ag = sb("ag", [128, 8], F32, SN)
            rstdb = sb("a_rstdb", [128, NT], F32, SN)
            sqb = [sb("a_sqb%d" % i, [128, NT], BF16, SN) for i in range(2)]
            pss = [ps("a_pss%d" % hf, [128, 512], F32, SN) for hf in range(2)]
            with nc.allow_non_contiguous_dma(reason="tiny"):
                P.dma(lambda e: e.dma_start(out=ag[:], in_=W["attn_out_g"].rearrange("o (b p) -> p (o b)", p=128), allow_slow_non_contiguous=True), writes=["ag"])
            for h in range(8):
                i = h % 2
                P.act(lambda e, h=h, i=i: e.activation(out=sqb[i][:], in_=oaT[:, h, :], func=AF.Square), reads=[("oaT", h)], writes=["a_sqb%d" % i])
                for hf in range(2):
                    P.pe(lambda e, h=h, i=i, hf=hf: e.matmul(pss[hf][:], lhsT=ones_bf[:], rhs=sqb[i][:, hf * 512:(hf + 1) * 512], start=(h == 0), stop=(h == 7)),
                         reads=["ones_bf", "a_sqb%d" % i], writes=["a_pss%d" % hf])
            for hf in range(2):
                sl = slice(hf * 512, (hf + 1) * 512)
                P.act(lambda e, hf=hf, sl=sl: e.activation(out=rstdb[:, sl], in_=pss[hf][:], func=AF.Sqrt, scale=1.0 / 1024, bias=epsT[:, 0:1]), reads=["a_pss%d" % hf, "m_eps"], writes=["a_rstdb"])
            P.dve(lambda e: e.reciprocal(out=rstdb[:], in_=rstdb[:]), reads=["a_rstdb"], writes=["a_rstdb"])
            ATall = [("AT", t) for t in range(NTILE)]
            for h in range(8):
                P.dve(lambda e, h=h: e.scalar_tensor_tensor(out=AT[:, h, :], in0=oaT[:, h, :], scalar=ag[:, h:h + 1], in1=rstdb[:], op0=ALU.mult, op1=ALU.mult),
                      reads=[("oaT", h), "ag", "a_rstdb"], writes=ATall)
                P.pool(lambda e, h=h: e.tensor_copy(out=AT[:, 8 + h, :], in_=orecT[:, h, :]), reads=[("orecT", h)], writes=ATall)
    P.barrier()


def outproj(C):
    nc, P, W, sb, ps, AT = C.nc, C.P, C.W, C.sb, C.ps, C.AT
    contextlib = C.contextlib
    ATall = [("AT", t) for t in range(NTILE)]
    with contextlib.ExitStack() as SO:
        YT = sb("YT", [128, KD, NT], BF16, SO)
        with contextlib.ExitStack() as S1:
            wp = [sb("wo%d" % i, [128, KD, 512], BF16, S1) for i in range(2)]
            py = [[ps("po%d%d" % (i, hf), [128, 512], F32, S1) for hf in range(2)] for i in range(2)]
            wv = W["w_out"].rearrange("(kc p) m -> p kc m", p=128)
            cnt = 0
            for pi in range(4):
                b = pi % 2
                P.dma(lambda e, pi=pi, b=b: e.dma_start(out=wp[b][:], in_=wv[:, :, pi * 512:(pi + 1) * 512]), writes=["wo%d" % b], q="pool")
                for mc in range(4):
                    m = pi * 4 + mc
                    r = cnt % 2
                    cnt += 1
                    for hf in range(2):
                        for k in range(KD):
                            P.pe(lambda e, k=k, hf=hf, r=r, b=b, mc=mc: e.matmul(py[r][hf][:], lhsT=wp[b][:, k, mc * 128:(mc + 1) * 128], rhs=AT[:, k, hf * 512:(hf + 1) * 512],
                                                                         start=(k == 0), stop=(k == KD - 1)), reads=["wo%d" % b] + ATall[hf * 4:hf * 4 + 4], writes=["po%d%d" % (r, hf)])
                        P.act(lambda e, m=m, hf=hf, r=r: e.copy(out=YT[:, m, hf * 512:(hf + 1) * 512], in_=py[r][hf][:]), reads=["po%d%d" % (r, hf)],
                              writes=[("YT", hf * 4 + q) for q in range(4)])
        C.post("mx", YT, C.h1d, W["mix_post_g"], 1.0, C.h2d, W["ff2_pre_g"])


def ple(C):
    nc, P, W, sb, ps, AT, ident = C.nc, C.P, C.W, C.sb, C.ps, C.AT, C.ident
    contextlib = C.contextlib
    ATall = [("AT", t) for t in range(NTILE)]
    with contextlib.ExitStack() as SO:
        YT = sb("YTp", [128, KD, NT], BF16, SO)
        with contextlib.ExitStack() as S1:
            pT = sb("pTp", [128, 2, NT], BF16, S1)
            ptl = [sb("ptl%d" % i, [128, 256], F32, S1) for i in range(2)]
            ptb = [sb("ptb%d" % i, [128, 256], BF16, S1) for i in range(2)]
            wpj = sb("wpj", [128, 2, D], BF16, S1)
            sg = [sb("psg%d" % i, [128, 512], F32, S1) for i in range(2)]
            wp = [sb("wg%d" % i, [128, KD, 512], BF16, S1) for i in range(2)]
            ppt = [ps("ppt%d" % i, [128, 2, 128], BF16, S1) for i in range(2)]
            pa = [[ps("pa%d%d" % (i, hf), [128, 512], F32, S1) for hf in range(2)] for i in range(1)]
            pb = [[ps("pbp%d%d" % (i, hf), [128, 512], F32, S1) for hf in range(2)] for i in range(1)]
            P.dma(lambda e: e.dma_start(out=wpj[:], in_=W["w_ple_proj"].rearrange("(c p) m -> p c m", p=128)), writes=["wpj"], q="pool")
            for t in range(NTILE):
                i = t % 2
                P.dma(lambda e, t=t, i=i: e.dma_start(out=ptl[i][:], in_=C.pin[t * 128:(t + 1) * 128, :]), writes=["ptl%d" % i])
                P.dve(lambda e, i=i: e.tensor_copy(out=ptb[i][:], in_=ptl[i][:]), reads=["ptl%d" % i], writes=["ptb%d" % i])
                for c in range(2):
                    P.pe(lambda e, i=i, c=c: e.transpose(out=ppt[i][:, c, :], in_=ptb[i][:, c * 128:(c + 1) * 128], identity=ident[:]), reads=["ptb%d" % i, "ident"], writes=["ppt%d" % i])
                P.act(lambda e, i=i, t=t: e.copy(out=pT[:, :, t * 128:(t + 1) * 128], in_=ppt[i][:]), reads=["ppt%d" % i], writes=[("pTp", t)])
            wv = W["w_ple_gate"].rearrange("(kc p) m -> p kc m", p=128)
            pTall = [("pTp", t) for t in range(NTILE)]
            for pi in range(4):
                b = pi % 2
                P.dma(lambda e, pi=pi, b=b: e.dma_start(out=wp[b][:], in_=wv[:, :, pi * 512:(pi + 1) * 512]), writes=["wg%d" % b], q="pool")
                for mc in range(4):
                    m = pi * 4 + mc
                    for hf in range(2):
                        for k in range(KD):
                            P.pe(lambda e, k=k, hf=hf, b=b, mc=mc: e.matmul(pa[0][hf][:], lhsT=wp[b][:, k, mc * 128:(mc + 1) * 128], rhs=AT[:, k, hf * 512:(hf + 1) * 512],
                                                                    start=(k == 0), stop=(k == KD - 1)), reads=["wg%d" % b] + ATall[hf * 4:hf * 4 + 4], writes=["pa0%d" % hf])
                        for c in range(2):
                            P.pe(lambda e, c=c, hf=hf, m=m: e.matmul(pb[0][hf][:], lhsT=wpj[:, c, m * 128:(m + 1) * 128], rhs=pT[:, c, hf * 512:(hf + 1) * 512],
                                                              start=(c == 0), stop=(c == 1)), reads=["wpj"] + pTall[hf * 4:hf * 4 + 4], writes=["pbp0%d" % hf])
                        P.act(lambda e, hf=hf: e.activation(out=sg[hf][:], in_=pa[0][hf][:], func=AF.Sigmoid), reads=["pa0%d" % hf], writes=["psg%d" % hf])
                        P.dve(lambda e, hf=hf, m=m: e.tensor_tensor(out=YT[:, m, hf * 512:(hf + 1) * 512], in0=sg[hf][:], in1=pb[0][hf][:], op=ALU.mult),
                              reads=["psg%d" % hf, "pbp0%d" % hf], writes=[("YTp", hf * 4 + q) for q in range(4)])
        C.post("pl", YT, C.h3d, W["ple_post_g"], 1.0, C.out, None)


def host_consts(j):
    import ml_dtypes
    bf = ml_dtypes.bfloat16
    c = {}
    d = np.arange(128)
    half = 16
    inv_freq = (500000.0 ** (-np.arange(half, dtype=np.float32) / half)).astype(np.float32)
    rope = np.zeros((128, 4), np.float32)
    rope[:32, 0] = inv_freq[d[:32] % 16]
    rope[:16, 1] = -1.0
    rope[16:32, 1] = 1.0
    rope[:, 2] = 1.0
    c["c_rope"] = rope
    perm = np.zeros((128, 128), np.float32)
    for m in range(128):
        k = m + 16 if m < 16 else (m - 16 if m < 32 else m)
        perm[k, m] = 1.0
    c["c_perm"] = perm
    oh = np.zeros((128, 4), np.float32)
    oh[:, j] = 1.0
    c["c_oh"] = oh
    q = np.arange(128)
    kk = np.arange(128)
    cm = np.zeros((128, 2, 8, 128), np.float32)
    for ct in range(2):
        cg = ct * 128 + kk
        for l in range(8):
            t = (4 * l + j) * 128 + q
            cm[:, ct, l, :] = ((16 * cg[:, None] + 31 <= t[None, :]) & (cg[:, None] < 255))
    c["c_cmpm"] = cm.reshape(128, -1).astype(bf)
    tri = (kk[:, None] <= q[None, :]).astype(np.float32)
    triu = (kk[:, None] > q[None, :]).astype(np.float32)
    sc = np.zeros((128, 4, 128), np.float32)
    for m in range(4):
        sc[:, m, :] = 1.0 if m < j else (tri if m == j else 0.0)
    c["c_selcm"] = sc.reshape(128, -1).astype(bf)
    wm = np.zeros((128, 8, 128), np.float32)
    for m in range(8):
        dd = m - 4 - j
        if dd == 0:
            wm[:, m, :] = tri
        elif dd == -4:
            wm[:, m, :] = triu
        elif -4 < dd < 0:
            wm[:, m, :] = 1.0
    c["c_winm"] = wm.reshape(128, -1).astype(bf)
    sA = np.zeros((128, 8, 64), np.float32)
    sB = np.zeros((128, 8, 64), np.float32)
    sF = np.zeros((128, 8, 64), np.float32)
    s = np.arange(64)
    for l in range(8):
        t = (4 * l + j) * 128 + q
        valid = (64 * s[None, :] <= t[:, None])
        forced = (s[None, :] == (t // 64)[:, None]) | (s[None, :] == 0)
        sA[:, l, :] = valid
        sB[:, l, :] = (valid.astype(np.float32) - 1.0) * 1e4
        sF[:, l, :] = np.where(forced, 1e4, -3e4)
    c["c_selA"] = sA.reshape(128, -1)
    c["c_selB"] = sB.reshape(128, -1)
    c["c_selF"] = sF.reshape(128, -1)
    key = np.arange(4096)
    c["c_E"] = (key[None, :] // 64 == s[:, None]).astype(np.float32).astype(bf)
    ov = np.zeros((128, 2, 64), np.float32)
    for ct in range(2):
        cg = ct * 128 + kk
        st_ = 16 * cg
        en_ = st_ + 31
        ov[:, ct, :] = ((st_[:, None] < 64 * s[None, :] + 64) & (en_[:, None] >= 64 * s[None, :]) & (cg[:, None] < 255))
    c["c_ovl"] = ov.reshape(128, -1)
    sm = np.zeros((24, 24, 128), np.float32)
    for n in range(24):
        sm[n, n, :] = 1.0
    c["c_selmat"] = sm.reshape(24, -1)
    return c


_orig_make_in_maps = make_in_maps


def make_in_maps(inputs):
    maps = _orig_make_in_maps(inputs)
    for c in range(8):
        maps[c].update(host_consts(c % 4))
    return maps


_NC_CACHE = {}


def kernel(**inputs):
    maps = make_in_maps(inputs)
    if "nc" not in _NC_CACHE:
        _NC_CACHE["nc"] = build()
    nc = _NC_CACHE["nc"]
    res = run_bass_kernel_spmd(nc, maps, core_ids=list(range(8)))
    return unshard([r["out"] for r in res.results])
```

```python
import numpy as np
import concourse.bass as bass
import concourse.mybir as mybir
from concourse.bass_utils import run_bass_kernel_spmd

F32 = mybir.dt.float32
BF16 = mybir.dt.bfloat16
I32 = mybir.dt.int32
AF = mybir.ActivationFunctionType
ALU = mybir.AluOpType
AX = mybir.AxisListType

COMPUTE = ("pe", "act", "dve", "pool")
NRING = 12


class Op:
    __slots__ = ("eng", "fn", "deps", "is_dma", "signal", "idx", "ring", "ringval", "prev_ring", "is_cc", "seg", "dur", "fin", "bdeps")

    def __init__(self, eng, fn, is_dma):
        self.eng = eng
        self.fn = fn
        self.is_dma = is_dma
        self.deps = []
        self.bdeps = []
        self.signal = 0
        self.ring = None
        self.ringval = 0
        self.prev_ring = None
        self.idx = 0
        self.is_cc = False
        self.seg = 0
        self.dur = 0.5
        self.fin = 0.0


DEFAULT_DUR = {"pe": 0.28, "act": 0.7, "dve": 0.9, "pool": 1.2, "sp": 2.5}
SCHED = True
WINDOW = 48


class Prog:
    def __init__(self, nc):
        self.nc = nc
        self.ops = {k: [] for k in ("pe", "act", "dve", "pool", "sp")}
        self.last_w = {}
        self.readers = {}
        self.nops = 0
        self.seg = 0

    def barrier(self):
        self.seg += 1

    def _add(self, eng, fn, reads, writes, is_dma, d=None):
        op = Op(eng, fn, is_dma)
        op.idx = self.nops
        op.seg = self.seg
        op.dur = DEFAULT_DUR[eng] if d is None else d
        self.nops += 1
        deps = {}
        for r in reads:
            w = self.last_w.get(r)
            if w is not None:
                deps[id(w)] = w
        for wr in writes:
            w = self.last_w.get(wr)
            if w is not None:
                deps[id(w)] = w
            for rd in self.readers.get(wr, ()):
                deps[id(rd)] = rd
        for dd in deps.values():
            if dd is op:
                continue
            op.deps.append(dd)
        for r in reads:
            self.readers.setdefault(r, []).append(op)
        for wr in writes:
            self.last_w[wr] = op
            self.readers[wr] = []
        self.ops[eng].append(op)
        return op

    def pe(self, fn, reads=(), writes=(), d=None):
        return self._add("pe", fn, reads, writes, False, d)

    def act(self, fn, reads=(), writes=(), d=None):
        return self._add("act", fn, reads, writes, False, d)

    def dve(self, fn, reads=(), writes=(), d=None):
        return self._add("dve", fn, reads, writes, False, d)

    def pool(self, fn, reads=(), writes=(), d=None):
        return self._add("pool", fn, reads, writes, False, d)

    def dma(self, fn, reads=(), writes=(), q="sp", d=None):
        return self._add(q, fn, reads, writes, True, d)

    def cc(self, fn, reads=(), writes=()):
        op = self._add("pool", fn, reads, writes, True, 15.0)
        op.is_cc = True
        return op

    def _schedule(self):
        nseg = self.seg + 1
        byseg = [{e: [] for e in self.ops} for _ in range(nseg)]
        for e, lst in self.ops.items():
            for op in lst:
                byseg[op.seg][e].append(op)
        new = {e: [] for e in self.ops}
        LAT_X, LAT_S = 1.2, 0.25
        t_base = 0.0
        for sg in range(nseg):
            queues = byseg[sg]
            if not SCHED:
                for e in queues:
                    new[e].extend(queues[e])
                continue
            pos = {e: 0 for e in queues}
            done = set()
            sched_flag = {e: [False] * len(queues[e]) for e in queues}
            free_at = {e: t_base for e in queues}
            remaining = sum(len(q) for q in queues.values())
            tmax = t_base
            while remaining:
                best = None
                for e, q in queues.items():
                    n = len(q)
                    p = pos[e]
                    while p < n and sched_flag[e][p]:
                        p += 1
                    pos[e] = p
                    cnt = 0
                    i = p
                    while i < n and cnt < WINDOW:
                        if not sched_flag[e][i]:
                            cnt += 1
                            op = q[i]
                            ok = True
                            rdy = free_at[e]
                            for dd in op.deps:
                                if dd.seg == sg:
                                    if id(dd) not in done:
                                        ok = False
                                        break
                                    lat = LAT_S if (dd.eng == e and not dd.is_dma) else LAT_X
                                    if dd.eng == "pe" and e == "pe":
                                        lat = 0.0
                                    tt = dd.fin + lat
                                    if tt > rdy:
                                        rdy = tt
                            if ok:
                                key = (rdy, op.idx)
                                if best is None or key < best[0]:
                                    best = (key, e, i, op)
                                if rdy <= free_at[e]:
                                    break
                        i += 1
                (rdy, _), e, i, op = best
                if op.is_dma:
                    free_at[e] = rdy + 0.15
                    op.fin = rdy + op.dur
                else:
                    free_at[e] = rdy + op.dur
                    op.fin = free_at[e]
                tmax = max(tmax, op.fin)
                sched_flag[e][i] = True
                done.add(id(op))
                new[e].append(op)
                remaining -= 1
            t_base = tmax + 2.0
        self.ops = new

    def emit(self, final_wait_ops=()):
        nc = self.nc
        self._schedule()
        nseg = self.seg + 1
        last_compute = {}
        first_in_seg = {}
        dmas_in_seg = [[] for _ in range(nseg)]
        ccs_in_seg = [[] for _ in range(nseg)]
        lastc_upto = [dict() for _ in range(nseg)]
        for e, lst in self.ops.items():
            for op in lst:
                if (op.seg, e) not in first_in_seg:
                    first_in_seg[(op.seg, e)] = op
                if op.is_cc:
                    ccs_in_seg[op.seg].append(op)
                elif op.is_dma:
                    dmas_in_seg[op.seg].append(op)
                else:
                    lastc_upto[op.seg][e] = op
        run_last = {}
        pend = {e: [] for e in self.ops}
        for sg in range(1, nseg):
            for e2, op2 in lastc_upto[sg - 1].items():
                run_last[e2] = op2
            bd = list(run_last.values()) + dmas_in_seg[sg - 1]
            for e in self.ops:
                pend[e] = pend[e] + bd + (ccs_in_seg[sg - 1] if e == "pool" else [])
                f = first_in_seg.get((sg, e))
                if f is not None:
                    f.bdeps = [d for d in pend[e] if not (d.eng == e and not d.is_dma and not f.is_dma)]
                    pend[e] = []
        needed = set()
        for e, lst in self.ops.items():
            for op in lst:
                for d in op.deps:
                    if not (d.eng == "pe" and e == "pe" and not d.is_dma):
                        needed.add(id(d))
                for d in op.bdeps:
                    needed.add(id(d))
        for op in final_wait_ops:
            needed.add(id(op))
        ringcount = {}
        for e, lst in self.ops.items():
            n = 0
            k = 0
            last_on_ring = {}
            for op in lst:
                if id(op) not in needed:
                    continue
                if op.is_cc:
                    op.ring = ("cc", op.idx)
                    op.ringval = 1
                    op.prev_ring = None
                elif op.is_dma:
                    slot = k % NRING
                    k += 1
                    op.ring = (e, slot)
                    ringcount[(e, slot)] = ringcount.get((e, slot), 0) + 16
                    op.ringval = ringcount[(e, slot)]
                    op.prev_ring = last_on_ring.get(slot)
                    last_on_ring[slot] = op
                else:
                    n += 1
                    op.signal = n
        import contextlib
        with contextlib.ExitStack() as st:
            csem = {e: st.enter_context(nc.semaphore("s_" + e)) for e in ("pe", "act", "dve", "pool")}
            rsem = {}
            for e in ("sp", "pool"):
                for s_ in range(NRING):
                    rsem[(e, s_)] = st.enter_context(nc.semaphore("r_%s_%d" % (e, s_)))
            for e, lst in self.ops.items():
                for op in lst:
                    if op.is_cc and op.ring is not None:
                        rsem[op.ring] = st.enter_context(nc.semaphore("cc_%d" % op.idx))
            block = st.enter_context(nc.Block())

            def run(ename):
                def body(eng):
                    waited = {}
                    lst = self.ops[ename]
                    for op in lst:
                        deps = [d for d in op.deps if not (d.eng == "pe" and ename == "pe" and not d.is_dma)] + list(op.bdeps)
                        if op.is_dma and op.prev_ring is not None:
                            deps.append(op.prev_ring)
                        need = {}
                        for d in deps:
                            if d.is_dma:
                                key = ("r",) + d.ring
                                val = d.ringval
                                sem = rsem[d.ring]
                            else:
                                key = ("c", d.eng)
                                val = d.signal
                                sem = csem[d.eng]
                            assert val > 0, (ename, d.eng)
                            if need.get(key, (0, None))[0] < val:
                                need[key] = (val, sem)
                        for key, (val, sem) in need.items():
                            if waited.get(key, 0) >= val:
                                continue
                            waited[key] = val
                            eng.wait_ge(sem, val)
                        ins = op.fn(eng)
                        if op.is_cc:
                            if op.ring is not None:
                                ins.then_inc(rsem[op.ring], 1)
                        elif op.is_dma:
                            if op.ring is not None:
                                ins.then_inc(rsem[op.ring], 16)
                        elif op.signal:
                            ins.then_inc(csem[ename], 1)
                    if ename == "sp":
                        for d in final_wait_ops:
                            if d.is_dma:
                                eng.wait_ge(rsem[d.ring], d.ringval)
                            else:
                                eng.wait_ge(csem[d.eng], d.signal)
                return body

            block.tensor(run("pe"))
            block.scalar(run("act"))
            block.vector(run("dve"))
            block.gpsimd(run("pool"))
            block.sync(run("sp"))


D = 2048
NT = 1024
NTILE = 8
DFF = 5632
NF = DFF // 128
KD = D // 128
EPS = 1e-6
IN_WIDTH = 4632


def bcast_rows(ap, n, parts=128):
    return bass.AP(ap.tensor, ap.offset, [[0, parts], [1, n]])


class Ctx:
    pass


def build(stop=99, debug=False):
    import contextlib
    nc = bass.Bass("TRN2", target_bir_lowering=False)
    C = Ctx()
    C.nc = nc
    P = Prog(nc)
    C.P = P

    def din(name, shape, dt=F32):
        return nc.dram_tensor(name, list(shape), dt, kind="ExternalInput").ap()

    x = din("x", [NT, D])
    pin = din("p", [NT, 256])
    pos = din("pos", [1, NT], I32)
    WSHAPES = dict([("ff1_pre_g", [1, D]), ("ff1_post_g", [1, D]), ("ff1_w_gate", [D, DFF]), ("ff1_w_up", [D, DFF]),
                    ("ff1_w_down", [DFF, D]), ("mix_pre_g", [1, D]), ("mix_post_g", [1, D]), ("w_in", [D, IN_WIDTH]),
                    ("cmp_pos_k", [32, 128]), ("cmp_pos_v", [32, 128]), ("cmp_k_w1", [4096, 256]), ("cmp_k_w2", [256, 128]),
                    ("cmp_v_w1", [4096, 256]), ("cmp_v_w2", [256, 128]), ("nsa_gate_b", [1, 24]),
                    ("conv_w", [4, 1024]), ("conv_b", [1, 1024]), ("rg_w_a", [8, 128, 128]), ("rg_b_a", [1, 1024]),
                    ("rg_w_i", [8, 128, 128]), ("rg_b_i", [1, 1024]), ("rg_lambda", [1, 1024]),
                    ("attn_out_g", [1, 1024]), ("rec_out_g", [1, 1024]), ("w_out", [D, D]),
                    ("ff2_pre_g", [1, D]), ("ff2_post_g", [1, D]), ("ff2_w_gate", [D, DFF]), ("ff2_w_up", [D, DFF]),
                    ("ff2_w_down", [DFF, D]), ("ple_pre_g", [1, D]), ("ple_post_g", [1, D]),
                    ("w_ple_gate", [D, D]), ("w_ple_proj", [256, D])])

    class LazyW(dict):
        def __missing__(self, nm):
            v = din(nm, WSHAPES[nm])
            self[nm] = v
            return v
    W = LazyW()
    C.W = W
    out = nc.dram_tensor("out", [NT, D], F32, kind="ExternalOutput").ap()
    h1d = nc.dram_tensor("h1d", [NT, D], F32, kind="Internal").ap()
    h2d = nc.dram_tensor("h2d", [NT, D], F32, kind="Internal").ap()
    h3d = nc.dram_tensor("h3d", [NT, D], F32, kind="Internal").ap()
    dbg = {}
    if debug:
        dbg["uT"] = nc.dram_tensor("dbg_uT", [128, KD, NT], BF16, kind="ExternalOutput").ap()

    finals = []
    with contextlib.ExitStack() as st:
        used_names = {}

        def uniq(name):
            n = used_names.get(name, 0)
            used_names[name] = n + 1
            return name if n == 0 else "%s_v%d" % (name, n)

        def sb(name, shape, dt, stack=st):
            return stack.enter_context(nc.sbuf_tensor(uniq(name), list(shape), dt))

        def ps(name, shape, dt, stack=st):
            return stack.enter_context(nc.psum_tensor(uniq(name), list(shape), dt))

        identf = sb("identf", [128, 128], F32)
        ident = sb("ident", [128, 128], BF16)
        AT = sb("AT", [128, KD, NT], BF16)
        P.pool(lambda e: e.memset(identf[:], 1.0), writes=["identf"])
        P.pool(lambda e: e.affine_select(out=identf[:], in_=identf[:], pattern=[[-1, 128]], compare_op=ALU.is_equal,
                                         fill=0.0, base=0, channel_multiplier=1), reads=["identf"], writes=["identf"])
        P.dve(lambda e: e.tensor_copy(out=ident[:], in_=identf[:]), reads=["identf"], writes=["ident"])
        C.ident, C.identf, C.AT = ident, identf, AT
        C.sb, C.ps = sb, ps

        def norm_transpose(S, tg, ht, hkey, gb, gkey, t, scr):
            sq, ss, ub, pT = scr["sq"], scr["ss"], scr["ub"], scr["pT"]
            P.act(lambda e: e.activation(out=sq[:], in_=ht[:], func=AF.Square, accum_out=ss[:, 0:1]),
                  reads=[hkey], writes=[tg + "sq", tg + "ss"])
            P.act(lambda e: e.activation(out=ss[:, 1:2], in_=ss[:, 0:1], func=AF.Sqrt, scale=1.0 / D, bias=scr["eps"][:, 0:1]),
                  reads=[tg + "ss", scr["epskey"]], writes=[tg + "ss1"])
            P.dve(lambda e: e.reciprocal(out=ss[:, 2:3], in_=ss[:, 1:2]), reads=[tg + "ss1"], writes=[tg + "ss2"])
            P.dve(lambda e: e.scalar_tensor_tensor(out=ub[:], in0=ht[:], scalar=ss[:, 2:3], in1=gb[:], op0=ALU.mult, op1=ALU.mult),
                  reads=[hkey, tg + "ss2", gkey], writes=[tg + "ub"])
            for k in range(KD):
                P.pe(lambda e, k=k: e.transpose(out=pT[:, k, :], in_=ub[:, k * 128:(k + 1) * 128], identity=ident[:]),
                     reads=[tg + "ub", "ident"], writes=[tg + "pT"])
            P.act(lambda e: e.copy(out=AT[:, :, t * 128:(t + 1) * 128], in_=pT[:]), reads=[tg + "pT"], writes=[("AT", t)])

        C.norm_transpose = norm_transpose

        def ffn(tg, h_src, pre_g, post_g, wg, wu, wd, h_dst, next_g, first):
            with contextlib.ExitStack() as S:
                hid = sb(tg + "hid", [128, NF, NT], BF16, S)
                epsT = sb(tg + "eps", [128, 1], F32, S)
                P.pool(lambda e: e.memset(epsT[:], EPS), writes=[tg + "eps"])
                if first:
                    with contextlib.ExitStack() as S0:
                        gb = sb(tg + "gb", [128, D], F32, S0)
                        P.dma(lambda e: e.dma_start(out=gb[:], in_=bcast_rows(pre_g, D)), writes=[tg + "gb"])
                        scr = [dict(sq=sb(tg + "sq%d" % i, [128, D], BF16, S0), ss=sb(tg + "ss%d" % i, [128, 4], F32, S0),
                                    ub=sb(tg + "ub%d" % i, [128, D], BF16, S0), pT=ps(tg + "pT%d" % i, [128, KD, 128], BF16, S0),
                                    eps=epsT, epskey=tg + "eps") for i in range(2)]
                        hts = [sb(tg + "ht%d" % i, [128, D], F32, S0) for i in range(2)]
                        for t in range(NTILE):
                            i = t % 2
                            P.dma(lambda e, t=t, i=i: e.dma_start(out=hts[i][:], in_=h_src[t * 128:(t + 1) * 128, :]),
                                  writes=[tg + "ht%d" % i])
                            norm_transpose(S0, tg + "n%d" % i, hts[i], tg + "ht%d" % i, gb, tg + "gb", t, scr[i])
                P.barrier()
                with contextlib.ExitStack() as S1:
                    wgp = [sb(tg + "wgp%d" % i, [128, KD, 512], BF16, S1) for i in range(2)]
                    wup = [sb(tg + "wup%d" % i, [128, KD, 512], BF16, S1) for i in range(2)]
                    sgt = [sb(tg + "sg%d" % i, [128, 512], BF16, S1) for i in range(2)]
                    pg = [[ps(tg + "pg%d%d" % (i, hf), [128, 512], F32, S1) for hf in range(2)] for i in range(2)]
                    pu = [[ps(tg + "pu%d%d" % (i, hf), [128, 512], F32, S1) for hf in range(2)] for i in range(2)]
                    wgv = wg.rearrange("(kc p) m -> p kc m", p=128)
                    wuv = wu.rearrange("(kc p) m -> p kc m", p=128)
                    ATall = [("AT", t) for t in range(NTILE)]
                    for pi in range(NF // 4):
                        b = pi % 2
                        P.dma(lambda e, pi=pi, b=b: e.dma_start(out=wgp[b][:], in_=wgv[:, :, pi * 512:(pi + 1) * 512]),
                              writes=[tg + "wgp%d" % b], q="pool")
                        P.dma(lambda e, pi=pi, b=b: e.dma_start(out=wup[b][:], in_=wuv[:, :, pi * 512:(pi + 1) * 512]),
                              writes=[tg + "wup%d" % b], q="pool")
                        for fl in range(4):
                            f = pi * 4 + fl
                            r = f % 2
                            for hf in range(2):
                                for k in range(KD):
                                    P.pe(lambda e, k=k, hf=hf, r=r, b=b, fl=fl: e.matmul(
                                        pg[r][hf][:], lhsT=wgp[b][:, k, fl * 128:(fl + 1) * 128], rhs=AT[:, k, hf * 512:(hf + 1) * 512],
                                        start=(k == 0), stop=(k == KD - 1)),
                                        reads=[tg + "wgp%d" % b] + ATall[hf * 4:hf * 4 + 4], writes=[tg + "pg%d%d" % (r, hf)])
                                for k in range(KD):
                                    P.pe(lambda e, k=k, hf=hf, r=r, b=b, fl=fl: e.matmul(
                                        pu[r][hf][:], lhsT=wup[b][:, k, fl * 128:(fl + 1) * 128], rhs=AT[:, k, hf * 512:(hf + 1) * 512],
                                        start=(k == 0), stop=(k == KD - 1)),
                                        reads=[tg + "wup%d" % b] + ATall[hf * 4:hf * 4 + 4], writes=[tg + "pu%d%d" % (r, hf)])
                                P.act(lambda e, hf=hf, r=r: e.activation(out=sgt[hf][:], in_=pg[r][hf][:], func=AF.Silu),
                                      reads=[tg + "pg%d%d" % (r, hf)], writes=[tg + "sg%d" % hf])
                                P.dve(lambda e, hf=hf, r=r, f=f: e.tensor_tensor(out=hid[:, f, hf * 512:(hf + 1) * 512], in0=sgt[hf][:],
                                                                               in1=pu[r][hf][:], op=ALU.mult),
                                      reads=[tg + "sg%d" % hf, tg + "pu%d%d" % (r, hf)], writes=[(tg + "hid", f)])
                P.barrier()
                if debug and tg == "f1":
                    dbg["hid"] = nc.dram_tensor("dbg_hid", [128, NF, NT], BF16, kind="ExternalOutput").ap()
                    C.dbg_extra = [P.dma(lambda e: e.dma_start(out=dbg["hid"][:, :, :], in_=hid[:]), reads=[(tg + "hid", f) for f in range(NF)], writes=["dbg_hid"])]
                with contextlib.ExitStack() as S2:
                    NP = 4
                    FP = NF // NP
                    wdp = [sb(tg + "wdp%d" % i, [128, FP, 256], BF16, S2) for i in range(4)]
                    py = [[[ps(tg + "py%d%d%d" % (i, mc, hf), [128, 512], F32, S2) for hf in range(2)] for mc in range(2)] for i in range(2)]
                    wdv = wd.rearrange("(fc p) m -> p fc m", p=128)
                    cnt = 0
                    for cb in range(8):
                        r = cb % 2
                        for pc in range(NP):
                            bi = cnt % 4
                            cnt += 1
                            P.dma(lambda e, cb=cb, pc=pc, bi=bi: e.dma_start(out=wdp[bi][:], in_=wdv[:, pc * FP:(pc + 1) * FP, cb * 256:(cb + 1) * 256]),
                                  writes=[tg + "wdp%d" % bi], q="pool")
                            for mc in range(2):
                                for hf in range(2):
                                    for fi in range(FP):
                                        f = pc * FP + fi
                                        P.pe(lambda e, mc=mc, hf=hf, fi=fi, f=f, bi=bi, r=r: e.matmul(
                                            py[r][mc][hf][:], lhsT=wdp[bi][:, fi, mc * 128:(mc + 1) * 128], rhs=hid[:, f, hf * 512:(hf + 1) * 512],
                                            start=(f == 0), stop=(f == NF - 1)),
                                            reads=[tg + "wdp%d" % bi, (tg + "hid", f)], writes=[tg + "py%d%d%d" % (r, mc, hf)])
                        for mc in range(2):
                            for hf in range(2):
                                m = cb * 2 + mc
                                P.act(lambda e, m=m, mc=mc, hf=hf, r=r: e.copy(out=AT[:, m, hf * 512:(hf + 1) * 512], in_=py[r][mc][hf][:]),
                                      reads=[tg + "py%d%d%d" % (r, mc, hf)], writes=[("AT", hf * 4 + q) for q in range(4)])
            if debug and tg == "f1":
                P.barrier()
                dbg["yT"] = nc.dram_tensor("dbg_yT", [128, KD, NT], BF16, kind="ExternalOutput").ap()
                C.dbg_extra.append(P.dma(lambda e: e.dma_start(out=dbg["yT"][:, :, :], in_=AT[:]), reads=[("AT", t) for t in range(NTILE)], writes=["dbg_yT"]))
            post(tg, AT, h_src, post_g, 0.5, h_dst, next_g)

        def post(tg, YT, h_src, post_g, coef, h_dst, next_g):
            P.barrier()
            with contextlib.ExitStack() as S3:
                epsT = sb(tg + "eps3", [128, 1], F32, S3)
                P.pool(lambda e: e.memset(epsT[:], EPS), writes=[tg + "eps3"])
                gpo = sb(tg + "gpo", [128, D], F32, S3)
                P.dma(lambda e: e.dma_start(out=gpo[:], in_=bcast_rows(post_g, D)), writes=[tg + "gpo"])
                gnx = None
                if next_g is not None:
                    gnx = sb(tg + "gnx", [128, D], F32, S3)
                    P.dma(lambda e: e.dma_start(out=gnx[:], in_=bcast_rows(next_g, D)), writes=[tg + "gnx"])
                scr = [dict(sq=sb(tg + "psq%d" % i, [128, D], BF16, S3), ss=sb(tg + "pss%d" % i, [128, 4], F32, S3),
                            ub=sb(tg + "pub%d" % i, [128, D], BF16, S3), pT=ps(tg + "ppT%d" % i, [128, KD, 128], BF16, S3),
                            eps=epsT, epskey=tg + "eps3") for i in range(2)]
                xt = [sb(tg + "xt%d" % i, [128, D], F32, S3) for i in range(2)]
                tt = [sb(tg + "tt%d" % i, [128, D], F32, S3) for i in range(2)]
                s2 = [sb(tg + "s2%d" % i, [128, 4], F32, S3) for i in range(2)]
                pyT = [ps(tg + "pyT%d" % i, [128, KD, 128], BF16, S3) for i in range(2)]
                ykey = YT.name
                for t in range(NTILE):
                    i = t % 2
                    P.dma(lambda e, t=t, i=i: e.dma_start(out=xt[i][:], in_=h_src[t * 128:(t + 1) * 128, :]),
                          reads=[("hd", h_src.tensor.name, t)], writes=[tg + "xt%d" % i])
                    for m in range(KD):
                        P.pe(lambda e, m=m, t=t, i=i: e.transpose(out=pyT[i][:, m, :], in_=YT[:, m, t * 128:(t + 1) * 128], identity=ident[:]),
                             reads=[(ykey, t), "ident"], writes=[tg + "pyT%d" % i])
                    yv = pyT[i][:].rearrange("p k c -> p (k c)")
                    P.act(lambda e, i=i, yv=yv: e.activation(out=scr[i]["sq"][:], in_=yv, func=AF.Square, accum_out=s2[i][:, 0:1]),
                          reads=[tg + "pyT%d" % i], writes=[tg + "n%dsq" % i, tg + "s2a%d" % i])
                    P.act(lambda e, i=i: e.activation(out=s2[i][:, 1:2], in_=s2[i][:, 0:1], func=AF.Sqrt, scale=1.0 / D, bias=epsT[:, 0:1]),
                          reads=[tg + "s2a%d" % i, tg + "eps3"], writes=[tg + "s2b%d" % i])
                    P.dve(lambda e, i=i: e.reciprocal(out=s2[i][:, 2:3], in_=s2[i][:, 1:2]), reads=[tg + "s2b%d" % i], writes=[tg + "s2c%d" % i])
                    P.dve(lambda e, i=i, yv=yv: e.scalar_tensor_tensor(out=tt[i][:], in0=yv, scalar=s2[i][:, 2:3], in1=gpo[:], op0=ALU.mult, op1=ALU.mult),
                          reads=[tg + "pyT%d" % i, tg + "s2c%d" % i, tg + "gpo"], writes=[tg + "tt%d" % i])
                    P.dve(lambda e, i=i: e.scalar_tensor_tensor(out=xt[i][:], in0=tt[i][:], scalar=float(coef), in1=xt[i][:], op0=ALU.mult, op1=ALU.add),
                          reads=[tg + "tt%d" % i, tg + "xt%d" % i], writes=[tg + "xt%d" % i])
                    o = P.dma(lambda e, t=t, i=i: e.dma_start(out=h_dst[t * 128:(t + 1) * 128, :], in_=xt[i][:]), reads=[tg + "xt%d" % i],
                              writes=[("hd", h_dst.tensor.name, t)])
                    if next_g is not None:
                        norm_transpose(S3, tg + "n%d" % i, xt[i], tg + "xt%d" % i, gnx, tg + "gnx", t, scr[i])
                    else:
                        finals.append(o)
            P.barrier()

        C.post = post
        C.x, C.pin, C.pos, C.out, C.h1d, C.h2d, C.h3d, C.dbg, C.finals = x, pin, pos, out, h1d, h2d, h3d, dbg, finals
        C.bcast_rows = bcast_rows
        C.contextlib = contextlib
        C.stop = stop
        C.debug = debug

        if stop not in (20, 21, 22):
            ffn("f1", x, W["ff1_pre_g"], W["ff1_post_g"], W["ff1_w_gate"], W["ff1_w_up"], W["ff1_w_down"], h1d, W["mix_pre_g"], True)
        if stop <= 1:
            C.dbg_extra = getattr(C, "dbg_extra", [])
            o = P.dma(lambda e: e.dma_start(out=dbg["uT"][:, :, :], in_=AT[:]), reads=[("AT", t) for t in range(NTILE)], writes=["dbg_uT"])
            o2 = P.dma(lambda e: e.dma_start(out=out[:, :], in_=h1d[:, :]), reads=[("hd", "h1d", t) for t in range(NTILE)], writes=["out"])
            P.emit(final_wait_ops=[o, o2] + C.dbg_extra)
            return nc
        if stop in (20, 21, 22):
            with contextlib.ExitStack() as S0:
                epsT = sb("eps0", [128, 1], F32, S0)
                P.pool(lambda e: e.memset(epsT[:], EPS), writes=["eps0"])
                gb = sb("gb0", [128, D], F32, S0)
                P.dma(lambda e: e.dma_start(out=gb[:], in_=bcast_rows(W["mix_pre_g"], D)), writes=["gb0"])
                scr = [dict(sq=sb("sq0%d" % i, [128, D], BF16, S0), ss=sb("ss0%d" % i, [128, 4], F32, S0), ub=sb("ub0%d" % i, [128, D], BF16, S0),
                            pT=ps("pT0%d" % i, [128, KD, 128], BF16, S0), eps=epsT, epskey="eps0") for i in range(2)]
                hts = [sb("ht0%d" % i, [128, D], F32, S0) for i in range(2)]
                for t in range(NTILE):
                    i = t % 2
                    P.dma(lambda e, t=t, i=i: e.dma_start(out=hts[i][:], in_=x[t * 128:(t + 1) * 128, :]), writes=["ht0%d" % i])
                    P.dma(lambda e, t=t, i=i: e.dma_start(out=h1d[t * 128:(t + 1) * 128, :], in_=hts[i][:]), reads=["ht0%d" % i], writes=[("hd", "h1d", t)])
                    norm_transpose(S0, "n0%d" % i, hts[i], "ht0%d" % i, gb, "gb0", t, scr[i])
            P.barrier()
        if mixer(C) == "stop":
            return nc
        if stop <= 2 or stop in (20, 22):
            o2 = P.dma(lambda e: e.dma_start(out=out[:, :], in_=h2d[:, :]), reads=[("hd", "h2d", t) for t in range(NTILE)], writes=["out"])
            P.emit(final_wait_ops=[o2] + C.dbg_ops)
            return nc
        ffn("f2", h2d, None, W["ff2_post_g"], W["ff2_w_gate"], W["ff2_w_up"], W["ff2_w_down"], h3d, W["ple_pre_g"], False)
        ple(C)
        P.emit(final_wait_ops=finals)
    return nc


WNAMES = ["ff1_pre_g", "ff1_post_g", "ff1_w_gate", "ff1_w_up", "ff1_w_down", "mix_pre_g", "mix_post_g", "w_in",
          "cmp_pos_k", "cmp_pos_v", "cmp_k_w1", "cmp_k_w2", "cmp_v_w1", "cmp_v_w2", "nsa_gate_b",
          "conv_w", "conv_b", "rg_w_a", "rg_b_a", "rg_w_i", "rg_b_i", "rg_lambda",
          "attn_out_g", "rec_out_g", "w_out", "ff2_pre_g", "ff2_post_g", "ff2_w_gate", "ff2_w_up", "ff2_w_down",
          "ple_pre_g", "ple_post_g", "w_ple_gate", "w_ple_proj"]


def shard_tokens(a, b, j):
    T = a.shape[0]
    r = a.reshape(T // 512, 4, 128, *a.shape[1:])[:, j]
    return np.ascontiguousarray(r.reshape(T // 4, *a.shape[1:]))


def make_in_maps(inputs):
    shared = {}
    for nm in WNAMES:
        a = np.asarray(inputs[nm], dtype=np.float32)[0]
        if a.ndim == 1:
            a = a.reshape(1, -1)
        if nm == "nsa_gate_b":
            a = a.reshape(1, 24)
        shared[nm] = np.ascontiguousarray(a)
    maps = []
    x = np.asarray(inputs["x"], dtype=np.float32)
    p = np.asarray(inputs["p"], dtype=np.float32)[0]
    positions = np.asarray(inputs["positions"]).astype(np.int32)
    for c in range(8):
        b, j = c // 4, c % 4
        m = dict(shared)
        m["x"] = shard_tokens(x[b], b, j)
        m["p"] = shard_tokens(p[b], b, j)
        m["pos"] = shard_tokens(positions[b], b, j).reshape(1, NT)
        maps.append(m)
    return maps


def unshard(outs):
    res = np.zeros((2, 4096, D), dtype=np.float32)
    for c in range(8):
        b, j = c // 4, c % 4
        res[b].reshape(8, 4, 128, D)[:, j] = np.asarray(outs[c]).reshape(8, 128, D)
    return res


BIGNEG = -30000.0
SCALE = 128 ** -0.5
RG = [[0, 1, 2, 3], [4, 5, 6, 7]]
GROWS = 12 * 128


def mixer(C):
    nc, P, W, sb, ps, AT, ident, identf = C.nc, C.P, C.W, C.sb, C.ps, C.AT, C.ident, C.identf
    contextlib = C.contextlib
    C.dbg_ops = []

    def cin(name, shape, dt=F32):
        return nc.dram_tensor(name, list(shape), dt, kind="ExternalInput").ap()

    c_rope = cin("c_rope", [128, 4])
    c_perm = cin("c_perm", [128, 128])
    c_oh = cin("c_oh", [128, 4])
    c_cmpm = cin("c_cmpm", [128, 2 * 8 * 128], BF16)
    c_selcm = cin("c_selcm", [128, 4 * 128], BF16)
    c_winm = cin("c_winm", [128, 8 * 128], BF16)
    c_selA = cin("c_selA", [128, 8 * 64])
    c_selB = cin("c_selB", [128, 8 * 64])
    c_selF = cin("c_selF", [128, 8 * 64])
    c_E = cin("c_E", [64, 4096], BF16)
    c_ovl = cin("c_ovl", [128, 2 * 64])
    c_selmat = cin("c_selmat", [24, 24 * 128])

    gin = nc.dram_tensor("gin", [GROWS, NT], BF16, kind="Internal").ap()
    gbuf = nc.dram_tensor("gbuf", [4 * GROWS, NT], BF16, kind="Internal").ap()
    hin = nc.dram_tensor("hin", [128, 192], F32, kind="Internal").ap()
    hbuf = nc.dram_tensor("hbuf", [4 * 128, 192], F32, kind="Internal").ap()
    sin_ = nc.dram_tensor("sin", [128, 128], F32, kind="Internal").ap()
    sbuf_ = nc.dram_tensor("sbuf", [4 * 128, 128], F32, kind="Internal").ap()

    def load(dst, src, key, q="sp", reads=()):
        return P.dma(lambda e: e.dma_start(out=dst, in_=src), reads=list(reads), writes=[key], q=q)

    with contextlib.ExitStack() as SM:
        qT = sb("qT", [128, 8, NT], BF16, SM)
        gT = sb("gT", [24, NT], F32, SM)
        orecT = sb("orecT", [128, 8, NT], BF16, SM)
        ones_bf = sb("ones_bf", [128, 128], BF16, SM)
        ones_f = sb("ones_f", [128, 128], F32, SM)
        onec = sb("onec", [128, 1], F32, SM)
        epsT = sb("m_eps", [128, 1], F32, SM)
        oh = sb("oh", [128, 4], F32, SM)
        P.pool(lambda e: e.memset(ones_bf[:], 1.0), writes=["ones_bf"])
        P.pool(lambda e: e.memset(ones_f[:], 1.0), writes=["ones_f"])
        P.pool(lambda e: e.memset(onec[:], 1.0), writes=["onec"])
        P.pool(lambda e: e.memset(epsT[:], EPS), writes=["m_eps"])
        load(oh[:], c_oh[:, :], "oh")
        C.ones_bf, C.ones_f, C.epsT = ones_bf, ones_f, epsT
        ATall = [("AT", t) for t in range(NTILE)]

        with contextlib.ExitStack() as S2:
            ZX = sb("ZX", [128, 8, NT], F32, S2)
            gy = sb("gy", [128, 8, NT], BF16, S2)
            with contextlib.ExitStack() as S2a:
                rope = sb("rope", [128, 4], F32, S2a)
                perm = sb("perm", [128, 128], F32, S2a)
                posi = sb("posi", [128, NT], I32, S2a)
                ang = sb("ang", [128, NT], F32, S2a)
                kf = sb("kf", [128, NT], F32, S2a)
                ki = sb("ki", [128, NT], I32, S2a)
                Ct = sb("Ct", [128, NT], F32, S2a)
                St = sb("St", [128, NT], F32, S2a)
                gbias = sb("gbias", [24, 1], F32, S2a)
                load(rope[:], c_rope[:, :], "rope")
                load(perm[:], c_perm[:, :], "perm")
                load(posi[:], C.bcast_rows(C.pos, NT), "posi")
                with nc.allow_non_contiguous_dma(reason="tiny param"):
                    P.dma(lambda e: e.dma_start(out=gbias[:], in_=W["nsa_gate_b"].rearrange("o n -> n o"), allow_slow_non_contiguous=True), writes=["gbias"])
                P.dve(lambda e: e.tensor_copy(out=ang[:], in_=posi[:]), reads=["posi"], writes=["ang"])
                P.dve(lambda e: e.tensor_scalar(out=ang[:], in0=ang[:], scalar1=rope[:, 0:1], scalar2=None, op0=ALU.mult), reads=["ang", "rope"], writes=["ang"])
                TWO_PI = 6.283185307179586
                C1 = 6.28125
                C2 = TWO_PI - C1
                P.dve(lambda e: e.tensor_scalar(out=kf[:], in0=ang[:], scalar1=1.0 / TWO_PI, scalar2=None, op0=ALU.mult), reads=["ang"], writes=["kf"])
                P.dve(lambda e: e.tensor_copy(out=ki[:], in_=kf[:]), reads=["kf"], writes=["ki"])
                P.dve(lambda e: e.tensor_copy(out=kf[:], in_=ki[:]), reads=["ki"], writes=["kf"])
                P.dve(lambda e: e.scalar_tensor_tensor(out=ang[:], in0=kf[:], scalar=-C1, in1=ang[:], op0=ALU.mult, op1=ALU.add), reads=["kf", "ang"], writes=["ang"])
                P.dve(lambda e: e.scalar_tensor_tensor(out=ang[:], in0=kf[:], scalar=-C2, in1=ang[:], op0=ALU.mult, op1=ALU.add), reads=["kf", "ang"], writes=["ang"])
                P.dve(lambda e: e.tensor_scalar(out=kf[:], in0=ang[:], scalar1=3.141592653589793, scalar2=-TWO_PI, op0=ALU.is_gt, op1=ALU.mult), reads=["ang"], writes=["kf"])
                P.dve(lambda e: e.tensor_tensor(out=ang[:], in0=ang[:], in1=kf[:], op=ALU.add), reads=["ang", "kf"], writes=["ang"])
                P.dve(lambda e: e.tensor_scalar(out=kf[:], in0=ang[:], scalar1=-3.141592653589793, scalar2=TWO_PI, op0=ALU.is_lt, op1=ALU.mult), reads=["ang"], writes=["kf"])
                P.dve(lambda e: e.tensor_tensor(out=ang[:], in0=ang[:], in1=kf[:], op=ALU.add), reads=["ang", "kf"], writes=["ang"])
                P.act(lambda e: e.activation(out=St[:], in_=ang[:], func=AF.Sin), reads=["ang"], writes=["St"])
                P.dve(lambda e: e.tensor_scalar(out=St[:], in0=St[:], scalar1=rope[:, 1:2], scalar2=None, op0=ALU.mult), reads=["St", "rope"], writes=["St"])
                P.act(lambda e: e.activation(out=kf[:], in_=ang[:], func=AF.Abs), reads=["ang"], writes=["kf"])
                P.dve(lambda e: e.tensor_scalar(out=kf[:], in0=kf[:], scalar1=-1.0, scalar2=1.5707963267948966, op0=ALU.mult, op1=ALU.add), reads=["kf"], writes=["kf"])
                P.act(lambda e: e.activation(out=Ct[:], in_=kf[:], func=AF.Sin), reads=["kf"], writes=["Ct"])

                wpan = [sb("wpan%d" % i, [128, KD, 512], BF16, S2a) for i in range(2)]
                zf = [sb("zf%d" % i, [128, NT], F32, S2a) for i in range(2)]
                t1 = [sb("t1%d" % i, [128, 512], F32, S2a) for i in range(2)]
                stg = [sb("stg%d" % i, [128, NT], BF16, S2a) for i in range(2)]
                pz = [[ps("pz%d%d" % (i, hf), [128, 512], F32, S2a) for hf in range(2)] for i in range(2)]
                psw = [ps("psw%d" % i, [128, 512], F32, S2a) for i in range(2)]
                pvt = [ps("pvt%d" % i, [128, 8, 128], BF16, S2a) for i in range(2)]
                wv = W["w_in"].rearrange("(kc p) m -> p kc m", p=128)
                panels = [(0, 512), (512, 512), (1024, 512), (1536, 512), (2048, 512), (2560, 24), (2584, 512), (3096, 512), (3608, 512), (4120, 512)]
                kinds = {}
                for h in range(8):
                    kinds[h * 128] = ("q", h)
                for g in range(2):
                    kinds[1024 + g * 128] = ("kf", 0 + g)
                    kinds[1280 + g * 128] = ("vf", 2 + g)
                    kinds[1536 + g * 128] = ("kf", 4 + g)
                    kinds[1792 + g * 128] = ("vt", 8 + g)
                    kinds[2048 + g * 128] = ("kf", 6 + g)
                    kinds[2304 + g * 128] = ("vt", 10 + g)
                kinds[2560] = ("g", 0)
                for h in range(8):
                    kinds[2584 + h * 128] = ("zx", h)
                    kinds[3608 + h * 128] = ("zy", h)
                cnt = 0
                for pi, (c0, wdt) in enumerate(panels):
                    b = pi % 2
                    P.dma(lambda e, b=b, c0=c0, wdt=wdt: e.dma_start(out=wpan[b][:, :, 0:wdt], in_=wv[:, :, c0:c0 + wdt]), writes=["wpan%d" % b], q="pool")
                    for ci in range(max(1, wdt // 128)):
                        col = c0 + ci * 128
                        kind, idx = kinds[col]
                        cw = 24 if kind == "g" else 128
                        r = cnt % 2
                        cnt += 1
                        for hf in range(2):
                            for k in range(KD):
                                P.pe(lambda e, k=k, hf=hf, r=r, b=b, ci=ci, cw=cw: e.matmul(
                                    pz[r][hf][0:cw, :], lhsT=wpan[b][:, k, ci * 128:ci * 128 + cw], rhs=AT[:, k, hf * 512:(hf + 1) * 512],
                                    start=(k == 0), stop=(k == KD - 1)), reads=["wpan%d" % b] + ATall[hf * 4:hf * 4 + 4], writes=["pz%d%d" % (r, hf)])
                        pzk = ["pz%d%d" % (r, 0), "pz%d%d" % (r, 1)]
                        if kind == "g":
                            for hf in range(2):
                                P.act(lambda e, hf=hf, r=r: e.activation(out=gT[:, hf * 512:(hf + 1) * 512], in_=pz[r][hf][0:24, :], func=AF.Sigmoid, bias=gbias[:, 0:1]),
                                      reads=[pzk[hf], "gbias"], writes=["gT"])
                        elif kind == "zx":
                            for hf in range(2):
                                P.act(lambda e, hf=hf, r=r, idx=idx: e.copy(out=ZX[:, idx, hf * 512:(hf + 1) * 512], in_=pz[r][hf][:]),
                                      reads=[pzk[hf]], writes=[("ZX", idx)])
                        elif kind == "zy":
                            for hf in range(2):
                                sl = slice(hf * 512, (hf + 1) * 512)
                                P.act(lambda e, hf=hf, r=r, sl=sl: e.activation(out=zf[r][:, sl], in_=pz[r][hf][:], func=AF.Square), reads=[pzk[hf]], writes=[("zf%d" % r, hf)])
                                P.dve(lambda e, r=r, sl=sl, hf=hf: e.tensor_scalar(out=zf[r][:, sl], in0=zf[r][:, sl], scalar1=0.044715, scalar2=1.0, op0=ALU.mult, op1=ALU.add), reads=[("zf%d" % r, hf)], writes=[("zf%d" % r, hf)])
                                P.dve(lambda e, hf=hf, r=r, sl=sl: e.tensor_tensor(out=zf[r][:, sl], in0=zf[r][:, sl], in1=pz[r][hf][:], op=ALU.mult), reads=[("zf%d" % r, hf), pzk[hf]], writes=[("zf%d" % r, hf)])
                                P.act(lambda e, r=r, sl=sl, hf=hf: e.activation(out=zf[r][:, sl], in_=zf[r][:, sl], func=AF.Sigmoid, scale=1.5957691216057308), reads=[("zf%d" % r, hf)], writes=[("zf%d" % r, hf)])
                                P.dve(lambda e, hf=hf, r=r, sl=sl, idx=idx: e.tensor_tensor(out=gy[:, idx, sl], in0=zf[r][:, sl], in1=pz[r][hf][:], op=ALU.mult), reads=[("zf%d" % r, hf), pzk[hf]], writes=[("gy", idx)])
                        else:
                            for hf in range(2):
                                P.act(lambda e, hf=hf, r=r: e.copy(out=zf[r][:, hf * 512:(hf + 1) * 512], in_=pz[r][hf][:]), reads=[pzk[hf]], writes=[("zf%d" % r, hf)])
                            if kind in ("q", "kf"):
                                dst = qT[:, idx, :] if kind == "q" else stg[r][:]
                                dkey = ("qT", idx) if kind == "q" else "stg%d" % r
                                for hf in range(2):
                                    sl = slice(hf * 512, (hf + 1) * 512)
                                    P.pe(lambda e, hf=hf, r=r, sl=sl: e.matmul(psw[hf][:], lhsT=perm[:], rhs=zf[r][:, sl], start=True, stop=True),
                                         reads=["perm", ("zf%d" % r, hf)], writes=["psw%d" % hf])
                                    P.dve(lambda e, hf=hf, r=r, sl=sl: e.tensor_tensor(out=t1[hf][:], in0=zf[r][:, sl], in1=Ct[:, sl], op=ALU.mult),
                                          reads=[("zf%d" % r, hf), "Ct"], writes=["t1%d" % hf])
                                    P.dve(lambda e, hf=hf, r=r, sl=sl: e.tensor_tensor(out=zf[r][:, sl], in0=psw[hf][:], in1=St[:, sl], op=ALU.mult),
                                          reads=["psw%d" % hf, "St", ("zf%d" % r, hf)], writes=[("zf%d" % r, hf)])
                                    P.dve(lambda e, hf=hf, r=r, sl=sl, dst=dst: e.tensor_tensor(out=dst[:, sl], in0=zf[r][:, sl], in1=t1[hf][:], op=ALU.add),
                                          reads=[("zf%d" % r, hf), "t1%d" % hf], writes=[dkey])
                                if kind == "kf":
                                    P.dma(lambda e, r=r, idx=idx: e.dma_start(out=gin[idx * 128:(idx + 1) * 128, :], in_=stg[r][:]), reads=["stg%d" % r], writes=[("gin", idx)])
                            elif kind == "vf":
                                P.dve(lambda e, r=r: e.tensor_copy(out=stg[r][:], in_=zf[r][:]), reads=[("zf%d" % r, 0), ("zf%d" % r, 1)], writes=["stg%d" % r])
                                P.dma(lambda e, r=r, idx=idx: e.dma_start(out=gin[idx * 128:(idx + 1) * 128, :], in_=stg[r][:]), reads=["stg%d" % r], writes=[("gin", idx)])
                            elif kind == "vt":
                                P.dve(lambda e, r=r: e.tensor_copy(out=stg[r][:], in_=zf[r][:]), reads=[("zf%d" % r, 0), ("zf%d" % r, 1)], writes=["stg%d" % r])
                                for l in range(8):
                                    P.pe(lambda e, r=r, l=l: e.transpose(out=pvt[r][:, l, :], in_=stg[r][:, l * 128:(l + 1) * 128], identity=ident[:]),
                                         reads=["stg%d" % r, "ident"], writes=["pvt%d" % r])
                                P.act(lambda e, r=r: e.copy(out=stg[r][:], in_=pvt[r][:].rearrange("p l d -> p (l d)")), reads=["pvt%d" % r], writes=["stg%d" % r])
                                P.dma(lambda e, r=r, idx=idx: e.dma_start(out=gin[idx * 128:(idx + 1) * 128, :], in_=stg[r][:]), reads=["stg%d" % r], writes=[("gin", idx)])
            P.barrier()
            with contextlib.ExitStack() as S2b:
                hst = sb("hst", [128, 8, 8, 3], F32, S2b)
                for blk in range(8):
                    P.dve(lambda e, blk=blk: e.tensor_copy(out=hst[:, blk, :, :], in_=ZX[:, blk, :].rearrange("p (l t) -> p l t", t=128)[:, :, 125:128]),
                          reads=[("ZX", blk)], writes=["hst"])
                P.dma(lambda e: e.dma_start(out=hin[:, :], in_=hst[:].rearrange("p b l t -> p (b l t)")), reads=["hst"], writes=["hin"])
                cc2 = P.cc(lambda e: e.collective_compute("AllGather", ALU.bypass, replica_groups=RG, ins=[hin[:, :]], outs=[hbuf[:, :]]), reads=["hin"], writes=["hbuf"])
                for kq in range(4):
                    P.cc(lambda e, kq=kq: e.collective_compute("AllGather", ALU.bypass, replica_groups=RG, ins=[gin[kq * 384:(kq + 1) * 384, :]], outs=[gbuf[kq * 1536:(kq + 1) * 1536, :]]),
                         reads=[("gin", it) for it in range(3 * kq, 3 * kq + 3)] + ["hin", "hbuf"], writes=[("gbuf", kq)])
            P.barrier()
            if C.stop == 21:
                C.dbg["qT"] = nc.dram_tensor("dbg_qT", [128, 8, NT], BF16, kind="ExternalOutput").ap()
                C.dbg["gT"] = nc.dram_tensor("dbg_gT", [24, NT], F32, kind="ExternalOutput").ap()
                C.dbg["gbuf"] = nc.dram_tensor("dbg_gbuf", [4 * GROWS, NT], BF16, kind="ExternalOutput").ap()
                C.dbg["ZX"] = nc.dram_tensor("dbg_ZX", [128, 8, NT], F32, kind="ExternalOutput").ap()
                C.dbg["gy"] = nc.dram_tensor("dbg_gy", [128, 8, NT], BF16, kind="ExternalOutput").ap()
                C.dbg_ops.append(P.dma(lambda e: e.dma_start(out=C.dbg["qT"][:, :, :], in_=qT[:]), reads=[("qT", h) for h in range(8)], writes=["d1"]))
                C.dbg_ops.append(P.dma(lambda e: e.dma_start(out=C.dbg["gT"][:, :], in_=gT[:]), reads=["gT"], writes=["d2"]))
                C.dbg_ops.append(P.dma(lambda e: e.dma_start(out=C.dbg["gbuf"][:, :], in_=gbuf[:, :]), reads=[("gbuf", it) for it in range(4)], writes=["d3"]))
                C.dbg_ops.append(P.dma(lambda e: e.dma_start(out=C.dbg["ZX"][:, :, :], in_=ZX[:]), reads=[("ZX", h) for h in range(8)], writes=["d4"]))
                C.dbg_ops.append(P.dma(lambda e: e.dma_start(out=C.dbg["gy"][:, :, :], in_=gy[:]), reads=[("gy", h) for h in range(8)], writes=["d5"]))
                P.emit(final_wait_ops=C.dbg_ops)
                return "stop"
            lru(C, SM, ZX, gy, orecT, hbuf, sin_, sbuf_, oh, onec)
        P.barrier()
        attention(C, SM, qT, gT, orecT, gbuf, ones_bf, ones_f, epsT,
                  dict(cmpm=c_cmpm, selcm=c_selcm, winm=c_winm, selA=c_selA, selB=c_selB, selF=c_selF, E=c_E, ovl=c_ovl, selmat=c_selmat))
    P.barrier()
    if C.stop == 22:
        C.dbg["cat"] = nc.dram_tensor("dbg_cat", [128, KD, NT], BF16, kind="ExternalOutput").ap()
        C.dbg_ops.append(P.dma(lambda e: e.dma_start(out=C.dbg["cat"][:, :, :], in_=AT[:]), reads=[("AT", t) for t in range(NTILE)], writes=["dcat"]))
        P.barrier()
    outproj(C)


def lru(C, SM, ZX, gy, orecT, hbuf, sin_, sbuf_, oh, onec):
    nc, P, W, sb, ps = C.nc, C.P, C.W, C.sb, C.ps
    contextlib = C.contextlib
    ones_bf, epsT = C.ones_bf, C.epsT
    with contextlib.ExitStack() as SL:
        Pc = sb("Pc", [128, 8, NT], F32, SL)
        cw = sb("cw", [128, 4, 8], F32, SL)
        cb = sb("cb", [128, 8], F32, SL)
        ba = sb("ba", [128, 8], F32, SL)
        bi = sb("bi", [128, 8], F32, SL)
        lam = sb("lam", [128, 8], F32, SL)
        rg = sb("rg", [128, 8], F32, SL)
        cch = sb("cch", [128, 8], F32, SL)
        wa = sb("wa", [128, 8, 128], BF16, SL)
        wi = sb("wi", [128, 8, 128], BF16, SL)
        G2 = sb("G2", [128, 4, 192], F32, SL)
        halo = sb("halo", [128, 8, 8, 3], F32, SL)
        zeros = sb("zeros", [128, 128], F32, SL)
        P.dve(lambda e: e.memset(zeros[:], 0.0), writes=["zeros"])
        with nc.allow_non_contiguous_dma(reason="tiny params"):
            for w_ in range(4):
                P.dma(lambda e, w_=w_: e.dma_start(out=cw[:, w_, :], in_=W["conv_w"][w_:w_ + 1, :].rearrange("o (b p) -> p (o b)", p=128), allow_slow_non_contiguous=True), writes=[("cw", w_)])
            for t_, nm in [(cb, "conv_b"), (ba, "rg_b_a"), (bi, "rg_b_i"), (lam, "rg_lambda"), (rg, "rec_out_g")]:
                P.dma(lambda e, t_=t_, nm=nm: e.dma_start(out=t_[:], in_=W[nm].rearrange("o (b p) -> p (o b)", p=128), allow_slow_non_contiguous=True), writes=[nm])
        wstage = sb("wstage", [128, 8, 128], F32, SL)
        P.dma(lambda e: e.dma_start(out=wstage[:], in_=W["rg_w_a"].rearrange("b i j -> i b j")), writes=["wstage"])
        P.act(lambda e: e.copy(out=wa[:], in_=wstage[:]), reads=["wstage"], writes=["wa"])
        P.dma(lambda e: e.dma_start(out=wstage[:], in_=W["rg_w_i"].rearrange("b i j -> i b j")), writes=["wstage"])
        P.act(lambda e: e.copy(out=wi[:], in_=wstage[:]), reads=["wstage"], writes=["wi"])
        P.dma(lambda e: e.dma_start(out=G2[:], in_=hbuf.rearrange("(r p) c -> p r c", p=128)), reads=["hbuf"], writes=["G2"])
        P.act(lambda e: e.activation(out=cch[:], in_=lam[:], func=AF.Sigmoid), reads=["rg_lambda"], writes=["cch"])
        P.act(lambda e: e.activation(out=cch[:], in_=cch[:], func=AF.Ln), reads=["cch"], writes=["cch"])
        P.dve(lambda e: e.tensor_scalar(out=cch[:], in0=cch[:], scalar1=8.0, scalar2=None, op0=ALU.mult), reads=["cch"], writes=["cch"])
        hv = halo[:].rearrange("p b l t -> p (b l t)")
        P.dve(lambda e: e.tensor_scalar(out=hv, in0=G2[:, 0, :], scalar1=oh[:, 1:2], scalar2=None, op0=ALU.mult), reads=["G2", "oh"], writes=["halo"])
        P.dve(lambda e: e.scalar_tensor_tensor(out=hv, in0=G2[:, 1, :], scalar=oh[:, 2:3], in1=hv, op0=ALU.mult, op1=ALU.add), reads=["G2", "oh", "halo"], writes=["halo"])
        P.dve(lambda e: e.scalar_tensor_tensor(out=hv, in0=G2[:, 2, :], scalar=oh[:, 3:4], in1=hv, op0=ALU.mult, op1=ALU.add), reads=["G2", "oh", "halo"], writes=["halo"])
        g3 = G2[:, 3, :].rearrange("p (b l t) -> p b l t", b=8, l=8)
        P.dve(lambda e: e.scalar_tensor_tensor(out=halo[:, :, 1:8, :], in0=g3[:, :, 0:7, :], scalar=oh[:, 0:1], in1=halo[:, :, 1:8, :], op0=ALU.mult, op1=ALU.add),
              reads=["G2", "oh", "halo"], writes=["halo"])
        with contextlib.ExitStack() as SB:
            NB = 2
            xpad = [sb("xpad%d" % i, [128, 8, 131], F32, SB) for i in range(NB)]
            xc = [sb("xc%d" % i, [128, 8, 128], F32, SB) for i in range(NB)]
            xcb = [sb("xcb%d" % i, [128, NT], BF16, SB) for i in range(NB)]
            rr = [sb("rr%d" % i, [128, NT], F32, SB) for i in range(NB)]
            ig0 = sb("ig0", [128, NT], F32, SB)
            ig = [ig0, ig0]
            aa = [sb("aa%d" % i, [128, NT], F32, SB) for i in range(NB)]
            uu0 = sb("uu0", [128, NT], F32, SB)
            uu = [uu0, uu0]
            pr = [[ps("pr%d%d" % (i, hf), [128, 512], F32, SB) for hf in range(2)] for i in range(NB)]
            pg = [[ps("pi%d%d" % (i, hf), [128, 512], F32, SB) for hf in range(2)] for i in range(NB)]
            for blk in range(8):
                i = blk % NB
                k = lambda s: "%s%d" % (s, i)
                P.act(lambda e, i=i, blk=blk: e.copy(out=xpad[i][:, :, 0:3], in_=halo[:, blk, :, :]), reads=["halo"], writes=[k("xpad")])
                P.act(lambda e, i=i, blk=blk: e.copy(out=xpad[i][:, :, 3:131], in_=ZX[:, blk, :].rearrange("p (l t) -> p l t", t=128)), reads=[("ZX", blk)], writes=[k("xpad")])
                P.dve(lambda e, i=i, blk=blk: e.tensor_scalar(out=xc[i][:], in0=xpad[i][:, :, 0:128], scalar1=cw[:, 0, blk:blk + 1], scalar2=cb[:, blk:blk + 1], op0=ALU.mult, op1=ALU.add),
                      reads=[k("xpad"), ("cw", 0), "conv_b"], writes=[k("xc")])
                for w in range(1, 4):
                    P.dve(lambda e, i=i, blk=blk, w=w: e.scalar_tensor_tensor(out=xc[i][:], in0=xpad[i][:, :, w:w + 128], scalar=cw[:, w, blk:blk + 1], in1=xc[i][:], op0=ALU.mult, op1=ALU.add),
                          reads=[k("xpad"), ("cw", w), k("xc")], writes=[k("xc")])
                xcf = xc[i][:].rearrange("p l t -> p (l t)")
                P.act(lambda e, i=i, xcf=xcf: e.copy(out=xcb[i][:], in_=xcf), reads=[k("xc")], writes=[k("xcb")])
                for hf in range(2):
                    sl = slice(hf * 512, (hf + 1) * 512)
                    P.pe(lambda e, i=i, blk=blk, hf=hf, sl=sl: e.matmul(pr[i][hf][:], lhsT=wa[:, blk, :], rhs=xcb[i][:, sl], start=True, stop=True), reads=["wa", k("xcb")], writes=["pr%d%d" % (i, hf)])
                    P.pe(lambda e, i=i, blk=blk, hf=hf, sl=sl: e.matmul(pg[i][hf][:], lhsT=wi[:, blk, :], rhs=xcb[i][:, sl], start=True, stop=True), reads=["wi", k("xcb")], writes=["pi%d%d" % (i, hf)])
                    P.act(lambda e, i=i, blk=blk, hf=hf, sl=sl: e.activation(out=rr[i][:, sl], in_=pr[i][hf][:], func=AF.Sigmoid, bias=ba[:, blk:blk + 1]), reads=["pr%d%d" % (i, hf), "rg_b_a"], writes=[k("rr")])
                    P.act(lambda e, i=i, blk=blk, hf=hf, sl=sl: e.activation(out=ig[i][:, sl], in_=pg[i][hf][:], func=AF.Sigmoid, bias=bi[:, blk:blk + 1]), reads=["pi%d%d" % (i, hf), "rg_b_i"], writes=["ig0"])
                P.act(lambda e, i=i, blk=blk: e.activation(out=aa[i][:], in_=rr[i][:], func=AF.Exp, scale=cch[:, blk:blk + 1]), reads=[k("rr"), "cch"], writes=[k("aa")])
                P.dve(lambda e, i=i: e.tensor_tensor(out=uu[i][:], in0=aa[i][:], in1=aa[i][:], op=ALU.mult), reads=[k("aa")], writes=["uu0"])
                P.act(lambda e, i=i: e.activation(out=uu[i][:], in_=uu[i][:], func=AF.Sqrt, scale=-1.0, bias=onec[:, 0:1]), reads=["uu0", "onec"], writes=["uu0"])
                P.dve(lambda e, i=i: e.tensor_tensor(out=uu[i][:], in0=uu[i][:], in1=ig[i][:], op=ALU.mult), reads=["uu0", "ig0"], writes=["uu0"])
                P.dve(lambda e, i=i, xcf=xcf: e.tensor_tensor(out=uu[i][:], in0=uu[i][:], in1=xcf, op=ALU.mult), reads=["uu0", k("xc")], writes=["uu0"])
                for l in range(8):
                    sl = slice(l * 128, (l + 1) * 128)
                    P.dve(lambda e, i=i, blk=blk, sl=sl: e.tensor_tensor_scan(out=ZX[:, blk, sl], data0=aa[i][:, sl], data1=uu[i][:, sl], initial=0.0, op0=ALU.mult, op1=ALU.add),
                          reads=[k("aa"), "uu0", k("xpad")], writes=[("ZX", blk)])
                    P.dve(lambda e, i=i, blk=blk, sl=sl: e.tensor_tensor_scan(out=Pc[:, blk, sl], data0=aa[i][:, sl], data1=zeros[:], initial=1.0, op0=ALU.mult, op1=ALU.add),
                          reads=[k("aa"), "zeros"], writes=[("Pc", blk)])
        P.barrier()
        with contextlib.ExitStack() as SC:
            sst = sb("sst", [128, 8, 8, 2], F32, SC)
            SG = sb("SG", [128, 4, 128], F32, SC)
            Aall = sb("Aall", [128, 8, 32], F32, SC)
            Hall = sb("Hall", [128, 8, 32], F32, SC)
            Spad = sb("Spad", [128, 8, 33], F32, SC)
            hinit = sb("hinit", [128, 8, 8], F32, SC)
            rstdb = sb("rstdb", [128, NT], F32, SC)
            sqb = [sb("sqb%d" % i, [128, NT], BF16, SC) for i in range(2)]
            pss = [ps("pss%d" % hf, [128, 512], F32, SC) for hf in range(2)]
            allZX = [("ZX", b) for b in range(8)]
            allPc = [("Pc", b) for b in range(8)]
            P.dve(lambda e: e.tensor_copy(out=sst[:, :, :, 0], in_=Pc[:].rearrange("p b (l t) -> p b l t", t=128)[:, :, :, 127]), reads=allPc, writes=["sst"])
            P.dve(lambda e: e.tensor_copy(out=sst[:, :, :, 1], in_=ZX[:].rearrange("p b (l t) -> p b l t", t=128)[:, :, :, 127]), reads=allZX, writes=["sst"])
            P.dma(lambda e: e.dma_start(out=sin_[:, :], in_=sst[:].rearrange("p b l t -> p (b l t)")), reads=["sst"], writes=["sin"])
            P.cc(lambda e: e.collective_compute("AllGather", ALU.bypass, replica_groups=RG, ins=[sin_[:, :]], outs=[sbuf_[:, :]]), reads=["sin"], writes=["sbufd"])
            P.dma(lambda e: e.dma_start(out=SG[:], in_=sbuf_.rearrange("(r p) c -> p r c", p=128)), reads=["sbufd"], writes=["SG"])
            for j in range(4):
                sgv = SG[:, j, :].rearrange("p (b l t) -> p b l t", b=8, l=8)
                P.dve(lambda e, j=j, sgv=sgv: e.tensor_copy(out=Aall[:, :, j:j + 29:4], in_=sgv[:, :, :, 0]), reads=["SG"], writes=["Aall"])
                P.dve(lambda e, j=j, sgv=sgv: e.tensor_copy(out=Hall[:, :, j:j + 29:4], in_=sgv[:, :, :, 1]), reads=["SG"], writes=["Hall"])
            P.pool(lambda e: e.memset(Spad[:], 0.0), writes=["Spad"])
            for blk in range(8):
                P.dve(lambda e, blk=blk: e.tensor_tensor_scan(out=Spad[:, blk, 1:33], data0=Aall[:, blk, :], data1=Hall[:, blk, :], initial=0.0, op0=ALU.mult, op1=ALU.add),
                      reads=["Aall", "Hall", "Spad"], writes=["Spad"])
            P.dve(lambda e: e.tensor_scalar(out=hinit[:], in0=Spad[:, :, 0:29:4], scalar1=oh[:, 0:1], scalar2=None, op0=ALU.mult), reads=["Spad", "oh"], writes=["hinit"])
            for m in range(1, 4):
                P.dve(lambda e, m=m: e.scalar_tensor_tensor(out=hinit[:], in0=Spad[:, :, m:m + 29:4], scalar=oh[:, m:m + 1], in1=hinit[:], op0=ALU.mult, op1=ALU.add),
                      reads=["Spad", "oh", "hinit"], writes=["hinit"])
            for blk in range(8):
                for l in range(8):
                    sl = slice(l * 128, (l + 1) * 128)
                    P.dve(lambda e, blk=blk, l=l, sl=sl: e.scalar_tensor_tensor(out=ZX[:, blk, sl], in0=Pc[:, blk, sl], scalar=hinit[:, blk, l:l + 1], in1=ZX[:, blk, sl], op0=ALU.mult, op1=ALU.add),
                          reads=[("Pc", blk), "hinit", ("ZX", blk)], writes=[("ZX", blk)])
                P.dve(lambda e, blk=blk: e.tensor_tensor(out=ZX[:, blk, :], in0=ZX[:, blk, :], in1=gy[:, blk, :], op=ALU.mult), reads=[("ZX", blk), ("gy", blk)], writes=[("ZX", blk)])
                i = blk % 2
                P.act(lambda e, blk=blk, i=i: e.activation(out=sqb[i][:], in_=ZX[:, blk, :], func=AF.Square), reads=[("ZX", blk)], writes=["sqb%d" % i])
                for hf in range(2):
                    P.pe(lambda e, blk=blk, i=i, hf=hf: e.matmul(pss[hf][:], lhsT=ones_bf[:], rhs=sqb[i][:, hf * 512:(hf + 1) * 512], start=(blk == 0), stop=(blk == 7)),
                         reads=["ones_bf", "sqb%d" % i], writes=["pss%d" % hf])
            for hf in range(2):
                sl = slice(hf * 512, (hf + 1) * 512)
                P.act(lambda e, hf=hf, sl=sl: e.activation(out=rstdb[:, sl], in_=pss[hf][:], func=AF.Sqrt, scale=1.0 / 1024, bias=epsT[:, 0:1]), reads=["pss%d" % hf, "m_eps"], writes=["rstdb"])
            P.dve(lambda e: e.reciprocal(out=rstdb[:], in_=rstdb[:]), reads=["rstdb"], writes=["rstdb"])
            for blk in range(8):
                P.dve(lambda e, blk=blk: e.scalar_tensor_tensor(out=orecT[:, blk, :], in0=ZX[:, blk, :], scalar=rg[:, blk:blk + 1], in1=rstdb[:], op0=ALU.mult, op1=ALU.mult),
                      reads=[("ZX", blk), "rec_out_g", "rstdb"], writes=[("orecT", blk)])
        P.barrier()


def attention(C, SM, qT, gT, orecT, gbuf, ones_bf, ones_f, epsT, K):
    nc, P, W, sb, ps, AT, ident = C.nc, C.P, C.W, C.sb, C.ps, C.AT, C.ident
    contextlib = C.contextlib

    def bc4(ap, n):
        return ap.unsqueeze(1).broadcast_to([n, 4, 128])

    with contextlib.ExitStack() as SA:
        cmpm = sb("cmpm", [128, 2, 8, 128], BF16, SA)
        selcm = sb("selcm", [128, 4, 128], BF16, SA)
        winm = sb("winm", [128, 8, 128], BF16, SA)
        selA = sb("selA", [128, 8, 64], F32, SA)
        selB = sb("selB", [128, 8, 64], F32, SA)
        selF = sb("selF", [128, 8, 64], F32, SA)
        E = sb("E", [64, 4096], BF16, SA)
        ovl = sb("ovl", [128, 2, 64], F32, SA)
        selmat = sb("selmat", [24, 24, 128], F32, SA)
        oaT = sb("oaT", [128, 8, NT], BF16, SA)
        for t_, src, nm in [(cmpm, K["cmpm"], "cmpm"), (selcm, K["selcm"], "selcm"), (winm, K["winm"], "winm"), (selA, K["selA"], "selA"),
                            (selB, K["selB"], "selB"), (selF, K["selF"], "selF"), (E, K["E"], "E"), (ovl, K["ovl"], "ovl"), (selmat, K["selmat"], "selmat")]:
            fl = t_[:]
            if len(t_.shape) == 3:
                fl = t_[:].rearrange("p a b -> p (a b)")
            elif len(t_.shape) == 4:
                fl = t_[:].rearrange("p a b c -> p (a b c)")
            P.dma(lambda e, fl=fl, src=src: e.dma_start(out=fl, in_=src[:, :]), writes=[nm])

        gview = gbuf.rearrange("(k j il d) (l p) -> d k il l j p", k=4, j=4, il=3, d=128, l=8, p=128)
        for g in range(2):
            with contextlib.ExitStack() as SG_:
                KV = {}
                for nm, item in [("kc", 0 + g), ("vc", 2 + g), ("ks", 4 + g), ("kw", 6 + g), ("vs", 8 + g), ("vw", 10 + g)]:
                    t_ = sb("%s%d" % (nm, g), [128, 4096], BF16, SG_)
                    KV[nm] = t_
                    for l in range(8):
                        P.dma(lambda e, t_=t_, item=item, l=l: e.dma_start(out=t_[:, l * 512:(l + 1) * 512].rearrange("d (j p) -> d j p", j=4), in_=gview[:, item // 3, item % 3, l, :, :]),
                              reads=[("gbuf", item // 3)], writes=[("kv", nm, l)])
                kvall = lambda nm: [("kv", nm, l) for l in range(8)]
                kcT = sb("kcT%d" % g, [128, 256], BF16, SG_)
                vcm = sb("vcm%d" % g, [128, 2, 128], BF16, SG_)
                with contextlib.ExitStack() as SCm:
                    w1t = sb("w1t", [128, 32, 256], BF16, SCm)
                    w2t = sb("w2t", [128, 2, 128], BF16, SCm)
                    posf = sb("posf", [128, 32], F32, SCm)
                    posb = sb("posb", [128, 32], BF16, SCm)
                    b1 = sb("b1", [128, 2], F32, SCm)
                    hx = sb("hx", [128, 256], F32, SCm)
                    hy = sb("hy", [128, 256], F32, SCm)
                    ghid = sb("ghid", [128, 2, 256], BF16, SCm)
                    ph = [ps("ph%d" % i, [128, 256], F32, SCm) for i in range(2)]
                    pb = [ps("pb%d" % i, [128, 2], F32, SCm) for i in range(2)]
                    pk = ps("pk", [128, 256], F32, SCm)
                    for kvn, src, w1n, w2n, posn in [("k", "kc", "cmp_k_w1", "cmp_k_w2", "cmp_pos_k"), ("v", "vc", "cmp_v_w1", "cmp_v_w2", "cmp_pos_v")]:
                        P.dma(lambda e, w1n=w1n: e.dma_start(out=w1t[:], in_=W[w1n].rearrange("(l d) m -> d l m", d=128)), writes=["w1t"], q="pool")
                        P.dma(lambda e, w2n=w2n: e.dma_start(out=w2t[:], in_=W[w2n].rearrange("(c h) d -> h c d", h=128)), writes=["w2t"], q="pool")
                        with nc.allow_non_contiguous_dma(reason="tiny"):
                            P.dma(lambda e, posn=posn: e.dma_start(out=posf[:], in_=W[posn].rearrange("l d -> d l"), allow_slow_non_contiguous=True), writes=["posf"])
                        P.dve(lambda e: e.tensor_copy(out=posb[:], in_=posf[:]), reads=["posf"], writes=["posb"])
                        xT = KV[src]
                        for hc in range(2):
                            for l in range(32):
                                P.pe(lambda e, hc=hc, l=l, xT=xT: e.matmul(ph[hc][:, 0:255], lhsT=w1t[:, l, hc * 128:(hc + 1) * 128], rhs=xT[:, l:l + 16 * 254 + 1:16],
                                                                   start=(l == 0), stop=(l == 31)), reads=["w1t"] + kvall(src), writes=["ph%d" % hc])
                            for l in range(32):
                                P.pe(lambda e, hc=hc, l=l: e.matmul(pb[hc][:, 0:1], lhsT=w1t[:, l, hc * 128:(hc + 1) * 128], rhs=posb[:, l:l + 1],
                                                             start=(l == 0), stop=(l == 31)), reads=["w1t", "posb"], writes=["pb%d" % hc])
                            P.act(lambda e, hc=hc: e.copy(out=b1[:, hc:hc + 1], in_=pb[hc][:, 0:1]), reads=["pb%d" % hc], writes=["b1"])
                            P.act(lambda e, hc=hc: e.activation(out=hx[:, 0:255], in_=ph[hc][:, 0:255], func=AF.Identity, bias=b1[:, hc:hc + 1]), reads=["ph%d" % hc, "b1"], writes=["hx"])
                            P.act(lambda e: e.activation(out=hy[:, 0:255], in_=hx[:, 0:255], func=AF.Square), reads=["hx"], writes=["hy"])
                            P.dve(lambda e: e.tensor_scalar(out=hy[:, 0:255], in0=hy[:, 0:255], scalar1=0.044715, scalar2=1.0, op0=ALU.mult, op1=ALU.add), reads=["hy"], writes=["hy"])
                            P.dve(lambda e: e.tensor_tensor(out=hy[:, 0:255], in0=hy[:, 0:255], in1=hx[:, 0:255], op=ALU.mult), reads=["hy", "hx"], writes=["hy"])
                            P.act(lambda e: e.activation(out=hy[:, 0:255], in_=hy[:, 0:255], func=AF.Sigmoid, scale=1.5957691216057308), reads=["hy"], writes=["hy"])
                            P.dve(lambda e, hc=hc: e.tensor_tensor(out=ghid[:, hc, 0:255], in0=hy[:, 0:255], in1=hx[:, 0:255], op=ALU.mult), reads=["hy", "hx"], writes=[("ghid", hc)])
                        if kvn == "k":
                            for hc in range(2):
                                P.pe(lambda e, hc=hc: e.matmul(pk[:, 0:255], lhsT=w2t[:, hc, :], rhs=ghid[:, hc, 0:255], start=(hc == 0), stop=(hc == 1)),
                                     reads=["w2t", ("ghid", 0), ("ghid", 1)], writes=["pk"])
                            P.act(lambda e: e.copy(out=kcT[:, 0:255], in_=pk[:, 0:255]), reads=["pk"], writes=["kcT"])
                        else:
                            for ct in range(2):
                                cn = 128 if ct == 0 else 127
                                for hc in range(2):
                                    P.pe(lambda e, hc=hc, ct=ct, cn=cn: e.matmul(pk[0:cn, ct * 128:(ct + 1) * 128], lhsT=ghid[:, hc, ct * 128:ct * 128 + cn], rhs=w2t[:, hc, :], start=(hc == 0), stop=(hc == 1)),
                                         reads=["w2t", ("ghid", 0), ("ghid", 1)], writes=["pk"])
                            P.act(lambda e: e.copy(out=vcm[:, 0, :], in_=pk[:, 0:128]), reads=["pk"], writes=["vcm"])
                            P.act(lambda e: e.copy(out=vcm[0:127, 1, :], in_=pk[0:127, 128:256]), reads=["pk"], writes=["vcm"])
                P.barrier()
                with contextlib.ExitStack() as SQ:
                    Pcf = [sb("Pcf%d" % ct, [128, 512], F32, SQ) for ct in range(2)]
                    pcb = [sb("pcb%d" % ct, [128, 512], BF16, SQ) for ct in range(2)]
                    rd = sb("rd", [128, 512], F32, SQ)
                    gS = sb("gS", [128, 512], F32, SQ)
                    wgt = sb("wgt", [128, 512], F32, SQ)
                    accs = [sb("acc%d" % i, [128, 512], F32, SQ) for i in range(2)]
                    rdc = sb("rdc", [128, 512], F32, SQ)
                    gSc = sb("gSc", [128, 512], F32, SQ)
                    t0 = sb("t0", [128, 64], F32, SQ)
                    t1 = sb("t1s", [128, 64], F32, SQ)
                    m8a = sb("m8a", [128, 8], F32, SQ)
                    m8b = sb("m8b", [128, 8], F32, SQ)
                    selbb = sb("selbb", [128, 64], BF16, SQ)
                    selbTs = [sb("selbT%d" % i, [64, 4, 128], BF16, SQ) for i in range(2)]
                    Pt = [sb("Pt%d" % i, [128, 512], BF16, SQ) for i in range(3)]
                    pS = [ps("pS%d" % i, [128, 512], F32, SQ) for i in range(2)]
                    pO = ps("pO", [128, 512], F32, SQ)
                    pD = ps("pD", [128, 512], F32, SQ)
                    pG = ps("pG", [128, 512], F32, SQ)
                    pI = ps("pI", [128, 64], F32, SQ)
                    pTt = ps("pTt", [64, 128], BF16, SQ)
                    pC = ps("pC", [128, 512], F32, SQ)
                    scnt = [0]
                    pcnt = [0]

                    def dump(nm, tile_, key, shape, dt=F32):
                        C.dbg[nm] = nc.dram_tensor("dbg_" + nm, list(shape), dt, kind="ExternalOutput").ap()
                        C.dbg_ops.append(P.dma(lambda e: e.dma_start(out=C.dbg[nm][:, :], in_=tile_), reads=[key], writes=["dd_" + nm]))

                    def gates(br, l, dst=None, dkey="gS"):
                        dst = gS if dst is None else dst
                        for h in range(4):
                            n = (4 * g + h) * 3 + br
                            P.pe(lambda e, h=h, n=n, l=l: e.matmul(pG[:, h * 128:(h + 1) * 128], lhsT=selmat[:, n, :], rhs=gT[:, l * 128:(l + 1) * 128], start=True, stop=True),
                                 reads=["selmat", "gT"], writes=["pG"])
                        P.act(lambda e, dst=dst: e.copy(out=dst[:], in_=pG[:]), reads=["pG"], writes=[dkey])

                    def branch(br, l, keyT, vtok, kts, maskfn, use_sel):
                        acc = accs[l % 2]
                        akey = "acc%d" % (l % 2)
                        selbT = selbTs[l % 2]
                        skey = "selbT%d" % (l % 2)
                        qg = qT[:, 4 * g:4 * g + 4, l * 128:(l + 1) * 128]
                        qkeys = [("qT", 4 * g + h) for h in range(4)]
                        nk = len(kts)
                        for ii, kt in enumerate(kts):
                            si = scnt[0] % 2
                            scnt[0] += 1
                            pi_ = pcnt[0] % 3
                            pcnt[0] += 1
                            lk = ("kv", keyT[1], kt // 4)
                            P.pe(lambda e, si=si, kt=kt: e.matmul(pS[si][:], lhsT=keyT[0][:, kt * 128:(kt + 1) * 128], rhs=qg, start=True, stop=(not use_sel)),
                                 reads=[lk] + qkeys, writes=["pS%d" % si])
                            if use_sel:
                                P.pe(lambda e, si=si, kt=kt, selbT=selbT: e.matmul(pS[si][:], lhsT=E[:, kt * 128:(kt + 1) * 128], rhs=selbT[:].rearrange("s h q -> s (h q)"), start=False, stop=True),
                                     reads=["E", skey], writes=["pS%d" % si])
                            P.act(lambda e, si=si, pi_=pi_: e.activation(out=Pt[pi_][:], in_=pS[si][:], func=AF.Exp, scale=SCALE), reads=["pS%d" % si], writes=["Pt%d" % pi_])
                            mk = maskfn(kt)
                            if mk is not None:
                                P.dve(lambda e, pi_=pi_, mk=mk: e.tensor_tensor(out=Pt[pi_][:].rearrange("k (h q) -> k h q", h=4), in0=Pt[pi_][:].rearrange("k (h q) -> k h q", h=4),
                                                                          in1=bc4(mk[0], 128), op=ALU.mult), reads=["Pt%d" % pi_, mk[1]], writes=["Pt%d" % pi_])
                            vk = ("kv", vtok[1], kt // 4)
                            P.pe(lambda e, pi_=pi_, kt=kt, ii=ii: e.matmul(pO[:], lhsT=vtok[0][:, kt * 128:(kt + 1) * 128], rhs=Pt[pi_][:], start=(ii == 0), stop=(ii == nk - 1)),
                                 reads=[vk, "Pt%d" % pi_], writes=["pO"])
                            P.pe(lambda e, pi_=pi_, ii=ii: e.matmul(pD[:], lhsT=ones_bf[:], rhs=Pt[pi_][:], start=(ii == 0), stop=(ii == nk - 1)),
                                 reads=["ones_bf", "Pt%d" % pi_], writes=["pD"])
                        gates(br, l)
                        P.dve(lambda e: e.tensor_scalar(out=rd[:], in0=pD[:], scalar1=1e-30, scalar2=None, op0=ALU.max), reads=["pD"], writes=["rd"])
                        P.dve(lambda e: e.reciprocal(out=rd[:], in_=rd[:]), reads=["rd"], writes=["rd"])
                        P.dve(lambda e: e.tensor_tensor(out=wgt[:], in0=rd[:], in1=gS[:], op=ALU.mult), reads=["rd", "gS"], writes=["wgt"])
                        P.dve(lambda e: e.tensor_tensor(out=wgt[:], in0=pO[:], in1=wgt[:], op=ALU.mult), reads=["pO", "wgt"], writes=["wgt"])
                        P.dve(lambda e, acc=acc: e.tensor_tensor(out=acc[:], in0=acc[:], in1=wgt[:], op=ALU.add), reads=[akey, "wgt"], writes=[akey])

                    def cmp_stage(l):
                        acc = accs[l % 2]
                        akey = "acc%d" % (l % 2)
                        selbT = selbTs[l % 2]
                        skey = "selbT%d" % (l % 2)
                        qg = qT[:, 4 * g:4 * g + 4, l * 128:(l + 1) * 128]
                        qkeys = [("qT", 4 * g + h) for h in range(4)]
                        for ct in range(2):
                            cn = 128 if ct == 0 else 127
                            si = scnt[0] % 2
                            scnt[0] += 1
                            P.pe(lambda e, si=si, ct=ct, cn=cn, qg=qg: e.matmul(pS[si][0:cn, :], lhsT=kcT[:, ct * 128:ct * 128 + cn], rhs=qg, start=True, stop=True),
                                 reads=["kcT"] + qkeys, writes=["pS%d" % si])
                            P.act(lambda e, si=si, ct=ct, cn=cn: e.activation(out=Pcf[ct][0:cn, :], in_=pS[si][0:cn, :], func=AF.Exp, scale=SCALE), reads=["pS%d" % si], writes=["Pcf%d" % ct])
                            P.dve(lambda e, ct=ct, cn=cn, l=l: e.tensor_tensor(out=Pcf[ct][0:cn, :].rearrange("k (h q) -> k h q", h=4), in0=Pcf[ct][0:cn, :].rearrange("k (h q) -> k h q", h=4),
                                                                     in1=bc4(cmpm[0:cn, ct, l, :], cn), op=ALU.mult), reads=["Pcf%d" % ct, "cmpm"], writes=["Pcf%d" % ct])
                        for ct in range(2):
                            cn = 128 if ct == 0 else 127
                            P.pe(lambda e, ct=ct, cn=cn: e.matmul(pC[:], lhsT=ones_f[0:cn, :], rhs=Pcf[ct][0:cn, :], start=(ct == 0), stop=(ct == 1)),
                                 reads=["ones_f", "Pcf%d" % ct], writes=["pC"])
                        P.dve(lambda e: e.tensor_scalar(out=rdc[:], in0=pC[:], scalar1=1e-30, scalar2=None, op0=ALU.max), reads=["pC"], writes=["rdc"])
                        P.dve(lambda e: e.reciprocal(out=rdc[:], in_=rdc[:]), reads=["rdc"], writes=["rdc"])
                        for ct in range(2):
                            cn = 128 if ct == 0 else 127
                            P.dve(lambda e, ct=ct, cn=cn: e.tensor_tensor(out=Pcf[ct][0:cn, :], in0=Pcf[ct][0:cn, :], in1=rdc[0:cn, :], op=ALU.mult), reads=["Pcf%d" % ct, "rdc"], writes=["Pcf%d" % ct])
                            P.act(lambda e, ct=ct, cn=cn: e.copy(out=pcb[ct][0:cn, :], in_=Pcf[ct][0:cn, :]), reads=["Pcf%d" % ct], writes=["pcb%d" % ct])
                        for ct in range(2):
                            cn = 128 if ct == 0 else 127
                            P.pe(lambda e, ct=ct, cn=cn: e.matmul(pC[:], lhsT=vcm[0:cn, ct, :], rhs=pcb[ct][0:cn, :], start=(ct == 0), stop=(ct == 1)),
                                 reads=["vcm", "pcb%d" % ct, "rdc"], writes=["pC"])
                        cnt8 = 0
                        for r in range(4):
                            for ct in range(2):
                                cn = 128 if ct == 0 else 127
                                P.pe(lambda e, ct=ct, cn=cn, r=r, cnt8=cnt8: e.matmul(pI[:], lhsT=Pcf[ct][0:cn, r * 128:(r + 1) * 128], rhs=ovl[0:cn, ct, :], start=(cnt8 == 0), stop=(cnt8 == 7)),
                                     reads=["Pcf%d" % ct, "ovl"], writes=["pI"])
                                cnt8 += 1
                        gates(0, l, gSc, "gSc")
                        P.dve(lambda e, acc=acc: e.tensor_tensor(out=acc[:], in0=pC[:], in1=gSc[:], op=ALU.mult), reads=["pC", "gSc"], writes=[akey])
                        dbg_here = (C.stop == 22 and g == 0 and l == 1)
                        if dbg_here:
                            dump("acc0", acc[:], akey, [128, 512])
                            dump("kcT", kcT[:], "kcT", [128, 256], BF16)
                            dump("vcm", vcm[:].rearrange("p a b -> p (a b)"), "vcm", [128, 256], BF16)
                            dump("gS0", gSc[:], "gSc", [128, 512])
                        P.dve(lambda e, l=l: e.tensor_tensor(out=t0[:], in0=pI[:], in1=selA[:, l, :], op=ALU.mult), reads=["pI", "selA"], writes=["t0"])
                        P.dve(lambda e, l=l: e.tensor_tensor(out=t0[:], in0=t0[:], in1=selB[:, l, :], op=ALU.add), reads=["t0", "selB"], writes=["t0"])
                        P.dve(lambda e, l=l: e.tensor_tensor(out=t0[:], in0=t0[:], in1=selF[:, l, :], op=ALU.max), reads=["t0", "selF"], writes=["t0"])
                        P.dve(lambda e: e.max(out=m8a[:], in_=t0[:]), reads=["t0"], writes=["m8a"])
                        P.dve(lambda e: e.match_replace(out=t1[:], in_to_replace=m8a[:], in_values=t0[:], imm_value=-1e30), reads=["t0", "m8a"], writes=["t1s"])
                        P.dve(lambda e: e.max(out=m8b[:], in_=t1[:]), reads=["t1s"], writes=["m8b"])
                        P.dve(lambda e: e.tensor_scalar(out=t1[:], in0=t0[:], scalar1=m8b[:, 7:8], scalar2=-1.0, op0=ALU.is_ge, op1=ALU.add), reads=["t0", "m8b"], writes=["t1s"])
                        P.dve(lambda e: e.tensor_scalar(out=selbb[:], in0=t1[:], scalar1=-BIGNEG, scalar2=None, op0=ALU.mult), reads=["t1s"], writes=["selbb"])
                        P.pe(lambda e: e.transpose(out=pTt[:], in_=selbb[:], identity=ident[:]), reads=["selbb", "ident"], writes=["pTt"])
                        for h in range(4):
                            P.act(lambda e, h=h, selbT=selbT: e.copy(out=selbT[:, h, :], in_=pTt[:]), reads=["pTt"], writes=[skey])
                        if C.stop == 22 and g == 0 and l == 1:
                            C.dbg["t0"] = nc.dram_tensor("dbg_t0", [128, 64], F32, kind="ExternalOutput").ap()
                            C.dbg["selb"] = nc.dram_tensor("dbg_selb", [128, 64], BF16, kind="ExternalOutput").ap()
                            C.dbg_ops.append(P.dma(lambda e: e.dma_start(out=C.dbg["t0"][:, :], in_=t0[:]), reads=["t0"], writes=["dd1"]))
                            C.dbg_ops.append(P.dma(lambda e: e.dma_start(out=C.dbg["selb"][:, :], in_=selbb[:]), reads=["selbb"], writes=["dd2"]))
                    def rest_stage(l):
                        acc = accs[l % 2]
                        akey = "acc%d" % (l % 2)
                        dbg_here = (C.stop == 22 and g == 0 and l == 1)
                        branch(1, l, (KV["ks"], "ks"), (KV["vs"], "vs"), list(range(4 * l + 4)),
                               lambda kt, l=l: ((selcm[:, kt - 4 * l, :], "selcm") if kt >= 4 * l else None), True)
                        if dbg_here:
                            dump("acc1", acc[:], akey, [128, 512])
                        branch(2, l, (KV["kw"], "kw"), (KV["vw"], "vw"), list(range(max(0, 4 * l - 4), 4 * l + 4)),
                               lambda kt, l=l: (winm[:, kt - 4 * l + 4, :], "winm"), False)
                        if dbg_here:
                            dump("acc2", acc[:], akey, [128, 512])
                        for h in range(4):
                            P.act(lambda e, h=h, l=l, g=g, acc=acc: e.copy(out=oaT[:, 4 * g + h, l * 128:(l + 1) * 128], in_=acc[:, h * 128:(h + 1) * 128]), reads=[akey], writes=[("oaT", 4 * g + h)])
                    cmp_stage(0)
                    for l in range(8):
                        if l + 1 < 8:
                            cmp_stage(l + 1)
                        rest_stage(l)
                P.barrier()
        with contextlib.ExitStack() as SN:
            ag = sb("ag", [128, 8], F32, SN)
            rstdb = sb("a_rstdb", [128, NT], F32, SN)
            sqb = [sb("a_sqb%d" % i, [128, NT], BF16, SN) for i in range(2)]
            pss = [ps("a_pss%d" % hf, [128, 512], F32, SN) for hf in range(2)]
            with nc.allow_non_contiguous_dma(reason="tiny"):
                P.dma(lambda e: e.dma_start(out=ag[:], in_=W["attn_out_g"].rearrange("o (b p) -> p (o b)", p=128), allow_slow_non_contiguous=True), writes=["ag"])
            for h in range(8):
                i = h % 2
                P.act(lambda e, h=h, i=i: e.activation(out=sqb[i][:], in_=oaT[:, h, :], func=AF.Square), reads=[("oaT", h)], writes=["a_sqb%d" % i])
                for hf in range(2):
                    P.pe(lambda e, h=h, i=i, hf=hf: e.matmul(pss[hf][:], lhsT=ones_bf[:], rhs=sqb[i][:, hf * 512:(hf + 1) * 512], start=(h == 0), stop=(h == 7)),
                         reads=["ones_bf", "a_sqb%d" % i], writes=["a_pss%d" % hf])
            for hf in range(2):
                sl = slice(hf * 512, (hf + 1) * 512)
                P.act(lambda e, hf=hf, sl=sl: e.activation(out=rstdb[:, sl], in_=pss[hf][:], func=AF.Sqrt, scale=1.0 / 1024, bias=epsT[:, 0:1]), reads=["a_pss%d" % hf, "m_eps"], writes=["a_rstdb"])
            P.dve(lambda e: e.reciprocal(out=rstdb[:], in_=rstdb[:]), reads=["a_rstdb"], writes=["a_rstdb"])
            ATall = [("AT", t) for t in range(NTILE)]
            for h in range(8):
                P.dve(lambda e, h=h: e.scalar_tensor_tensor(out=AT[:, h, :], in0=oaT[:, h, :], scalar=ag[:, h:h + 1], in1=rstdb[:], op0=ALU.mult, op1=ALU.mult),
                      reads=[("oaT", h), "ag", "a_rstdb"], writes=ATall)
                P.pool(lambda e, h=h: e.tensor_copy(out=AT[:, 8 + h, :], in_=orecT[:, h, :]), reads=[("orecT", h)], writes=ATall)
    P.barrier()


def outproj(C):
    nc, P, W, sb, ps, AT = C.nc, C.P, C.W, C.sb, C.ps, C.AT
    contextlib = C.contextlib
    ATall = [("AT", t) for t in range(NTILE)]
    with contextlib.ExitStack() as SO:
        YT = sb("YT", [128, KD, NT], BF16, SO)
        with contextlib.ExitStack() as S1:
            wp = [sb("wo%d" % i, [128, KD, 512], BF16, S1) for i in range(2)]
            py = [[ps("po%d%d" % (i, hf), [128, 512], F32, S1) for hf in range(2)] for i in range(2)]
            wv = W["w_out"].rearrange("(kc p) m -> p kc m", p=128)
            cnt = 0
            for pi in range(4):
                b = pi % 2
                P.dma(lambda e, pi=pi, b=b: e.dma_start(out=wp[b][:], in_=wv[:, :, pi * 512:(pi + 1) * 512]), writes=["wo%d" % b], q="pool")
                for mc in range(4):
                    m = pi * 4 + mc
                    r = cnt % 2
                    cnt += 1
                    for hf in range(2):
                        for k in range(KD):
                            P.pe(lambda e, k=k, hf=hf, r=r, b=b, mc=mc: e.matmul(py[r][hf][:], lhsT=wp[b][:, k, mc * 128:(mc + 1) * 128], rhs=AT[:, k, hf * 512:(hf + 1) * 512],
                                                                         start=(k == 0), stop=(k == KD - 1)), reads=["wo%d" % b] + ATall[hf * 4:hf * 4 + 4], writes=["po%d%d" % (r, hf)])
                        P.act(lambda e, m=m, hf=hf, r=r: e.copy(out=YT[:, m, hf * 512:(hf + 1) * 512], in_=py[r][hf][:]), reads=["po%d%d" % (r, hf)],
                              writes=[("YT", hf * 4 + q) for q in range(4)])
        C.post("mx", YT, C.h1d, W["mix_post_g"], 1.0, C.h2d, W["ff2_pre_g"])


def ple(C):
    nc, P, W, sb, ps, AT, ident = C.nc, C.P, C.W, C.sb, C.ps, C.AT, C.ident
    contextlib = C.contextlib
    ATall = [("AT", t) for t in range(NTILE)]
    with contextlib.ExitStack() as SO:
        YT = sb("YTp", [128, KD, NT], BF16, SO)
        with contextlib.ExitStack() as S1:
            pT = sb("pTp", [128, 2, NT], BF16, S1)
            ptl = [sb("ptl%d" % i, [128, 256], F32, S1) for i in range(2)]
            ptb = [sb("ptb%d" % i, [128, 256], BF16, S1) for i in range(2)]
            wpj = sb("wpj", [128, 2, D], BF16, S1)
            sg = [sb("psg%d" % i, [128, 512], F32, S1) for i in range(2)]
            wp = [sb("wg%d" % i, [128, KD, 512], BF16, S1) for i in range(2)]
            ppt = [ps("ppt%d" % i, [128, 2, 128], BF16, S1) for i in range(2)]
            pa = [[ps("pa%d%d" % (i, hf), [128, 512], F32, S1) for hf in range(2)] for i in range(1)]
            pb = [[ps("pbp%d%d" % (i, hf), [128, 512], F32, S1) for hf in range(2)] for i in range(1)]
            P.dma(lambda e: e.dma_start(out=wpj[:], in_=W["w_ple_proj"].rearrange("(c p) m -> p c m", p=128)), writes=["wpj"], q="pool")
            for t in range(NTILE):
                i = t % 2
                P.dma(lambda e, t=t, i=i: e.dma_start(out=ptl[i][:], in_=C.pin[t * 128:(t + 1) * 128, :]), writes=["ptl%d" % i])
                P.dve(lambda e, i=i: e.tensor_copy(out=ptb[i][:], in_=ptl[i][:]), reads=["ptl%d" % i], writes=["ptb%d" % i])
                for c in range(2):
                    P.pe(lambda e, i=i, c=c: e.transpose(out=ppt[i][:, c, :], in_=ptb[i][:, c * 128:(c + 1) * 128], identity=ident[:]), reads=["ptb%d" % i, "ident"], writes=["ppt%d" % i])
                P.act(lambda e, i=i, t=t: e.copy(out=pT[:, :, t * 128:(t + 1) * 128], in_=ppt[i][:]), reads=["ppt%d" % i], writes=[("pTp", t)])
            wv = W["w_ple_gate"].rearrange("(kc p) m -> p kc m", p=128)
            pTall = [("pTp", t) for t in range(NTILE)]
            for pi in range(4):
                b = pi % 2
                P.dma(lambda e, pi=pi, b=b: e.dma_start(out=wp[b][:], in_=wv[:, :, pi * 512:(pi + 1) * 512]), writes=["wg%d" % b], q="pool")
                for mc in range(4):
                    m = pi * 4 + mc
                    for hf in range(2):
                        for k in range(KD):
                            P.pe(lambda e, k=k, hf=hf, b=b, mc=mc: e.matmul(pa[0][hf][:], lhsT=wp[b][:, k, mc * 128:(mc + 1) * 128], rhs=AT[:, k, hf * 512:(hf + 1) * 512],
                                                                    start=(k == 0), stop=(k == KD - 1)), reads=["wg%d" % b] + ATall[hf * 4:hf * 4 + 4], writes=["pa0%d" % hf])
                        for c in range(2):
                            P.pe(lambda e, c=c, hf=hf, m=m: e.matmul(pb[0][hf][:], lhsT=wpj[:, c, m * 128:(m + 1) * 128], rhs=pT[:, c, hf * 512:(hf + 1) * 512],
                                                              start=(c == 0), stop=(c == 1)), reads=["wpj"] + pTall[hf * 4:hf * 4 + 4], writes=["pbp0%d" % hf])
                        P.act(lambda e, hf=hf: e.activation(out=sg[hf][:], in_=pa[0][hf][:], func=AF.Sigmoid), reads=["pa0%d" % hf], writes=["psg%d" % hf])
                        P.dve(lambda e, hf=hf, m=m: e.tensor_tensor(out=YT[:, m, hf * 512:(hf + 1) * 512], in0=sg[hf][:], in1=pb[0][hf][:], op=ALU.mult),
                              reads=["psg%d" % hf, "pbp0%d" % hf], writes=[("YTp", hf * 4 + q) for q in range(4)])
        C.post("pl", YT, C.h3d, W["ple_post_g"], 1.0, C.out, None)


def host_consts(j):
    import ml_dtypes
    bf = ml_dtypes.bfloat16
    c = {}
    d = np.arange(128)
    half = 16
    inv_freq = (500000.0 ** (-np.arange(half, dtype=np.float32) / half)).astype(np.float32)
    rope = np.zeros((128, 4), np.float32)
    rope[:32, 0] = inv_freq[d[:32] % 16]
    rope[:16, 1] = -1.0
    rope[16:32, 1] = 1.0
    rope[:, 2] = 1.0
    c["c_rope"] = rope
    perm = np.zeros((128, 128), np.float32)
    for m in range(128):
        k = m + 16 if m < 16 else (m - 16 if m < 32 else m)
        perm[k, m] = 1.0
    c["c_perm"] = perm
    oh = np.zeros((128, 4), np.float32)
    oh[:, j] = 1.0
    c["c_oh"] = oh
    q = np.arange(128)
    kk = np.arange(128)
    cm = np.zeros((128, 2, 8, 128), np.float32)
    for ct in range(2):
        cg = ct * 128 + kk
        for l in range(8):
            t = (4 * l + j) * 128 + q
            cm[:, ct, l, :] = ((16 * cg[:, None] + 31 <= t[None, :]) & (cg[:, None] < 255))
    c["c_cmpm"] = cm.reshape(128, -1).astype(bf)
    tri = (kk[:, None] <= q[None, :]).astype(np.float32)
    triu = (kk[:, None] > q[None, :]).astype(np.float32)
    sc = np.zeros((128, 4, 128), np.float32)
    for m in range(4):
        sc[:, m, :] = 1.0 if m < j else (tri if m == j else 0.0)
    c["c_selcm"] = sc.reshape(128, -1).astype(bf)
    wm = np.zeros((128, 8, 128), np.float32)
    for m in range(8):
        dd = m - 4 - j
        if dd == 0:
            wm[:, m, :] = tri
        elif dd == -4:
            wm[:, m, :] = triu
        elif -4 < dd < 0:
            wm[:, m, :] = 1.0
    c["c_winm"] = wm.reshape(128, -1).astype(bf)
    sA = np.zeros((128, 8, 64), np.float32)
    sB = np.zeros((128, 8, 64), np.float32)
    sF = np.zeros((128, 8, 64), np.float32)
    s = np.arange(64)
    for l in range(8):
        t = (4 * l + j) * 128 + q
        valid = (64 * s[None, :] <= t[:, None])
        forced = (s[None, :] == (t // 64)[:, None]) | (s[None, :] == 0)
        sA[:, l, :] = valid
        sB[:, l, :] = (valid.astype(np.float32) - 1.0) * 1e4
        sF[:, l, :] = np.where(forced, 1e4, -3e4)
    c["c_selA"] = sA.reshape(128, -1)
    c["c_selB"] = sB.reshape(128, -1)
    c["c_selF"] = sF.reshape(128, -1)
    key = np.arange(4096)
    c["c_E"] = (key[None, :] // 64 == s[:, None]).astype(np.float32).astype(bf)
    ov = np.zeros((128, 2, 64), np.float32)
    for ct in range(2):
        cg = ct * 128 + kk
        st_ = 16 * cg
        en_ = st_ + 31
        ov[:, ct, :] = ((st_[:, None] < 64 * s[None, :] + 64) & (en_[:, None] >= 64 * s[None, :]) & (cg[:, None] < 255))
    c["c_ovl"] = ov.reshape(128, -1)
    sm = np.zeros((24, 24, 128), np.float32)
    for n in range(24):
        sm[n, n, :] = 1.0
    c["c_selmat"] = sm.reshape(24, -1)
    return c


_orig_make_in_maps = make_in_maps


def make_in_maps(inputs):
    maps = _orig_make_in_maps(inputs)
    for c in range(8):
        maps[c].update(host_consts(c % 4))
    return maps


_NC_CACHE = {}


def kernel(**inputs):
    maps = make_in_maps(inputs)
    if "nc" not in _NC_CACHE:
        _NC_CACHE["nc"] = build()
    nc = _NC_CACHE["nc"]
    res = run_bass_kernel_spmd(nc, maps, core_ids=list(range(8)))
    return unshard([r["out"] for r in res.results])
```
